# Optimizing a Trainium2 kernel written in Bass

```python
import jax, jax.numpy as jnp
from jax import lax
import numpy as np

D_MODEL = 1024
BATCH = 4
SEQ = 8192
DEPTH = 4

GRID_W = 64
CTX_LEN = 256
HEAD_DIM = 64
A_HEADS = 8
A_KV_HEADS = 2
A_GROUP = A_HEADS // A_KV_HEADS
B_HEADS = 4
C_CHANNELS = 256
CONV_WIDTH = 31
NA_WIN_ROWS = 8
NA_WIN_COLS = 16
Q_BLOCK = 128
N_EXPERTS = 16
EXPERT_DIM = 2048
CAPACITY_FACTOR = 2
ROPE_THETA = 10000.0
EPS = 1e-6
A_WIDTH = A_HEADS * HEAD_DIM
A_KV_WIDTH = A_KV_HEADS * HEAD_DIM
B_WIDTH = B_HEADS * HEAD_DIM
MIX_WIDTH = A_WIDTH + B_WIDTH + C_CHANNELS
IN_WIDTH = A_WIDTH + 2 * A_KV_WIDTH + 3 * B_WIDTH + 2 * C_CHANNELS

kernel_name = "hybrid_gqa_natten_conformer_ecmoe_dit"


def rms_norm(x, g):
    xf = x.astype(jnp.float32)
    y = xf * lax.rsqrt(jnp.mean(xf * xf, axis=-1, keepdims=True) + EPS)
    return (y * g.astype(jnp.float32)).astype(x.dtype)


def layer_norm(x, g, b):
    xf = x.astype(jnp.float32)
    mu = jnp.mean(xf, axis=-1, keepdims=True)
    var = jnp.mean(jnp.square(xf - mu), axis=-1, keepdims=True)
    y = (xf - mu) * lax.rsqrt(var + EPS)
    return (y * g.astype(jnp.float32) + b.astype(jnp.float32)).astype(x.dtype)


def modulate(h, shift, scale):
    return h * (1 + scale) + shift


def rope_axis(x, pos):
    half = x.shape[-1] // 2
    inv = ROPE_THETA ** (-jnp.arange(half, dtype=jnp.float32) / half)
    ang = pos.astype(jnp.float32)[:, None] * inv[None, :]
    cos = jnp.cos(ang)[:, None, :]
    sin = jnp.sin(ang)[:, None, :]
    xf = x.astype(jnp.float32)
    x1, x2 = xf[..., :half], xf[..., half:]
    return jnp.concatenate([x1 * cos - x2 * sin, x2 * cos + x1 * sin], axis=-1).astype(x.dtype)


def rope_2d(x, pos_row, pos_col):
    h = x.shape[-1] // 2
    return jnp.concatenate([rope_axis(x[..., :h], pos_row), rope_axis(x[..., h:], pos_col)], axis=-1)


def gqa_attend(q, k, v):
    b, nq = q.shape[0], q.shape[1]
    s = jnp.einsum('bqkgd,btkd->bkgqt', q, k).astype(jnp.float32) * (q.shape[-1] ** -0.5)
    p = jax.nn.softmax(s, axis=-1).astype(v.dtype)
    o = jnp.einsum('bkgqt,btkd->bqkgd', p, v)
    return o.reshape(b, nq, -1)


def axial_gqa_mixer(qa_l, ka_l, va_l, qa_c, ka_c, va_c, q_norm_g, k_norm_g, pos_row, pos_col, ctx_queries):
    b, n, _ = qa_l.shape
    nc = ka_c.shape[1]
    q_l = rope_2d(rms_norm(qa_l.reshape(b, n, A_HEADS, HEAD_DIM), q_norm_g), pos_row, pos_col)
    k_l = rope_2d(rms_norm(ka_l.reshape(b, n, A_KV_HEADS, HEAD_DIM), k_norm_g), pos_row, pos_col)
    v_l = va_l.reshape(b, n, A_KV_HEADS, HEAD_DIM)
    k_c = rms_norm(ka_c.reshape(b, nc, A_KV_HEADS, HEAD_DIM), k_norm_g)
    v_c = va_c.reshape(b, nc, A_KV_HEADS, HEAD_DIM)
    k_all = jnp.concatenate([k_c, k_l], axis=1)
    v_all = jnp.concatenate([v_c, v_l], axis=1)
    q_blocks = jnp.moveaxis(q_l.reshape(b, n // Q_BLOCK, Q_BLOCK, A_KV_HEADS, A_GROUP, HEAD_DIM), 1, 0)
    out = lax.map(lambda qb: gqa_attend(qb, k_all, v_all), q_blocks)
    a_lat = jnp.moveaxis(out, 0, 1).reshape(b, n, A_WIDTH)
    a_ctx = None
    if ctx_queries:
        q_c = rms_norm(qa_c.reshape(b, nc, A_HEADS, HEAD_DIM), q_norm_g)
        a_ctx = gqa_attend(q_c.reshape(b, nc, A_KV_HEADS, A_GROUP, HEAD_DIM), k_c, v_c)
    return a_lat, a_ctx


def neighbourhood_mixer(qb_l, kb_l, vb_l, qb_c, kb_c, vb_c, rpb, ctx_queries):
    b, n, _ = qb_l.shape
    nc = kb_c.shape[1]
    rows = n // GRID_W
    wr = min(NA_WIN_ROWS, rows)
    wc = NA_WIN_COLS
    scale = HEAD_DIM ** -0.5
    qg = qb_l.reshape(b, rows, GRID_W, B_HEADS, HEAD_DIM)
    kg = kb_l.reshape(b, rows, GRID_W, B_HEADS, HEAD_DIM)
    vg = vb_l.reshape(b, rows, GRID_W, B_HEADS, HEAD_DIM)
    k_c = kb_c.reshape(b, nc, B_HEADS, HEAD_DIM)
    v_c = vb_c.reshape(b, nc, B_HEADS, HEAD_DIM)
    col = jnp.arange(GRID_W)
    col_start = jnp.clip(col - wc // 2, 0, GRID_W - wc)
    col_idx = col_start[:, None] + jnp.arange(wc)[None, :]
    dc = col_idx - col[:, None] + (NA_WIN_COLS - 1)

    def row_block(args):
        r, q_row = args
        rs = jnp.clip(r - wr // 2, 0, rows - wr)
        k_rows = lax.dynamic_slice_in_dim(kg, rs, wr, axis=1)
        v_rows = lax.dynamic_slice_in_dim(vg, rs, wr, axis=1)
        k_nb = k_rows[:, :, col_idx]
        v_nb = v_rows[:, :, col_idx]
        dr = rs + jnp.arange(wr) - r + (NA_WIN_ROWS - 1)
        bias = rpb[:, dr[None, :, None], dc[:, None, :]].astype(jnp.float32)
        s_nb = jnp.einsum('bqhd,bwqjhd->bhqwj', q_row, k_nb).astype(jnp.float32) * scale + bias[None]
        s_nb = s_nb.reshape(b, B_HEADS, GRID_W, wr * wc)
        s_cx = jnp.einsum('bqhd,bchd->bhqc', q_row, k_c).astype(jnp.float32) * scale
        p = jax.nn.softmax(jnp.concatenate([s_nb, s_cx], axis=-1), axis=-1).astype(v_c.dtype)
        p_nb = p[..., :wr * wc].reshape(b, B_HEADS, GRID_W, wr, wc)
        p_cx = p[..., wr * wc:]
        return (jnp.einsum('bhqwj,bwqjhd->bqhd', p_nb, v_nb)
                + jnp.einsum('bhqc,bchd->bqhd', p_cx, v_c))

    out = lax.map(row_block, (jnp.arange(rows), jnp.moveaxis(qg, 1, 0)))
    b_lat = jnp.moveaxis(out, 0, 1).reshape(b, n, B_WIDTH)
    b_ctx = None
    if ctx_queries:
        q_c = qb_c.reshape(b, nc, B_HEADS, 1, HEAD_DIM)
        b_ctx = gqa_attend(q_c, k_c, v_c)
    return b_lat, b_ctx


def conformer_conv(val, gate, conv_w, conv_b, ln_g, ln_b):
    u = val * jax.nn.sigmoid(gate)
    y = lax.conv_general_dilated(
        u, conv_w[:, None, :].astype(u.dtype), window_strides=(1,),
        padding=[(CONV_WIDTH // 2, CONV_WIDTH // 2)],
        dimension_numbers=('NWC', 'WIO', 'NWC'), feature_group_count=C_CHANNELS)
    y = y + conv_b
    return jax.nn.silu(layer_norm(y, ln_g, ln_b))


def expert_choice_moe(h, w_router, w_gate, w_up, w_down):
    b, n, d = h.shape
    cap = CAPACITY_FACTOR * n // N_EXPERTS
    aff = jax.nn.softmax(jnp.einsum('bnd,de->bne', h, w_router).astype(jnp.float32), axis=-1)
    gate, idx = lax.top_k(jnp.swapaxes(aff, 1, 2), cap)

    def one_expert(args):
        wg, wu, wd, idx_e, gate_e = args
        xe = jax.vmap(lambda hb, ib: hb[ib])(h, idx_e)
        ye = (jax.nn.silu(xe @ wg) * (xe @ wu)) @ wd
        return ye * gate_e[..., None].astype(ye.dtype)

    y = lax.map(one_expert, (w_gate, w_up, w_down, jnp.swapaxes(idx, 0, 1), jnp.swapaxes(gate, 0, 1)))
    y = jnp.swapaxes(y, 0, 1).reshape(b, N_EXPERTS * cap, d)
    idx_flat = idx.reshape(b, N_EXPERTS * cap)
    return jax.vmap(lambda yb, ib: jnp.zeros((n, d), yb.dtype).at[ib].add(yb))(y, idx_flat)


def split_in_proj(p):
    sizes = (A_WIDTH, A_KV_WIDTH, A_KV_WIDTH, B_WIDTH, B_WIDTH, B_WIDTH, C_CHANNELS, C_CHANNELS)
    offsets = [int(v) for v in np.cumsum(sizes)[:-1]]
    return jnp.split(p, offsets, axis=-1)


def hybrid_layer(x_lat, x_ctx, mod_lat, mod_ctx, pos_row, pos_col, norm1_g, norm2_g, w_in, q_norm_g, k_norm_g,
                 na_rpb, conv_w, conv_b, conv_ln_g, conv_ln_b, w_out, w_router, w_gate, w_up, w_down, last):
    s1, sc1, g1, s2, sc2, g2 = jnp.split(mod_lat, 6, axis=-1)
    cs1, csc1, cg1, cs2, csc2, cg2 = jnp.split(mod_ctx, 6, axis=-1)
    ctx_queries = not last

    h_lat = modulate(rms_norm(x_lat, norm1_g), s1, sc1)
    h_ctx = modulate(rms_norm(x_ctx, norm1_g), cs1, csc1)
    qa_l, ka_l, va_l, qb_l, kb_l, vb_l, cv_l, cg_l = split_in_proj(h_lat @ w_in)
    qa_c, ka_c, va_c, qb_c, kb_c, vb_c, cv_c, cg_c = split_in_proj(h_ctx @ w_in)

    a_lat, a_ctx = axial_gqa_mixer(qa_l, ka_l, va_l, qa_c, ka_c, va_c, q_norm_g, k_norm_g,
                                   pos_row, pos_col, ctx_queries)
    b_lat, b_ctx = neighbourhood_mixer(qb_l, kb_l, vb_l, qb_c, kb_c, vb_c, na_rpb, ctx_queries)
    c_lat = conformer_conv(cv_l, cg_l, conv_w, conv_b, conv_ln_g, conv_ln_b)

    x_lat = x_lat + g1 * (jnp.concatenate([a_lat, b_lat, c_lat], axis=-1) @ w_out)
    h2 = modulate(rms_norm(x_lat, norm2_g), s2, sc2)
    x_lat = x_lat + g2 * expert_choice_moe(h2, w_router, w_gate, w_up, w_down)

    if ctx_queries:
        c_ctx_out = conformer_conv(cv_c, cg_c, conv_w, conv_b, conv_ln_g, conv_ln_b)
        x_ctx = x_ctx + cg1 * (jnp.concatenate([a_ctx, b_ctx, c_ctx_out], axis=-1) @ w_out)
        hc2 = modulate(rms_norm(x_ctx, norm2_g), cs2, csc2)
        x_ctx = x_ctx + cg2 * expert_choice_moe(hc2, w_router, w_gate, w_up, w_down)
    return x_lat, x_ctx


def setup_inputs(seed: int = 0) -> dict:
    key = jax.random.key(seed)
    ks = jax.random.split(key, 24)
    f32 = jnp.float32

    def nrm(k, shape, scale):
        return jax.random.normal(k, shape, f32) * scale

    d = D_MODEL
    return {
        "x": nrm(ks[0], (BATCH, SEQ, d), 1.0),
        "c": nrm(ks[1], (BATCH, d), 1.0),
        "ctx": nrm(ks[2], (BATCH, CTX_LEN, d), 1.0),
        "c_ctx": nrm(ks[3], (d,), 1.0),
        "w_ada": nrm(ks[4], (DEPTH, d, 6 * d), 0.5 * d ** -0.5),
        "b_ada": nrm(ks[5], (DEPTH, 6 * d), 0.02),
        "norm1_g": 1.0 + nrm(ks[6], (DEPTH, d), 0.05),
        "norm2_g": 1.0 + nrm(ks[7], (DEPTH, d), 0.05),
        "w_in": nrm(ks[8], (DEPTH, d, IN_WIDTH), d ** -0.5),
        "q_norm_g": 1.0 + nrm(ks[9], (DEPTH, HEAD_DIM), 0.05),
        "k_norm_g": 1.0 + nrm(ks[10], (DEPTH, HEAD_DIM), 0.05),
        "na_rpb": nrm(ks[11], (DEPTH, B_HEADS, 2 * NA_WIN_ROWS - 1, 2 * NA_WIN_COLS - 1), 0.1),
        "conv_w": nrm(ks[12], (DEPTH, CONV_WIDTH, C_CHANNELS), CONV_WIDTH ** -0.5),
        "conv_b": nrm(ks[13], (DEPTH, C_CHANNELS), 0.02),
        "conv_ln_g": 1.0 + nrm(ks[14], (DEPTH, C_CHANNELS), 0.05),
        "conv_ln_b": nrm(ks[15], (DEPTH, C_CHANNELS), 0.02),
        "w_out": nrm(ks[16], (DEPTH, MIX_WIDTH, d), MIX_WIDTH ** -0.5),
        "w_router": nrm(ks[17], (DEPTH, d, N_EXPERTS), d ** -0.5),
        "w_gate": nrm(ks[18], (DEPTH, N_EXPERTS, d, EXPERT_DIM), d ** -0.5),
        "w_up": nrm(ks[19], (DEPTH, N_EXPERTS, d, EXPERT_DIM), d ** -0.5),
        "w_down": nrm(ks[20], (DEPTH, N_EXPERTS, EXPERT_DIM, d), EXPERT_DIM ** -0.5),
        "final_norm_g": 1.0 + nrm(ks[21], (d,), 0.05),
    }


def reference(x, c, ctx, c_ctx, w_ada, b_ada, norm1_g, norm2_g, w_in, q_norm_g, k_norm_g, na_rpb,
              conv_w, conv_b, conv_ln_g, conv_ln_b, w_out, w_router, w_gate, w_up, w_down, final_norm_g):
    n = x.shape[1]
    t = jnp.arange(n)
    pos_row = t // GRID_W
    pos_col = t % GRID_W
    silu_c = jax.nn.silu(c)
    silu_cc = jax.nn.silu(c_ctx)
    x_lat, x_ctx = x, ctx
    for i in range(DEPTH):
        mod_lat = (silu_c @ w_ada[i] + b_ada[i])[:, None, :]
        mod_ctx = (silu_cc @ w_ada[i] + b_ada[i])[None, None, :]
        x_lat, x_ctx = hybrid_layer(
            x_lat, x_ctx, mod_lat, mod_ctx, pos_row, pos_col, norm1_g[i], norm2_g[i], w_in[i],
            q_norm_g[i], k_norm_g[i], na_rpb[i], conv_w[i], conv_b[i], conv_ln_g[i], conv_ln_b[i],
            w_out[i], w_router[i], w_gate[i], w_up[i], w_down[i], i == DEPTH - 1)
    return rms_norm(x_lat, final_norm_g)
```

```python
import contextlib
import numpy as np
import concourse.bass as bass
import concourse.mybir as mybir
from concourse.bass_utils import run_bass_kernel_spmd

F32 = mybir.dt.float32
BF16 = mybir.dt.bfloat16
I32 = mybir.dt.int32
AF = mybir.ActivationFunctionType
ALU = mybir.AluOpType
AX = mybir.AxisListType

D = 1024
T = 8192
CT = 256
TT = T + CT
NTL = T // 128
NTT = TT // 128
DEPTH = 4
NE = 16
FF = 2048
CAP = 1024
CCAP = 32
NPAD = 96
MS = CAP + CCAP + NPAD
EPS = 1e-6
NEG = -30000.0


class Buf:
    def __init__(self, t, name, space):
        self.t = t
        self.name = name
        self.space = space
        self.writes = {}
        self.reads = {}
        self.sem = None
        self.total = 0

    def __getitem__(self, idx):
        return self.t[idx]

    def ap(self):
        return self.t.ap()


class Sched:
    def __init__(self, nc):
        self.nc = nc
        self.eng = {"pe": nc.tensor, "act": nc.scalar, "dve": nc.vector,
                    "pool": nc.gpsimd, "sp": nc.sync}
        self.psem = {k: nc.alloc_semaphore("prog_" + k) for k in self.eng}
        self.cnt = {k: 0 for k in self.eng}
        self.seen = {k: {} for k in self.eng}
        self.sem_pool = []
        self.live = []
        self.nbuf = 0
        self.nsem = 0

    def sb(self, shape, dtype, name=None, stack=None):
        self.nbuf += 1
        name = (name or "sb") + "_%d" % self.nbuf
        if stack is None:
            t = self.nc.alloc_sbuf_tensor(name, list(shape), dtype)
        else:
            t = stack.enter_context(self.nc.sbuf_tensor(name, list(shape), dtype))
        b = Buf(t, name, "sb")
        b.local = stack is not None
        return b

    def ps(self, shape, dtype=F32, name=None, stack=None):
        self.nbuf += 1
        name = (name or "ps") + "_%d" % self.nbuf
        if stack is None:
            t = self.nc.alloc_psum_tensor(name, list(shape), dtype)
        else:
            t = stack.enter_context(self.nc.psum_tensor(name, list(shape), dtype))
        b = Buf(t, name, "ps")
        b.local = stack is not None
        return b

    def dram(self, name, shape, dtype, kind="Internal"):
        b = Buf(self.nc.dram_tensor(name, list(shape), dtype, kind=kind), name, "dram")
        b.local = False
        return b

    def _wait(self, e, tok):
        sem, v = tok
        key = id(sem)
        if self.seen[e].get(key, 0) >= v:
            return
        self.seen[e][key] = v
        self.eng[e].wait_ge(sem, v)

    def _deps(self, e, reads, writes):
        own = id(self.psem[e])
        toks = []
        for r in reads:
            if r.space == "dram":
                continue
            for t in r.writes.values():
                if id(t[0]) == own and e == "pe":
                    continue
                toks.append(t)
        for w in writes:
            if w.space == "dram":
                continue
            for t in list(w.writes.values()) + list(w.reads.values()):
                if id(t[0]) == own:
                    continue
                toks.append(t)
        for t in toks:
            self._wait(e, t)

    def _record(self, tok, reads, writes, partial):
        k = id(tok[0])
        for r in reads:
            if r.space != "dram":
                r.reads[k] = tok
        for w in writes:
            if w.space == "dram":
                continue
            if partial:
                w.writes[k] = tok
            else:
                w.writes = {k: tok}
                w.reads = {}

    def op(self, e, ins, reads=(), writes=(), partial=False):
        self._deps(e, reads, writes)
        i = ins()
        self.cnt[e] += 1
        i.then_inc(self.psem[e], 1)
        tok = (self.psem[e], self.cnt[e])
        self._record(tok, reads, writes, partial)
        return tok

    def dma(self, q, mk, reads=(), writes=(), partial=False, after=()):
        self._deps(q, reads, writes)
        for t in after:
            self._wait(q, t)
        owner = None
        for b in list(writes) + list(reads):
            if b.space != "dram":
                owner = b
                break
        if owner is None:
            owner = (list(writes) + list(reads))[0]
        if owner.sem is None:
            if self.sem_pool:
                owner.sem, owner.total = self.sem_pool.pop()
            else:
                self.nsem += 1
                owner.sem = self.nc.alloc_semaphore("dsem%d" % self.nsem)
                owner.total = 0
            self.live.append(owner)
        i = mk()
        owner.total += 16
        i.then_inc(owner.sem, 16)
        tok = (owner.sem, owner.total)
        self._record(tok, reads, writes, partial)
        return tok

    def barrier(self):
        toks = [(self.psem[k], self.cnt[k]) for k in self.eng if self.cnt[k] > 0]
        toks += [(b.sem, b.total) for b in self.live]
        for e in self.eng:
            for t in toks:
                if id(t[0]) == id(self.psem[e]):
                    continue
                self._wait(e, t)

    def end_phase(self):
        self.barrier()
        keep = []
        for b in self.live:
            if b.local:
                self.sem_pool.append((b.sem, b.total))
                b.sem = None
            else:
                keep.append(b)
        self.live = keep


def build_program(n_layers=DEPTH, debug=(), stop_after=None):
    nc = bass.Bass("TRN2", target_bir_lowering=False)
    S = Sched(nc)
    ES = contextlib.ExitStack
    bc_reg = nc.gpsimd.to_reg(NE * MS - 1)
    L = n_layers

    def din(name, shape, dt=F32):
        return S.dram(name, shape, dt, kind="ExternalInput")

    x_in = din("x", [T, D])
    ctx_in = din("ctx", [CT, D])
    c_in = din("c", [D])
    cc_in = din("c_ctx", [D])
    w_ada = din("w_ada", [L, D, 6 * D])
    b_ada = din("b_ada", [L, 6 * D])
    norm1_g = din("norm1_g", [L, D])
    norm2_g = din("norm2_g", [L, D])
    w_in = din("w_in", [L, D, 2048])
    q_norm_g = din("q_norm_g", [L, 64])
    k_norm_g = din("k_norm_g", [L, 64])
    conv_w = din("conv_w", [L, 31, 256])
    conv_b = din("conv_b", [L, 256])
    conv_ln_g = din("conv_ln_g", [L, 256])
    conv_ln_b = din("conv_ln_b", [L, 256])
    w_out = din("w_out", [L, D, D])
    w_router = din("w_router", [L, D, NE])
    has_moe = stop_after is None or stop_after in ("7", "8")
    if has_moe:
        w_gate = din("w_gate", [L, NE, D, FF])
        w_up = din("w_up", [L, NE, D, FF])
        w_down = din("w_down", [L, NE, FF, D])
    final_g = din("final_norm_g", [D])
    ident_in = din("ident", [128, 128])
    rope_c = din("rope_c", [TT, 64])
    rope_s = din("rope_s", [TT, 64])
    nabias = din("nabias", [L, 8, 64, 4, 512])
    lpad = din("lpad", [NPAD, 2], I32)
    utri_in = din("utri", [128, 128])
    out = S.dram("out", [T, D], F32, kind="ExternalOutput")
    dbg = {}

    X = S.dram("X", [TT, D], F32)
    QT = S.dram("QT", [128, 4, TT], BF16)
    KT = S.dram("KT", [128, TT], BF16)
    VA = S.dram("VA", [TT, 130], BF16)
    QBT = S.dram("QBT", [128, 2, TT], BF16)
    KBT = S.dram("KBT", [128, 2, TT], BF16)
    VB = S.dram("VB", [TT, 256], BF16)
    UT = S.dram("UT", [256, TT], F32)
    AT = S.dram("AT", [512, TT], BF16)
    BT = S.dram("BT", [256, TT], BF16)
    CTs = S.dram("CTs", [256, TT], BF16)
    H2 = S.dram("H2", [TT + NPAD, D], BF16)
    YACC = S.dram("YACC", [TT + NPAD, D], F32)
    LST = S.dram("LST", [NE, MS, 2], I32)

    idf = S.sb([128, 128], F32, "idf")
    idb = S.sb([128, 128], BF16, "idb")
    ones_b = S.sb([128, 128], BF16, "ones_b")
    ones_f = S.sb([128, 128], F32, "ones_f")
    zeros_f = S.sb([128, D], F32, "zeros_f")
    mod_l = S.sb([128, 6 * D], F32, "mod_l")
    mod_c = S.sb([128, 6 * D], F32, "mod_c")
    crep_l = S.sb([128, 8, 128], BF16, "crep_l")
    crep_c = S.sb([128, 8, 128], BF16, "crep_c")
    AFF = S.sb([128, NTT, NE], F32, "AFF")

    def v_(e):
        return {"dve": nc.vector, "pool": nc.gpsimd}[e]

    S.dma("sp", lambda: nc.sync.dma_start(out=idf[:], in_=ident_in[:, :]), reads=[ident_in], writes=[idf])
    S.op("dve", lambda: nc.vector.tensor_copy(out=idb[:], in_=idf[:]), reads=[idf], writes=[idb])
    S.op("dve", lambda: nc.vector.memset(ones_b[:], 1.0), writes=[ones_b])
    S.op("dve", lambda: nc.vector.memset(ones_f[:], 1.0), writes=[ones_f])
    S.op("dve", lambda: nc.vector.memset(zeros_f[:], 0.0), writes=[zeros_f])
    with ES() as st:
        cT = S.sb([128, 2, 8], F32, "cT", st)
        with nc.allow_non_contiguous_dma(reason="tiny"):
            S.dma("sp", lambda: nc.sync.dma_start(out=cT[:, 0, :], in_=c_in.ap().rearrange("(k p) -> p k", p=128)),
                  reads=[c_in], writes=[cT], partial=True)
            S.dma("sp", lambda: nc.sync.dma_start(out=cT[:, 1, :], in_=cc_in.ap().rearrange("(k p) -> p k", p=128)),
                  reads=[cc_in], writes=[cT], partial=True)
        sT = S.sb([128, 2, 8], F32, "sT", st)
        S.op("act", lambda: nc.scalar.activation(out=sT[:], in_=cT[:], func=AF.Silu), reads=[cT], writes=[sT])
        for k in range(8):
            S.op("dve", lambda: nc.vector.tensor_scalar(out=crep_l[:, k, :], in0=ones_b[:], scalar1=sT[:, 0, k:k + 1],
                                                        scalar2=None, op0=ALU.mult),
                 reads=[ones_b, sT], writes=[crep_l], partial=True)
            S.op("dve", lambda: nc.vector.tensor_scalar(out=crep_c[:, k, :], in0=ones_b[:], scalar1=sT[:, 1, k:k + 1],
                                                        scalar2=None, op0=ALU.mult),
                 reads=[ones_b, sT], writes=[crep_c], partial=True)
        zb = S.sb([NPAD, D], BF16, "zb", st)
        S.op("dve", lambda: nc.vector.memset(zb[:], 0.0), writes=[zb])
        S.dma("sp", lambda: nc.sync.dma_start(out=H2[TT:TT + NPAD, :], in_=zb[:]), reads=[zb], writes=[H2])
        lp = S.sb([NPAD, 2], I32, "lp", st)
        S.dma("sp", lambda: nc.sync.dma_start(out=lp[:], in_=lpad[:, :]), reads=[lpad], writes=[lp])
        for e in range(NE):
            S.dma("sp", lambda: nc.sync.dma_start(out=LST[e, CAP + CCAP:MS, :], in_=lp[:]), reads=[lp], writes=[LST])
        xts = [S.sb([128, D], F32, "xcp", st) for _ in range(2)]
        for i in range(NTT):
            xt = xts[i % 2]
            src = x_in[i * 128:(i + 1) * 128, :] if i < NTL else ctx_in[(i - NTL) * 128:(i - NTL + 1) * 128, :]
            S.dma("sp", lambda: nc.sync.dma_start(out=xt[:], in_=src), reads=[x_in], writes=[xt])
            S.dma("sp", lambda: nc.sync.dma_start(out=X[i * 128:(i + 1) * 128, :], in_=xt[:]), reads=[xt], writes=[X])
        S.end_phase()

    for l in range(L):
        last = (l == DEPTH - 1)
        ntq = NTL if last else NTT

        with ES() as st:
            brep = S.sb([128, 6 * D], F32, "brep", st)
            S.dma("sp", lambda: nc.sync.dma_start(out=brep[:], in_=b_ada.ap()[l].partition_broadcast(128)),
                  reads=[b_ada], writes=[brep])
            g1rep = S.sb([128, D], F32, "g1rep", st)
            g2rep = S.sb([128, D], F32, "g2rep", st)
            S.dma("sp", lambda: nc.sync.dma_start(out=g1rep[:], in_=norm1_g.ap()[l].partition_broadcast(128)),
                  reads=[norm1_g], writes=[g1rep])
            S.dma("sp", lambda: nc.sync.dma_start(out=g2rep[:], in_=norm2_g.ap()[l].partition_broadcast(128)),
                  reads=[norm2_g], writes=[g2rep])
            was = [S.sb([128, 8, 512], BF16, "wa", st) for _ in range(2)]
            pms = [S.ps([128, 512], F32, "pm", st) for _ in range(2)]
            for cc in range(12):
                wa = was[cc % 2]
                S.dma("pool", lambda: nc.gpsimd.dma_start(
                    out=wa[:], in_=w_ada.ap()[l][:, cc * 512:(cc + 1) * 512].rearrange("(k p) n -> p k n", p=128)),
                    reads=[w_ada], writes=[wa])
                for which, (crep, mod) in enumerate(((crep_l, mod_l), (crep_c, mod_c))):
                    pm = pms[which]
                    for k in range(8):
                        S.op("pe", lambda: nc.tensor.matmul(pm[:], lhsT=crep[:, k, :], rhs=wa[:, k, :],
                                                            start=(k == 0), stop=(k == 7)),
                             reads=[crep, wa], writes=[pm], partial=(k > 0))
                    S.op("dve", lambda: nc.vector.tensor_tensor(out=mod[:, cc * 512:(cc + 1) * 512], in0=pm[:],
                                                                in1=brep[:, cc * 512:(cc + 1) * 512], op=ALU.add),
                         reads=[pm, brep], writes=[mod], partial=True)
            for mod in (mod_l, mod_c):
                S.op("dve", lambda: nc.vector.scalar_tensor_tensor(out=mod[:, D:2 * D], in0=mod[:, D:2 * D], scalar=1.0,
                                                                   in1=g1rep[:], op0=ALU.add, op1=ALU.mult),
                     reads=[mod, g1rep], writes=[mod], partial=True)
                S.op("dve", lambda: nc.vector.scalar_tensor_tensor(out=mod[:, 4 * D:5 * D], in0=mod[:, 4 * D:5 * D],
                                                                   scalar=1.0, in1=g2rep[:], op0=ALU.add, op1=ALU.mult),
                     reads=[mod, g2rep], writes=[mod], partial=True)
            S.end_phase()
        if "mod" in debug and l == 0:
            dbg["mod"] = S.dram("dbg_mod", [128, 6 * D], F32, kind="ExternalOutput")
            S.dma("sp", lambda: nc.sync.dma_start(out=dbg["mod"][:, :], in_=mod_l[:]), reads=[mod_l], writes=[dbg["mod"]])
            S.barrier()
        if stop_after == "M":
            break

        with ES() as st:
            win = S.sb([128, 8, 2048], BF16, "win", st)
            for h in range(4):
                S.dma("pool", lambda: nc.gpsimd.dma_start(
                    out=win[:, :, h * 512:(h + 1) * 512],
                    in_=w_in.ap()[l][:, h * 512:(h + 1) * 512].rearrange("(k p) n -> p k n", p=128)),
                    reads=[w_in], writes=[win], partial=True)
            gq = S.sb([128, 64], F32, "gq", st)
            gk = S.sb([128, 64], F32, "gk", st)
            S.dma("sp", lambda: nc.sync.dma_start(out=gq[:], in_=q_norm_g.ap()[l].partition_broadcast(128)),
                  reads=[q_norm_g], writes=[gq])
            S.dma("sp", lambda: nc.sync.dma_start(out=gk[:], in_=k_norm_g.ap()[l].partition_broadcast(128)),
                  reads=[k_norm_g], writes=[gk])
            S.op("dve", lambda: nc.vector.tensor_scalar(out=gq[:], in0=gq[:], scalar1=0.125, scalar2=None, op0=ALU.mult),
                 reads=[gq], writes=[gq])
            xts = [S.sb([128, D], F32, "xt", st) for _ in range(2)]
            sq = S.sb([128, D], F32, "sq", st)
            ss = S.sb([128, 1], F32, "ss", st)
            rstd = S.sb([128, 1], F32, "rstd", st)
            hf = S.sb([128, D], F32, "hf", st)
            hb = S.sb([128, D], BF16, "hb", st)
            hT = S.sb([128, 8, 512], BF16, "hT", st)
            rc = [S.sb([128, 64], F32, "rc", st) for _ in range(2)]
            rs_ = [S.sb([128, 64], F32, "rs", st) for _ in range(2)]
            qsq = S.sb([128, 640], F32, "qsq", st)
            qss = S.sb([128, 10], F32, "qss", st)
            qn = S.sb([128, 640], F32, "qn", st)
            t1 = S.sb([128, 640], F32, "t1", st)
            t2 = S.sb([128, 640], F32, "t2", st)
            qr = S.sb([128, 640], BF16, "qr", st)
            qbk = S.sb([128, 512], BF16, "qbk", st)
            qTs = S.sb([128, 4, 512], BF16, "qTs", st)
            kTs = S.sb([128, 512], BF16, "kTs", st)
            vas = S.sb([128, 4, 130], BF16, "vas", st)
            qbTs = S.sb([128, 2, 512], BF16, "qbTs", st)
            kbTs = S.sb([128, 2, 512], BF16, "kbTs", st)
            vbs = S.sb([128, 4, 256], BF16, "vbs", st)
            sg = S.sb([128, 512], F32, "sg", st)
            uTs = S.sb([128, 2, 512], F32, "uTs", st)
            pT = S.ps([128, 8, 128], BF16, "pT", st)
            pmm = [S.ps([128, 512], F32, "pmm", st) for _ in range(3)]
            pq = S.ps([128, 4, 128], BF16, "pq", st)
            pk = S.ps([128, 5, 128], BF16, "pk", st)
            pf = [S.ps([128, 512], F32, "pf", st) for _ in range(2)]
            S.op("dve", lambda: nc.vector.memset(vas[:], 1.0), writes=[vas])
            A1 = lambda mod: mod[:, D:2 * D]
            S1 = lambda mod: mod[:, 0:D]

            groups = [(g * 512, 512, mod_l) for g in range(T // 512)] + [(T, CT, mod_c)]
            for (t0, G, mod) in groups:
                nsub = G // 128
                for s in range(nsub):
                    r0 = t0 + s * 128
                    xt = xts[s % 2]
                    S.dma("sp", lambda: nc.sync.dma_start(out=xt[:], in_=X[r0:r0 + 128, :]), reads=[X], writes=[xt])
                    S.op("act", lambda: nc.scalar.activation(out=sq[:], in_=xt[:], func=AF.Square, accum_out=ss[:]),
                         reads=[xt], writes=[sq, ss])
                    S.op("dve", lambda: nc.vector.tensor_scalar(out=rstd[:], in0=ss[:], scalar1=1.0 / D, scalar2=EPS,
                                                                op0=ALU.mult, op1=ALU.add), reads=[ss], writes=[rstd])
                    S.op("act", lambda: nc.scalar.activation(out=rstd[:], in_=rstd[:], func=AF.Sqrt), reads=[rstd], writes=[rstd])
                    S.op("dve", lambda: nc.vector.reciprocal(out=rstd[:], in_=rstd[:]), reads=[rstd], writes=[rstd])
                    S.op("dve", lambda: nc.vector.scalar_tensor_tensor(out=hf[:], in0=xt[:], scalar=rstd[:, 0:1], in1=A1(mod),
                                                                       op0=ALU.mult, op1=ALU.mult),
                         reads=[xt, rstd, mod], writes=[hf])
                    S.op("pool", lambda: nc.gpsimd.tensor_tensor(out=hb[:], in0=hf[:], in1=S1(mod), op=ALU.add),
                         reads=[hf, mod], writes=[hb])
                    for k in range(8):
                        S.op("pe", lambda: nc.tensor.transpose(out=pT[:, k, :], in_=hb[:, k * 128:(k + 1) * 128], identity=idb[:]),
                             reads=[hb, idb], writes=[pT], partial=(k > 0))
                    S.op("act", lambda: nc.scalar.copy(out=hT[:, :, s * 128:(s + 1) * 128], in_=pT[:]),
                         reads=[pT], writes=[hT], partial=True)
                    for cg_ in range(3):
                        pm = pmm[cg_]
                        for k in range(8):
                            S.op("pe", lambda: nc.tensor.matmul(pm[:], lhsT=hT[:, k, s * 128:(s + 1) * 128],
                                                                rhs=win[:, k, cg_ * 512:(cg_ + 1) * 512],
                                                                start=(k == 0), stop=(k == 7)),
                                 reads=[hT, win], writes=[pm], partial=(k > 0))
                    rcb, rsb = rc[s % 2], rs_[s % 2]
                    S.dma("sp", lambda: nc.sync.dma_start(out=rcb[:], in_=rope_c[r0:r0 + 128, :]), reads=[rope_c], writes=[rcb])
                    S.dma("sp", lambda: nc.sync.dma_start(out=rsb[:], in_=rope_s[r0:r0 + 128, :]), reads=[rope_s], writes=[rsb])
                    S.op("act", lambda: nc.scalar.activation(out=qsq[:, 0:512], in_=pmm[0][:], func=AF.Square),
                         reads=[pmm[0]], writes=[qsq], partial=True)
                    S.op("act", lambda: nc.scalar.activation(out=qsq[:, 512:640], in_=pmm[1][:, 0:128], func=AF.Square),
                         reads=[pmm[1]], writes=[qsq], partial=True)
                    S.op("dve", lambda: nc.vector.tensor_reduce(out=qss[:], in_=qsq[:].rearrange("p (h d) -> p h d", d=64),
                                                                axis=AX.X, op=ALU.add), reads=[qsq], writes=[qss])
                    S.op("dve", lambda: nc.vector.tensor_scalar(out=qss[:], in0=qss[:], scalar1=1.0 / 64, scalar2=EPS,
                                                                op0=ALU.mult, op1=ALU.add), reads=[qss], writes=[qss])
                    S.op("act", lambda: nc.scalar.activation(out=qss[:], in_=qss[:], func=AF.Sqrt), reads=[qss], writes=[qss])
                    S.op("dve", lambda: nc.vector.reciprocal(out=qss[:], in_=qss[:]), reads=[qss], writes=[qss])
                    S.op("dve", lambda: nc.vector.tensor_tensor(
                        out=qn[:, 0:512].rearrange("p (h d) -> p h d", d=64), in0=pmm[0][:].rearrange("p (h d) -> p h d", d=64),
                        in1=qss[:, 0:8].unsqueeze(2).to_broadcast([128, 8, 64]), op=ALU.mult),
                        reads=[pmm[0], qss], writes=[qn], partial=True)
                    S.op("dve", lambda: nc.vector.tensor_tensor(
                        out=qn[:, 512:640].rearrange("p (h d) -> p h d", d=64),
                        in0=pmm[1][:, 0:128].rearrange("p (h d) -> p h d", d=64),
                        in1=qss[:, 8:10].unsqueeze(2).to_broadcast([128, 2, 64]), op=ALU.mult),
                        reads=[pmm[1], qss], writes=[qn], partial=True)
                    S.op("pool", lambda: nc.gpsimd.tensor_tensor(
                        out=qn[:, 0:512].rearrange("p (h d) -> p h d", d=64), in0=qn[:, 0:512].rearrange("p (h d) -> p h d", d=64),
                        in1=gq[:].unsqueeze(1).to_broadcast([128, 8, 64]), op=ALU.mult), reads=[qn, gq], writes=[qn], partial=True)
                    S.op("pool", lambda: nc.gpsimd.tensor_tensor(
                        out=qn[:, 512:640].rearrange("p (h d) -> p h d", d=64),
                        in0=qn[:, 512:640].rearrange("p (h d) -> p h d", d=64),
                        in1=gk[:].unsqueeze(1).to_broadcast([128, 2, 64]), op=ALU.mult), reads=[qn, gk], writes=[qn], partial=True)
                    S.op("dve", lambda: nc.vector.tensor_tensor(
                        out=t1[:].rearrange("p (h d) -> p h d", d=64), in0=qn[:].rearrange("p (h d) -> p h d", d=64),
                        in1=rcb[:].unsqueeze(1).to_broadcast([128, 10, 64]), op=ALU.mult), reads=[qn, rcb], writes=[t1])
                    qv = qn[:].rearrange("p (h a b c) -> p h a b c", h=10, a=2, b=2)
                    tv = t2[:].rearrange("p (h a b c) -> p h a b c", h=10, a=2, b=2)
                    sv = rsb[:].rearrange("p (a b c) -> p a b c", a=2, b=2)
                    for a in range(2):
                        for b_ in range(2):
                            S.op("pool", lambda: nc.gpsimd.tensor_tensor(
                                out=tv[:, :, a, b_, :], in0=qv[:, :, a, 1 - b_, :],
                                in1=sv[:, a, b_, :].unsqueeze(1).to_broadcast([128, 10, 16]), op=ALU.mult),
                                reads=[qn, rsb], writes=[t2], partial=True)
                    S.op("dve", lambda: nc.vector.tensor_tensor(
                        out=qr[:, 0:512].rearrange("p (g k d) -> p k g d", g=4, k=2),
                        in0=t1[:, 0:512].rearrange("p (k g d) -> p k g d", k=2, g=4),
                        in1=t2[:, 0:512].rearrange("p (k g d) -> p k g d", k=2, g=4), op=ALU.add),
                        reads=[t1, t2], writes=[qr])
                    S.op("dve", lambda: nc.vector.tensor_tensor(out=qr[:, 512:640], in0=t1[:, 512:640], in1=t2[:, 512:640],
                                                                op=ALU.add), reads=[t1, t2], writes=[qr], partial=True)
                    for g in range(4):
                        S.op("pe", lambda: nc.tensor.transpose(
                            out=pq[:, g, :], in_=qr[:, g * 128:(g + 1) * 128],
                            identity=idb[:]), reads=[qr, idb], writes=[pq], partial=(g > 0))
                    S.op("act", lambda: nc.scalar.copy(out=qTs[:, :, s * 128:(s + 1) * 128], in_=pq[:]),
                         reads=[pq], writes=[qTs], partial=True)
                    S.op("pe", lambda: nc.tensor.transpose(out=pk[:, 0, :], in_=qr[:, 512:640], identity=idb[:]),
                         reads=[qr, idb], writes=[pk], partial=False)
                    S.op("act", lambda: nc.scalar.copy(out=vas[:, s, :].rearrange("p (k e) -> p k e", e=65)[:, :, 0:64],
                                                       in_=pmm[1][:, 128:256].rearrange("p (k d) -> p k d", d=64)),
                         reads=[pmm[1]], writes=[vas], partial=True)
                    S.op("dve", lambda: nc.vector.tensor_scalar(out=qbk[:, 0:256], in0=pmm[1][:, 256:512], scalar1=0.125,
                                                                scalar2=None, op0=ALU.mult),
                         reads=[pmm[1]], writes=[qbk], partial=True)
                    S.op("act", lambda: nc.scalar.copy(out=qbk[:, 256:512], in_=pmm[2][:, 0:256]),
                         reads=[pmm[2]], writes=[qbk], partial=True)
                    S.op("act", lambda: nc.scalar.copy(out=vbs[:, s, :], in_=pmm[2][:, 256:512]),
                         reads=[pmm[2]], writes=[vbs], partial=True)
                    for j in range(4):
                        S.op("pe", lambda: nc.tensor.transpose(out=pk[:, 1 + j, :], in_=qbk[:, j * 128:(j + 1) * 128],
                                                               identity=idb[:]), reads=[qbk, idb], writes=[pk], partial=True)
                    S.op("dve", lambda: nc.vector.tensor_copy(out=kTs[:, s * 128:(s + 1) * 128], in_=pk[:, 0, :]),
                         reads=[pk], writes=[kTs], partial=True)
                    S.op("dve", lambda: nc.vector.tensor_copy(out=qbTs[:, :, s * 128:(s + 1) * 128], in_=pk[:, 1:3, :]),
                         reads=[pk], writes=[qbTs], partial=True)
                    S.op("dve", lambda: nc.vector.tensor_copy(out=kbTs[:, :, s * 128:(s + 1) * 128], in_=pk[:, 3:5, :]),
                         reads=[pk], writes=[kbTs], partial=True)
                for j in range(2):
                    for which in range(2):
                        c0 = 1536 + which * 256 + j * 128
                        for k in range(8):
                            S.op("pe", lambda: nc.tensor.matmul(pf[which][:, 0:G], lhsT=win[:, k, c0:c0 + 128], rhs=hT[:, k, 0:G],
                                                                start=(k == 0), stop=(k == 7)),
                                 reads=[win, hT], writes=[pf[which]], partial=(k > 0))
                    S.op("act", lambda: nc.scalar.activation(out=sg[:, 0:G], in_=pf[1][:, 0:G], func=AF.Sigmoid),
                         reads=[pf[1]], writes=[sg])
                    S.op("dve", lambda: nc.vector.tensor_tensor(out=uTs[:, j, 0:G], in0=pf[0][:, 0:G], in1=sg[:, 0:G], op=ALU.mult),
                         reads=[pf[0], sg], writes=[uTs], partial=True)
                S.dma("sp", lambda: nc.sync.dma_start(out=QT[:, :, t0:t0 + G], in_=qTs[:, :, 0:G]), reads=[qTs], writes=[QT])
                S.dma("sp", lambda: nc.sync.dma_start(out=KT[:, t0:t0 + G], in_=kTs[:, 0:G]), reads=[kTs], writes=[KT])
                S.dma("sp", lambda: nc.sync.dma_start(out=VA.ap()[t0:t0 + G, :].rearrange("(s p) e -> p s e", p=128),
                                                      in_=vas[:, 0:nsub, :]), reads=[vas], writes=[VA])
                S.dma("sp", lambda: nc.sync.dma_start(out=QBT[:, :, t0:t0 + G], in_=qbTs[:, :, 0:G]), reads=[qbTs], writes=[QBT])
                S.dma("sp", lambda: nc.sync.dma_start(out=KBT[:, :, t0:t0 + G], in_=kbTs[:, :, 0:G]), reads=[kbTs], writes=[KBT])
                S.dma("sp", lambda: nc.sync.dma_start(out=VB.ap()[t0:t0 + G, :].rearrange("(s p) e -> p s e", p=128),
                                                      in_=vbs[:, 0:nsub, :]), reads=[vbs], writes=[VB])
                S.dma("sp", lambda: nc.sync.dma_start(out=UT.ap()[:, t0:t0 + G].rearrange("(j p) t -> p j t", p=128),
                                                      in_=uTs[:, :, 0:G]), reads=[uTs], writes=[UT])
            S.end_phase()
        if stop_after == "1":
            break

        with ES() as st:
            ksb = S.sb([128, TT], BF16, "ksb", st)
            vsb = S.sb([128, NTT, 130], BF16, "vsb", st)
            S.dma("sp", lambda: nc.sync.dma_start(out=ksb[:], in_=KT[:, :]), reads=[KT], writes=[ksb])
            S.dma("sp", lambda: nc.sync.dma_start(out=vsb[:], in_=VA.ap().rearrange("(n p) e -> p n e", p=128)),
                  reads=[VA], writes=[vsb])
            gqk = S.sb([128, 2, 64], F32, "gqk", st)
            S.dma("sp", lambda: nc.sync.dma_start(out=gqk[:, 0, :], in_=q_norm_g.ap()[l].partition_broadcast(128)),
                  reads=[q_norm_g], writes=[gqk], partial=True)
            S.dma("sp", lambda: nc.sync.dma_start(out=gqk[:, 1, :], in_=k_norm_g.ap()[l].partition_broadcast(128)),
                  reads=[k_norm_g], writes=[gqk], partial=True)
            gmx = S.sb([128, 2], F32, "gmx", st)
            nb = S.sb([128, 1], F32, "nb", st)
            gng = S.sb([128, 2, 64], F32, "gng", st)
            S.op("dve", lambda: nc.vector.tensor_scalar(out=gng[:], in0=gqk[:], scalar1=-1.0, scalar2=None, op0=ALU.mult),
                 reads=[gqk], writes=[gng])
            S.op("dve", lambda: nc.vector.tensor_tensor(out=gqk[:], in0=gqk[:], in1=gng[:], op=ALU.max),
                 reads=[gqk, gng], writes=[gqk])
            S.op("dve", lambda: nc.vector.tensor_reduce(out=gmx[:], in_=gqk[:], axis=AX.X, op=ALU.max), reads=[gqk], writes=[gmx])
            S.op("dve", lambda: nc.vector.tensor_tensor(out=nb[:], in0=gmx[:, 0:1], in1=gmx[:, 1:2], op=ALU.mult),
                 reads=[gmx], writes=[nb])
            S.op("dve", lambda: nc.vector.tensor_scalar(out=nb[:], in0=nb[:], scalar1=-8.0, scalar2=None, op0=ALU.mult),
                 reads=[nb], writes=[nb])
            qsbs = [S.sb([128, 512], BF16, "qsb", st) for _ in range(2)]
            pbs = [S.sb([128, 512], BF16, "pb", st) for _ in range(3)]
            rsa = S.sb([128, 4], F32, "rsa", st)
            osb = [S.sb([128, 512], BF16, "osb", st) for _ in range(2)]
            aTs = [S.sb([128, 4, 128], BF16, "aTs", st) for _ in range(2)]
            pss = [S.ps([128, 512], F32, "pss", st) for _ in range(3)]
            pos = [S.ps([128, 512], F32, "po", st) for _ in range(4)]
            pa = S.ps([128, 4, 128], BF16, "pa", st)
            it = 0
            for qi in range(ntq):
                q0 = qi * 128
                qsb = qsbs[qi % 2]
                S.dma("sp", lambda: nc.sync.dma_start(out=qsb[:].rearrange("p (g t) -> p g t", g=4), in_=QT[:, :, q0:q0 + 128]),
                      reads=[QT], writes=[qsb])
                ktiles = list(range(NTT)) if qi < NTL else [NTL, NTL + 1]
                ob = osb[qi % 2]
                for kh in range(2):
                    for idx, kt in enumerate(ktiles):
                        ps = pss[it % 3]
                        pb = pbs[it % 3]
                        it += 1
                        S.op("pe", lambda: nc.tensor.matmul(ps[:], lhsT=ksb[kh * 64:(kh + 1) * 64, kt * 128:(kt + 1) * 128],
                                                            rhs=qsb[kh * 64:(kh + 1) * 64, :], start=True, stop=True),
                             reads=[ksb, qsb], writes=[ps])
                        S.op("act", lambda: nc.scalar.activation(out=pb[:], in_=ps[:], func=AF.Exp, bias=nb[:, 0:1], scale=1.0),
                             reads=[ps, nb], writes=[pb])
                        for g in range(4):
                            S.op("pe", lambda: nc.tensor.matmul(pos[g][:, 0:65], lhsT=pb[:, g * 128:(g + 1) * 128],
                                                                rhs=vsb[:, kt, kh * 65:(kh + 1) * 65],
                                                                start=(idx == 0), stop=(idx == len(ktiles) - 1)),
                                 reads=[pb, vsb], writes=[pos[g]], partial=(idx > 0))
                    for g in range(4):
                        S.op("dve", lambda: nc.vector.reciprocal(out=rsa[:, g:g + 1], in_=pos[g][:, 64:65]),
                             reads=[pos[g]], writes=[rsa], partial=True)
                        S.op("dve", lambda: nc.vector.tensor_scalar(
                            out=ob[:, kh * 256 + g * 64:kh * 256 + (g + 1) * 64], in0=pos[g][:, 0:64], scalar1=rsa[:, g:g + 1],
                            scalar2=None, op0=ALU.mult), reads=[pos[g], rsa], writes=[ob], partial=True)
                for j in range(4):
                    S.op("pe", lambda: nc.tensor.transpose(out=pa[:, j, :], in_=ob[:, j * 128:(j + 1) * 128], identity=idb[:]),
                         reads=[ob, idb], writes=[pa], partial=(j > 0))
                aT = aTs[qi % 2]
                S.op("dve", lambda: nc.vector.tensor_copy(out=aT[:], in_=pa[:]), reads=[pa], writes=[aT])
                S.dma("sp", lambda: nc.sync.dma_start(out=AT.ap()[:, q0:q0 + 128].rearrange("(j p) t -> p j t", p=128), in_=aT[:]),
                      reads=[aT], writes=[AT])
            S.end_phase()
        if stop_after == "2":
            break

        with ES() as st:
            kc = S.sb([128, 2, 256], BF16, "kc", st)
            vc = S.sb([128, 2, 256], BF16, "vc", st)
            S.dma("sp", lambda: nc.sync.dma_start(out=kc[:], in_=KBT[:, :, T:TT]), reads=[KBT], writes=[kc])
            S.dma("sp", lambda: nc.sync.dma_start(out=vc[:], in_=VB.ap()[T:TT, :].rearrange("(n p) e -> p n e", p=128)),
                  reads=[VB], writes=[vc])
            bint = S.sb([64, 4, 512], F32, "bint", st)
            bedge = S.sb([64, 4, 512], F32, "bedge", st)
            S.dma("sp", lambda: nc.sync.dma_start(out=bint[:], in_=nabias[l, 3]), reads=[nabias], writes=[bint])
            qrows = [S.sb([128, 2, 64], BF16, "qrow", st) for _ in range(2)]
            kwins = [S.sb([128, 2, 512], BF16, "kwin", st) for _ in range(2)]
            vwins = [S.sb([64, 8, 256], BF16, "vwin", st) for _ in range(2)]
            ssb = S.sb([64, 768], F32, "ssb", st)
            mx = S.sb([64, 1], F32, "mx", st)
            sm = S.sb([64, 1], F32, "sm", st)
            pexp = S.sb([64, 768], BF16, "pexp", st)
            ptw_s = S.sb([64, 8, 64], BF16, "ptw_s", st)
            ptc_s = S.sb([128, 2, 64], BF16, "ptc_s", st)
            brow = S.sb([64, 256], BF16, "brow", st)
            bts = [S.sb([128, 2, 64], BF16, "bts", st) for _ in range(2)]
            psn = [S.ps([64, 1024], F32, "psn", st) for _ in range(2)]
            ptw = S.ps([64, 8, 64], BF16, "ptw", st)
            ptc = S.ps([128, 2, 64], BF16, "ptc", st)
            pon = S.ps([64, 64], F32, "pon", st)
            pbt = S.ps([128, 2, 64], BF16, "pbt", st)
            it = 0
            for r in range(T // 64):
                rs0 = min(max(r - 4, 0), 120)
                off = rs0 - r + 7
                if off == 3:
                    bias = bint
                else:
                    bias = bedge
                    S.dma("sp", lambda: nc.sync.dma_start(out=bedge[:], in_=nabias[l, off]), reads=[nabias], writes=[bedge])
                qrow, kwin, vwin = qrows[r % 2], kwins[r % 2], vwins[r % 2]
                S.dma("sp", lambda: nc.sync.dma_start(out=qrow[:], in_=QBT[:, :, r * 64:(r + 1) * 64]), reads=[QBT], writes=[qrow])
                S.dma("sp", lambda: nc.sync.dma_start(out=kwin[:], in_=KBT[:, :, rs0 * 64:(rs0 + 8) * 64]), reads=[KBT], writes=[kwin])
                S.dma("sp", lambda: nc.sync.dma_start(out=vwin[:], in_=VB.ap()[rs0 * 64:(rs0 + 8) * 64, :].rearrange("(j p) e -> p j e", p=64)),
                      reads=[VB], writes=[vwin])
                for hb in range(4):
                    hp, pr = hb // 2, (hb % 2) * 64
                    ps = psn[it % 2]
                    it += 1
                    S.op("pe", lambda: nc.tensor.matmul(ps[:, 0:512], lhsT=qrow[pr:pr + 64, hp, :], rhs=kwin[pr:pr + 64, hp, :],
                                                        start=True, stop=True), reads=[qrow, kwin], writes=[ps])
                    S.op("pe", lambda: nc.tensor.matmul(ps[:, 512:768], lhsT=qrow[pr:pr + 64, hp, :], rhs=kc[pr:pr + 64, hp, :],
                                                        start=True, stop=True), reads=[qrow, kc], writes=[ps], partial=True)
                    S.op("dve", lambda: nc.vector.tensor_tensor(out=ssb[:, 0:512], in0=ps[:, 0:512], in1=bias[:, hb, :], op=ALU.add),
                         reads=[ps, bias], writes=[ssb])
                    S.op("act", lambda: nc.scalar.copy(out=ssb[:, 512:768], in_=ps[:, 512:768]), reads=[ps], writes=[ssb], partial=True)
                    S.op("dve", lambda: nc.vector.tensor_reduce(out=mx[:], in_=ssb[:], axis=AX.X, op=ALU.max), reads=[ssb], writes=[mx])
                    S.op("dve", lambda: nc.vector.tensor_scalar(out=mx[:], in0=mx[:], scalar1=-1.0, scalar2=None, op0=ALU.mult),
                         reads=[mx], writes=[mx])
                    S.op("act", lambda: nc.scalar.activation(out=pexp[:], in_=ssb[:], func=AF.Exp, bias=mx[:, 0:1], scale=1.0,
                                                             accum_out=sm[:]), reads=[ssb, mx], writes=[pexp, sm])
                    for j in range(8):
                        S.op("pe", lambda: nc.tensor.transpose(out=ptw[:, j, :], in_=pexp[:, j * 64:(j + 1) * 64], identity=idb[0:64, 0:64]),
                             reads=[pexp, idb], writes=[ptw], partial=(j > 0))
                    for j in range(2):
                        S.op("pe", lambda: nc.tensor.transpose(out=ptc[:, j, :], in_=pexp[:, 512 + j * 128:512 + (j + 1) * 128],
                                                               identity=idb[0:64, 0:64]), reads=[pexp, idb], writes=[ptc], partial=(j > 0))
                    S.op("dve", lambda: nc.vector.tensor_copy(out=ptw_s[:], in_=ptw[:]), reads=[ptw], writes=[ptw_s])
                    S.op("act", lambda: nc.scalar.copy(out=ptc_s[:], in_=ptc[:]), reads=[ptc], writes=[ptc_s])
                    for j in range(8):
                        S.op("pe", lambda: nc.tensor.matmul(pon[:], lhsT=ptw_s[:, j, :], rhs=vwin[:, j, hb * 64:(hb + 1) * 64],
                                                            start=(j == 0), stop=False), reads=[ptw_s, vwin], writes=[pon], partial=(j > 0))
                    for j in range(2):
                        S.op("pe", lambda: nc.tensor.matmul(pon[:], lhsT=ptc_s[:, j, :], rhs=vc[:, j, hb * 64:(hb + 1) * 64],
                                                            start=False, stop=(j == 1)), reads=[ptc_s, vc], writes=[pon], partial=True)
                    S.op("dve", lambda: nc.vector.reciprocal(out=sm[:], in_=sm[:]), reads=[sm], writes=[sm])
                    S.op("dve", lambda: nc.vector.tensor_scalar(out=brow[:, hb * 64:(hb + 1) * 64], in0=pon[:], scalar1=sm[:, 0:1],
                                                                scalar2=None, op0=ALU.mult), reads=[pon, sm], writes=[brow], partial=True)
                for j in range(2):
                    S.op("pe", lambda: nc.tensor.transpose(out=pbt[:, j, :], in_=brow[:, j * 128:(j + 1) * 128], identity=idb[0:64, 0:64]),
                         reads=[brow, idb], writes=[pbt], partial=(j > 0))
                bt = bts[r % 2]
                S.op("act", lambda: nc.scalar.copy(out=bt[:], in_=pbt[:]), reads=[pbt], writes=[bt])
                S.dma("sp", lambda: nc.sync.dma_start(out=BT.ap()[:, r * 64:(r + 1) * 64].rearrange("(j p) t -> p j t", p=128), in_=bt[:]),
                      reads=[bt], writes=[BT])
            S.end_phase()
        if not last:
            with ES() as st:
                kc = S.sb([128, 2, 256], BF16, "kc", st)
                vc = S.sb([128, 2, 256], BF16, "vc", st)
                S.dma("sp", lambda: nc.sync.dma_start(out=kc[:], in_=KBT[:, :, T:TT]), reads=[KBT], writes=[kc])
                S.dma("sp", lambda: nc.sync.dma_start(out=vc[:], in_=VB.ap()[T:TT, :].rearrange("(n p) e -> p n e", p=128)),
                      reads=[VB], writes=[vc])
                qc = S.sb([128, 2, 128], BF16, "qc", st)
                sc_ = S.sb([128, 256], F32, "sc", st)
                mxc = S.sb([128, 1], F32, "mxc", st)
                smc = S.sb([128, 1], F32, "smc", st)
                pxc = S.sb([128, 256], BF16, "pxc", st)
                ptcs = S.sb([128, 2, 128], BF16, "ptcs", st)
                browc = S.sb([128, 256], BF16, "browc", st)
                btc = S.sb([128, 2, 128], BF16, "btc", st)
                psc = S.ps([128, 256], F32, "psc", st)
                ptcp = S.ps([128, 2, 128], BF16, "ptcp", st)
                poc = S.ps([128, 64], F32, "poc", st)
                pbc = S.ps([128, 2, 128], BF16, "pbc", st)
                for ci in range(2):
                    c0 = T + ci * 128
                    S.dma("sp", lambda: nc.sync.dma_start(out=qc[:], in_=QBT[:, :, c0:c0 + 128]), reads=[QBT], writes=[qc])
                    for hb in range(4):
                        hp, pr = hb // 2, (hb % 2) * 64
                        S.op("pe", lambda: nc.tensor.matmul(psc[:], lhsT=qc[pr:pr + 64, hp, :], rhs=kc[pr:pr + 64, hp, :],
                                                            start=True, stop=True), reads=[qc, kc], writes=[psc])
                        S.op("act", lambda: nc.scalar.copy(out=sc_[:], in_=psc[:]), reads=[psc], writes=[sc_])
                        S.op("dve", lambda: nc.vector.tensor_reduce(out=mxc[:], in_=sc_[:], axis=AX.X, op=ALU.max), reads=[sc_], writes=[mxc])
                        S.op("dve", lambda: nc.vector.tensor_scalar(out=mxc[:], in0=mxc[:], scalar1=-1.0, scalar2=None, op0=ALU.mult),
                             reads=[mxc], writes=[mxc])
                        S.op("act", lambda: nc.scalar.activation(out=pxc[:], in_=sc_[:], func=AF.Exp, bias=mxc[:, 0:1], scale=1.0,
                                                                 accum_out=smc[:]), reads=[sc_, mxc], writes=[pxc, smc])
                        for j in range(2):
                            S.op("pe", lambda: nc.tensor.transpose(out=ptcp[:, j, :], in_=pxc[:, j * 128:(j + 1) * 128], identity=idb[:]),
                                 reads=[pxc, idb], writes=[ptcp], partial=(j > 0))
                        S.op("dve", lambda: nc.vector.tensor_copy(out=ptcs[:], in_=ptcp[:]), reads=[ptcp], writes=[ptcs])
                        for j in range(2):
                            S.op("pe", lambda: nc.tensor.matmul(poc[:], lhsT=ptcs[:, j, :], rhs=vc[:, j, hb * 64:(hb + 1) * 64],
                                                                start=(j == 0), stop=(j == 1)), reads=[ptcs, vc], writes=[poc], partial=(j > 0))
                        S.op("dve", lambda: nc.vector.reciprocal(out=smc[:], in_=smc[:]), reads=[smc], writes=[smc])
                        S.op("dve", lambda: nc.vector.tensor_scalar(out=browc[:, hb * 64:(hb + 1) * 64], in0=poc[:], scalar1=smc[:, 0:1],
                                                                    scalar2=None, op0=ALU.mult), reads=[poc, smc], writes=[browc], partial=True)
                    for j in range(2):
                        S.op("pe", lambda: nc.tensor.transpose(out=pbc[:, j, :], in_=browc[:, j * 128:(j + 1) * 128], identity=idb[:]),
                             reads=[browc, idb], writes=[pbc], partial=(j > 0))
                    S.op("act", lambda: nc.scalar.copy(out=btc[:], in_=pbc[:]), reads=[pbc], writes=[btc])
                    S.dma("sp", lambda: nc.sync.dma_start(out=BT.ap()[:, c0:c0 + 128].rearrange("(j p) t -> p j t", p=128), in_=btc[:]),
                          reads=[btc], writes=[BT])
                S.end_phase()
        if stop_after == "3":
            break

        seqs = [(0, T)] + ([] if last else [(T, CT)])
        for (t0, Ls) in seqs:
            with ES() as st:
                Y = S.sb([128, 2, Ls], F32, "Y", st)
                up = S.sb([128, Ls + 30], F32, "up", st)
                cw = S.sb([128, 2, 31], F32, "cw", st)
                cb = S.sb([128, 2], F32, "cb", st)
                lng = S.sb([128, 2], F32, "lng", st)
                lnb = S.sb([128, 2], F32, "lnb", st)
                ones_s = S.sb([128, 128], F32, "ones_s", st)
                S.op("dve", lambda: nc.vector.memset(ones_s[:], 1.0 / 256), writes=[ones_s])
                with nc.allow_non_contiguous_dma(reason="tiny"):
                    for j in range(2):
                        S.dma("sp", lambda: nc.sync.dma_start(out=cw[:, j, :], in_=conv_w.ap()[l][:, j * 128:(j + 1) * 128].rearrange("w c -> c w")),
                              reads=[conv_w], writes=[cw], partial=True)
                    for (dst, src) in ((cb, conv_b), (lng, conv_ln_g), (lnb, conv_ln_b)):
                        S.dma("sp", lambda: nc.sync.dma_start(out=dst[:], in_=src.ap()[l].rearrange("(j p) -> p j", p=128)),
                              reads=[src], writes=[dst])
                S.op("dve", lambda: nc.vector.memset(up[:, 0:15], 0.0), writes=[up], partial=True)
                S.op("dve", lambda: nc.vector.memset(up[:, Ls + 15:Ls + 30], 0.0), writes=[up], partial=True)
                for j in range(2):
                    S.dma("sp", lambda: nc.sync.dma_start(out=up[:, 15:15 + Ls], in_=UT[j * 128:(j + 1) * 128, t0:t0 + Ls]),
                          reads=[UT], writes=[up], partial=True)
                    S.op("dve", lambda: nc.vector.tensor_scalar(out=Y[:, j, :], in0=up[:, 0:Ls], scalar1=cw[:, j, 0:1], scalar2=cb[:, j:j + 1],
                                                                op0=ALU.mult, op1=ALU.add), reads=[up, cw, cb], writes=[Y], partial=True)
                    for w in range(1, 31):
                        S.op("dve", lambda: nc.vector.scalar_tensor_tensor(out=Y[:, j, :], in0=up[:, w:w + Ls], scalar=cw[:, j, w:w + 1],
                                                                           in1=Y[:, j, :], op0=ALU.mult, op1=ALU.add),
                             reads=[up, cw, Y], writes=[Y], partial=True)
                BL = min(512, Ls)
                ysq = S.sb([128, 2, BL], F32, "ysq", st)
                mean = S.sb([128, BL], F32, "mean", st)
                var = S.sb([128, BL], F32, "var", st)
                tmpc = S.sb([128, 2, BL], F32, "tmpc", st)
                ctsb = [S.sb([128, 2, BL], BF16, "ctsb", st) for _ in range(2)]
                pmn = S.ps([128, BL], F32, "pmn", st)
                pe2 = S.ps([128, BL], F32, "pe2", st)
                for bi in range(Ls // BL):
                    b0 = bi * BL
                    S.op("act", lambda: nc.scalar.activation(out=ysq[:], in_=Y[:, :, b0:b0 + BL], func=AF.Square), reads=[Y], writes=[ysq])
                    for j in range(2):
                        S.op("pe", lambda: nc.tensor.matmul(pmn[:], lhsT=ones_s[:], rhs=Y[:, j, b0:b0 + BL], start=(j == 0), stop=(j == 1)),
                             reads=[ones_s, Y], writes=[pmn], partial=(j > 0))
                    for j in range(2):
                        S.op("pe", lambda: nc.tensor.matmul(pe2[:], lhsT=ones_s[:], rhs=ysq[:, j, :], start=(j == 0), stop=(j == 1)),
                             reads=[ones_s, ysq], writes=[pe2], partial=(j > 0))
                    S.op("act", lambda: nc.scalar.copy(out=mean[:], in_=pmn[:]), reads=[pmn], writes=[mean])
                    S.op("dve", lambda: nc.vector.tensor_tensor(out=var[:], in0=mean[:], in1=mean[:], op=ALU.mult), reads=[mean], writes=[var])
                    S.op("dve", lambda: nc.vector.tensor_tensor(out=var[:], in0=pe2[:], in1=var[:], op=ALU.subtract), reads=[pe2, var], writes=[var])
                    S.op("dve", lambda: nc.vector.tensor_scalar(out=var[:], in0=var[:], scalar1=EPS, scalar2=None, op0=ALU.add),
                         reads=[var], writes=[var])
                    S.op("act", lambda: nc.scalar.activation(out=var[:], in_=var[:], func=AF.Sqrt), reads=[var], writes=[var])
                    S.op("dve", lambda: nc.vector.reciprocal(out=var[:], in_=var[:]), reads=[var], writes=[var])
                    cts_ = ctsb[bi % 2]
                    for j in range(2):
                        S.op("dve", lambda: nc.vector.tensor_tensor(out=tmpc[:, j, :], in0=Y[:, j, b0:b0 + BL], in1=mean[:], op=ALU.subtract),
                             reads=[Y, mean], writes=[tmpc], partial=True)
                        S.op("dve", lambda: nc.vector.tensor_tensor(out=tmpc[:, j, :], in0=tmpc[:, j, :], in1=var[:], op=ALU.mult),
                             reads=[tmpc, var], writes=[tmpc], partial=True)
                        S.op("act", lambda: nc.scalar.activation(out=cts_[:, j, :], in_=tmpc[:, j, :], func=AF.Silu,
                                                                 bias=lnb[:, j:j + 1], scale=lng[:, j:j + 1]),
                             reads=[tmpc, lnb, lng], writes=[cts_], partial=True)
                    S.dma("sp", lambda: nc.sync.dma_start(out=CTs.ap()[:, t0 + b0:t0 + b0 + BL].rearrange("(j p) t -> p j t", p=128), in_=cts_[:]),
                          reads=[cts_], writes=[CTs])
                S.end_phase()
        if stop_after == "4":
            break

        with ES() as st:
            wo = S.sb([128, 8, D], BF16, "wo", st)
            for h in range(2):
                S.dma("pool", lambda: nc.gpsimd.dma_start(out=wo[:, :, h * 512:(h + 1) * 512],
                                                          in_=w_out.ap()[l][:, h * 512:(h + 1) * 512].rearrange("(k p) n -> p k n", p=128)),
                      reads=[w_out], writes=[wo], partial=True)
            wr = S.sb([128, 8, NE], F32, "wr", st)
            S.dma("sp", lambda: nc.sync.dma_start(out=wr[:], in_=w_router.ap()[l].rearrange("(k p) e -> p k e", p=128)),
                  reads=[w_router], writes=[wr])
            cats = [S.sb([128, 8, 128], BF16, "cat", st) for _ in range(2)]
            xts = [S.sb([128, D], F32, "xt5", st) for _ in range(2)]
            tmp5 = S.sb([128, D], F32, "tmp5", st)
            x1s = [S.sb([128, D], F32, "x1", st) for _ in range(2)]
            sq5 = S.sb([128, D], F32, "sq5", st)
            ss5 = S.sb([128, 1], F32, "ss5", st)
            rstd5 = S.sb([128, 1], F32, "rstd5", st)
            h2f = S.sb([128, D], F32, "h2f", st)
            h2b = [S.sb([128, D], BF16, "h2b", st) for _ in range(2)]
            h2T = S.sb([128, 8, 128], F32, "h2T", st)
            lg = S.sb([128, NE], F32, "lg", st)
            mx5 = S.sb([128, 1], F32, "mx5", st)
            se5 = S.sb([128, 1], F32, "se5", st)
            ps5 = S.ps([128, D], F32, "ps5", st)
            pt5 = S.ps([128, 8, 128], F32, "pt5", st)
            pl5 = S.ps([128, NE], F32, "pl5", st)
            for i in range(NTT + 1):
                r0 = i * 128
                nr = 128 if i < NTT else NPAD
                S.dma("sp", lambda: nc.sync.dma_start(out=YACC[r0:r0 + nr, :], in_=zeros_f[0:nr, :]), reads=[zeros_f], writes=[YACC])
            for ti in range(ntq):
                r0 = ti * 128
                mod = mod_l if ti < NTL else mod_c
                cat, xt, x1, hb2 = cats[ti % 2], xts[ti % 2], x1s[ti % 2], h2b[ti % 2]
                S.dma("sp", lambda: nc.sync.dma_start(out=cat[:, 0:4, :], in_=AT.ap()[:, r0:r0 + 128].rearrange("(j p) t -> p j t", p=128)),
                      reads=[AT], writes=[cat], partial=True)
                S.dma("sp", lambda: nc.sync.dma_start(out=cat[:, 4:6, :], in_=BT.ap()[:, r0:r0 + 128].rearrange("(j p) t -> p j t", p=128)),
                      reads=[BT], writes=[cat], partial=True)
                S.dma("sp", lambda: nc.sync.dma_start(out=cat[:, 6:8, :], in_=CTs.ap()[:, r0:r0 + 128].rearrange("(j p) t -> p j t", p=128)),
                      reads=[CTs], writes=[cat], partial=True)
                S.dma("sp", lambda: nc.sync.dma_start(out=xt[:], in_=X[r0:r0 + 128, :]), reads=[X], writes=[xt])
                for hh in range(2):
                    for k in range(8):
                        S.op("pe", lambda: nc.tensor.matmul(ps5[:, hh * 512:(hh + 1) * 512], lhsT=cat[:, k, :], rhs=wo[:, k, hh * 512:(hh + 1) * 512],
                                                            start=(k == 0), stop=(k == 7)), reads=[cat, wo], writes=[ps5],
                             partial=not (hh == 0 and k == 0))
                S.op("dve", lambda: nc.vector.tensor_tensor(out=tmp5[:], in0=ps5[:], in1=mod[:, 2 * D:3 * D], op=ALU.mult),
                     reads=[ps5, mod], writes=[tmp5])
                S.op("pool", lambda: nc.gpsimd.tensor_tensor(out=x1[:], in0=tmp5[:], in1=xt[:], op=ALU.add), reads=[tmp5, xt], writes=[x1])
                S.dma("sp", lambda: nc.sync.dma_start(out=X[r0:r0 + 128, :], in_=x1[:]), reads=[x1], writes=[X])
                S.op("act", lambda: nc.scalar.activation(out=sq5[:], in_=x1[:], func=AF.Square, accum_out=ss5[:]), reads=[x1], writes=[sq5, ss5])
                S.op("dve", lambda: nc.vector.tensor_scalar(out=rstd5[:], in0=ss5[:], scalar1=1.0 / D, scalar2=EPS, op0=ALU.mult, op1=ALU.add),
                     reads=[ss5], writes=[rstd5])
                S.op("act", lambda: nc.scalar.activation(out=rstd5[:], in_=rstd5[:], func=AF.Sqrt), reads=[rstd5], writes=[rstd5])
                S.op("dve", lambda: nc.vector.reciprocal(out=rstd5[:], in_=rstd5[:]), reads=[rstd5], writes=[rstd5])
                S.op("dve", lambda: nc.vector.scalar_tensor_tensor(out=h2f[:], in0=x1[:], scalar=rstd5[:, 0:1], in1=mod[:, 4 * D:5 * D],
                                                                   op0=ALU.mult, op1=ALU.mult), reads=[x1, rstd5, mod], writes=[h2f])
                S.op("pool", lambda: nc.gpsimd.tensor_tensor(out=h2f[:], in0=h2f[:], in1=mod[:, 3 * D:4 * D], op=ALU.add),
                     reads=[h2f, mod], writes=[h2f])
                S.op("act", lambda: nc.scalar.copy(out=hb2[:], in_=h2f[:]), reads=[h2f], writes=[hb2])
                S.dma("sp", lambda: nc.sync.dma_start(out=H2[r0:r0 + 128, :], in_=hb2[:]), reads=[hb2], writes=[H2])
                for k in range(8):
                    S.op("pe", lambda: nc.tensor.transpose(out=pt5[:, k, :], in_=h2f[:, k * 128:(k + 1) * 128], identity=idf[:]),
                         reads=[h2f, idf], writes=[pt5], partial=(k > 0))
                S.op("dve", lambda: nc.vector.tensor_copy(out=h2T[:], in_=pt5[:]), reads=[pt5], writes=[h2T])
                for k in range(8):
                    S.op("pe", lambda: nc.tensor.matmul(pl5[:], lhsT=h2T[:, k, :], rhs=wr[:, k, :], start=(k == 0), stop=(k == 7)),
                         reads=[h2T, wr], writes=[pl5], partial=(k > 0))
                S.op("dve", lambda: nc.vector.tensor_reduce(out=mx5[:], in_=pl5[:], axis=AX.X, op=ALU.max), reads=[pl5], writes=[mx5])
                S.op("dve", lambda: nc.vector.tensor_scalar(out=mx5[:], in0=mx5[:], scalar1=-1.0, scalar2=None, op0=ALU.mult),
                     reads=[mx5], writes=[mx5])
                S.op("act", lambda: nc.scalar.activation(out=lg[:], in_=pl5[:], func=AF.Exp, bias=mx5[:, 0:1], scale=1.0, accum_out=se5[:]),
                     reads=[pl5, mx5], writes=[lg, se5])
                S.op("dve", lambda: nc.vector.reciprocal(out=se5[:], in_=se5[:]), reads=[se5], writes=[se5])
                S.op("dve", lambda: nc.vector.tensor_scalar(out=AFF[:, ti, :], in0=lg[:], scalar1=se5[:, 0:1], scalar2=None, op0=ALU.mult),
                     reads=[lg, se5], writes=[AFF], partial=True)
            S.end_phase()
        if stop_after == "5":
            break

        nrt = ntq
        with ES() as st:
            utb = S.sb([128, 128], BF16, "utb", st)
            utf = S.sb([128, 128], F32, "utf", st)
            S.dma("sp", lambda: nc.sync.dma_start(out=utf[:], in_=utri_in[:, :]), reads=[utri_in], writes=[utf])
            S.op("dve", lambda: nc.vector.tensor_copy(out=utb[:], in_=utf[:]), reads=[utf], writes=[utb])
            lo = S.sb([128, 32], F32, "lo", st)
            hi = S.sb([128, 32], F32, "hi", st)
            mid = S.sb([128, 32], F32, "mid", st)
            tgt = S.sb([128, 32], F32, "tgt", st)
            ge = S.sb([128, 32], F32, "ge", st)
            d1 = S.sb([128, 32], F32, "d1", st)
            cmpb = S.sb([128, NTT, NE], F32, "cmpb", st)
            cntb = S.sb([128, 32], F32, "cntb", st)
            pc = S.ps([128, 32], F32, "pc", st)
            S.op("dve", lambda: nc.vector.memset(lo[:], 0.0), writes=[lo])
            S.op("dve", lambda: nc.vector.memset(hi[:], 1.0), writes=[hi])
            S.op("dve", lambda: nc.vector.memset(tgt[:, 0:16], float(CAP)), writes=[tgt], partial=True)
            S.op("dve", lambda: nc.vector.memset(tgt[:, 16:32], float(CCAP)), writes=[tgt], partial=True)
            S.op("dve", lambda: nc.vector.memset(cntb[:], 0.0), writes=[cntb])
            parts = [(0, NTL, 0)] + ([] if last else [(NTL, NTT, 16)])

            def compare(dst, thr):
                for (a, b_, c0) in parts:
                    S.op("dve", lambda: nc.vector.tensor_tensor(
                        out=dst[:, a:b_, :], in0=AFF[:, a:b_, :],
                        in1=thr[:, c0:c0 + 16].unsqueeze(1).to_broadcast([128, b_ - a, NE]), op=ALU.is_ge),
                        reads=[AFF, thr], writes=[dst], partial=True)

            for itn in range(40):
                S.op("dve", lambda: nc.vector.tensor_tensor(out=mid[:], in0=lo[:], in1=hi[:], op=ALU.add), reads=[lo, hi], writes=[mid])
                S.op("dve", lambda: nc.vector.tensor_scalar(out=mid[:], in0=mid[:], scalar1=0.5, scalar2=None, op0=ALU.mult),
                     reads=[mid], writes=[mid])
                compare(cmpb, mid)
                for (a, b_, c0) in parts:
                    S.op("dve", lambda: nc.vector.tensor_reduce(out=cntb[:, c0:c0 + 16], in_=cmpb[:, a:b_, :].rearrange("p t e -> p e t"),
                                                                axis=AX.X, op=ALU.add), reads=[cmpb], writes=[cntb], partial=True)
                S.op("pe", lambda: nc.tensor.matmul(pc[:], lhsT=ones_f[:], rhs=cntb[:], start=True, stop=True), reads=[ones_f, cntb], writes=[pc])
                S.op("dve", lambda: nc.vector.tensor_tensor(out=ge[:], in0=pc[:], in1=tgt[:], op=ALU.is_ge), reads=[pc, tgt], writes=[ge])
                S.op("dve", lambda: nc.vector.tensor_tensor(out=d1[:], in0=mid[:], in1=lo[:], op=ALU.subtract), reads=[mid, lo], writes=[d1])
                S.op("dve", lambda: nc.vector.tensor_tensor(out=d1[:], in0=d1[:], in1=ge[:], op=ALU.mult), reads=[d1, ge], writes=[d1])
                S.op("dve", lambda: nc.vector.tensor_tensor(out=lo[:], in0=lo[:], in1=d1[:], op=ALU.add), reads=[lo, d1], writes=[lo])
                S.op("dve", lambda: nc.vector.tensor_tensor(out=d1[:], in0=hi[:], in1=mid[:], op=ALU.subtract), reads=[hi, mid], writes=[d1])
                S.op("dve", lambda: nc.vector.tensor_tensor(out=d1[:], in0=d1[:], in1=ge[:], op=ALU.mult), reads=[d1, ge], writes=[d1])
                S.op("dve", lambda: nc.vector.tensor_tensor(out=hi[:], in0=mid[:], in1=d1[:], op=ALU.add), reads=[mid, d1], writes=[hi])
            maskb = S.sb([128, NTT, NE], BF16, "maskb", st)
            pref = S.sb([128, NTT, NE], F32, "pref", st)
            tcnt = S.sb([128, NTT, NE], F32, "tcnt", st)
            tcn2 = S.sb([128, NTT, NE], F32, "tcn2", st)
            S.op("dve", lambda: nc.vector.memset(cmpb[:], 0.0), writes=[cmpb])
            compare(cmpb, lo)
            S.op("dve", lambda: nc.vector.tensor_copy(out=maskb[:], in_=cmpb[:]), reads=[cmpb], writes=[maskb])
            ncol = NTT * NE
            mflat = maskb[:].rearrange("p t e -> p (t e)")
            pflat = pref[:].rearrange("p t e -> p (t e)")
            tflat = tcnt[:].rearrange("p t e -> p (t e)")
            pp = [S.ps([128, 512], F32, "pp", st) for _ in range(2)]
            for ci, c0 in enumerate(range(0, ncol, 512)):
                n = min(512, ncol - c0)
                S.op("pe", lambda: nc.tensor.matmul(pp[0][:, 0:n], lhsT=utb[:], rhs=mflat[:, c0:c0 + n], start=True, stop=True),
                     reads=[utb, maskb], writes=[pp[0]])
                S.op("pe", lambda: nc.tensor.matmul(pp[1][:, 0:n], lhsT=ones_b[:], rhs=mflat[:, c0:c0 + n], start=True, stop=True),
                     reads=[ones_b, maskb], writes=[pp[1]])
                S.op("dve", lambda: nc.vector.tensor_copy(out=pflat[:, c0:c0 + n], in_=pp[0][:, 0:n]), reads=[pp[0]], writes=[pref], partial=True)
                S.op("act", lambda: nc.scalar.copy(out=tflat[:, c0:c0 + n], in_=pp[1][:, 0:n]), reads=[pp[1]], writes=[tcnt], partial=True)
            src, dst = tcnt, tcn2
            S.op("dve", lambda: nc.vector.tensor_copy(out=tcn2[:], in_=tcnt[:]), reads=[tcnt], writes=[tcn2])
            cum = S.sb([128, NTT, NE], F32, "cum", st)
            S.op("dve", lambda: nc.vector.tensor_copy(out=cum[:], in_=tcnt[:]), reads=[tcnt], writes=[cum])
            a_, b2 = cum, tcn2
            sft = 1
            while sft < NTL:
                S.op("dve", lambda: nc.vector.tensor_tensor(out=b2[:, sft:NTL, :], in0=a_[:, sft:NTL, :], in1=a_[:, 0:NTL - sft, :], op=ALU.add),
                     reads=[a_], writes=[b2], partial=True)
                S.op("dve", lambda: nc.vector.tensor_copy(out=b2[:, 0:sft, :], in_=a_[:, 0:sft, :]), reads=[a_], writes=[b2], partial=True)
                a_, b2 = b2, a_
                sft *= 2
            inc = a_
            slot = S.sb([128, NTT, NE], F32, "slot", st)
            S.op("dve", lambda: nc.vector.tensor_tensor(out=slot[:, 0:NTL, :], in0=inc[:, 0:NTL, :], in1=tcnt[:, 0:NTL, :], op=ALU.subtract),
                 reads=[inc, tcnt], writes=[slot], partial=True)
            if not last:
                S.op("dve", lambda: nc.vector.memset(slot[:, NTL, :], float(CAP)), writes=[slot], partial=True)
                S.op("dve", lambda: nc.vector.tensor_scalar(out=slot[:, NTL + 1, :], in0=tcnt[:, NTL, :], scalar1=float(CAP), scalar2=None,
                                                            op0=ALU.add), reads=[tcnt], writes=[slot], partial=True)
            S.op("dve", lambda: nc.vector.tensor_tensor(out=slot[:, 0:nrt, :], in0=slot[:, 0:nrt, :], in1=pref[:, 0:nrt, :], op=ALU.add),
                 reads=[slot, pref], writes=[slot], partial=True)
            ebase = S.sb([128, NE], F32, "ebase", st)
            for e in range(NE):
                S.op("dve", lambda: nc.vector.memset(ebase[:, e:e + 1], float(e * MS)), writes=[ebase], partial=True)
            S.op("dve", lambda: nc.vector.tensor_tensor(out=slot[:, 0:nrt, :], in0=slot[:, 0:nrt, :],
                                                        in1=ebase[:].unsqueeze(1).to_broadcast([128, nrt, NE]), op=ALU.add),
                 reads=[slot, ebase], writes=[slot], partial=True)
            BIGI = float(1 << 20)
            S.op("dve", lambda: nc.vector.scalar_tensor_tensor(out=slot[:, 0:nrt, :].rearrange("p t e -> p (t e)"),
                                                               in0=slot[:, 0:nrt, :].rearrange("p t e -> p (t e)"), scalar=-BIGI,
                                                               in1=cmpb[:, 0:nrt, :].rearrange("p t e -> p (t e)"), op0=ALU.add, op1=ALU.mult),
                 reads=[slot, cmpb], writes=[slot], partial=True)
            S.op("dve", lambda: nc.vector.tensor_scalar(out=slot[:, 0:nrt, :], in0=slot[:, 0:nrt, :], scalar1=BIGI, scalar2=None, op0=ALU.add),
                 reads=[slot], writes=[slot], partial=True)
            idxi = S.sb([128, NTT, NE], I32, "idxi", st)
            S.op("dve", lambda: nc.vector.tensor_copy(out=idxi[:, 0:nrt, :], in_=slot[:, 0:nrt, :]), reads=[slot], writes=[idxi])
            tid = S.sb([128, NTT], I32, "tid", st)
            S.op("pool", lambda: nc.gpsimd.iota(tid[:], pattern=[[128, NTT]], base=0, channel_multiplier=1), writes=[tid])
            pay = S.sb([128, NTT, NE, 2], I32, "pay", st)
            S.op("dve", lambda: nc.vector.tensor_copy(out=pay[:, :, :, 0], in_=tid[:].unsqueeze(2).to_broadcast([128, NTT, NE])),
                 reads=[tid], writes=[pay], partial=True)
            S.op("dve", lambda: nc.vector.tensor_copy(out=pay[:].bitcast(F32)[:, :, :, 1], in_=AFF[:]), reads=[AFF], writes=[pay], partial=True)
            lflat = LST.ap().rearrange("e s c -> (e s) c")
            for ti in range(nrt):
                for e in range(NE):
                    S.dma("pool", lambda: nc.gpsimd.indirect_dma_start(
                        out=lflat, out_offset=bass.IndirectOffsetOnAxis(ap=idxi[:, ti, e:e + 1], axis=0),
                        in_=pay[:, ti, e, :], in_offset=None, bounds_check=bc_reg, oob_is_err=False),
                        reads=[pay, idxi], writes=[LST])
            if "route" in debug and l == 0:
                dbg["thr"] = S.dram("dbg_thr", [128, 32], F32, kind="ExternalOutput")
                S.dma("sp", lambda: nc.sync.dma_start(out=dbg["thr"][:, :], in_=lo[:]), reads=[lo], writes=[dbg["thr"]])
                dbg["idx"] = S.dram("dbg_idx", [128, NTT, NE], I32, kind="ExternalOutput")
                S.dma("sp", lambda: nc.sync.dma_start(out=dbg["idx"].ap(), in_=idxi[:]), reads=[idxi], writes=[dbg["idx"]])
            S.end_phase()
        if stop_after == "6":
            break

        with ES() as st:
            wg = S.sb([128, 8, FF], BF16, "wg", st)
            wu = S.sb([128, 8, FF], BF16, "wu", st)
            wd = S.sb([128, 16, D], BF16, "wd", st)
            xeT = S.sb([128, 8, 512], BF16, "xeT", st)
            hTe = S.sb([128, 16, 512], BF16, "hTe", st)
            idts = [S.sb([128, 2], I32, "idt", st) for _ in range(4)]
            xgs = [S.sb([128, D], BF16, "xg", st) for _ in range(2)]
            ysbs = [S.sb([128, D], F32, "ysb", st) for _ in range(2)]
            sgl = [S.sb([128, 512], F32, "sgl", st) for _ in range(2)]
            ptx = S.ps([128, 8, 128], BF16, "ptx", st)
            pgs = [S.ps([128, 512], F32, "pg", st) for _ in range(2)]
            pus = [S.ps([128, 512], F32, "pu", st) for _ in range(2)]
            pys = [S.ps([128, 512], F32, "py", st) for _ in range(2)]
            groups = [(0, 512), (512, 512)] + ([] if last else [(1024, 128)])
            prev_sc = []
            nfc = 0
            nys = 0
            for e in range(NE):
                for h in range(4):
                    S.dma("pool", lambda: nc.gpsimd.dma_start(out=wg[:, :, h * 512:(h + 1) * 512],
                                                              in_=w_gate.ap()[l, e][:, h * 512:(h + 1) * 512].rearrange("(k p) f -> p k f", p=128)),
                          reads=[w_gate], writes=[wg], partial=(h > 0))
                for h in range(4):
                    S.dma("pool", lambda: nc.gpsimd.dma_start(out=wu[:, :, h * 512:(h + 1) * 512],
                                                              in_=w_up.ap()[l, e][:, h * 512:(h + 1) * 512].rearrange("(k p) f -> p k f", p=128)),
                          reads=[w_up], writes=[wu], partial=(h > 0))
                for h in range(2):
                    S.dma("pool", lambda: nc.gpsimd.dma_start(out=wd[:, :, h * 512:(h + 1) * 512],
                                                              in_=w_down.ap()[l, e][:, h * 512:(h + 1) * 512].rearrange("(k p) n -> p k n", p=128)),
                          reads=[w_down], writes=[wd], partial=(h > 0))
                cur_sc = []
                for (s0, N) in groups:
                    nst = N // 128
                    for si in range(nst):
                        idt = idts[si]
                        xg = xgs[si % 2]
                        S.dma("sp", lambda: nc.sync.dma_start(out=idt[:], in_=LST[e, s0 + si * 128:s0 + (si + 1) * 128, :]),
                              reads=[LST], writes=[idt])
                        S.dma("pool", lambda: nc.gpsimd.indirect_dma_start(
                            out=xg[:], out_offset=None, in_=H2.ap(),
                            in_offset=bass.IndirectOffsetOnAxis(ap=idt[:, 0:1], axis=0)), reads=[H2, idt], writes=[xg])
                        for k in range(8):
                            S.op("pe", lambda: nc.tensor.transpose(out=ptx[:, k, :], in_=xg[:, k * 128:(k + 1) * 128], identity=idb[:]),
                                 reads=[xg, idb], writes=[ptx], partial=(k > 0))
                        S.op("dve", lambda: nc.vector.tensor_copy(out=xeT[:, :, si * 128:(si + 1) * 128], in_=ptx[:]),
                             reads=[ptx], writes=[xeT], partial=True)
                    for fc in range(16):
                        pg, pu, sg_ = pgs[nfc % 2], pus[nfc % 2], sgl[nfc % 2]
                        nfc += 1
                        for k in range(8):
                            S.op("pe", lambda: nc.tensor.matmul(pg[:, 0:N], lhsT=wg[:, k, fc * 128:(fc + 1) * 128], rhs=xeT[:, k, 0:N],
                                                                start=(k == 0), stop=(k == 7)), reads=[wg, xeT], writes=[pg], partial=(k > 0))
                        for k in range(8):
                            S.op("pe", lambda: nc.tensor.matmul(pu[:, 0:N], lhsT=wu[:, k, fc * 128:(fc + 1) * 128], rhs=xeT[:, k, 0:N],
                                                                start=(k == 0), stop=(k == 7)), reads=[wu, xeT], writes=[pu], partial=(k > 0))
                        S.op("act", lambda: nc.scalar.activation(out=sg_[:, 0:N], in_=pg[:, 0:N], func=AF.Silu), reads=[pg], writes=[sg_])
                        S.op("dve", lambda: nc.vector.tensor_tensor(out=hTe[:, fc, 0:N], in0=sg_[:, 0:N], in1=pu[:, 0:N], op=ALU.mult),
                             reads=[sg_, pu], writes=[hTe], partial=True)
                    for si in range(nst):
                        idt = idts[si]
                        ysb = ysbs[nys % 2]
                        nys += 1
                        for dh in range(2):
                            py = pys[dh]
                            for fc in range(16):
                                S.op("pe", lambda: nc.tensor.matmul(py[:], lhsT=hTe[:, fc, si * 128:(si + 1) * 128], rhs=wd[:, fc, dh * 512:(dh + 1) * 512],
                                                                    start=(fc == 0), stop=(fc == 15)), reads=[hTe, wd], writes=[py], partial=(fc > 0))
                            if dh == 0:
                                S.op("dve", lambda: nc.vector.tensor_scalar(out=ysb[:, 0:512], in0=py[:], scalar1=idt[:].bitcast(F32)[:, 1:2],
                                                                            scalar2=None, op0=ALU.mult), reads=[py, idt], writes=[ysb], partial=True)
                            else:
                                S.op("act", lambda: nc.scalar.activation(out=ysb[:, 512:1024], in_=py[:], func=AF.Copy,
                                                                         scale=idt[:].bitcast(F32)[:, 1:2]), reads=[py, idt], writes=[ysb], partial=True)
                        tok = S.dma("pool", lambda: nc.gpsimd.indirect_dma_start(
                            out=YACC.ap(), out_offset=bass.IndirectOffsetOnAxis(ap=idt[:, 0:1], axis=0),
                            in_=ysb[:], in_offset=None, compute_op=ALU.add), reads=[ysb, idt], writes=[YACC], after=prev_sc)
                        cur_sc.append(tok)
                prev_sc = cur_sc[-2:]
            S.end_phase()
        if stop_after == "7":
            break

        with ES() as st:
            xt8 = [S.sb([128, D], F32, "xt8", st) for _ in range(2)]
            yt8 = [S.sb([128, D], F32, "yt8", st) for _ in range(2)]
            xo8 = [S.sb([128, D], F32, "xo8", st) for _ in range(2)]
            sq8 = S.sb([128, D], F32, "sq8", st)
            ss8 = S.sb([128, 1], F32, "ss8", st)
            fg = S.sb([128, D], F32, "fg", st)
            if last:
                S.dma("sp", lambda: nc.sync.dma_start(out=fg[:], in_=final_g.ap().partition_broadcast(128)), reads=[final_g], writes=[fg])
            for ti in range(ntq):
                r0 = ti * 128
                mod = mod_l if ti < NTL else mod_c
                xt, yt, xo = xt8[ti % 2], yt8[ti % 2], xo8[ti % 2]
                S.dma("sp", lambda: nc.sync.dma_start(out=xt[:], in_=X[r0:r0 + 128, :]), reads=[X], writes=[xt])
                S.dma("sp", lambda: nc.sync.dma_start(out=yt[:], in_=YACC[r0:r0 + 128, :]), reads=[YACC], writes=[yt])
                S.op("dve", lambda: nc.vector.tensor_tensor(out=yt[:], in0=yt[:], in1=mod[:, 5 * D:6 * D], op=ALU.mult), reads=[yt, mod], writes=[yt])
                S.op("pool", lambda: nc.gpsimd.tensor_tensor(out=xo[:], in0=yt[:], in1=xt[:], op=ALU.add), reads=[yt, xt], writes=[xo])
                if not last:
                    S.dma("sp", lambda: nc.sync.dma_start(out=X[r0:r0 + 128, :], in_=xo[:]), reads=[xo], writes=[X])
                else:
                    S.op("act", lambda: nc.scalar.activation(out=sq8[:], in_=xo[:], func=AF.Square, accum_out=ss8[:]), reads=[xo], writes=[sq8, ss8])
                    S.op("dve", lambda: nc.vector.tensor_scalar(out=ss8[:], in0=ss8[:], scalar1=1.0 / D, scalar2=EPS, op0=ALU.mult, op1=ALU.add),
                         reads=[ss8], writes=[ss8])
                    S.op("act", lambda: nc.scalar.activation(out=ss8[:], in_=ss8[:], func=AF.Sqrt), reads=[ss8], writes=[ss8])
                    S.op("dve", lambda: nc.vector.reciprocal(out=ss8[:], in_=ss8[:]), reads=[ss8], writes=[ss8])
                    S.op("dve", lambda: nc.vector.scalar_tensor_tensor(out=xt[:], in0=xo[:], scalar=ss8[:, 0:1], in1=fg[:], op0=ALU.mult, op1=ALU.mult),
                         reads=[xo, ss8, fg], writes=[xt])
                    S.dma("sp", lambda: nc.sync.dma_start(out=out[r0:r0 + 128, :], in_=xt[:]), reads=[xt], writes=[out])
            S.end_phase()
        if stop_after == "8":
            break

    def tap(name, buf, shape, dt):
        o = S.dram("dbg_" + name, shape, dt, kind="ExternalOutput")
        S.dma("sp", lambda: nc.sync.dma_start(out=o.ap(), in_=buf.ap()), reads=[buf], writes=[o])

    for name in debug:
        if name == "QT":
            tap("QT", QT, [128, 4, TT], BF16)
        if name == "KT":
            tap("KT", KT, [128, TT], BF16)
        if name == "VA":
            tap("VA", VA, [TT, 130], BF16)
        if name == "QBT":
            tap("QBT", QBT, [128, 2, TT], BF16)
        if name == "KBT":
            tap("KBT", KBT, [128, 2, TT], BF16)
        if name == "VB":
            tap("VB", VB, [TT, 256], BF16)
        if name == "UT":
            tap("UT", UT, [256, TT], F32)
        if name == "X":
            tap("X", X, [TT, D], F32)
        if name == "AT":
            tap("AT", AT, [512, TT], BF16)
        if name == "BT":
            tap("BT", BT, [256, TT], BF16)
        if name == "CTs":
            tap("CTs", CTs, [256, TT], BF16)
        if name == "H2":
            tap("H2", H2, [TT + NPAD, D], BF16)
        if name == "LST":
            tap("LST", LST, [NE, MS, 2], I32)
        if name == "YACC":
            tap("YACC", YACC, [TT + NPAD, D], F32)
        if name == "AFF":
            o_ = S.dram("dbg_AFF", [128, NTT, NE], F32, kind="ExternalOutput")
            S.dma("sp", lambda: nc.sync.dma_start(out=o_.ap(), in_=AFF[:]), reads=[AFF], writes=[o_])
    S.barrier()
    return nc


def host_consts(na_rpb, n_layers):
    t = np.arange(T)
    row = (t // 64).astype(np.float64)
    col = (t % 64).astype(np.float64)
    inv = 10000.0 ** (-np.arange(16, dtype=np.float64) / 16)
    rc = np.ones((TT, 64), np.float32)
    rs = np.zeros((TT, 64), np.float32)
    for a, pos in enumerate((row, col)):
        ang = (pos.astype(np.float32)[:, None] * inv.astype(np.float32)[None, :]).astype(np.float32)
        cs, sn = np.cos(ang), np.sin(ang)
        rc[:T, a * 32:a * 32 + 16] = cs
        rc[:T, a * 32 + 16:a * 32 + 32] = cs
        rs[:T, a * 32:a * 32 + 16] = -sn
        rs[:T, a * 32 + 16:a * 32 + 32] = sn
    q = np.arange(64)
    cs0 = np.clip(q - 8, 0, 48)
    c = np.arange(64)
    valid = (c[None, :] >= cs0[:, None]) & (c[None, :] < cs0[:, None] + 16)
    dc = np.clip(c[None, :] - q[:, None] + 15, 0, 30)
    nab = np.full((n_layers, 8, 64, 4, 8, 64), NEG, np.float32)
    for off in range(8):
        for j in range(8):
            g = na_rpb[:n_layers, :, off + j, :][:, :, dc]
            g = np.where(valid[None, None], g, np.float32(NEG))
            nab[:, off, :, :, j, :] = np.transpose(g, (0, 2, 1, 3))
    lpad = np.zeros((NPAD, 2), np.int32)
    lpad[:, 0] = TT + np.arange(NPAD)
    return {"ident": np.eye(128, dtype=np.float32), "utri": np.triu(np.ones((128, 128), np.float32), 1), "rope_c": rc, "rope_s": rs,
            "nabias": nab.reshape(n_layers, 8, 64, 4, 512), "lpad": lpad}


_WNAMES = ["w_ada", "b_ada", "norm1_g", "norm2_g", "w_in", "q_norm_g", "k_norm_g", "conv_w", "conv_b",
           "conv_ln_g", "conv_ln_b", "w_out", "w_router", "w_gate", "w_up", "w_down"]


def make_in_maps(inputs, n_layers, samples):
    consts = host_consts(np.asarray(inputs["na_rpb"]), n_layers)
    maps = []
    for b in samples:
        m = {"x": np.ascontiguousarray(inputs["x"][b]), "ctx": np.ascontiguousarray(inputs["ctx"][b]),
             "c": np.ascontiguousarray(inputs["c"][b]), "c_ctx": np.ascontiguousarray(inputs["c_ctx"]),
             "final_norm_g": np.ascontiguousarray(inputs["final_norm_g"])}
        for k in _WNAMES:
            m[k] = np.ascontiguousarray(inputs[k][:n_layers])
        m.update(consts)
        maps.append(m)
    return maps


def kernel(**inputs):
    inputs = {k: np.asarray(v) for k, v in inputs.items()}
    nc = build_program(DEPTH)
    maps = make_in_maps(inputs, DEPTH, range(4))
    res = run_bass_kernel_spmd(nc, maps, core_ids=list(range(4)))
    return np.stack([r["out"] for r in res.results], 0).astype(np.float32)
```

```python
import contextlib
import numpy as np
import concourse.bass as bass
import concourse.mybir as mybir
from concourse.bass_utils import run_bass_kernel_spmd

F32 = mybir.dt.float32
BF16 = mybir.dt.bfloat16
I32 = mybir.dt.int32
AF = mybir.ActivationFunctionType
ALU = mybir.AluOpType
AX = mybir.AxisListType

D = 1024
T = 8192
CT = 256
TT = T + CT
NTL = T // 128
NTT = TT // 128
DEPTH = 4
NE = 16
FF = 2048
CAP = 1024
CCAP = 32
NPAD = 96
MS = CAP + CCAP + NPAD
EPS = 1e-6
NEG = -30000.0
P2_LIMIT = 0


class Buf:
    def __init__(self, t, name, space):
        self.t = t
        self.name = name
        self.space = space
        self.writes = {}
        self.reads = {}
        self.sem = None
        self.total = 0

    def __getitem__(self, idx):
        return self.t[idx]

    def ap(self):
        return self.t.ap()


class Sched:
    def __init__(self, nc):
        self.nc = nc
        self.eng = {"pe": nc.tensor, "act": nc.scalar, "dve": nc.vector,
                    "pool": nc.gpsimd, "sp": nc.sync}
        self.psem = {k: nc.alloc_semaphore("prog_" + k) for k in self.eng}
        self.cnt = {k: 0 for k in self.eng}
        self.seen = {k: {} for k in self.eng}
        self.sem_pool = []
        self.live = []
        self.nbuf = 0
        self.nsem = 0

    def sb(self, shape, dtype, name=None, stack=None):
        self.nbuf += 1
        name = (name or "sb") + "_%d" % self.nbuf
        if stack is None:
            t = self.nc.alloc_sbuf_tensor(name, list(shape), dtype)
        else:
            t = stack.enter_context(self.nc.sbuf_tensor(name, list(shape), dtype))
        b = Buf(t, name, "sb")
        b.local = stack is not None
        return b

    def ps(self, shape, dtype=F32, name=None, stack=None):
        self.nbuf += 1
        name = (name or "ps") + "_%d" % self.nbuf
        if stack is None:
            t = self.nc.alloc_psum_tensor(name, list(shape), dtype)
        else:
            t = stack.enter_context(self.nc.psum_tensor(name, list(shape), dtype))
        b = Buf(t, name, "ps")
        b.local = stack is not None
        return b

    def dram(self, name, shape, dtype, kind="Internal"):
        b = Buf(self.nc.dram_tensor(name, list(shape), dtype, kind=kind), name, "dram")
        b.local = False
        return b

    def _wait(self, e, tok):
        sem, v = tok
        key = id(sem)
        if self.seen[e].get(key, 0) >= v:
            return
        self.seen[e][key] = v
        self.eng[e].wait_ge(sem, v)

    def _deps(self, e, reads, writes):
        own = id(self.psem[e])
        toks = []
        for r in reads:
            if r.space == "dram":
                continue
            for t in r.writes.values():
                if id(t[0]) == own and e == "pe":
                    continue
                toks.append(t)
        for w in writes:
            if w.space == "dram":
                continue
            for t in list(w.writes.values()) + list(w.reads.values()):
                if id(t[0]) == own:
                    continue
                toks.append(t)
        for t in toks:
            self._wait(e, t)

    def _record(self, tok, reads, writes, partial):
        k = id(tok[0])
        for r in reads:
            if r.space != "dram":
                r.reads[k] = tok
        for w in writes:
            if w.space == "dram":
                continue
            if partial:
                w.writes[k] = tok
            else:
                w.writes = {k: tok}
                w.reads = {}

    def op(self, e, ins, reads=(), writes=(), partial=False):
        self._deps(e, reads, writes)
        i = ins()
        self.cnt[e] += 1
        i.then_inc(self.psem[e], 1)
        tok = (self.psem[e], self.cnt[e])
        self._record(tok, reads, writes, partial)
        return tok

    def dma(self, q, mk, reads=(), writes=(), partial=False, after=()):
        self._deps(q, reads, writes)
        for t in after:
            self._wait(q, t)
        owner = None
        for b in list(writes) + list(reads):
            if b.space != "dram":
                owner = b
                break
        if owner is None:
            owner = (list(writes) + list(reads))[0]
        if owner.sem is None:
            if self.sem_pool:
                owner.sem, owner.total = self.sem_pool.pop()
            else:
                self.nsem += 1
                owner.sem = self.nc.alloc_semaphore("dsem%d" % self.nsem)
                owner.total = 0
            self.live.append(owner)
        i = mk()
        owner.total += 16
        i.then_inc(owner.sem, 16)
        tok = (owner.sem, owner.total)
        self._record(tok, reads, writes, partial)
        return tok

    def barrier(self):
        toks = [(self.psem[k], self.cnt[k]) for k in self.eng if self.cnt[k] > 0]
        toks += [(b.sem, b.total) for b in self.live]
        for e in self.eng:
            for t in toks:
                if id(t[0]) == id(self.psem[e]):
                    continue
                self._wait(e, t)

    def end_phase(self):
        self.barrier()
        keep = []
        for b in self.live:
            if b.local:
                self.sem_pool.append((b.sem, b.total))
                b.sem = None
            else:
                keep.append(b)
        self.live = keep


def build_program(n_layers=DEPTH, debug=(), stop_after=None):
    nc = bass.Bass("TRN2", target_bir_lowering=False)
    S = Sched(nc)
    ES = contextlib.ExitStack
    bc_reg = nc.gpsimd.to_reg(NE * MS - 1)
    L = n_layers

    def din(name, shape, dt=F32):
        return S.dram(name, shape, dt, kind="ExternalInput")

    x_in = din("x", [T, D])
    ctx_in = din("ctx", [CT, D])
    c_in = din("c", [D])
    cc_in = din("c_ctx", [D])
    w_ada = din("w_ada", [L, D, 6 * D])
    b_ada = din("b_ada", [L, 6 * D])
    norm1_g = din("norm1_g", [L, D])
    norm2_g = din("norm2_g", [L, D])
    w_in = din("w_in", [L, D, 2048])
    q_norm_g = din("q_norm_g", [L, 64])
    k_norm_g = din("k_norm_g", [L, 64])
    conv_w = din("conv_w", [L, 31, 256])
    conv_b = din("conv_b", [L, 256])
    conv_ln_g = din("conv_ln_g", [L, 256])
    conv_ln_b = din("conv_ln_b", [L, 256])
    w_out = din("w_out", [L, D, D])
    w_router = din("w_router", [L, D, NE])
    has_moe = stop_after is None or stop_after in ("7", "8")
    if has_moe:
        w_gate = din("w_gate", [L, NE, D, FF])
        w_up = din("w_up", [L, NE, D, FF])
        w_down = din("w_down", [L, NE, FF, D])
    final_g = din("final_norm_g", [D])
    ident_in = din("ident", [128, 128])
    rope_c = din("rope_c", [TT, 64])
    rope_s = din("rope_s", [TT, 64])
    nabias = din("nabias", [L, 8, 64, 4, 512])
    lpad = din("lpad", [NPAD, 2], I32)
    utri_in = din("utri", [128, 128])
    out = S.dram("out", [T, D], F32, kind="ExternalOutput")
    dbg = {}

    X = S.dram("X", [TT, D], F32)
    QT = S.dram("QT", [128, 4, TT], BF16)
    KT = S.dram("KT", [128, TT], BF16)
    VA = S.dram("VA", [TT, 130], BF16)
    QBT = S.dram("QBT", [128, 2, TT], BF16)
    KBT = S.dram("KBT", [128, 2, TT], BF16)
    VB = S.dram("VB", [TT, 256], BF16)
    UT = S.dram("UT", [256, TT], F32)
    AT = S.dram("AT", [512, TT], BF16)
    BT = S.dram("BT", [256, TT], BF16)
    CTs = S.dram("CTs", [256, TT], BF16)
    H2 = S.dram("H2", [TT + NPAD, D], BF16)
    YACC = S.dram("YACC", [TT + NPAD, D], F32)
    LST = S.dram("LST", [NE, MS, 2], I32)

    idf = S.sb([128, 128], F32, "idf")
    idb = S.sb([128, 128], BF16, "idb")
    ones_b = S.sb([128, 128], BF16, "ones_b")
    ones_f = S.sb([128, 128], F32, "ones_f")
    zeros_f = S.sb([128, D], F32, "zeros_f")
    mod_l = S.sb([128, 6 * D], F32, "mod_l")
    mod_c = S.sb([128, 6 * D], F32, "mod_c")
    crep_l = S.sb([128, 8, 128], BF16, "crep_l")
    crep_c = S.sb([128, 8, 128], BF16, "crep_c")
    AFF = S.sb([128, NTT, NE], F32, "AFF")

    def v_(e):
        return {"dve": nc.vector, "pool": nc.gpsimd}[e]

    S.dma("sp", lambda: nc.sync.dma_start(out=idf[:], in_=ident_in[:, :]), reads=[ident_in], writes=[idf])
    S.op("dve", lambda: nc.vector.tensor_copy(out=idb[:], in_=idf[:]), reads=[idf], writes=[idb])
    S.op("dve", lambda: nc.vector.memset(ones_b[:], 1.0), writes=[ones_b])
    S.op("dve", lambda: nc.vector.memset(ones_f[:], 1.0), writes=[ones_f])
    S.op("dve", lambda: nc.vector.memset(zeros_f[:], 0.0), writes=[zeros_f])
    with ES() as st:
        cT = S.sb([128, 2, 8], F32, "cT", st)
        with nc.allow_non_contiguous_dma(reason="tiny"):
            S.dma("sp", lambda: nc.sync.dma_start(out=cT[:, 0, :], in_=c_in.ap().rearrange("(k p) -> p k", p=128)),
                  reads=[c_in], writes=[cT], partial=True)
            S.dma("sp", lambda: nc.sync.dma_start(out=cT[:, 1, :], in_=cc_in.ap().rearrange("(k p) -> p k", p=128)),
                  reads=[cc_in], writes=[cT], partial=True)
        sT = S.sb([128, 2, 8], F32, "sT", st)
        S.op("act", lambda: nc.scalar.activation(out=sT[:], in_=cT[:], func=AF.Silu), reads=[cT], writes=[sT])
        for k in range(8):
            S.op("dve", lambda: nc.vector.tensor_scalar(out=crep_l[:, k, :], in0=ones_b[:], scalar1=sT[:, 0, k:k + 1],
                                                        scalar2=None, op0=ALU.mult),
                 reads=[ones_b, sT], writes=[crep_l], partial=True)
            S.op("dve", lambda: nc.vector.tensor_scalar(out=crep_c[:, k, :], in0=ones_b[:], scalar1=sT[:, 1, k:k + 1],
                                                        scalar2=None, op0=ALU.mult),
                 reads=[ones_b, sT], writes=[crep_c], partial=True)
        zb = S.sb([NPAD, D], BF16, "zb", st)
        S.op("dve", lambda: nc.vector.memset(zb[:], 0.0), writes=[zb])
        S.dma("sp", lambda: nc.sync.dma_start(out=H2[TT:TT + NPAD, :], in_=zb[:]), reads=[zb], writes=[H2])
        lp = S.sb([NPAD, 2], I32, "lp", st)
        S.dma("sp", lambda: nc.sync.dma_start(out=lp[:], in_=lpad[:, :]), reads=[lpad], writes=[lp])
        for e in range(NE):
            S.dma("sp", lambda: nc.sync.dma_start(out=LST[e, CAP + CCAP:MS, :], in_=lp[:]), reads=[lp], writes=[LST])
        xts = [S.sb([128, D], F32, "xcp", st) for _ in range(2)]
        for i in range(NTT):
            xt = xts[i % 2]
            src = x_in[i * 128:(i + 1) * 128, :] if i < NTL else ctx_in[(i - NTL) * 128:(i - NTL + 1) * 128, :]
            S.dma("sp", lambda: nc.sync.dma_start(out=xt[:], in_=src), reads=[x_in], writes=[xt])
            S.dma("sp", lambda: nc.sync.dma_start(out=X[i * 128:(i + 1) * 128, :], in_=xt[:]), reads=[xt], writes=[X])
        S.end_phase()

    for l in range(L):
        last = (l == DEPTH - 1)
        ntq = NTL if last else NTT

        with ES() as st:
            brep = S.sb([128, 6 * D], F32, "brep", st)
            S.dma("sp", lambda: nc.sync.dma_start(out=brep[:], in_=b_ada.ap()[l].partition_broadcast(128)),
                  reads=[b_ada], writes=[brep])
            g1rep = S.sb([128, D], F32, "g1rep", st)
            g2rep = S.sb([128, D], F32, "g2rep", st)
            S.dma("sp", lambda: nc.sync.dma_start(out=g1rep[:], in_=norm1_g.ap()[l].partition_broadcast(128)),
                  reads=[norm1_g], writes=[g1rep])
            S.dma("sp", lambda: nc.sync.dma_start(out=g2rep[:], in_=norm2_g.ap()[l].partition_broadcast(128)),
                  reads=[norm2_g], writes=[g2rep])
            was = [S.sb([128, 8, 512], BF16, "wa", st) for _ in range(2)]
            pms = [S.ps([128, 512], F32, "pm", st) for _ in range(2)]
            for cc in range(12):
                wa = was[cc % 2]
                S.dma("pool", lambda: nc.gpsimd.dma_start(
                    out=wa[:], in_=w_ada.ap()[l][:, cc * 512:(cc + 1) * 512].rearrange("(k p) n -> p k n", p=128)),
                    reads=[w_ada], writes=[wa])
                for which, (crep, mod) in enumerate(((crep_l, mod_l), (crep_c, mod_c))):
                    pm = pms[which]
                    for k in range(8):
                        S.op("pe", lambda: nc.tensor.matmul(pm[:], lhsT=crep[:, k, :], rhs=wa[:, k, :],
                                                            start=(k == 0), stop=(k == 7)),
                             reads=[crep, wa], writes=[pm], partial=(k > 0))
                    S.op("dve", lambda: nc.vector.tensor_tensor(out=mod[:, cc * 512:(cc + 1) * 512], in0=pm[:],
                                                                in1=brep[:, cc * 512:(cc + 1) * 512], op=ALU.add),
                         reads=[pm, brep], writes=[mod], partial=True)
            for mod in (mod_l, mod_c):
                S.op("dve", lambda: nc.vector.scalar_tensor_tensor(out=mod[:, D:2 * D], in0=mod[:, D:2 * D], scalar=1.0,
                                                                   in1=g1rep[:], op0=ALU.add, op1=ALU.mult),
                     reads=[mod, g1rep], writes=[mod], partial=True)
                S.op("dve", lambda: nc.vector.scalar_tensor_tensor(out=mod[:, 4 * D:5 * D], in0=mod[:, 4 * D:5 * D],
                                                                   scalar=1.0, in1=g2rep[:], op0=ALU.add, op1=ALU.mult),
                     reads=[mod, g2rep], writes=[mod], partial=True)
            S.end_phase()
        if "mod" in debug and l == 0:
            dbg["mod"] = S.dram("dbg_mod", [128, 6 * D], F32, kind="ExternalOutput")
            S.dma("sp", lambda: nc.sync.dma_start(out=dbg["mod"][:, :], in_=mod_l[:]), reads=[mod_l], writes=[dbg["mod"]])
            S.barrier()
        if stop_after == "M":
            break

        with ES() as st:
            win = S.sb([128, 8, 2048], BF16, "win", st)
            for h in range(4):
                S.dma("pool", lambda: nc.gpsimd.dma_start(
                    out=win[:, :, h * 512:(h + 1) * 512],
                    in_=w_in.ap()[l][:, h * 512:(h + 1) * 512].rearrange("(k p) n -> p k n", p=128)),
                    reads=[w_in], writes=[win], partial=True)
            gq = S.sb([128, 64], F32, "gq", st)
            gk = S.sb([128, 64], F32, "gk", st)
            S.dma("sp", lambda: nc.sync.dma_start(out=gq[:], in_=q_norm_g.ap()[l].partition_broadcast(128)),
                  reads=[q_norm_g], writes=[gq])
            S.dma("sp", lambda: nc.sync.dma_start(out=gk[:], in_=k_norm_g.ap()[l].partition_broadcast(128)),
                  reads=[k_norm_g], writes=[gk])
            S.op("dve", lambda: nc.vector.tensor_scalar(out=gq[:], in0=gq[:], scalar1=0.125, scalar2=None, op0=ALU.mult),
                 reads=[gq], writes=[gq])
            xts = [S.sb([128, D], F32, "xt", st) for _ in range(2)]
            sq = S.sb([128, D], F32, "sq", st)
            ss = S.sb([128, 1], F32, "ss", st)
            rstd = S.sb([128, 1], F32, "rstd", st)
            hf = S.sb([128, D], F32, "hf", st)
            hb = S.sb([128, D], BF16, "hb", st)
            hT = S.sb([128, 8, 512], BF16, "hT", st)
            rc = [S.sb([128, 64], F32, "rc", st) for _ in range(2)]
            rs_ = [S.sb([128, 64], F32, "rs", st) for _ in range(2)]
            qsq = S.sb([128, 640], F32, "qsq", st)
            qss = S.sb([128, 10], F32, "qss", st)
            qn = S.sb([128, 640], F32, "qn", st)
            t1 = S.sb([128, 640], F32, "t1", st)
            t2 = S.sb([128, 640], F32, "t2", st)
            qr = S.sb([128, 640], BF16, "qr", st)
            qbk = S.sb([128, 512], BF16, "qbk", st)
            qTs = S.sb([128, 4, 512], BF16, "qTs", st)
            kTs = S.sb([128, 512], BF16, "kTs", st)
            vas = S.sb([128, 4, 130], BF16, "vas", st)
            qbTs = S.sb([128, 2, 512], BF16, "qbTs", st)
            kbTs = S.sb([128, 2, 512], BF16, "kbTs", st)
            vbs = S.sb([128, 4, 256], BF16, "vbs", st)
            sg = S.sb([128, 512], F32, "sg", st)
            uTs = S.sb([128, 2, 512], F32, "uTs", st)
            pT = S.ps([128, 8, 128], BF16, "pT", st)
            pmm = [S.ps([128, 512], F32, "pmm", st) for _ in range(3)]
            pq = S.ps([128, 4, 128], BF16, "pq", st)
            pk = S.ps([128, 5, 128], BF16, "pk", st)
            pf = [S.ps([128, 512], F32, "pf", st) for _ in range(2)]
            S.op("dve", lambda: nc.vector.memset(vas[:], 1.0), writes=[vas])
            A1 = lambda mod: mod[:, D:2 * D]
            S1 = lambda mod: mod[:, 0:D]

            groups = [(g * 512, 512, mod_l) for g in range(T // 512)] + [(T, CT, mod_c)]
            for (t0, G, mod) in groups:
                nsub = G // 128
                for s in range(nsub):
                    r0 = t0 + s * 128
                    xt = xts[s % 2]
                    S.dma("sp", lambda: nc.sync.dma_start(out=xt[:], in_=X[r0:r0 + 128, :]), reads=[X], writes=[xt])
                    S.op("act", lambda: nc.scalar.activation(out=sq[:], in_=xt[:], func=AF.Square, accum_out=ss[:]),
                         reads=[xt], writes=[sq, ss])
                    S.op("dve", lambda: nc.vector.tensor_scalar(out=rstd[:], in0=ss[:], scalar1=1.0 / D, scalar2=EPS,
                                                                op0=ALU.mult, op1=ALU.add), reads=[ss], writes=[rstd])
                    S.op("act", lambda: nc.scalar.activation(out=rstd[:], in_=rstd[:], func=AF.Sqrt), reads=[rstd], writes=[rstd])
                    S.op("dve", lambda: nc.vector.reciprocal(out=rstd[:], in_=rstd[:]), reads=[rstd], writes=[rstd])
                    S.op("dve", lambda: nc.vector.scalar_tensor_tensor(out=hf[:], in0=xt[:], scalar=rstd[:, 0:1], in1=A1(mod),
                                                                       op0=ALU.mult, op1=ALU.mult),
                         reads=[xt, rstd, mod], writes=[hf])
                    S.op("pool", lambda: nc.gpsimd.tensor_tensor(out=hb[:], in0=hf[:], in1=S1(mod), op=ALU.add),
                         reads=[hf, mod], writes=[hb])
                    for k in range(8):
                        S.op("pe", lambda: nc.tensor.transpose(out=pT[:, k, :], in_=hb[:, k * 128:(k + 1) * 128], identity=idb[:]),
                             reads=[hb, idb], writes=[pT], partial=(k > 0))
                    S.op("act", lambda: nc.scalar.copy(out=hT[:, :, s * 128:(s + 1) * 128], in_=pT[:]),
                         reads=[pT], writes=[hT], partial=True)
                    for cg_ in range(3):
                        pm = pmm[cg_]
                        for k in range(8):
                            S.op("pe", lambda: nc.tensor.matmul(pm[:], lhsT=hT[:, k, s * 128:(s + 1) * 128],
                                                                rhs=win[:, k, cg_ * 512:(cg_ + 1) * 512],
                                                                start=(k == 0), stop=(k == 7)),
                                 reads=[hT, win], writes=[pm], partial=(k > 0))
                    rcb, rsb = rc[s % 2], rs_[s % 2]
                    S.dma("sp", lambda: nc.sync.dma_start(out=rcb[:], in_=rope_c[r0:r0 + 128, :]), reads=[rope_c], writes=[rcb])
                    S.dma("sp", lambda: nc.sync.dma_start(out=rsb[:], in_=rope_s[r0:r0 + 128, :]), reads=[rope_s], writes=[rsb])
                    S.op("act", lambda: nc.scalar.activation(out=qsq[:, 0:512], in_=pmm[0][:], func=AF.Square),
                         reads=[pmm[0]], writes=[qsq], partial=True)
                    S.op("act", lambda: nc.scalar.activation(out=qsq[:, 512:640], in_=pmm[1][:, 0:128], func=AF.Square),
                         reads=[pmm[1]], writes=[qsq], partial=True)
                    S.op("dve", lambda: nc.vector.tensor_reduce(out=qss[:], in_=qsq[:].rearrange("p (h d) -> p h d", d=64),
                                                                axis=AX.X, op=ALU.add), reads=[qsq], writes=[qss])
                    S.op("dve", lambda: nc.vector.tensor_scalar(out=qss[:], in0=qss[:], scalar1=1.0 / 64, scalar2=EPS,
                                                                op0=ALU.mult, op1=ALU.add), reads=[qss], writes=[qss])
                    S.op("act", lambda: nc.scalar.activation(out=qss[:], in_=qss[:], func=AF.Sqrt), reads=[qss], writes=[qss])
                    S.op("dve", lambda: nc.vector.reciprocal(out=qss[:], in_=qss[:]), reads=[qss], writes=[qss])
                    S.op("dve", lambda: nc.vector.tensor_tensor(
                        out=qn[:, 0:512].rearrange("p (h d) -> p h d", d=64), in0=pmm[0][:].rearrange("p (h d) -> p h d", d=64),
                        in1=qss[:, 0:8].unsqueeze(2).to_broadcast([128, 8, 64]), op=ALU.mult),
                        reads=[pmm[0], qss], writes=[qn], partial=True)
                    S.op("dve", lambda: nc.vector.tensor_tensor(
                        out=qn[:, 512:640].rearrange("p (h d) -> p h d", d=64),
                        in0=pmm[1][:, 0:128].rearrange("p (h d) -> p h d", d=64),
                        in1=qss[:, 8:10].unsqueeze(2).to_broadcast([128, 2, 64]), op=ALU.mult),
                        reads=[pmm[1], qss], writes=[qn], partial=True)
                    S.op("pool", lambda: nc.gpsimd.tensor_tensor(
                        out=qn[:, 0:512].rearrange("p (h d) -> p h d", d=64), in0=qn[:, 0:512].rearrange("p (h d) -> p h d", d=64),
                        in1=gq[:].unsqueeze(1).to_broadcast([128, 8, 64]), op=ALU.mult), reads=[qn, gq], writes=[qn], partial=True)
                    S.op("pool", lambda: nc.gpsimd.tensor_tensor(
                        out=qn[:, 512:640].rearrange("p (h d) -> p h d", d=64),
                        in0=qn[:, 512:640].rearrange("p (h d) -> p h d", d=64),
                        in1=gk[:].unsqueeze(1).to_broadcast([128, 2, 64]), op=ALU.mult), reads=[qn, gk], writes=[qn], partial=True)
                    S.op("dve", lambda: nc.vector.tensor_tensor(
                        out=t1[:].rearrange("p (h d) -> p h d", d=64), in0=qn[:].rearrange("p (h d) -> p h d", d=64),
                        in1=rcb[:].unsqueeze(1).to_broadcast([128, 10, 64]), op=ALU.mult), reads=[qn, rcb], writes=[t1])
                    qv = qn[:].rearrange("p (h a b c) -> p h a b c", h=10, a=2, b=2)
                    tv = t2[:].rearrange("p (h a b c) -> p h a b c", h=10, a=2, b=2)
                    sv = rsb[:].rearrange("p (a b c) -> p a b c", a=2, b=2)
                    for a in range(2):
                        for b_ in range(2):
                            S.op("pool", lambda: nc.gpsimd.tensor_tensor(
                                out=tv[:, :, a, b_, :], in0=qv[:, :, a, 1 - b_, :],
                                in1=sv[:, a, b_, :].unsqueeze(1).to_broadcast([128, 10, 16]), op=ALU.mult),
                                reads=[qn, rsb], writes=[t2], partial=True)
                    S.op("dve", lambda: nc.vector.tensor_tensor(
                        out=qr[:, 0:512].rearrange("p (g k d) -> p k g d", g=4, k=2),
                        in0=t1[:, 0:512].rearrange("p (k g d) -> p k g d", k=2, g=4),
                        in1=t2[:, 0:512].rearrange("p (k g d) -> p k g d", k=2, g=4), op=ALU.add),
                        reads=[t1, t2], writes=[qr])
                    S.op("dve", lambda: nc.vector.tensor_tensor(out=qr[:, 512:640], in0=t1[:, 512:640], in1=t2[:, 512:640],
                                                                op=ALU.add), reads=[t1, t2], writes=[qr], partial=True)
                    for g in range(4):
                        S.op("pe", lambda: nc.tensor.transpose(
                            out=pq[:, g, :], in_=qr[:, g * 128:(g + 1) * 128],
                            identity=idb[:]), reads=[qr, idb], writes=[pq], partial=(g > 0))
                    S.op("act", lambda: nc.scalar.copy(out=qTs[:, :, s * 128:(s + 1) * 128], in_=pq[:]),
                         reads=[pq], writes=[qTs], partial=True)
                    S.op("pe", lambda: nc.tensor.transpose(out=pk[:, 0, :], in_=qr[:, 512:640], identity=idb[:]),
                         reads=[qr, idb], writes=[pk], partial=False)
                    S.op("act", lambda: nc.scalar.copy(out=vas[:, s, :].rearrange("p (k e) -> p k e", e=65)[:, :, 0:64],
                                                       in_=pmm[1][:, 128:256].rearrange("p (k d) -> p k d", d=64)),
                         reads=[pmm[1]], writes=[vas], partial=True)
                    S.op("dve", lambda: nc.vector.tensor_scalar(out=qbk[:, 0:256], in0=pmm[1][:, 256:512], scalar1=0.125,
                                                                scalar2=None, op0=ALU.mult),
                         reads=[pmm[1]], writes=[qbk], partial=True)
                    S.op("act", lambda: nc.scalar.copy(out=qbk[:, 256:512], in_=pmm[2][:, 0:256]),
                         reads=[pmm[2]], writes=[qbk], partial=True)
                    S.op("act", lambda: nc.scalar.copy(out=vbs[:, s, :], in_=pmm[2][:, 256:512]),
                         reads=[pmm[2]], writes=[vbs], partial=True)
                    for j in range(4):
                        S.op("pe", lambda: nc.tensor.transpose(out=pk[:, 1 + j, :], in_=qbk[:, j * 128:(j + 1) * 128],
                                                               identity=idb[:]), reads=[qbk, idb], writes=[pk], partial=True)
                    S.op("dve", lambda: nc.vector.tensor_copy(out=kTs[:, s * 128:(s + 1) * 128], in_=pk[:, 0, :]),
                         reads=[pk], writes=[kTs], partial=True)
                    S.op("dve", lambda: nc.vector.tensor_copy(out=qbTs[:, :, s * 128:(s + 1) * 128], in_=pk[:, 1:3, :]),
                         reads=[pk], writes=[qbTs], partial=True)
                    S.op("dve", lambda: nc.vector.tensor_copy(out=kbTs[:, :, s * 128:(s + 1) * 128], in_=pk[:, 3:5, :]),
                         reads=[pk], writes=[kbTs], partial=True)
                for j in range(2):
                    for which in range(2):
                        c0 = 1536 + which * 256 + j * 128
                        for k in range(8):
                            S.op("pe", lambda: nc.tensor.matmul(pf[which][:, 0:G], lhsT=win[:, k, c0:c0 + 128], rhs=hT[:, k, 0:G],
                                                                start=(k == 0), stop=(k == 7)),
                                 reads=[win, hT], writes=[pf[which]], partial=(k > 0))
                    S.op("act", lambda: nc.scalar.activation(out=sg[:, 0:G], in_=pf[1][:, 0:G], func=AF.Sigmoid),
                         reads=[pf[1]], writes=[sg])
                    S.op("dve", lambda: nc.vector.tensor_tensor(out=uTs[:, j, 0:G], in0=pf[0][:, 0:G], in1=sg[:, 0:G], op=ALU.mult),
                         reads=[pf[0], sg], writes=[uTs], partial=True)
                S.dma("sp", lambda: nc.sync.dma_start(out=QT[:, :, t0:t0 + G], in_=qTs[:, :, 0:G]), reads=[qTs], writes=[QT])
                S.dma("sp", lambda: nc.sync.dma_start(out=KT[:, t0:t0 + G], in_=kTs[:, 0:G]), reads=[kTs], writes=[KT])
                S.dma("sp", lambda: nc.sync.dma_start(out=VA.ap()[t0:t0 + G, :].rearrange("(s p) e -> p s e", p=128),
                                                      in_=vas[:, 0:nsub, :]), reads=[vas], writes=[VA])
                S.dma("sp", lambda: nc.sync.dma_start(out=QBT[:, :, t0:t0 + G], in_=qbTs[:, :, 0:G]), reads=[qbTs], writes=[QBT])
                S.dma("sp", lambda: nc.sync.dma_start(out=KBT[:, :, t0:t0 + G], in_=kbTs[:, :, 0:G]), reads=[kbTs], writes=[KBT])
                S.dma("sp", lambda: nc.sync.dma_start(out=VB.ap()[t0:t0 + G, :].rearrange("(s p) e -> p s e", p=128),
                                                      in_=vbs[:, 0:nsub, :]), reads=[vbs], writes=[VB])
                S.dma("sp", lambda: nc.sync.dma_start(out=UT.ap()[:, t0:t0 + G].rearrange("(j p) t -> p j t", p=128),
                                                      in_=uTs[:, :, 0:G]), reads=[uTs], writes=[UT])
            S.end_phase()
        if stop_after == "1":
            break

        with ES() as st:
            ksb = S.sb([128, TT], BF16, "ksb", st)
            vsb = S.sb([128, NTT, 130], BF16, "vsb", st)
            S.dma("sp", lambda: nc.sync.dma_start(out=ksb[:], in_=KT[:, :]), reads=[KT], writes=[ksb])
            S.dma("sp", lambda: nc.sync.dma_start(out=vsb[:], in_=VA.ap().rearrange("(n p) e -> p n e", p=128)),
                  reads=[VA], writes=[vsb])
            gqk = S.sb([128, 2, 64], F32, "gqk", st)
            S.dma("sp", lambda: nc.sync.dma_start(out=gqk[:, 0, :], in_=q_norm_g.ap()[l].partition_broadcast(128)),
                  reads=[q_norm_g], writes=[gqk], partial=True)
            S.dma("sp", lambda: nc.sync.dma_start(out=gqk[:, 1, :], in_=k_norm_g.ap()[l].partition_broadcast(128)),
                  reads=[k_norm_g], writes=[gqk], partial=True)
            gmx = S.sb([128, 2], F32, "gmx", st)
            nb = S.sb([128, 1], F32, "nb", st)
            gng = S.sb([128, 2, 64], F32, "gng", st)
            S.op("dve", lambda: nc.vector.tensor_scalar(out=gng[:], in0=gqk[:], scalar1=-1.0, scalar2=None, op0=ALU.mult),
                 reads=[gqk], writes=[gng])
            S.op("dve", lambda: nc.vector.tensor_tensor(out=gqk[:], in0=gqk[:], in1=gng[:], op=ALU.max),
                 reads=[gqk, gng], writes=[gqk])
            S.op("dve", lambda: nc.vector.tensor_reduce(out=gmx[:], in_=gqk[:], axis=AX.X, op=ALU.max), reads=[gqk], writes=[gmx])
            S.op("dve", lambda: nc.vector.tensor_tensor(out=nb[:], in0=gmx[:, 0:1], in1=gmx[:, 1:2], op=ALU.mult),
                 reads=[gmx], writes=[nb])
            S.op("dve", lambda: nc.vector.tensor_scalar(out=nb[:], in0=nb[:], scalar1=-8.0, scalar2=None, op0=ALU.mult),
                 reads=[nb], writes=[nb])
            qsbs = [S.sb([128, 2, 512], BF16, "qsb", st) for _ in range(2)]
            for qb_ in qsbs:
                S.op("dve", lambda: nc.vector.memset(qb_[:], 0.0), writes=[qb_])
            pbs = [S.sb([128, 512], BF16, "pb", st) for _ in range(3)]
            rsa = S.sb([128, 4], F32, "rsa", st)
            osb = [S.sb([128, 512], BF16, "osb", st) for _ in range(2)]
            aTs = [S.sb([128, 4, 128], BF16, "aTs", st) for _ in range(2)]
            pss = [S.ps([128, 512], F32, "pss", st) for _ in range(3)]
            pos = [S.ps([128, 512], F32, "po", st) for _ in range(4)]
            pa = S.ps([128, 4, 128], BF16, "pa", st)
            steps = []
            nq2 = min(ntq, P2_LIMIT) if P2_LIMIT else ntq
            for qi in range(nq2):
                ktiles = list(range(NTT)) if qi < NTL else [NTL, NTL + 1]
                for kh in range(2):
                    for idx, kt in enumerate(ktiles):
                        steps.append((qi, kh, idx, kt, len(ktiles)))
            nst_ = len(steps)

            def load_q(qi):
                q0 = qi * 128
                qsb = qsbs[qi % 2]
                for kh_ in range(2):
                    S.dma("sp", lambda: nc.sync.dma_start(
                        out=qsb[kh_ * 64:(kh_ + 1) * 64, kh_, :].rearrange("p (g t) -> p g t", g=4),
                        in_=QT[kh_ * 64:(kh_ + 1) * 64, :, q0:q0 + 128]), reads=[QT], writes=[qsb], partial=True)

            def emit_S(i):
                qi, kh, idx, kt, nk = steps[i]
                if kh == 0 and idx == 0 and qi + 1 < nq2:
                    load_q(qi + 1)
                qsb = qsbs[qi % 2]
                ps = pss[i % 3]
                S.op("pe", lambda: nc.tensor.matmul(ps[:], lhsT=ksb[:, kt * 128:(kt + 1) * 128],
                                                    rhs=qsb[:, kh, :], start=True, stop=True),
                     reads=[ksb, qsb], writes=[ps])

            def finish_q(qi):
                q0 = qi * 128
                ob = osb[qi % 2]
                for j in range(4):
                    S.op("pe", lambda: nc.tensor.transpose(out=pa[:, j, :], in_=ob[:, j * 128:(j + 1) * 128], identity=idb[:]),
                         reads=[ob, idb], writes=[pa], partial=(j > 0))
                aT = aTs[qi % 2]
                S.op("dve", lambda: nc.vector.tensor_copy(out=aT[:], in_=pa[:]), reads=[pa], writes=[aT])
                S.dma("sp", lambda: nc.sync.dma_start(out=AT.ap()[:, q0:q0 + 128].rearrange("(j p) t -> p j t", p=128), in_=aT[:]),
                      reads=[aT], writes=[AT])

            load_q(0)
            emit_S(0)
            if nst_ > 1:
                emit_S(1)
            deferred = {}
            for i in range(nst_):
                qi, kh, idx, kt, nk = steps[i]
                ps, pb = pss[i % 3], pbs[i % 3]
                ob = osb[qi % 2]
                S.op("act", lambda: nc.scalar.activation(out=pb[:], in_=ps[:], func=AF.Exp, bias=nb[:, 0:1], scale=1.0),
                     reads=[ps, nb], writes=[pb])
                for g in range(4):
                    S.op("pe", lambda: nc.tensor.matmul(pos[g][:, 0:65], lhsT=pb[:, g * 128:(g + 1) * 128],
                                                        rhs=vsb[:, kt, kh * 65:(kh + 1) * 65],
                                                        start=(idx == 0), stop=(idx == nk - 1)),
                         reads=[pb, vsb], writes=[pos[g]], partial=(idx > 0))
                if i + 2 < nst_:
                    emit_S(i + 2)
                if idx == nk - 1:
                    for g in range(4):
                        S.op("dve", lambda: nc.vector.reciprocal(out=rsa[:, g:g + 1], in_=pos[g][:, 64:65]),
                             reads=[pos[g]], writes=[rsa], partial=True)
                        S.op("dve", lambda: nc.vector.tensor_scalar(
                            out=ob[:, kh * 256 + g * 64:kh * 256 + (g + 1) * 64], in0=pos[g][:, 0:64], scalar1=rsa[:, g:g + 1],
                            scalar2=None, op0=ALU.mult), reads=[pos[g], rsa], writes=[ob], partial=True)
                    if kh == 1:
                        deferred[min(i + 4, nst_ - 1)] = deferred.get(min(i + 4, nst_ - 1), []) + [qi]
                for qd in deferred.pop(i, []):
                    finish_q(qd)
            S.end_phase()
        if stop_after == "2":
            break

        with ES() as st:
            kc = S.sb([128, 2, 256], BF16, "kc", st)
            vc = S.sb([128, 2, 256], BF16, "vc", st)
            S.dma("sp", lambda: nc.sync.dma_start(out=kc[:], in_=KBT[:, :, T:TT]), reads=[KBT], writes=[kc])
            S.dma("sp", lambda: nc.sync.dma_start(out=vc[:], in_=VB.ap()[T:TT, :].rearrange("(n p) e -> p n e", p=128)),
                  reads=[VB], writes=[vc])
            bint = S.sb([64, 4, 512], F32, "bint", st)
            bedge = S.sb([64, 4, 512], F32, "bedge", st)
            S.dma("sp", lambda: nc.sync.dma_start(out=bint[:], in_=nabias[l, 3]), reads=[nabias], writes=[bint])
            qrows = [S.sb([128, 2, 64], BF16, "qrow", st) for _ in range(2)]
            kwins = [S.sb([128, 2, 512], BF16, "kwin", st) for _ in range(2)]
            vwins = [S.sb([64, 8, 256], BF16, "vwin", st) for _ in range(2)]
            ssb = S.sb([64, 768], F32, "ssb", st)
            mx = S.sb([64, 1], F32, "mx", st)
            sm = S.sb([64, 1], F32, "sm", st)
            pexp = S.sb([64, 768], BF16, "pexp", st)
            ptw_s = S.sb([64, 8, 64], BF16, "ptw_s", st)
            ptc_s = S.sb([128, 2, 64], BF16, "ptc_s", st)
            brow = S.sb([64, 256], BF16, "brow", st)
            bts = [S.sb([128, 2, 64], BF16, "bts", st) for _ in range(2)]
            psn = [S.ps([64, 1024], F32, "psn", st) for _ in range(2)]
            ptw = S.ps([64, 8, 64], BF16, "ptw", st)
            ptc = S.ps([128, 2, 64], BF16, "ptc", st)
            pon = S.ps([64, 64], F32, "pon", st)
            pbt = S.ps([128, 2, 64], BF16, "pbt", st)
            it = 0
            for r in range(T // 64):
                rs0 = min(max(r - 4, 0), 120)
                off = rs0 - r + 7
                if off == 3:
                    bias = bint
                else:
                    bias = bedge
                    S.dma("sp", lambda: nc.sync.dma_start(out=bedge[:], in_=nabias[l, off]), reads=[nabias], writes=[bedge])
                qrow, kwin, vwin = qrows[r % 2], kwins[r % 2], vwins[r % 2]
                S.dma("sp", lambda: nc.sync.dma_start(out=qrow[:], in_=QBT[:, :, r * 64:(r + 1) * 64]), reads=[QBT], writes=[qrow])
                S.dma("sp", lambda: nc.sync.dma_start(out=kwin[:], in_=KBT[:, :, rs0 * 64:(rs0 + 8) * 64]), reads=[KBT], writes=[kwin])
                S.dma("sp", lambda: nc.sync.dma_start(out=vwin[:], in_=VB.ap()[rs0 * 64:(rs0 + 8) * 64, :].rearrange("(j p) e -> p j e", p=64)),
                      reads=[VB], writes=[vwin])
                for hb in range(4):
                    hp, pr = hb // 2, (hb % 2) * 64
                    ps = psn[it % 2]
                    it += 1
                    S.op("pe", lambda: nc.tensor.matmul(ps[:, 0:512], lhsT=qrow[pr:pr + 64, hp, :], rhs=kwin[pr:pr + 64, hp, :],
                                                        start=True, stop=True), reads=[qrow, kwin], writes=[ps])
                    S.op("pe", lambda: nc.tensor.matmul(ps[:, 512:768], lhsT=qrow[pr:pr + 64, hp, :], rhs=kc[pr:pr + 64, hp, :],
                                                        start=True, stop=True), reads=[qrow, kc], writes=[ps], partial=True)
                    S.op("dve", lambda: nc.vector.tensor_tensor(out=ssb[:, 0:512], in0=ps[:, 0:512], in1=bias[:, hb, :], op=ALU.add),
                         reads=[ps, bias], writes=[ssb])
                    S.op("act", lambda: nc.scalar.copy(out=ssb[:, 512:768], in_=ps[:, 512:768]), reads=[ps], writes=[ssb], partial=True)
                    S.op("dve", lambda: nc.vector.tensor_reduce(out=mx[:], in_=ssb[:], axis=AX.X, op=ALU.max), reads=[ssb], writes=[mx])
                    S.op("dve", lambda: nc.vector.tensor_scalar(out=mx[:], in0=mx[:], scalar1=-1.0, scalar2=None, op0=ALU.mult),
                         reads=[mx], writes=[mx])
                    S.op("act", lambda: nc.scalar.activation(out=pexp[:], in_=ssb[:], func=AF.Exp, bias=mx[:, 0:1], scale=1.0,
                                                             accum_out=sm[:]), reads=[ssb, mx], writes=[pexp, sm])
                    for j in range(8):
                        S.op("pe", lambda: nc.tensor.transpose(out=ptw[:, j, :], in_=pexp[:, j * 64:(j + 1) * 64], identity=idb[0:64, 0:64]),
                             reads=[pexp, idb], writes=[ptw], partial=(j > 0))
                    for j in range(2):
                        S.op("pe", lambda: nc.tensor.transpose(out=ptc[:, j, :], in_=pexp[:, 512 + j * 128:512 + (j + 1) * 128],
                                                               identity=idb[0:64, 0:64]), reads=[pexp, idb], writes=[ptc], partial=(j > 0))
                    S.op("dve", lambda: nc.vector.tensor_copy(out=ptw_s[:], in_=ptw[:]), reads=[ptw], writes=[ptw_s])
                    S.op("act", lambda: nc.scalar.copy(out=ptc_s[:], in_=ptc[:]), reads=[ptc], writes=[ptc_s])
                    for j in range(8):
                        S.op("pe", lambda: nc.tensor.matmul(pon[:], lhsT=ptw_s[:, j, :], rhs=vwin[:, j, hb * 64:(hb + 1) * 64],
                                                            start=(j == 0), stop=False), reads=[ptw_s, vwin], writes=[pon], partial=(j > 0))
                    for j in range(2):
                        S.op("pe", lambda: nc.tensor.matmul(pon[:], lhsT=ptc_s[:, j, :], rhs=vc[:, j, hb * 64:(hb + 1) * 64],
                                                            start=False, stop=(j == 1)), reads=[ptc_s, vc], writes=[pon], partial=True)
                    S.op("dve", lambda: nc.vector.reciprocal(out=sm[:], in_=sm[:]), reads=[sm], writes=[sm])
                    S.op("dve", lambda: nc.vector.tensor_scalar(out=brow[:, hb * 64:(hb + 1) * 64], in0=pon[:], scalar1=sm[:, 0:1],
                                                                scalar2=None, op0=ALU.mult), reads=[pon, sm], writes=[brow], partial=True)
                for j in range(2):
                    S.op("pe", lambda: nc.tensor.transpose(out=pbt[:, j, :], in_=brow[:, j * 128:(j + 1) * 128], identity=idb[0:64, 0:64]),
                         reads=[brow, idb], writes=[pbt], partial=(j > 0))
                bt = bts[r % 2]
                S.op("act", lambda: nc.scalar.copy(out=bt[:], in_=pbt[:]), reads=[pbt], writes=[bt])
                S.dma("sp", lambda: nc.sync.dma_start(out=BT.ap()[:, r * 64:(r + 1) * 64].rearrange("(j p) t -> p j t", p=128), in_=bt[:]),
                      reads=[bt], writes=[BT])
            S.end_phase()
        if not last:
            with ES() as st:
                kc = S.sb([128, 2, 256], BF16, "kc", st)
                vc = S.sb([128, 2, 256], BF16, "vc", st)
                S.dma("sp", lambda: nc.sync.dma_start(out=kc[:], in_=KBT[:, :, T:TT]), reads=[KBT], writes=[kc])
                S.dma("sp", lambda: nc.sync.dma_start(out=vc[:], in_=VB.ap()[T:TT, :].rearrange("(n p) e -> p n e", p=128)),
                      reads=[VB], writes=[vc])
                qc = S.sb([128, 2, 128], BF16, "qc", st)
                sc_ = S.sb([128, 256], F32, "sc", st)
                mxc = S.sb([128, 1], F32, "mxc", st)
                smc = S.sb([128, 1], F32, "smc", st)
                pxc = S.sb([128, 256], BF16, "pxc", st)
                ptcs = S.sb([128, 2, 128], BF16, "ptcs", st)
                browc = S.sb([128, 256], BF16, "browc", st)
                btc = S.sb([128, 2, 128], BF16, "btc", st)
                psc = S.ps([128, 256], F32, "psc", st)
                ptcp = S.ps([128, 2, 128], BF16, "ptcp", st)
                poc = S.ps([128, 64], F32, "poc", st)
                pbc = S.ps([128, 2, 128], BF16, "pbc", st)
                for ci in range(2):
                    c0 = T + ci * 128
                    S.dma("sp", lambda: nc.sync.dma_start(out=qc[:], in_=QBT[:, :, c0:c0 + 128]), reads=[QBT], writes=[qc])
                    for hb in range(4):
                        hp, pr = hb // 2, (hb % 2) * 64
                        S.op("pe", lambda: nc.tensor.matmul(psc[:], lhsT=qc[pr:pr + 64, hp, :], rhs=kc[pr:pr + 64, hp, :],
                                                            start=True, stop=True), reads=[qc, kc], writes=[psc])
                        S.op("act", lambda: nc.scalar.copy(out=sc_[:], in_=psc[:]), reads=[psc], writes=[sc_])
                        S.op("dve", lambda: nc.vector.tensor_reduce(out=mxc[:], in_=sc_[:], axis=AX.X, op=ALU.max), reads=[sc_], writes=[mxc])
                        S.op("dve", lambda: nc.vector.tensor_scalar(out=mxc[:], in0=mxc[:], scalar1=-1.0, scalar2=None, op0=ALU.mult),
                             reads=[mxc], writes=[mxc])
                        S.op("act", lambda: nc.scalar.activation(out=pxc[:], in_=sc_[:], func=AF.Exp, bias=mxc[:, 0:1], scale=1.0,
                                                                 accum_out=smc[:]), reads=[sc_, mxc], writes=[pxc, smc])
                        for j in range(2):
                            S.op("pe", lambda: nc.tensor.transpose(out=ptcp[:, j, :], in_=pxc[:, j * 128:(j + 1) * 128], identity=idb[:]),
                                 reads=[pxc, idb], writes=[ptcp], partial=(j > 0))
                        S.op("dve", lambda: nc.vector.tensor_copy(out=ptcs[:], in_=ptcp[:]), reads=[ptcp], writes=[ptcs])
                        for j in range(2):
                            S.op("pe", lambda: nc.tensor.matmul(poc[:], lhsT=ptcs[:, j, :], rhs=vc[:, j, hb * 64:(hb + 1) * 64],
                                                                start=(j == 0), stop=(j == 1)), reads=[ptcs, vc], writes=[poc], partial=(j > 0))
                        S.op("dve", lambda: nc.vector.reciprocal(out=smc[:], in_=smc[:]), reads=[smc], writes=[smc])
                        S.op("dve", lambda: nc.vector.tensor_scalar(out=browc[:, hb * 64:(hb + 1) * 64], in0=poc[:], scalar1=smc[:, 0:1],
                                                                    scalar2=None, op0=ALU.mult), reads=[poc, smc], writes=[browc], partial=True)
                    for j in range(2):
                        S.op("pe", lambda: nc.tensor.transpose(out=pbc[:, j, :], in_=browc[:, j * 128:(j + 1) * 128], identity=idb[:]),
                             reads=[browc, idb], writes=[pbc], partial=(j > 0))
                    S.op("act", lambda: nc.scalar.copy(out=btc[:], in_=pbc[:]), reads=[pbc], writes=[btc])
                    S.dma("sp", lambda: nc.sync.dma_start(out=BT.ap()[:, c0:c0 + 128].rearrange("(j p) t -> p j t", p=128), in_=btc[:]),
                          reads=[btc], writes=[BT])
                S.end_phase()
        if stop_after == "3":
            break

        seqs = [(0, T)] + ([] if last else [(T, CT)])
        for (t0, Ls) in seqs:
            with ES() as st:
                Y = S.sb([128, 2, Ls], F32, "Y", st)
                up = S.sb([128, Ls + 30], F32, "up", st)
                cw = S.sb([128, 2, 31], F32, "cw", st)
                cb = S.sb([128, 2], F32, "cb", st)
                lng = S.sb([128, 2], F32, "lng", st)
                lnb = S.sb([128, 2], F32, "lnb", st)
                ones_s = S.sb([128, 128], F32, "ones_s", st)
                S.op("dve", lambda: nc.vector.memset(ones_s[:], 1.0 / 256), writes=[ones_s])
                with nc.allow_non_contiguous_dma(reason="tiny"):
                    for j in range(2):
                        S.dma("sp", lambda: nc.sync.dma_start(out=cw[:, j, :], in_=conv_w.ap()[l][:, j * 128:(j + 1) * 128].rearrange("w c -> c w")),
                              reads=[conv_w], writes=[cw], partial=True)
                    for (dst, src) in ((cb, conv_b), (lng, conv_ln_g), (lnb, conv_ln_b)):
                        S.dma("sp", lambda: nc.sync.dma_start(out=dst[:], in_=src.ap()[l].rearrange("(j p) -> p j", p=128)),
                              reads=[src], writes=[dst])
                S.op("dve", lambda: nc.vector.memset(up[:, 0:15], 0.0), writes=[up], partial=True)
                S.op("dve", lambda: nc.vector.memset(up[:, Ls + 15:Ls + 30], 0.0), writes=[up], partial=True)
                for j in range(2):
                    S.dma("sp", lambda: nc.sync.dma_start(out=up[:, 15:15 + Ls], in_=UT[j * 128:(j + 1) * 128, t0:t0 + Ls]),
                          reads=[UT], writes=[up], partial=True)
                    S.op("dve", lambda: nc.vector.tensor_scalar(out=Y[:, j, :], in0=up[:, 0:Ls], scalar1=cw[:, j, 0:1], scalar2=cb[:, j:j + 1],
                                                                op0=ALU.mult, op1=ALU.add), reads=[up, cw, cb], writes=[Y], partial=True)
                    for w in range(1, 31):
                        S.op("dve", lambda: nc.vector.scalar_tensor_tensor(out=Y[:, j, :], in0=up[:, w:w + Ls], scalar=cw[:, j, w:w + 1],
                                                                           in1=Y[:, j, :], op0=ALU.mult, op1=ALU.add),
                             reads=[up, cw, Y], writes=[Y], partial=True)
                BL = min(512, Ls)
                ysq = S.sb([128, 2, BL], F32, "ysq", st)
                mean = S.sb([128, BL], F32, "mean", st)
                var = S.sb([128, BL], F32, "var", st)
                tmpc = S.sb([128, 2, BL], F32, "tmpc", st)
                ctsb = [S.sb([128, 2, BL], BF16, "ctsb", st) for _ in range(2)]
                pmn = S.ps([128, BL], F32, "pmn", st)
                pe2 = S.ps([128, BL], F32, "pe2", st)
                for bi in range(Ls // BL):
                    b0 = bi * BL
                    S.op("act", lambda: nc.scalar.activation(out=ysq[:], in_=Y[:, :, b0:b0 + BL], func=AF.Square), reads=[Y], writes=[ysq])
                    for j in range(2):
                        S.op("pe", lambda: nc.tensor.matmul(pmn[:], lhsT=ones_s[:], rhs=Y[:, j, b0:b0 + BL], start=(j == 0), stop=(j == 1)),
                             reads=[ones_s, Y], writes=[pmn], partial=(j > 0))
                    for j in range(2):
                        S.op("pe", lambda: nc.tensor.matmul(pe2[:], lhsT=ones_s[:], rhs=ysq[:, j, :], start=(j == 0), stop=(j == 1)),
                             reads=[ones_s, ysq], writes=[pe2], partial=(j > 0))
                    S.op("act", lambda: nc.scalar.copy(out=mean[:], in_=pmn[:]), reads=[pmn], writes=[mean])
                    S.op("dve", lambda: nc.vector.tensor_tensor(out=var[:], in0=mean[:], in1=mean[:], op=ALU.mult), reads=[mean], writes=[var])
                    S.op("dve", lambda: nc.vector.tensor_tensor(out=var[:], in0=pe2[:], in1=var[:], op=ALU.subtract), reads=[pe2, var], writes=[var])
                    S.op("dve", lambda: nc.vector.tensor_scalar(out=var[:], in0=var[:], scalar1=EPS, scalar2=None, op0=ALU.add),
                         reads=[var], writes=[var])
                    S.op("act", lambda: nc.scalar.activation(out=var[:], in_=var[:], func=AF.Sqrt), reads=[var], writes=[var])
                    S.op("dve", lambda: nc.vector.reciprocal(out=var[:], in_=var[:]), reads=[var], writes=[var])
                    cts_ = ctsb[bi % 2]
                    for j in range(2):
                        S.op("dve", lambda: nc.vector.tensor_tensor(out=tmpc[:, j, :], in0=Y[:, j, b0:b0 + BL], in1=mean[:], op=ALU.subtract),
                             reads=[Y, mean], writes=[tmpc], partial=True)
                        S.op("dve", lambda: nc.vector.tensor_tensor(out=tmpc[:, j, :], in0=tmpc[:, j, :], in1=var[:], op=ALU.mult),
                             reads=[tmpc, var], writes=[tmpc], partial=True)
                        S.op("act", lambda: nc.scalar.activation(out=cts_[:, j, :], in_=tmpc[:, j, :], func=AF.Silu,
                                                                 bias=lnb[:, j:j + 1], scale=lng[:, j:j + 1]),
                             reads=[tmpc, lnb, lng], writes=[cts_], partial=True)
                    S.dma("sp", lambda: nc.sync.dma_start(out=CTs.ap()[:, t0 + b0:t0 + b0 + BL].rearrange("(j p) t -> p j t", p=128), in_=cts_[:]),
                          reads=[cts_], writes=[CTs])
                S.end_phase()
        if stop_after == "4":
            break

        with ES() as st:
            wo = S.sb([128, 8, D], BF16, "wo", st)
            for h in range(2):
                S.dma("pool", lambda: nc.gpsimd.dma_start(out=wo[:, :, h * 512:(h + 1) * 512],
                                                          in_=w_out.ap()[l][:, h * 512:(h + 1) * 512].rearrange("(k p) n -> p k n", p=128)),
                      reads=[w_out], writes=[wo], partial=True)
            wr = S.sb([128, 8, NE], F32, "wr", st)
            S.dma("sp", lambda: nc.sync.dma_start(out=wr[:], in_=w_router.ap()[l].rearrange("(k p) e -> p k e", p=128)),
                  reads=[w_router], writes=[wr])
            cats = [S.sb([128, 8, 128], BF16, "cat", st) for _ in range(2)]
            xts = [S.sb([128, D], F32, "xt5", st) for _ in range(2)]
            tmp5 = S.sb([128, D], F32, "tmp5", st)
            x1s = [S.sb([128, D], F32, "x1", st) for _ in range(2)]
            sq5 = S.sb([128, D], F32, "sq5", st)
            ss5 = S.sb([128, 1], F32, "ss5", st)
            rstd5 = S.sb([128, 1], F32, "rstd5", st)
            h2f = S.sb([128, D], F32, "h2f", st)
            h2b = [S.sb([128, D], BF16, "h2b", st) for _ in range(2)]
            h2T = S.sb([128, 8, 128], F32, "h2T", st)
            lg = S.sb([128, NE], F32, "lg", st)
            mx5 = S.sb([128, 1], F32, "mx5", st)
            se5 = S.sb([128, 1], F32, "se5", st)
            ps5 = S.ps([128, D], F32, "ps5", st)
            pt5 = S.ps([128, 8, 128], F32, "pt5", st)
            pl5 = S.ps([128, NE], F32, "pl5", st)
            for i in range(NTT + 1):
                r0 = i * 128
                nr = 128 if i < NTT else NPAD
                S.dma("sp", lambda: nc.sync.dma_start(out=YACC[r0:r0 + nr, :], in_=zeros_f[0:nr, :]), reads=[zeros_f], writes=[YACC])
            for ti in range(ntq):
                r0 = ti * 128
                mod = mod_l if ti < NTL else mod_c
                cat, xt, x1, hb2 = cats[ti % 2], xts[ti % 2], x1s[ti % 2], h2b[ti % 2]
                S.dma("sp", lambda: nc.sync.dma_start(out=cat[:, 0:4, :], in_=AT.ap()[:, r0:r0 + 128].rearrange("(j p) t -> p j t", p=128)),
                      reads=[AT], writes=[cat], partial=True)
                S.dma("sp", lambda: nc.sync.dma_start(out=cat[:, 4:6, :], in_=BT.ap()[:, r0:r0 + 128].rearrange("(j p) t -> p j t", p=128)),
                      reads=[BT], writes=[cat], partial=True)
                S.dma("sp", lambda: nc.sync.dma_start(out=cat[:, 6:8, :], in_=CTs.ap()[:, r0:r0 + 128].rearrange("(j p) t -> p j t", p=128)),
                      reads=[CTs], writes=[cat], partial=True)
                S.dma("sp", lambda: nc.sync.dma_start(out=xt[:], in_=X[r0:r0 + 128, :]), reads=[X], writes=[xt])
                for hh in range(2):
                    for k in range(8):
                        S.op("pe", lambda: nc.tensor.matmul(ps5[:, hh * 512:(hh + 1) * 512], lhsT=cat[:, k, :], rhs=wo[:, k, hh * 512:(hh + 1) * 512],
                                                            start=(k == 0), stop=(k == 7)), reads=[cat, wo], writes=[ps5],
                             partial=not (hh == 0 and k == 0))
                S.op("dve", lambda: nc.vector.tensor_tensor(out=tmp5[:], in0=ps5[:], in1=mod[:, 2 * D:3 * D], op=ALU.mult),
                     reads=[ps5, mod], writes=[tmp5])
                S.op("pool", lambda: nc.gpsimd.tensor_tensor(out=x1[:], in0=tmp5[:], in1=xt[:], op=ALU.add), reads=[tmp5, xt], writes=[x1])
                S.dma("sp", lambda: nc.sync.dma_start(out=X[r0:r0 + 128, :], in_=x1[:]), reads=[x1], writes=[X])
                S.op("act", lambda: nc.scalar.activation(out=sq5[:], in_=x1[:], func=AF.Square, accum_out=ss5[:]), reads=[x1], writes=[sq5, ss5])
                S.op("dve", lambda: nc.vector.tensor_scalar(out=rstd5[:], in0=ss5[:], scalar1=1.0 / D, scalar2=EPS, op0=ALU.mult, op1=ALU.add),
                     reads=[ss5], writes=[rstd5])
                S.op("act", lambda: nc.scalar.activation(out=rstd5[:], in_=rstd5[:], func=AF.Sqrt), reads=[rstd5], writes=[rstd5])
                S.op("dve", lambda: nc.vector.reciprocal(out=rstd5[:], in_=rstd5[:]), reads=[rstd5], writes=[rstd5])
                S.op("dve", lambda: nc.vector.scalar_tensor_tensor(out=h2f[:], in0=x1[:], scalar=rstd5[:, 0:1], in1=mod[:, 4 * D:5 * D],
                                                                   op0=ALU.mult, op1=ALU.mult), reads=[x1, rstd5, mod], writes=[h2f])
                S.op("pool", lambda: nc.gpsimd.tensor_tensor(out=h2f[:], in0=h2f[:], in1=mod[:, 3 * D:4 * D], op=ALU.add),
                     reads=[h2f, mod], writes=[h2f])
                S.op("act", lambda: nc.scalar.copy(out=hb2[:], in_=h2f[:]), reads=[h2f], writes=[hb2])
                S.dma("sp", lambda: nc.sync.dma_start(out=H2[r0:r0 + 128, :], in_=hb2[:]), reads=[hb2], writes=[H2])
                for k in range(8):
                    S.op("pe", lambda: nc.tensor.transpose(out=pt5[:, k, :], in_=h2f[:, k * 128:(k + 1) * 128], identity=idf[:]),
                         reads=[h2f, idf], writes=[pt5], partial=(k > 0))
                S.op("dve", lambda: nc.vector.tensor_copy(out=h2T[:], in_=pt5[:]), reads=[pt5], writes=[h2T])
                for k in range(8):
                    S.op("pe", lambda: nc.tensor.matmul(pl5[:], lhsT=h2T[:, k, :], rhs=wr[:, k, :], start=(k == 0), stop=(k == 7)),
                         reads=[h2T, wr], writes=[pl5], partial=(k > 0))
                S.op("dve", lambda: nc.vector.tensor_reduce(out=mx5[:], in_=pl5[:], axis=AX.X, op=ALU.max), reads=[pl5], writes=[mx5])
                S.op("dve", lambda: nc.vector.tensor_scalar(out=mx5[:], in0=mx5[:], scalar1=-1.0, scalar2=None, op0=ALU.mult),
                     reads=[mx5], writes=[mx5])
                S.op("act", lambda: nc.scalar.activation(out=lg[:], in_=pl5[:], func=AF.Exp, bias=mx5[:, 0:1], scale=1.0, accum_out=se5[:]),
                     reads=[pl5, mx5], writes=[lg, se5])
                S.op("dve", lambda: nc.vector.reciprocal(out=se5[:], in_=se5[:]), reads=[se5], writes=[se5])
                S.op("dve", lambda: nc.vector.tensor_scalar(out=AFF[:, ti, :], in0=lg[:], scalar1=se5[:, 0:1], scalar2=None, op0=ALU.mult),
                     reads=[lg, se5], writes=[AFF], partial=True)
            S.end_phase()
        if stop_after == "5":
            break

        nrt = ntq
        with ES() as st:
            utb = S.sb([128, 128], BF16, "utb", st)
            utf = S.sb([128, 128], F32, "utf", st)
            S.dma("sp", lambda: nc.sync.dma_start(out=utf[:], in_=utri_in[:, :]), reads=[utri_in], writes=[utf])
            S.op("dve", lambda: nc.vector.tensor_copy(out=utb[:], in_=utf[:]), reads=[utf], writes=[utb])
            lo = S.sb([128, 32], F32, "lo", st)
            hi = S.sb([128, 32], F32, "hi", st)
            mid = S.sb([128, 32], F32, "mid", st)
            tgt = S.sb([128, 32], F32, "tgt", st)
            ge = S.sb([128, 32], F32, "ge", st)
            d1 = S.sb([128, 32], F32, "d1", st)
            cmpb = S.sb([128, NTT, NE], F32, "cmpb", st)
            cntb = S.sb([128, 32], F32, "cntb", st)
            pc = S.ps([128, 32], F32, "pc", st)
            S.op("dve", lambda: nc.vector.memset(lo[:], 0.0), writes=[lo])
            S.op("dve", lambda: nc.vector.memset(hi[:], 1.0), writes=[hi])
            S.op("dve", lambda: nc.vector.memset(tgt[:, 0:16], float(CAP)), writes=[tgt], partial=True)
            S.op("dve", lambda: nc.vector.memset(tgt[:, 16:32], float(CCAP)), writes=[tgt], partial=True)
            S.op("dve", lambda: nc.vector.memset(cntb[:], 0.0), writes=[cntb])
            parts = [(0, NTL, 0)] + ([] if last else [(NTL, NTT, 16)])

            def compare(dst, thr):
                for (a, b_, c0) in parts:
                    S.op("dve", lambda: nc.vector.tensor_tensor(
                        out=dst[:, a:b_, :], in0=AFF[:, a:b_, :],
                        in1=thr[:, c0:c0 + 16].unsqueeze(1).to_broadcast([128, b_ - a, NE]), op=ALU.is_ge),
                        reads=[AFF, thr], writes=[dst], partial=True)

            for itn in range(40):
                S.op("dve", lambda: nc.vector.tensor_tensor(out=mid[:], in0=lo[:], in1=hi[:], op=ALU.add), reads=[lo, hi], writes=[mid])
                S.op("dve", lambda: nc.vector.tensor_scalar(out=mid[:], in0=mid[:], scalar1=0.5, scalar2=None, op0=ALU.mult),
                     reads=[mid], writes=[mid])
                compare(cmpb, mid)
                for (a, b_, c0) in parts:
                    S.op("dve", lambda: nc.vector.tensor_reduce(out=cntb[:, c0:c0 + 16], in_=cmpb[:, a:b_, :].rearrange("p t e -> p e t"),
                                                                axis=AX.X, op=ALU.add), reads=[cmpb], writes=[cntb], partial=True)
                S.op("pe", lambda: nc.tensor.matmul(pc[:], lhsT=ones_f[:], rhs=cntb[:], start=True, stop=True), reads=[ones_f, cntb], writes=[pc])
                S.op("dve", lambda: nc.vector.tensor_tensor(out=ge[:], in0=pc[:], in1=tgt[:], op=ALU.is_ge), reads=[pc, tgt], writes=[ge])
                S.op("dve", lambda: nc.vector.tensor_tensor(out=d1[:], in0=mid[:], in1=lo[:], op=ALU.subtract), reads=[mid, lo], writes=[d1])
                S.op("dve", lambda: nc.vector.tensor_tensor(out=d1[:], in0=d1[:], in1=ge[:], op=ALU.mult), reads=[d1, ge], writes=[d1])
                S.op("dve", lambda: nc.vector.tensor_tensor(out=lo[:], in0=lo[:], in1=d1[:], op=ALU.add), reads=[lo, d1], writes=[lo])
                S.op("dve", lambda: nc.vector.tensor_tensor(out=d1[:], in0=hi[:], in1=mid[:], op=ALU.subtract), reads=[hi, mid], writes=[d1])
                S.op("dve", lambda: nc.vector.tensor_tensor(out=d1[:], in0=d1[:], in1=ge[:], op=ALU.mult), reads=[d1, ge], writes=[d1])
                S.op("dve", lambda: nc.vector.tensor_tensor(out=hi[:], in0=mid[:], in1=d1[:], op=ALU.add), reads=[mid, d1], writes=[hi])
            maskb = S.sb([128, NTT, NE], BF16, "maskb", st)
            pref = S.sb([128, NTT, NE], F32, "pref", st)
            tcnt = S.sb([128, NTT, NE], F32, "tcnt", st)
            tcn2 = S.sb([128, NTT, NE], F32, "tcn2", st)
            S.op("dve", lambda: nc.vector.memset(cmpb[:], 0.0), writes=[cmpb])
            compare(cmpb, lo)
            S.op("dve", lambda: nc.vector.tensor_copy(out=maskb[:], in_=cmpb[:]), reads=[cmpb], writes=[maskb])
            ncol = NTT * NE
            mflat = maskb[:].rearrange("p t e -> p (t e)")
            pflat = pref[:].rearrange("p t e -> p (t e)")
            tflat = tcnt[:].rearrange("p t e -> p (t e)")
            pp = [S.ps([128, 512], F32, "pp", st) for _ in range(2)]
            for ci, c0 in enumerate(range(0, ncol, 512)):
                n = min(512, ncol - c0)
                S.op("pe", lambda: nc.tensor.matmul(pp[0][:, 0:n], lhsT=utb[:], rhs=mflat[:, c0:c0 + n], start=True, stop=True),
                     reads=[utb, maskb], writes=[pp[0]])
                S.op("pe", lambda: nc.tensor.matmul(pp[1][:, 0:n], lhsT=ones_b[:], rhs=mflat[:, c0:c0 + n], start=True, stop=True),
                     reads=[ones_b, maskb], writes=[pp[1]])
                S.op("dve", lambda: nc.vector.tensor_copy(out=pflat[:, c0:c0 + n], in_=pp[0][:, 0:n]), reads=[pp[0]], writes=[pref], partial=True)
                S.op("act", lambda: nc.scalar.copy(out=tflat[:, c0:c0 + n], in_=pp[1][:, 0:n]), reads=[pp[1]], writes=[tcnt], partial=True)
            src, dst = tcnt, tcn2
            S.op("dve", lambda: nc.vector.tensor_copy(out=tcn2[:], in_=tcnt[:]), reads=[tcnt], writes=[tcn2])
            cum = S.sb([128, NTT, NE], F32, "cum", st)
            S.op("dve", lambda: nc.vector.tensor_copy(out=cum[:], in_=tcnt[:]), reads=[tcnt], writes=[cum])
            a_, b2 = cum, tcn2
            sft = 1
            while sft < NTL:
                S.op("dve", lambda: nc.vector.tensor_tensor(out=b2[:, sft:NTL, :], in0=a_[:, sft:NTL, :], in1=a_[:, 0:NTL - sft, :], op=ALU.add),
                     reads=[a_], writes=[b2], partial=True)
                S.op("dve", lambda: nc.vector.tensor_copy(out=b2[:, 0:sft, :], in_=a_[:, 0:sft, :]), reads=[a_], writes=[b2], partial=True)
                a_, b2 = b2, a_
                sft *= 2
            inc = a_
            slot = S.sb([128, NTT, NE], F32, "slot", st)
            S.op("dve", lambda: nc.vector.tensor_tensor(out=slot[:, 0:NTL, :], in0=inc[:, 0:NTL, :], in1=tcnt[:, 0:NTL, :], op=ALU.subtract),
                 reads=[inc, tcnt], writes=[slot], partial=True)
            if not last:
                S.op("dve", lambda: nc.vector.memset(slot[:, NTL, :], float(CAP)), writes=[slot], partial=True)
                S.op("dve", lambda: nc.vector.tensor_scalar(out=slot[:, NTL + 1, :], in0=tcnt[:, NTL, :], scalar1=float(CAP), scalar2=None,
                                                            op0=ALU.add), reads=[tcnt], writes=[slot], partial=True)
            S.op("dve", lambda: nc.vector.tensor_tensor(out=slot[:, 0:nrt, :], in0=slot[:, 0:nrt, :], in1=pref[:, 0:nrt, :], op=ALU.add),
                 reads=[slot, pref], writes=[slot], partial=True)
            ebase = S.sb([128, NE], F32, "ebase", st)
            for e in range(NE):
                S.op("dve", lambda: nc.vector.memset(ebase[:, e:e + 1], float(e * MS)), writes=[ebase], partial=True)
            S.op("dve", lambda: nc.vector.tensor_tensor(out=slot[:, 0:nrt, :], in0=slot[:, 0:nrt, :],
                                                        in1=ebase[:].unsqueeze(1).to_broadcast([128, nrt, NE]), op=ALU.add),
                 reads=[slot, ebase], writes=[slot], partial=True)
            BIGI = float(1 << 20)
            S.op("dve", lambda: nc.vector.scalar_tensor_tensor(out=slot[:, 0:nrt, :].rearrange("p t e -> p (t e)"),
                                                               in0=slot[:, 0:nrt, :].rearrange("p t e -> p (t e)"), scalar=-BIGI,
                                                               in1=cmpb[:, 0:nrt, :].rearrange("p t e -> p (t e)"), op0=ALU.add, op1=ALU.mult),
                 reads=[slot, cmpb], writes=[slot], partial=True)
            S.op("dve", lambda: nc.vector.tensor_scalar(out=slot[:, 0:nrt, :], in0=slot[:, 0:nrt, :], scalar1=BIGI, scalar2=None, op0=ALU.add),
                 reads=[slot], writes=[slot], partial=True)
            idxi = S.sb([128, NTT, NE], I32, "idxi", st)
            S.op("dve", lambda: nc.vector.tensor_copy(out=idxi[:, 0:nrt, :], in_=slot[:, 0:nrt, :]), reads=[slot], writes=[idxi])
            tid = S.sb([128, NTT], I32, "tid", st)
            S.op("pool", lambda: nc.gpsimd.iota(tid[:], pattern=[[128, NTT]], base=0, channel_multiplier=1), writes=[tid])
            pay = S.sb([128, NTT, NE, 2], I32, "pay", st)
            S.op("dve", lambda: nc.vector.tensor_copy(out=pay[:, :, :, 0], in_=tid[:].unsqueeze(2).to_broadcast([128, NTT, NE])),
                 reads=[tid], writes=[pay], partial=True)
            S.op("dve", lambda: nc.vector.tensor_copy(out=pay[:].bitcast(F32)[:, :, :, 1], in_=AFF[:]), reads=[AFF], writes=[pay], partial=True)
            lflat = LST.ap().rearrange("e s c -> (e s) c")
            for ti in range(nrt):
                for e in range(NE):
                    S.dma("pool", lambda: nc.gpsimd.indirect_dma_start(
                        out=lflat, out_offset=bass.IndirectOffsetOnAxis(ap=idxi[:, ti, e:e + 1], axis=0),
                        in_=pay[:, ti, e, :], in_offset=None, bounds_check=bc_reg, oob_is_err=False),
                        reads=[pay, idxi], writes=[LST])
            if "route" in debug and l == 0:
                dbg["thr"] = S.dram("dbg_thr", [128, 32], F32, kind="ExternalOutput")
                S.dma("sp", lambda: nc.sync.dma_start(out=dbg["thr"][:, :], in_=lo[:]), reads=[lo], writes=[dbg["thr"]])
                dbg["idx"] = S.dram("dbg_idx", [128, NTT, NE], I32, kind="ExternalOutput")
                S.dma("sp", lambda: nc.sync.dma_start(out=dbg["idx"].ap(), in_=idxi[:]), reads=[idxi], writes=[dbg["idx"]])
            S.end_phase()
        if stop_after == "6":
            break

        with ES() as st:
            wg = S.sb([128, 8, FF], BF16, "wg", st)
            wu = S.sb([128, 8, FF], BF16, "wu", st)
            wd = S.sb([128, 16, D], BF16, "wd", st)
            xeT = S.sb([128, 8, 512], BF16, "xeT", st)
            hTe = S.sb([128, 16, 512], BF16, "hTe", st)
            idts = [S.sb([128, 2], I32, "idt", st) for _ in range(4)]
            xgs = [S.sb([128, D], BF16, "xg", st) for _ in range(2)]
            ysbs = [S.sb([128, D], F32, "ysb", st) for _ in range(2)]
            sgl = [S.sb([128, 512], F32, "sgl", st) for _ in range(2)]
            ptx = S.ps([128, 8, 128], BF16, "ptx", st)
            pgs = [S.ps([128, 512], F32, "pg", st) for _ in range(2)]
            pus = [S.ps([128, 512], F32, "pu", st) for _ in range(2)]
            pys = [S.ps([128, 512], F32, "py", st) for _ in range(2)]
            groups = [(0, 512), (512, 512)] + ([] if last else [(1024, 128)])
            prev_sc = []
            nfc = 0
            nys = 0
            for e in range(NE):
                for h in range(4):
                    S.dma("pool", lambda: nc.gpsimd.dma_start(out=wg[:, :, h * 512:(h + 1) * 512],
                                                              in_=w_gate.ap()[l, e][:, h * 512:(h + 1) * 512].rearrange("(k p) f -> p k f", p=128)),
                          reads=[w_gate], writes=[wg], partial=(h > 0))
                for h in range(4):
                    S.dma("pool", lambda: nc.gpsimd.dma_start(out=wu[:, :, h * 512:(h + 1) * 512],
                                                              in_=w_up.ap()[l, e][:, h * 512:(h + 1) * 512].rearrange("(k p) f -> p k f", p=128)),
                          reads=[w_up], writes=[wu], partial=(h > 0))
                for h in range(2):
                    S.dma("pool", lambda: nc.gpsimd.dma_start(out=wd[:, :, h * 512:(h + 1) * 512],
                                                              in_=w_down.ap()[l, e][:, h * 512:(h + 1) * 512].rearrange("(k p) n -> p k n", p=128)),
                          reads=[w_down], writes=[wd], partial=(h > 0))
                cur_sc = []
                for (s0, N) in groups:
                    nst = N // 128
                    for si in range(nst):
                        idt = idts[si]
                        xg = xgs[si % 2]
                        S.dma("sp", lambda: nc.sync.dma_start(out=idt[:], in_=LST[e, s0 + si * 128:s0 + (si + 1) * 128, :]),
                              reads=[LST], writes=[idt])
                        S.dma("pool", lambda: nc.gpsimd.indirect_dma_start(
                            out=xg[:], out_offset=None, in_=H2.ap(),
                            in_offset=bass.IndirectOffsetOnAxis(ap=idt[:, 0:1], axis=0)), reads=[H2, idt], writes=[xg])
                        for k in range(8):
                            S.op("pe", lambda: nc.tensor.transpose(out=ptx[:, k, :], in_=xg[:, k * 128:(k + 1) * 128], identity=idb[:]),
                                 reads=[xg, idb], writes=[ptx], partial=(k > 0))
                        S.op("dve", lambda: nc.vector.tensor_copy(out=xeT[:, :, si * 128:(si + 1) * 128], in_=ptx[:]),
                             reads=[ptx], writes=[xeT], partial=True)
                    for fc in range(16):
                        pg, pu, sg_ = pgs[nfc % 2], pus[nfc % 2], sgl[nfc % 2]
                        nfc += 1
                        for k in range(8):
                            S.op("pe", lambda: nc.tensor.matmul(pg[:, 0:N], lhsT=wg[:, k, fc * 128:(fc + 1) * 128], rhs=xeT[:, k, 0:N],
                                                                start=(k == 0), stop=(k == 7)), reads=[wg, xeT], writes=[pg], partial=(k > 0))
                        for k in range(8):
                            S.op("pe", lambda: nc.tensor.matmul(pu[:, 0:N], lhsT=wu[:, k, fc * 128:(fc + 1) * 128], rhs=xeT[:, k, 0:N],
                                                                start=(k == 0), stop=(k == 7)), reads=[wu, xeT], writes=[pu], partial=(k > 0))
                        S.op("act", lambda: nc.scalar.activation(out=sg_[:, 0:N], in_=pg[:, 0:N], func=AF.Silu), reads=[pg], writes=[sg_])
                        S.op("dve", lambda: nc.vector.tensor_tensor(out=hTe[:, fc, 0:N], in0=sg_[:, 0:N], in1=pu[:, 0:N], op=ALU.mult),
                             reads=[sg_, pu], writes=[hTe], partial=True)
                    for si in range(nst):
                        idt = idts[si]
                        ysb = ysbs[nys % 2]
                        nys += 1
                        for dh in range(2):
                            py = pys[dh]
                            for fc in range(16):
                                S.op("pe", lambda: nc.tensor.matmul(py[:], lhsT=hTe[:, fc, si * 128:(si + 1) * 128], rhs=wd[:, fc, dh * 512:(dh + 1) * 512],
                                                                    start=(fc == 0), stop=(fc == 15)), reads=[hTe, wd], writes=[py], partial=(fc > 0))
                            if dh == 0:
                                S.op("dve", lambda: nc.vector.tensor_scalar(out=ysb[:, 0:512], in0=py[:], scalar1=idt[:].bitcast(F32)[:, 1:2],
                                                                            scalar2=None, op0=ALU.mult), reads=[py, idt], writes=[ysb], partial=True)
                            else:
                                S.op("act", lambda: nc.scalar.activation(out=ysb[:, 512:1024], in_=py[:], func=AF.Copy,
                                                                         scale=idt[:].bitcast(F32)[:, 1:2]), reads=[py, idt], writes=[ysb], partial=True)
                        tok = S.dma("pool", lambda: nc.gpsimd.indirect_dma_start(
                            out=YACC.ap(), out_offset=bass.IndirectOffsetOnAxis(ap=idt[:, 0:1], axis=0),
                            in_=ysb[:], in_offset=None, compute_op=ALU.add), reads=[ysb, idt], writes=[YACC], after=prev_sc)
                        cur_sc.append(tok)
                prev_sc = cur_sc[-2:]
            S.end_phase()
        if stop_after == "7":
            break

        with ES() as st:
            xt8 = [S.sb([128, D], F32, "xt8", st) for _ in range(2)]
            yt8 = [S.sb([128, D], F32, "yt8", st) for _ in range(2)]
            xo8 = [S.sb([128, D], F32, "xo8", st) for _ in range(2)]
            sq8 = S.sb([128, D], F32, "sq8", st)
            ss8 = S.sb([128, 1], F32, "ss8", st)
            fg = S.sb([128, D], F32, "fg", st)
            if last:
                S.dma("sp", lambda: nc.sync.dma_start(out=fg[:], in_=final_g.ap().partition_broadcast(128)), reads=[final_g], writes=[fg])
            for ti in range(ntq):
                r0 = ti * 128
                mod = mod_l if ti < NTL else mod_c
                xt, yt, xo = xt8[ti % 2], yt8[ti % 2], xo8[ti % 2]
                S.dma("sp", lambda: nc.sync.dma_start(out=xt[:], in_=X[r0:r0 + 128, :]), reads=[X], writes=[xt])
                S.dma("sp", lambda: nc.sync.dma_start(out=yt[:], in_=YACC[r0:r0 + 128, :]), reads=[YACC], writes=[yt])
                S.op("dve", lambda: nc.vector.tensor_tensor(out=yt[:], in0=yt[:], in1=mod[:, 5 * D:6 * D], op=ALU.mult), reads=[yt, mod], writes=[yt])
                S.op("pool", lambda: nc.gpsimd.tensor_tensor(out=xo[:], in0=yt[:], in1=xt[:], op=ALU.add), reads=[yt, xt], writes=[xo])
                if not last:
                    S.dma("sp", lambda: nc.sync.dma_start(out=X[r0:r0 + 128, :], in_=xo[:]), reads=[xo], writes=[X])
                else:
                    S.op("act", lambda: nc.scalar.activation(out=sq8[:], in_=xo[:], func=AF.Square, accum_out=ss8[:]), reads=[xo], writes=[sq8, ss8])
                    S.op("dve", lambda: nc.vector.tensor_scalar(out=ss8[:], in0=ss8[:], scalar1=1.0 / D, scalar2=EPS, op0=ALU.mult, op1=ALU.add),
                         reads=[ss8], writes=[ss8])
                    S.op("act", lambda: nc.scalar.activation(out=ss8[:], in_=ss8[:], func=AF.Sqrt), reads=[ss8], writes=[ss8])
                    S.op("dve", lambda: nc.vector.reciprocal(out=ss8[:], in_=ss8[:]), reads=[ss8], writes=[ss8])
                    S.op("dve", lambda: nc.vector.scalar_tensor_tensor(out=xt[:], in0=xo[:], scalar=ss8[:, 0:1], in1=fg[:], op0=ALU.mult, op1=ALU.mult),
                         reads=[xo, ss8, fg], writes=[xt])
                    S.dma("sp", lambda: nc.sync.dma_start(out=out[r0:r0 + 128, :], in_=xt[:]), reads=[xt], writes=[out])
            S.end_phase()
        if stop_after == "8":
            break

    def tap(name, buf, shape, dt):
        o = S.dram("dbg_" + name, shape, dt, kind="ExternalOutput")
        S.dma("sp", lambda: nc.sync.dma_start(out=o.ap(), in_=buf.ap()), reads=[buf], writes=[o])

    for name in debug:
        if name == "QT":
            tap("QT", QT, [128, 4, TT], BF16)
        if name == "KT":
            tap("KT", KT, [128, TT], BF16)
        if name == "VA":
            tap("VA", VA, [TT, 130], BF16)
        if name == "QBT":
            tap("QBT", QBT, [128, 2, TT], BF16)
        if name == "KBT":
            tap("KBT", KBT, [128, 2, TT], BF16)
        if name == "VB":
            tap("VB", VB, [TT, 256], BF16)
        if name == "UT":
            tap("UT", UT, [256, TT], F32)
        if name == "X":
            tap("X", X, [TT, D], F32)
        if name == "AT":
            tap("AT", AT, [512, TT], BF16)
        if name == "BT":
            tap("BT", BT, [256, TT], BF16)
        if name == "CTs":
            tap("CTs", CTs, [256, TT], BF16)
        if name == "H2":
            tap("H2", H2, [TT + NPAD, D], BF16)
        if name == "LST":
            tap("LST", LST, [NE, MS, 2], I32)
        if name == "YACC":
            tap("YACC", YACC, [TT + NPAD, D], F32)
        if name == "AFF":
            o_ = S.dram("dbg_AFF", [128, NTT, NE], F32, kind="ExternalOutput")
            S.dma("sp", lambda: nc.sync.dma_start(out=o_.ap(), in_=AFF[:]), reads=[AFF], writes=[o_])
    S.barrier()
    return nc


def host_consts(na_rpb, n_layers):
    t = np.arange(T)
    row = (t // 64).astype(np.float64)
    col = (t % 64).astype(np.float64)
    inv = 10000.0 ** (-np.arange(16, dtype=np.float64) / 16)
    rc = np.ones((TT, 64), np.float32)
    rs = np.zeros((TT, 64), np.float32)
    for a, pos in enumerate((row, col)):
        ang = (pos.astype(np.float32)[:, None] * inv.astype(np.float32)[None, :]).astype(np.float32)
        cs, sn = np.cos(ang), np.sin(ang)
        rc[:T, a * 32:a * 32 + 16] = cs
        rc[:T, a * 32 + 16:a * 32 + 32] = cs
        rs[:T, a * 32:a * 32 + 16] = -sn
        rs[:T, a * 32 + 16:a * 32 + 32] = sn
    q = np.arange(64)
    cs0 = np.clip(q - 8, 0, 48)
    c = np.arange(64)
    valid = (c[None, :] >= cs0[:, None]) & (c[None, :] < cs0[:, None] + 16)
    dc = np.clip(c[None, :] - q[:, None] + 15, 0, 30)
    nab = np.full((n_layers, 8, 64, 4, 8, 64), NEG, np.float32)
    for off in range(8):
        for j in range(8):
            g = na_rpb[:n_layers, :, off + j, :][:, :, dc]
            g = np.where(valid[None, None], g, np.float32(NEG))
            nab[:, off, :, :, j, :] = np.transpose(g, (0, 2, 1, 3))
    lpad = np.zeros((NPAD, 2), np.int32)
    lpad[:, 0] = TT + np.arange(NPAD)
    return {"ident": np.eye(128, dtype=np.float32), "utri": np.triu(np.ones((128, 128), np.float32), 1), "rope_c": rc, "rope_s": rs,
            "nabias": nab.reshape(n_layers, 8, 64, 4, 512), "lpad": lpad}


_WNAMES = ["w_ada", "b_ada", "norm1_g", "norm2_g", "w_in", "q_norm_g", "k_norm_g", "conv_w", "conv_b",
           "conv_ln_g", "conv_ln_b", "w_out", "w_router", "w_gate", "w_up", "w_down"]


def make_in_maps(inputs, n_layers, samples):
    consts = host_consts(np.asarray(inputs["na_rpb"]), n_layers)
    maps = []
    for b in samples:
        m = {"x": np.ascontiguousarray(inputs["x"][b]), "ctx": np.ascontiguousarray(inputs["ctx"][b]),
             "c": np.ascontiguousarray(inputs["c"][b]), "c_ctx": np.ascontiguousarray(inputs["c_ctx"]),
             "final_norm_g": np.ascontiguousarray(inputs["final_norm_g"])}
        for k in _WNAMES:
            m[k] = np.ascontiguousarray(inputs[k][:n_layers])
        m.update(consts)
        maps.append(m)
    return maps


def kernel(**inputs):
    inputs = {k: np.asarray(v) for k, v in inputs.items()}
    nc = build_program(DEPTH)
    maps = make_in_maps(inputs, DEPTH, range(4))
    res = run_bass_kernel_spmd(nc, maps, core_ids=list(range(4)))
    return np.stack([r["out"] for r in res.results], 0).astype(np.float32)
```

```python
import contextlib
import numpy as np
import concourse.bass as bass
import concourse.mybir as mybir
from concourse.bass_utils import run_bass_kernel_spmd

F32 = mybir.dt.float32
BF16 = mybir.dt.bfloat16
I32 = mybir.dt.int32
AF = mybir.ActivationFunctionType
ALU = mybir.AluOpType
AX = mybir.AxisListType

D = 1024
T = 8192
CT = 256
TT = T + CT
NTL = T // 128
NTT = TT // 128
DEPTH = 4
NE = 16
FF = 2048
CAP = 1024
CCAP = 32
NPAD = 96
MS = CAP + CCAP + NPAD
EPS = 1e-6
NEG = -30000.0
P2_LIMIT = 0


class Buf:
    def __init__(self, t, name, space):
        self.t = t
        self.name = name
        self.space = space
        self.writes = {}
        self.reads = {}
        self.sem = None
        self.total = 0

    def __getitem__(self, idx):
        return self.t[idx]

    def ap(self):
        return self.t.ap()


class ModSlice(Buf):
    def __getitem__(self, idx):
        p, sl = idx
        return self.t[p, sl.start - self.off:sl.stop - self.off]


class Sched:
    def __init__(self, nc):
        self.nc = nc
        self.eng = {"pe": nc.tensor, "act": nc.scalar, "dve": nc.vector,
                    "pool": nc.gpsimd, "sp": nc.sync}
        self.psem = {k: nc.alloc_semaphore("prog_" + k) for k in self.eng}
        self.cnt = {k: 0 for k in self.eng}
        self.seen = {k: {} for k in self.eng}
        self.sem_pool = []
        self.live = []
        self.nbuf = 0
        self.nsem = 0

    def sb(self, shape, dtype, name=None, stack=None):
        self.nbuf += 1
        name = (name or "sb") + "_%d" % self.nbuf
        if stack is None:
            t = self.nc.alloc_sbuf_tensor(name, list(shape), dtype)
        else:
            t = stack.enter_context(self.nc.sbuf_tensor(name, list(shape), dtype))
        b = Buf(t, name, "sb")
        b.local = stack is not None
        return b

    def ps(self, shape, dtype=F32, name=None, stack=None):
        self.nbuf += 1
        name = (name or "ps") + "_%d" % self.nbuf
        if stack is None:
            t = self.nc.alloc_psum_tensor(name, list(shape), dtype)
        else:
            t = stack.enter_context(self.nc.psum_tensor(name, list(shape), dtype))
        b = Buf(t, name, "ps")
        b.local = stack is not None
        return b

    def dram(self, name, shape, dtype, kind="Internal"):
        b = Buf(self.nc.dram_tensor(name, list(shape), dtype, kind=kind), name, "dram")
        b.local = False
        return b

    def _wait(self, e, tok):
        sem, v = tok
        key = id(sem)
        if self.seen[e].get(key, 0) >= v:
            return
        self.seen[e][key] = v
        self.eng[e].wait_ge(sem, v)

    def _deps(self, e, reads, writes):
        own = id(self.psem[e])
        toks = []
        for r in reads:
            if r.space == "dram":
                continue
            for t in r.writes.values():
                if id(t[0]) == own and e == "pe":
                    continue
                toks.append(t)
        for w in writes:
            if w.space == "dram":
                continue
            for t in list(w.writes.values()) + list(w.reads.values()):
                if id(t[0]) == own:
                    continue
                toks.append(t)
        for t in toks:
            self._wait(e, t)

    def _record(self, tok, reads, writes, partial):
        k = id(tok[0])
        for r in reads:
            if r.space != "dram":
                r.reads[k] = tok
        for w in writes:
            if w.space == "dram":
                continue
            if partial:
                w.writes[k] = tok
            else:
                w.writes = {k: tok}
                w.reads = {}

    def op(self, e, ins, reads=(), writes=(), partial=False):
        self._deps(e, reads, writes)
        i = ins()
        self.cnt[e] += 1
        i.then_inc(self.psem[e], 1)
        tok = (self.psem[e], self.cnt[e])
        self._record(tok, reads, writes, partial)
        return tok

    def dma(self, q, mk, reads=(), writes=(), partial=False, after=()):
        self._deps(q, reads, writes)
        for t in after:
            self._wait(q, t)
        owner = None
        for b in list(writes) + list(reads):
            if b.space != "dram":
                owner = b
                break
        if owner is None:
            owner = (list(writes) + list(reads))[0]
        if owner.sem is None:
            if self.sem_pool:
                owner.sem, owner.total = self.sem_pool.pop()
            else:
                self.nsem += 1
                owner.sem = self.nc.alloc_semaphore("dsem%d" % self.nsem)
                owner.total = 0
            self.live.append(owner)
        i = mk()
        owner.total += 16
        i.then_inc(owner.sem, 16)
        tok = (owner.sem, owner.total)
        self._record(tok, reads, writes, partial)
        return tok

    def barrier(self):
        toks = [(self.psem[k], self.cnt[k]) for k in self.eng if self.cnt[k] > 0]
        toks += [(b.sem, b.total) for b in self.live]
        for e in self.eng:
            for t in toks:
                if id(t[0]) == id(self.psem[e]):
                    continue
                self._wait(e, t)

    def end_phase(self):
        self.barrier()
        keep = []
        for b in self.live:
            if b.local:
                self.sem_pool.append((b.sem, b.total))
                b.sem = None
            else:
                keep.append(b)
        self.live = keep


def build_program(n_layers=DEPTH, debug=(), stop_after=None):
    nc = bass.Bass("TRN2", target_bir_lowering=False)
    S = Sched(nc)
    ES = contextlib.ExitStack
    bc_reg = nc.gpsimd.to_reg(NE * MS - 1)
    L = n_layers

    def din(name, shape, dt=F32):
        return S.dram(name, shape, dt, kind="ExternalInput")

    x_in = din("x", [T, D])
    ctx_in = din("ctx", [CT, D])
    c_in = din("c", [D])
    cc_in = din("c_ctx", [D])
    w_ada = din("w_ada", [L, D, 6 * D])
    b_ada = din("b_ada", [L, 6 * D])
    norm1_g = din("norm1_g", [L, D])
    norm2_g = din("norm2_g", [L, D])
    w_in = din("w_in", [L, D, 2048])
    q_norm_g = din("q_norm_g", [L, 64])
    k_norm_g = din("k_norm_g", [L, 64])
    conv_w = din("conv_w", [L, 31, 256])
    conv_b = din("conv_b", [L, 256])
    conv_ln_g = din("conv_ln_g", [L, 256])
    conv_ln_b = din("conv_ln_b", [L, 256])
    w_out = din("w_out", [L, D, D])
    w_router = din("w_router", [L, D, NE])
    has_moe = stop_after is None or stop_after in ("7", "8")
    if has_moe:
        w_gate = din("w_gate", [L, NE, D, FF])
        w_up = din("w_up", [L, NE, D, FF])
        w_down = din("w_down", [L, NE, FF, D])
    final_g = din("final_norm_g", [D])
    ident_in = din("ident", [128, 128])
    rope_c = din("rope_c", [TT, 64])
    rope_s = din("rope_s", [TT, 64])
    nabias = din("nabias", [L, 8, 64, 4, 512])
    lpad = din("lpad", [NPAD, 2], I32)
    utri_in = din("utri", [128, 128])
    out = S.dram("out", [T, D], F32, kind="ExternalOutput")
    dbg = {}

    X = S.dram("X", [TT, D], F32)
    QT = S.dram("QT", [128, 4, TT], BF16)
    KT = S.dram("KT", [128, TT], BF16)
    VA = S.dram("VA", [TT, 130], BF16)
    QBT = S.dram("QBT", [128, 2, TT], BF16)
    KBT = S.dram("KBT", [128, 2, TT], BF16)
    VB = S.dram("VB", [TT, 256], BF16)
    UT = S.dram("UT", [256, TT], F32)
    AT = S.dram("AT", [512, TT], BF16)
    BT = S.dram("BT", [256, TT], BF16)
    CTs = S.dram("CTs", [256, TT], BF16)
    H2 = S.dram("H2", [TT + NPAD, D], BF16)
    YACC = S.dram("YACC", [TT + NPAD, D], F32)
    LST = S.dram("LST", [NE, MS, 2], I32)

    idf = S.sb([128, 128], F32, "idf")
    idb = S.sb([128, 128], BF16, "idb")
    ones_b = S.sb([128, 128], BF16, "ones_b")
    ones_f = S.sb([128, 128], F32, "ones_f")
    zeros_f = S.sb([128, D], F32, "zeros_f")
    MODS = S.dram("MODS", [2, 128, 6 * D], F32)

    def load_mod(st, off, n):
        res_ = []
        for w_ in range(2):
            b_ = S.sb([128, n], F32, "modw", st)
            b_.__class__ = ModSlice
            b_.off = off
            S.dma("sp", lambda: nc.sync.dma_start(out=b_.t[:], in_=MODS[w_, :, off:off + n]), reads=[MODS], writes=[b_])
            res_.append(b_)
        return res_
    crep_l = S.sb([128, 8, 128], BF16, "crep_l")
    crep_c = S.sb([128, 8, 128], BF16, "crep_c")
    AFF = S.sb([128, NTT, NE], F32, "AFF")

    def v_(e):
        return {"dve": nc.vector, "pool": nc.gpsimd}[e]

    S.dma("sp", lambda: nc.sync.dma_start(out=idf[:], in_=ident_in[:, :]), reads=[ident_in], writes=[idf])
    S.op("dve", lambda: nc.vector.tensor_copy(out=idb[:], in_=idf[:]), reads=[idf], writes=[idb])
    S.op("dve", lambda: nc.vector.memset(ones_b[:], 1.0), writes=[ones_b])
    S.op("dve", lambda: nc.vector.memset(ones_f[:], 1.0), writes=[ones_f])
    S.op("dve", lambda: nc.vector.memset(zeros_f[:], 0.0), writes=[zeros_f])
    with ES() as st:
        cT = S.sb([128, 2, 8], F32, "cT", st)
        with nc.allow_non_contiguous_dma(reason="tiny"):
            S.dma("sp", lambda: nc.sync.dma_start(out=cT[:, 0, :], in_=c_in.ap().rearrange("(k p) -> p k", p=128)),
                  reads=[c_in], writes=[cT], partial=True)
            S.dma("sp", lambda: nc.sync.dma_start(out=cT[:, 1, :], in_=cc_in.ap().rearrange("(k p) -> p k", p=128)),
                  reads=[cc_in], writes=[cT], partial=True)
        sT = S.sb([128, 2, 8], F32, "sT", st)
        S.op("act", lambda: nc.scalar.activation(out=sT[:], in_=cT[:], func=AF.Silu), reads=[cT], writes=[sT])
        for k in range(8):
            S.op("dve", lambda: nc.vector.tensor_scalar(out=crep_l[:, k, :], in0=ones_b[:], scalar1=sT[:, 0, k:k + 1],
                                                        scalar2=None, op0=ALU.mult),
                 reads=[ones_b, sT], writes=[crep_l], partial=True)
            S.op("dve", lambda: nc.vector.tensor_scalar(out=crep_c[:, k, :], in0=ones_b[:], scalar1=sT[:, 1, k:k + 1],
                                                        scalar2=None, op0=ALU.mult),
                 reads=[ones_b, sT], writes=[crep_c], partial=True)
        zb = S.sb([NPAD, D], BF16, "zb", st)
        S.op("dve", lambda: nc.vector.memset(zb[:], 0.0), writes=[zb])
        S.dma("sp", lambda: nc.sync.dma_start(out=H2[TT:TT + NPAD, :], in_=zb[:]), reads=[zb], writes=[H2])
        lp = S.sb([NPAD, 2], I32, "lp", st)
        S.dma("sp", lambda: nc.sync.dma_start(out=lp[:], in_=lpad[:, :]), reads=[lpad], writes=[lp])
        for e in range(NE):
            S.dma("sp", lambda: nc.sync.dma_start(out=LST[e, CAP + CCAP:MS, :], in_=lp[:]), reads=[lp], writes=[LST])
        xts = [S.sb([128, D], F32, "xcp", st) for _ in range(2)]
        for i in range(NTT):
            xt = xts[i % 2]
            src = x_in[i * 128:(i + 1) * 128, :] if i < NTL else ctx_in[(i - NTL) * 128:(i - NTL + 1) * 128, :]
            S.dma("sp", lambda: nc.sync.dma_start(out=xt[:], in_=src), reads=[x_in], writes=[xt])
            S.dma("sp", lambda: nc.sync.dma_start(out=X[i * 128:(i + 1) * 128, :], in_=xt[:]), reads=[xt], writes=[X])
        S.end_phase()

    for l in range(L):
        last = (l == DEPTH - 1)
        ntq = NTL if last else NTT

        with ES() as st:
            mod_l = S.sb([128, 6 * D], F32, "mod_l", st)
            mod_c = S.sb([128, 6 * D], F32, "mod_c", st)
            brep = S.sb([128, 6 * D], F32, "brep", st)
            S.dma("sp", lambda: nc.sync.dma_start(out=brep[:], in_=b_ada.ap()[l].partition_broadcast(128)),
                  reads=[b_ada], writes=[brep])
            g1rep = S.sb([128, D], F32, "g1rep", st)
            g2rep = S.sb([128, D], F32, "g2rep", st)
            S.dma("sp", lambda: nc.sync.dma_start(out=g1rep[:], in_=norm1_g.ap()[l].partition_broadcast(128)),
                  reads=[norm1_g], writes=[g1rep])
            S.dma("sp", lambda: nc.sync.dma_start(out=g2rep[:], in_=norm2_g.ap()[l].partition_broadcast(128)),
                  reads=[norm2_g], writes=[g2rep])
            was = [S.sb([128, 8, 512], BF16, "wa", st) for _ in range(2)]
            pms = [S.ps([128, 512], F32, "pm", st) for _ in range(2)]
            for cc in range(12):
                wa = was[cc % 2]
                S.dma("pool", lambda: nc.gpsimd.dma_start(
                    out=wa[:], in_=w_ada.ap()[l][:, cc * 512:(cc + 1) * 512].rearrange("(k p) n -> p k n", p=128)),
                    reads=[w_ada], writes=[wa])
                for which, (crep, mod) in enumerate(((crep_l, mod_l), (crep_c, mod_c))):
                    pm = pms[which]
                    for k in range(8):
                        S.op("pe", lambda: nc.tensor.matmul(pm[:], lhsT=crep[:, k, :], rhs=wa[:, k, :],
                                                            start=(k == 0), stop=(k == 7)),
                             reads=[crep, wa], writes=[pm], partial=(k > 0))
                    S.op("dve", lambda: nc.vector.tensor_tensor(out=mod[:, cc * 512:(cc + 1) * 512], in0=pm[:],
                                                                in1=brep[:, cc * 512:(cc + 1) * 512], op=ALU.add),
                         reads=[pm, brep], writes=[mod], partial=True)
            for mod in (mod_l, mod_c):
                S.op("dve", lambda: nc.vector.scalar_tensor_tensor(out=mod[:, D:2 * D], in0=mod[:, D:2 * D], scalar=1.0,
                                                                   in1=g1rep[:], op0=ALU.add, op1=ALU.mult),
                     reads=[mod, g1rep], writes=[mod], partial=True)
                S.op("dve", lambda: nc.vector.scalar_tensor_tensor(out=mod[:, 4 * D:5 * D], in0=mod[:, 4 * D:5 * D],
                                                                   scalar=1.0, in1=g2rep[:], op0=ALU.add, op1=ALU.mult),
                     reads=[mod, g2rep], writes=[mod], partial=True)
            S.dma("sp", lambda: nc.sync.dma_start(out=MODS[0], in_=mod_l[:]), reads=[mod_l], writes=[MODS])
            S.dma("sp", lambda: nc.sync.dma_start(out=MODS[1], in_=mod_c[:]), reads=[mod_c], writes=[MODS])
            S.end_phase()
        if stop_after == "M":
            break

        with ES() as st:
            mod_l, mod_c = load_mod(st, 0, 2 * D)
            win = S.sb([128, 8, 2048], BF16, "win", st)
            for h in range(4):
                S.dma("pool", lambda: nc.gpsimd.dma_start(
                    out=win[:, :, h * 512:(h + 1) * 512],
                    in_=w_in.ap()[l][:, h * 512:(h + 1) * 512].rearrange("(k p) n -> p k n", p=128)),
                    reads=[w_in], writes=[win], partial=True)
            gq = S.sb([128, 64], F32, "gq", st)
            gk = S.sb([128, 64], F32, "gk", st)
            S.dma("sp", lambda: nc.sync.dma_start(out=gq[:], in_=q_norm_g.ap()[l].partition_broadcast(128)),
                  reads=[q_norm_g], writes=[gq])
            S.dma("sp", lambda: nc.sync.dma_start(out=gk[:], in_=k_norm_g.ap()[l].partition_broadcast(128)),
                  reads=[k_norm_g], writes=[gk])
            S.op("dve", lambda: nc.vector.tensor_scalar(out=gq[:], in0=gq[:], scalar1=0.125, scalar2=None, op0=ALU.mult),
                 reads=[gq], writes=[gq])
            xts = [S.sb([128, D], F32, "xt", st) for _ in range(2)]
            sq = S.sb([128, D], F32, "sq", st)
            ss = S.sb([128, 1], F32, "ss", st)
            rstd = S.sb([128, 1], F32, "rstd", st)
            hf = S.sb([128, D], F32, "hf", st)
            hb = S.sb([128, D], BF16, "hb", st)
            hT = S.sb([128, 8, 512], BF16, "hT", st)
            rc = [S.sb([128, 64], F32, "rc", st) for _ in range(2)]
            rs_ = [S.sb([128, 64], F32, "rs", st) for _ in range(2)]
            qsq = S.sb([128, 640], F32, "qsq", st)
            qss = S.sb([128, 10], F32, "qss", st)
            qn = S.sb([128, 640], F32, "qn", st)
            t1 = S.sb([128, 640], F32, "t1", st)
            t2 = S.sb([128, 640], F32, "t2", st)
            qr = S.sb([128, 640], BF16, "qr", st)
            qbk = S.sb([128, 512], BF16, "qbk", st)
            qTs = S.sb([128, 4, 512], BF16, "qTs", st)
            kTs = S.sb([128, 512], BF16, "kTs", st)
            vas = S.sb([128, 4, 130], BF16, "vas", st)
            qbTs = S.sb([128, 2, 512], BF16, "qbTs", st)
            kbTs = S.sb([128, 2, 512], BF16, "kbTs", st)
            vbs = S.sb([128, 4, 256], BF16, "vbs", st)
            sg = S.sb([128, 512], F32, "sg", st)
            uTs = S.sb([128, 2, 512], F32, "uTs", st)
            pT = S.ps([128, 8, 128], BF16, "pT", st)
            pmm = [S.ps([128, 512], F32, "pmm", st) for _ in range(3)]
            pq = S.ps([128, 4, 128], BF16, "pq", st)
            pk = S.ps([128, 5, 128], BF16, "pk", st)
            pf = [S.ps([128, 512], F32, "pf", st) for _ in range(2)]
            S.op("dve", lambda: nc.vector.memset(vas[:], 1.0), writes=[vas])
            A1 = lambda mod: mod[:, D:2 * D]
            S1 = lambda mod: mod[:, 0:D]

            groups = [(g * 512, 512, mod_l) for g in range(T // 512)] + [(T, CT, mod_c)]
            for (t0, G, mod) in groups:
                nsub = G // 128
                for s in range(nsub):
                    r0 = t0 + s * 128
                    xt = xts[s % 2]
                    S.dma("sp", lambda: nc.sync.dma_start(out=xt[:], in_=X[r0:r0 + 128, :]), reads=[X], writes=[xt])
                    S.op("act", lambda: nc.scalar.activation(out=sq[:], in_=xt[:], func=AF.Square, accum_out=ss[:]),
                         reads=[xt], writes=[sq, ss])
                    S.op("dve", lambda: nc.vector.tensor_scalar(out=rstd[:], in0=ss[:], scalar1=1.0 / D, scalar2=EPS,
                                                                op0=ALU.mult, op1=ALU.add), reads=[ss], writes=[rstd])
                    S.op("act", lambda: nc.scalar.activation(out=rstd[:], in_=rstd[:], func=AF.Sqrt), reads=[rstd], writes=[rstd])
                    S.op("dve", lambda: nc.vector.reciprocal(out=rstd[:], in_=rstd[:]), reads=[rstd], writes=[rstd])
                    S.op("dve", lambda: nc.vector.scalar_tensor_tensor(out=hf[:], in0=xt[:], scalar=rstd[:, 0:1], in1=A1(mod),
                                                                       op0=ALU.mult, op1=ALU.mult),
                         reads=[xt, rstd, mod], writes=[hf])
                    S.op("pool", lambda: nc.gpsimd.tensor_tensor(out=hb[:], in0=hf[:], in1=S1(mod), op=ALU.add),
                         reads=[hf, mod], writes=[hb])
                    for k in range(8):
                        S.op("pe", lambda: nc.tensor.transpose(out=pT[:, k, :], in_=hb[:, k * 128:(k + 1) * 128], identity=idb[:]),
                             reads=[hb, idb], writes=[pT], partial=(k > 0))
                    S.op("act", lambda: nc.scalar.copy(out=hT[:, :, s * 128:(s + 1) * 128], in_=pT[:]),
                         reads=[pT], writes=[hT], partial=True)
                    for cg_ in range(3):
                        pm = pmm[cg_]
                        for k in range(8):
                            S.op("pe", lambda: nc.tensor.matmul(pm[:], lhsT=hT[:, k, s * 128:(s + 1) * 128],
                                                                rhs=win[:, k, cg_ * 512:(cg_ + 1) * 512],
                                                                start=(k == 0), stop=(k == 7)),
                                 reads=[hT, win], writes=[pm], partial=(k > 0))
                    rcb, rsb = rc[s % 2], rs_[s % 2]
                    S.dma("sp", lambda: nc.sync.dma_start(out=rcb[:], in_=rope_c[r0:r0 + 128, :]), reads=[rope_c], writes=[rcb])
                    S.dma("sp", lambda: nc.sync.dma_start(out=rsb[:], in_=rope_s[r0:r0 + 128, :]), reads=[rope_s], writes=[rsb])
                    S.op("act", lambda: nc.scalar.activation(out=qsq[:, 0:512], in_=pmm[0][:], func=AF.Square),
                         reads=[pmm[0]], writes=[qsq], partial=True)
                    S.op("act", lambda: nc.scalar.activation(out=qsq[:, 512:640], in_=pmm[1][:, 0:128], func=AF.Square),
                         reads=[pmm[1]], writes=[qsq], partial=True)
                    S.op("dve", lambda: nc.vector.tensor_reduce(out=qss[:], in_=qsq[:].rearrange("p (h d) -> p h d", d=64),
                                                                axis=AX.X, op=ALU.add), reads=[qsq], writes=[qss])
                    S.op("dve", lambda: nc.vector.tensor_scalar(out=qss[:], in0=qss[:], scalar1=1.0 / 64, scalar2=EPS,
                                                                op0=ALU.mult, op1=ALU.add), reads=[qss], writes=[qss])
                    S.op("act", lambda: nc.scalar.activation(out=qss[:], in_=qss[:], func=AF.Sqrt), reads=[qss], writes=[qss])
                    S.op("dve", lambda: nc.vector.reciprocal(out=qss[:], in_=qss[:]), reads=[qss], writes=[qss])
                    S.op("dve", lambda: nc.vector.tensor_tensor(
                        out=qn[:, 0:512].rearrange("p (h d) -> p h d", d=64), in0=pmm[0][:].rearrange("p (h d) -> p h d", d=64),
                        in1=qss[:, 0:8].unsqueeze(2).to_broadcast([128, 8, 64]), op=ALU.mult),
                        reads=[pmm[0], qss], writes=[qn], partial=True)
                    S.op("dve", lambda: nc.vector.tensor_tensor(
                        out=qn[:, 512:640].rearrange("p (h d) -> p h d", d=64),
                        in0=pmm[1][:, 0:128].rearrange("p (h d) -> p h d", d=64),
                        in1=qss[:, 8:10].unsqueeze(2).to_broadcast([128, 2, 64]), op=ALU.mult),
                        reads=[pmm[1], qss], writes=[qn], partial=True)
                    S.op("pool", lambda: nc.gpsimd.tensor_tensor(
                        out=qn[:, 0:512].rearrange("p (h d) -> p h d", d=64), in0=qn[:, 0:512].rearrange("p (h d) -> p h d", d=64),
                        in1=gq[:].unsqueeze(1).to_broadcast([128, 8, 64]), op=ALU.mult), reads=[qn, gq], writes=[qn], partial=True)
                    S.op("pool", lambda: nc.gpsimd.tensor_tensor(
                        out=qn[:, 512:640].rearrange("p (h d) -> p h d", d=64),
                        in0=qn[:, 512:640].rearrange("p (h d) -> p h d", d=64),
                        in1=gk[:].unsqueeze(1).to_broadcast([128, 2, 64]), op=ALU.mult), reads=[qn, gk], writes=[qn], partial=True)
                    S.op("dve", lambda: nc.vector.tensor_tensor(
                        out=t1[:].rearrange("p (h d) -> p h d", d=64), in0=qn[:].rearrange("p (h d) -> p h d", d=64),
                        in1=rcb[:].unsqueeze(1).to_broadcast([128, 10, 64]), op=ALU.mult), reads=[qn, rcb], writes=[t1])
                    qv = qn[:].rearrange("p (h a b c) -> p h a b c", h=10, a=2, b=2)
                    tv = t2[:].rearrange("p (h a b c) -> p h a b c", h=10, a=2, b=2)
                    sv = rsb[:].rearrange("p (a b c) -> p a b c", a=2, b=2)
                    for a in range(2):
                        for b_ in range(2):
                            S.op("pool", lambda: nc.gpsimd.tensor_tensor(
                                out=tv[:, :, a, b_, :], in0=qv[:, :, a, 1 - b_, :],
                                in1=sv[:, a, b_, :].unsqueeze(1).to_broadcast([128, 10, 16]), op=ALU.mult),
                                reads=[qn, rsb], writes=[t2], partial=True)
                    S.op("dve", lambda: nc.vector.tensor_tensor(
                        out=qr[:, 0:512].rearrange("p (g k d) -> p k g d", g=4, k=2),
                        in0=t1[:, 0:512].rearrange("p (k g d) -> p k g d", k=2, g=4),
                        in1=t2[:, 0:512].rearrange("p (k g d) -> p k g d", k=2, g=4), op=ALU.add),
                        reads=[t1, t2], writes=[qr])
                    S.op("dve", lambda: nc.vector.tensor_tensor(out=qr[:, 512:640], in0=t1[:, 512:640], in1=t2[:, 512:640],
                                                                op=ALU.add), reads=[t1, t2], writes=[qr], partial=True)
                    for g in range(4):
                        S.op("pe", lambda: nc.tensor.transpose(
                            out=pq[:, g, :], in_=qr[:, g * 128:(g + 1) * 128],
                            identity=idb[:]), reads=[qr, idb], writes=[pq], partial=(g > 0))
                    S.op("act", lambda: nc.scalar.copy(out=qTs[:, :, s * 128:(s + 1) * 128], in_=pq[:]),
                         reads=[pq], writes=[qTs], partial=True)
                    S.op("pe", lambda: nc.tensor.transpose(out=pk[:, 0, :], in_=qr[:, 512:640], identity=idb[:]),
                         reads=[qr, idb], writes=[pk], partial=False)
                    S.op("act", lambda: nc.scalar.copy(out=vas[:, s, :].rearrange("p (k e) -> p k e", e=65)[:, :, 0:64],
                                                       in_=pmm[1][:, 128:256].rearrange("p (k d) -> p k d", d=64)),
                         reads=[pmm[1]], writes=[vas], partial=True)
                    S.op("dve", lambda: nc.vector.tensor_scalar(out=qbk[:, 0:256], in0=pmm[1][:, 256:512], scalar1=0.125,
                                                                scalar2=None, op0=ALU.mult),
                         reads=[pmm[1]], writes=[qbk], partial=True)
                    S.op("act", lambda: nc.scalar.copy(out=qbk[:, 256:512], in_=pmm[2][:, 0:256]),
                         reads=[pmm[2]], writes=[qbk], partial=True)
                    S.op("act", lambda: nc.scalar.copy(out=vbs[:, s, :], in_=pmm[2][:, 256:512]),
                         reads=[pmm[2]], writes=[vbs], partial=True)
                    for j in range(4):
                        S.op("pe", lambda: nc.tensor.transpose(out=pk[:, 1 + j, :], in_=qbk[:, j * 128:(j + 1) * 128],
                                                               identity=idb[:]), reads=[qbk, idb], writes=[pk], partial=True)
                    S.op("dve", lambda: nc.vector.tensor_copy(out=kTs[:, s * 128:(s + 1) * 128], in_=pk[:, 0, :]),
                         reads=[pk], writes=[kTs], partial=True)
                    S.op("dve", lambda: nc.vector.tensor_copy(out=qbTs[:, :, s * 128:(s + 1) * 128], in_=pk[:, 1:3, :]),
                         reads=[pk], writes=[qbTs], partial=True)
                    S.op("dve", lambda: nc.vector.tensor_copy(out=kbTs[:, :, s * 128:(s + 1) * 128], in_=pk[:, 3:5, :]),
                         reads=[pk], writes=[kbTs], partial=True)
                for j in range(2):
                    for which in range(2):
                        c0 = 1536 + which * 256 + j * 128
                        for k in range(8):
                            S.op("pe", lambda: nc.tensor.matmul(pf[which][:, 0:G], lhsT=win[:, k, c0:c0 + 128], rhs=hT[:, k, 0:G],
                                                                start=(k == 0), stop=(k == 7)),
                                 reads=[win, hT], writes=[pf[which]], partial=(k > 0))
                    S.op("act", lambda: nc.scalar.activation(out=sg[:, 0:G], in_=pf[1][:, 0:G], func=AF.Sigmoid),
                         reads=[pf[1]], writes=[sg])
                    S.op("dve", lambda: nc.vector.tensor_tensor(out=uTs[:, j, 0:G], in0=pf[0][:, 0:G], in1=sg[:, 0:G], op=ALU.mult),
                         reads=[pf[0], sg], writes=[uTs], partial=True)
                S.dma("sp", lambda: nc.sync.dma_start(out=QT[:, :, t0:t0 + G], in_=qTs[:, :, 0:G]), reads=[qTs], writes=[QT])
                S.dma("sp", lambda: nc.sync.dma_start(out=KT[:, t0:t0 + G], in_=kTs[:, 0:G]), reads=[kTs], writes=[KT])
                S.dma("sp", lambda: nc.sync.dma_start(out=VA.ap()[t0:t0 + G, :].rearrange("(s p) e -> p s e", p=128),
                                                      in_=vas[:, 0:nsub, :]), reads=[vas], writes=[VA])
                S.dma("sp", lambda: nc.sync.dma_start(out=QBT[:, :, t0:t0 + G], in_=qbTs[:, :, 0:G]), reads=[qbTs], writes=[QBT])
                S.dma("sp", lambda: nc.sync.dma_start(out=KBT[:, :, t0:t0 + G], in_=kbTs[:, :, 0:G]), reads=[kbTs], writes=[KBT])
                S.dma("sp", lambda: nc.sync.dma_start(out=VB.ap()[t0:t0 + G, :].rearrange("(s p) e -> p s e", p=128),
                                                      in_=vbs[:, 0:nsub, :]), reads=[vbs], writes=[VB])
                S.dma("sp", lambda: nc.sync.dma_start(out=UT.ap()[:, t0:t0 + G].rearrange("(j p) t -> p j t", p=128),
                                                      in_=uTs[:, :, 0:G]), reads=[uTs], writes=[UT])
            S.end_phase()
        if stop_after == "1":
            break

        with ES() as st:
            ksb = S.sb([128, TT], BF16, "ksb", st)
            vsb = S.sb([128, NTT, 130], BF16, "vsb", st)
            S.dma("sp", lambda: nc.sync.dma_start(out=ksb[:], in_=KT[:, :]), reads=[KT], writes=[ksb])
            S.dma("sp", lambda: nc.sync.dma_start(out=vsb[:], in_=VA.ap().rearrange("(n p) e -> p n e", p=128)),
                  reads=[VA], writes=[vsb])
            gqk = S.sb([128, 2, 64], F32, "gqk", st)
            S.dma("sp", lambda: nc.sync.dma_start(out=gqk[:, 0, :], in_=q_norm_g.ap()[l].partition_broadcast(128)),
                  reads=[q_norm_g], writes=[gqk], partial=True)
            S.dma("sp", lambda: nc.sync.dma_start(out=gqk[:, 1, :], in_=k_norm_g.ap()[l].partition_broadcast(128)),
                  reads=[k_norm_g], writes=[gqk], partial=True)
            gmx = S.sb([128, 2], F32, "gmx", st)
            nb = S.sb([128, 1], F32, "nb", st)
            gng = S.sb([128, 2, 64], F32, "gng", st)
            S.op("dve", lambda: nc.vector.tensor_scalar(out=gng[:], in0=gqk[:], scalar1=-1.0, scalar2=None, op0=ALU.mult),
                 reads=[gqk], writes=[gng])
            S.op("dve", lambda: nc.vector.tensor_tensor(out=gqk[:], in0=gqk[:], in1=gng[:], op=ALU.max),
                 reads=[gqk, gng], writes=[gqk])
            S.op("dve", lambda: nc.vector.tensor_reduce(out=gmx[:], in_=gqk[:], axis=AX.X, op=ALU.max), reads=[gqk], writes=[gmx])
            S.op("dve", lambda: nc.vector.tensor_tensor(out=nb[:], in0=gmx[:, 0:1], in1=gmx[:, 1:2], op=ALU.mult),
                 reads=[gmx], writes=[nb])
            S.op("dve", lambda: nc.vector.tensor_scalar(out=nb[:], in0=nb[:], scalar1=-8.0, scalar2=None, op0=ALU.mult),
                 reads=[nb], writes=[nb])
            qsbs = [S.sb([128, 2, 512], BF16, "qsb", st) for _ in range(2)]
            for qb_ in qsbs:
                S.op("dve", lambda: nc.vector.memset(qb_[:], 0.0), writes=[qb_])
            pbs = [S.sb([128, 512], BF16, "pb", st) for _ in range(3)]
            rsa = S.sb([128, 4], F32, "rsa", st)
            osb = [S.sb([128, 512], BF16, "osb", st) for _ in range(2)]
            aTs = [S.sb([128, 4, 128], BF16, "aTs", st) for _ in range(2)]
            pss = [S.ps([128, 512], F32, "pss", st) for _ in range(3)]
            pos = [S.ps([128, 512], F32, "po", st) for _ in range(4)]
            pa = S.ps([128, 4, 128], BF16, "pa", st)
            steps = []
            nq2 = min(ntq, P2_LIMIT) if P2_LIMIT else ntq
            for qi in range(nq2):
                ktiles = list(range(NTT)) if qi < NTL else [NTL, NTL + 1]
                for kh in range(2):
                    for idx, kt in enumerate(ktiles):
                        steps.append((qi, kh, idx, kt, len(ktiles)))
            nst_ = len(steps)

            def load_q(qi):
                q0 = qi * 128
                qsb = qsbs[qi % 2]
                for kh_ in range(2):
                    S.dma("sp", lambda: nc.sync.dma_start(
                        out=qsb[kh_ * 64:(kh_ + 1) * 64, kh_, :].rearrange("p (g t) -> p g t", g=4),
                        in_=QT[kh_ * 64:(kh_ + 1) * 64, :, q0:q0 + 128]), reads=[QT], writes=[qsb], partial=True)

            def emit_S(i):
                qi, kh, idx, kt, nk = steps[i]
                if kh == 0 and idx == 0 and qi + 1 < nq2:
                    load_q(qi + 1)
                qsb = qsbs[qi % 2]
                ps = pss[i % 3]
                S.op("pe", lambda: nc.tensor.matmul(ps[:], lhsT=ksb[:, kt * 128:(kt + 1) * 128],
                                                    rhs=qsb[:, kh, :], start=True, stop=True),
                     reads=[ksb, qsb], writes=[ps])

            def finish_q(qi):
                q0 = qi * 128
                ob = osb[qi % 2]
                for j in range(4):
                    S.op("pe", lambda: nc.tensor.transpose(out=pa[:, j, :], in_=ob[:, j * 128:(j + 1) * 128], identity=idb[:]),
                         reads=[ob, idb], writes=[pa], partial=(j > 0))
                aT = aTs[qi % 2]
                S.op("dve", lambda: nc.vector.tensor_copy(out=aT[:], in_=pa[:]), reads=[pa], writes=[aT])
                S.dma("sp", lambda: nc.sync.dma_start(out=AT.ap()[:, q0:q0 + 128].rearrange("(j p) t -> p j t", p=128), in_=aT[:]),
                      reads=[aT], writes=[AT])

            load_q(0)
            emit_S(0)
            if nst_ > 1:
                emit_S(1)
            deferred = {}
            for i in range(nst_):
                qi, kh, idx, kt, nk = steps[i]
                ps, pb = pss[i % 3], pbs[i % 3]
                ob = osb[qi % 2]
                S.op("act", lambda: nc.scalar.activation(out=pb[:], in_=ps[:], func=AF.Exp, bias=nb[:, 0:1], scale=1.0),
                     reads=[ps, nb], writes=[pb])
                for g in range(4):
                    S.op("pe", lambda: nc.tensor.matmul(pos[g][:, 0:65], lhsT=pb[:, g * 128:(g + 1) * 128],
                                                        rhs=vsb[:, kt, kh * 65:(kh + 1) * 65],
                                                        start=(idx == 0), stop=(idx == nk - 1)),
                         reads=[pb, vsb], writes=[pos[g]], partial=(idx > 0))
                if i + 2 < nst_:
                    emit_S(i + 2)
                if idx == nk - 1:
                    for g in range(4):
                        S.op("dve", lambda: nc.vector.reciprocal(out=rsa[:, g:g + 1], in_=pos[g][:, 64:65]),
                             reads=[pos[g]], writes=[rsa], partial=True)
                        S.op("dve", lambda: nc.vector.tensor_scalar(
                            out=ob[:, kh * 256 + g * 64:kh * 256 + (g + 1) * 64], in0=pos[g][:, 0:64], scalar1=rsa[:, g:g + 1],
                            scalar2=None, op0=ALU.mult), reads=[pos[g], rsa], writes=[ob], partial=True)
                    if kh == 1:
                        deferred[min(i + 4, nst_ - 1)] = deferred.get(min(i + 4, nst_ - 1), []) + [qi]
                for qd in deferred.pop(i, []):
                    finish_q(qd)
            S.end_phase()
        if stop_after == "2":
            break

        with ES() as st:
            kc = S.sb([128, 2, 256], BF16, "kc", st)
            vc = S.sb([128, 2, 256], BF16, "vc", st)
            S.dma("sp", lambda: nc.sync.dma_start(out=kc[:], in_=KBT[:, :, T:TT]), reads=[KBT], writes=[kc])
            S.dma("sp", lambda: nc.sync.dma_start(out=vc[:], in_=VB.ap()[T:TT, :].rearrange("(n p) e -> p n e", p=128)),
                  reads=[VB], writes=[vc])
            bint = S.sb([64, 4, 512], F32, "bint", st)
            bedge = S.sb([64, 4, 512], F32, "bedge", st)
            S.dma("sp", lambda: nc.sync.dma_start(out=bint[:], in_=nabias[l, 3]), reads=[nabias], writes=[bint])
            qrows = [S.sb([128, 2, 64], BF16, "qrow", st) for _ in range(2)]
            kwins = [S.sb([128, 2, 512], BF16, "kwin", st) for _ in range(2)]
            vwins = [S.sb([64, 8, 256], BF16, "vwin", st) for _ in range(2)]
            ssb = S.sb([64, 768], F32, "ssb", st)
            mx = S.sb([64, 1], F32, "mx", st)
            sm = S.sb([64, 1], F32, "sm", st)
            pexp = S.sb([64, 768], BF16, "pexp", st)
            ptw_s = S.sb([64, 8, 64], BF16, "ptw_s", st)
            ptc_s = S.sb([128, 2, 64], BF16, "ptc_s", st)
            brow = S.sb([64, 256], BF16, "brow", st)
            bts = [S.sb([128, 2, 64], BF16, "bts", st) for _ in range(2)]
            psn = [S.ps([64, 1024], F32, "psn", st) for _ in range(2)]
            ptw = S.ps([64, 8, 64], BF16, "ptw", st)
            ptc = S.ps([128, 2, 64], BF16, "ptc", st)
            pon = S.ps([64, 64], F32, "pon", st)
            pbt = S.ps([128, 2, 64], BF16, "pbt", st)
            it = 0
            for r in range(T // 64):
                rs0 = min(max(r - 4, 0), 120)
                off = rs0 - r + 7
                if off == 3:
                    bias = bint
                else:
                    bias = bedge
                    S.dma("sp", lambda: nc.sync.dma_start(out=bedge[:], in_=nabias[l, off]), reads=[nabias], writes=[bedge])
                qrow, kwin, vwin = qrows[r % 2], kwins[r % 2], vwins[r % 2]
                S.dma("sp", lambda: nc.sync.dma_start(out=qrow[:], in_=QBT[:, :, r * 64:(r + 1) * 64]), reads=[QBT], writes=[qrow])
                S.dma("sp", lambda: nc.sync.dma_start(out=kwin[:], in_=KBT[:, :, rs0 * 64:(rs0 + 8) * 64]), reads=[KBT], writes=[kwin])
                S.dma("sp", lambda: nc.sync.dma_start(out=vwin[:], in_=VB.ap()[rs0 * 64:(rs0 + 8) * 64, :].rearrange("(j p) e -> p j e", p=64)),
                      reads=[VB], writes=[vwin])
                for hb in range(4):
                    hp, pr = hb // 2, (hb % 2) * 64
                    ps = psn[it % 2]
                    it += 1
                    S.op("pe", lambda: nc.tensor.matmul(ps[:, 0:512], lhsT=qrow[pr:pr + 64, hp, :], rhs=kwin[pr:pr + 64, hp, :],
                                                        start=True, stop=True), reads=[qrow, kwin], writes=[ps])
                    S.op("pe", lambda: nc.tensor.matmul(ps[:, 512:768], lhsT=qrow[pr:pr + 64, hp, :], rhs=kc[pr:pr + 64, hp, :],
                                                        start=True, stop=True), reads=[qrow, kc], writes=[ps], partial=True)
                    S.op("dve", lambda: nc.vector.tensor_tensor(out=ssb[:, 0:512], in0=ps[:, 0:512], in1=bias[:, hb, :], op=ALU.add),
                         reads=[ps, bias], writes=[ssb])
                    S.op("act", lambda: nc.scalar.copy(out=ssb[:, 512:768], in_=ps[:, 512:768]), reads=[ps], writes=[ssb], partial=True)
                    S.op("dve", lambda: nc.vector.tensor_reduce(out=mx[:], in_=ssb[:], axis=AX.X, op=ALU.max), reads=[ssb], writes=[mx])
                    S.op("dve", lambda: nc.vector.tensor_scalar(out=mx[:], in0=mx[:], scalar1=-1.0, scalar2=None, op0=ALU.mult),
                         reads=[mx], writes=[mx])
                    S.op("act", lambda: nc.scalar.activation(out=pexp[:], in_=ssb[:], func=AF.Exp, bias=mx[:, 0:1], scale=1.0,
                                                             accum_out=sm[:]), reads=[ssb, mx], writes=[pexp, sm])
                    for j in range(8):
                        S.op("pe", lambda: nc.tensor.transpose(out=ptw[:, j, :], in_=pexp[:, j * 64:(j + 1) * 64], identity=idb[0:64, 0:64]),
                             reads=[pexp, idb], writes=[ptw], partial=(j > 0))
                    for j in range(2):
                        S.op("pe", lambda: nc.tensor.transpose(out=ptc[:, j, :], in_=pexp[:, 512 + j * 128:512 + (j + 1) * 128],
                                                               identity=idb[0:64, 0:64]), reads=[pexp, idb], writes=[ptc], partial=(j > 0))
                    S.op("dve", lambda: nc.vector.tensor_copy(out=ptw_s[:], in_=ptw[:]), reads=[ptw], writes=[ptw_s])
                    S.op("act", lambda: nc.scalar.copy(out=ptc_s[:], in_=ptc[:]), reads=[ptc], writes=[ptc_s])
                    for j in range(8):
                        S.op("pe", lambda: nc.tensor.matmul(pon[:], lhsT=ptw_s[:, j, :], rhs=vwin[:, j, hb * 64:(hb + 1) * 64],
                                                            start=(j == 0), stop=False), reads=[ptw_s, vwin], writes=[pon], partial=(j > 0))
                    for j in range(2):
                        S.op("pe", lambda: nc.tensor.matmul(pon[:], lhsT=ptc_s[:, j, :], rhs=vc[:, j, hb * 64:(hb + 1) * 64],
                                                            start=False, stop=(j == 1)), reads=[ptc_s, vc], writes=[pon], partial=True)
                    S.op("dve", lambda: nc.vector.reciprocal(out=sm[:], in_=sm[:]), reads=[sm], writes=[sm])
                    S.op("dve", lambda: nc.vector.tensor_scalar(out=brow[:, hb * 64:(hb + 1) * 64], in0=pon[:], scalar1=sm[:, 0:1],
                                                                scalar2=None, op0=ALU.mult), reads=[pon, sm], writes=[brow], partial=True)
                for j in range(2):
                    S.op("pe", lambda: nc.tensor.transpose(out=pbt[:, j, :], in_=brow[:, j * 128:(j + 1) * 128], identity=idb[0:64, 0:64]),
                         reads=[brow, idb], writes=[pbt], partial=(j > 0))
                bt = bts[r % 2]
                S.op("act", lambda: nc.scalar.copy(out=bt[:], in_=pbt[:]), reads=[pbt], writes=[bt])
                S.dma("sp", lambda: nc.sync.dma_start(out=BT.ap()[:, r * 64:(r + 1) * 64].rearrange("(j p) t -> p j t", p=128), in_=bt[:]),
                      reads=[bt], writes=[BT])
            S.end_phase()
        if not last:
            with ES() as st:
                kc = S.sb([128, 2, 256], BF16, "kc", st)
                vc = S.sb([128, 2, 256], BF16, "vc", st)
                S.dma("sp", lambda: nc.sync.dma_start(out=kc[:], in_=KBT[:, :, T:TT]), reads=[KBT], writes=[kc])
                S.dma("sp", lambda: nc.sync.dma_start(out=vc[:], in_=VB.ap()[T:TT, :].rearrange("(n p) e -> p n e", p=128)),
                      reads=[VB], writes=[vc])
                qc = S.sb([128, 2, 128], BF16, "qc", st)
                sc_ = S.sb([128, 256], F32, "sc", st)
                mxc = S.sb([128, 1], F32, "mxc", st)
                smc = S.sb([128, 1], F32, "smc", st)
                pxc = S.sb([128, 256], BF16, "pxc", st)
                ptcs = S.sb([128, 2, 128], BF16, "ptcs", st)
                browc = S.sb([128, 256], BF16, "browc", st)
                btc = S.sb([128, 2, 128], BF16, "btc", st)
                psc = S.ps([128, 256], F32, "psc", st)
                ptcp = S.ps([128, 2, 128], BF16, "ptcp", st)
                poc = S.ps([128, 64], F32, "poc", st)
                pbc = S.ps([128, 2, 128], BF16, "pbc", st)
                for ci in range(2):
                    c0 = T + ci * 128
                    S.dma("sp", lambda: nc.sync.dma_start(out=qc[:], in_=QBT[:, :, c0:c0 + 128]), reads=[QBT], writes=[qc])
                    for hb in range(4):
                        hp, pr = hb // 2, (hb % 2) * 64
                        S.op("pe", lambda: nc.tensor.matmul(psc[:], lhsT=qc[pr:pr + 64, hp, :], rhs=kc[pr:pr + 64, hp, :],
                                                            start=True, stop=True), reads=[qc, kc], writes=[psc])
                        S.op("act", lambda: nc.scalar.copy(out=sc_[:], in_=psc[:]), reads=[psc], writes=[sc_])
                        S.op("dve", lambda: nc.vector.tensor_reduce(out=mxc[:], in_=sc_[:], axis=AX.X, op=ALU.max), reads=[sc_], writes=[mxc])
                        S.op("dve", lambda: nc.vector.tensor_scalar(out=mxc[:], in0=mxc[:], scalar1=-1.0, scalar2=None, op0=ALU.mult),
                             reads=[mxc], writes=[mxc])
                        S.op("act", lambda: nc.scalar.activation(out=pxc[:], in_=sc_[:], func=AF.Exp, bias=mxc[:, 0:1], scale=1.0,
                                                                 accum_out=smc[:]), reads=[sc_, mxc], writes=[pxc, smc])
                        for j in range(2):
                            S.op("pe", lambda: nc.tensor.transpose(out=ptcp[:, j, :], in_=pxc[:, j * 128:(j + 1) * 128], identity=idb[:]),
                                 reads=[pxc, idb], writes=[ptcp], partial=(j > 0))
                        S.op("dve", lambda: nc.vector.tensor_copy(out=ptcs[:], in_=ptcp[:]), reads=[ptcp], writes=[ptcs])
                        for j in range(2):
                            S.op("pe", lambda: nc.tensor.matmul(poc[:], lhsT=ptcs[:, j, :], rhs=vc[:, j, hb * 64:(hb + 1) * 64],
                                                                start=(j == 0), stop=(j == 1)), reads=[ptcs, vc], writes=[poc], partial=(j > 0))
                        S.op("dve", lambda: nc.vector.reciprocal(out=smc[:], in_=smc[:]), reads=[smc], writes=[smc])
                        S.op("dve", lambda: nc.vector.tensor_scalar(out=browc[:, hb * 64:(hb + 1) * 64], in0=poc[:], scalar1=smc[:, 0:1],
                                                                    scalar2=None, op0=ALU.mult), reads=[poc, smc], writes=[browc], partial=True)
                    for j in range(2):
                        S.op("pe", lambda: nc.tensor.transpose(out=pbc[:, j, :], in_=browc[:, j * 128:(j + 1) * 128], identity=idb[:]),
                             reads=[browc, idb], writes=[pbc], partial=(j > 0))
                    S.op("act", lambda: nc.scalar.copy(out=btc[:], in_=pbc[:]), reads=[pbc], writes=[btc])
                    S.dma("sp", lambda: nc.sync.dma_start(out=BT.ap()[:, c0:c0 + 128].rearrange("(j p) t -> p j t", p=128), in_=btc[:]),
                          reads=[btc], writes=[BT])
                S.end_phase()
        if stop_after == "3":
            break

        seqs = [(0, T)] + ([] if last else [(T, CT)])
        for (t0, Ls) in seqs:
            with ES() as st:
                Y = S.sb([128, 2, Ls], F32, "Y", st)
                up = S.sb([128, Ls + 30], F32, "up", st)
                cw = S.sb([128, 2, 31], F32, "cw", st)
                cb = S.sb([128, 2], F32, "cb", st)
                lng = S.sb([128, 2], F32, "lng", st)
                lnb = S.sb([128, 2], F32, "lnb", st)
                ones_s = S.sb([128, 128], F32, "ones_s", st)
                S.op("dve", lambda: nc.vector.memset(ones_s[:], 1.0 / 256), writes=[ones_s])
                with nc.allow_non_contiguous_dma(reason="tiny"):
                    for j in range(2):
                        S.dma("sp", lambda: nc.sync.dma_start(out=cw[:, j, :], in_=conv_w.ap()[l][:, j * 128:(j + 1) * 128].rearrange("w c -> c w")),
                              reads=[conv_w], writes=[cw], partial=True)
                    for (dst, src) in ((cb, conv_b), (lng, conv_ln_g), (lnb, conv_ln_b)):
                        S.dma("sp", lambda: nc.sync.dma_start(out=dst[:], in_=src.ap()[l].rearrange("(j p) -> p j", p=128)),
                              reads=[src], writes=[dst])
                S.op("dve", lambda: nc.vector.memset(up[:, 0:15], 0.0), writes=[up], partial=True)
                S.op("dve", lambda: nc.vector.memset(up[:, Ls + 15:Ls + 30], 0.0), writes=[up], partial=True)
                for j in range(2):
                    S.dma("sp", lambda: nc.sync.dma_start(out=up[:, 15:15 + Ls], in_=UT[j * 128:(j + 1) * 128, t0:t0 + Ls]),
                          reads=[UT], writes=[up], partial=True)
                    S.op("dve", lambda: nc.vector.tensor_scalar(out=Y[:, j, :], in0=up[:, 0:Ls], scalar1=cw[:, j, 0:1], scalar2=cb[:, j:j + 1],
                                                                op0=ALU.mult, op1=ALU.add), reads=[up, cw, cb], writes=[Y], partial=True)
                    for w in range(1, 31):
                        S.op("dve", lambda: nc.vector.scalar_tensor_tensor(out=Y[:, j, :], in0=up[:, w:w + Ls], scalar=cw[:, j, w:w + 1],
                                                                           in1=Y[:, j, :], op0=ALU.mult, op1=ALU.add),
                             reads=[up, cw, Y], writes=[Y], partial=True)
                BL = min(512, Ls)
                ysq = S.sb([128, 2, BL], F32, "ysq", st)
                mean = S.sb([128, BL], F32, "mean", st)
                var = S.sb([128, BL], F32, "var", st)
                tmpc = S.sb([128, 2, BL], F32, "tmpc", st)
                ctsb = [S.sb([128, 2, BL], BF16, "ctsb", st) for _ in range(2)]
                pmn = S.ps([128, BL], F32, "pmn", st)
                pe2 = S.ps([128, BL], F32, "pe2", st)
                for bi in range(Ls // BL):
                    b0 = bi * BL
                    S.op("act", lambda: nc.scalar.activation(out=ysq[:], in_=Y[:, :, b0:b0 + BL], func=AF.Square), reads=[Y], writes=[ysq])
                    for j in range(2):
                        S.op("pe", lambda: nc.tensor.matmul(pmn[:], lhsT=ones_s[:], rhs=Y[:, j, b0:b0 + BL], start=(j == 0), stop=(j == 1)),
                             reads=[ones_s, Y], writes=[pmn], partial=(j > 0))
                    for j in range(2):
                        S.op("pe", lambda: nc.tensor.matmul(pe2[:], lhsT=ones_s[:], rhs=ysq[:, j, :], start=(j == 0), stop=(j == 1)),
                             reads=[ones_s, ysq], writes=[pe2], partial=(j > 0))
                    S.op("act", lambda: nc.scalar.copy(out=mean[:], in_=pmn[:]), reads=[pmn], writes=[mean])
                    S.op("dve", lambda: nc.vector.tensor_tensor(out=var[:], in0=mean[:], in1=mean[:], op=ALU.mult), reads=[mean], writes=[var])
                    S.op("dve", lambda: nc.vector.tensor_tensor(out=var[:], in0=pe2[:], in1=var[:], op=ALU.subtract), reads=[pe2, var], writes=[var])
                    S.op("dve", lambda: nc.vector.tensor_scalar(out=var[:], in0=var[:], scalar1=EPS, scalar2=None, op0=ALU.add),
                         reads=[var], writes=[var])
                    S.op("act", lambda: nc.scalar.activation(out=var[:], in_=var[:], func=AF.Sqrt), reads=[var], writes=[var])
                    S.op("dve", lambda: nc.vector.reciprocal(out=var[:], in_=var[:]), reads=[var], writes=[var])
                    cts_ = ctsb[bi % 2]
                    for j in range(2):
                        S.op("dve", lambda: nc.vector.tensor_tensor(out=tmpc[:, j, :], in0=Y[:, j, b0:b0 + BL], in1=mean[:], op=ALU.subtract),
                             reads=[Y, mean], writes=[tmpc], partial=True)
                        S.op("dve", lambda: nc.vector.tensor_tensor(out=tmpc[:, j, :], in0=tmpc[:, j, :], in1=var[:], op=ALU.mult),
                             reads=[tmpc, var], writes=[tmpc], partial=True)
                        S.op("act", lambda: nc.scalar.activation(out=cts_[:, j, :], in_=tmpc[:, j, :], func=AF.Silu,
                                                                 bias=lnb[:, j:j + 1], scale=lng[:, j:j + 1]),
                             reads=[tmpc, lnb, lng], writes=[cts_], partial=True)
                    S.dma("sp", lambda: nc.sync.dma_start(out=CTs.ap()[:, t0 + b0:t0 + b0 + BL].rearrange("(j p) t -> p j t", p=128), in_=cts_[:]),
                          reads=[cts_], writes=[CTs])
                S.end_phase()
        if stop_after == "4":
            break

        with ES() as st:
            mod_l, mod_c = load_mod(st, 2 * D, 3 * D)
            wo = S.sb([128, 8, D], BF16, "wo", st)
            for h in range(2):
                S.dma("pool", lambda: nc.gpsimd.dma_start(out=wo[:, :, h * 512:(h + 1) * 512],
                                                          in_=w_out.ap()[l][:, h * 512:(h + 1) * 512].rearrange("(k p) n -> p k n", p=128)),
                      reads=[w_out], writes=[wo], partial=True)
            wr = S.sb([128, 8, NE], F32, "wr", st)
            S.dma("sp", lambda: nc.sync.dma_start(out=wr[:], in_=w_router.ap()[l].rearrange("(k p) e -> p k e", p=128)),
                  reads=[w_router], writes=[wr])
            cats = [S.sb([128, 8, 128], BF16, "cat", st) for _ in range(2)]
            xts = [S.sb([128, D], F32, "xt5", st) for _ in range(2)]
            tmp5 = S.sb([128, D], F32, "tmp5", st)
            x1s = [S.sb([128, D], F32, "x1", st) for _ in range(2)]
            sq5 = S.sb([128, D], F32, "sq5", st)
            ss5 = S.sb([128, 1], F32, "ss5", st)
            rstd5 = S.sb([128, 1], F32, "rstd5", st)
            h2f = S.sb([128, D], F32, "h2f", st)
            h2b = [S.sb([128, D], BF16, "h2b", st) for _ in range(2)]
            h2T = S.sb([128, 8, 128], F32, "h2T", st)
            lg = S.sb([128, NE], F32, "lg", st)
            mx5 = S.sb([128, 1], F32, "mx5", st)
            se5 = S.sb([128, 1], F32, "se5", st)
            ps5 = S.ps([128, D], F32, "ps5", st)
            pt5 = S.ps([128, 8, 128], F32, "pt5", st)
            pl5 = S.ps([128, NE], F32, "pl5", st)
            for i in range(NTT + 1):
                r0 = i * 128
                nr = 128 if i < NTT else NPAD
                S.dma("sp", lambda: nc.sync.dma_start(out=YACC[r0:r0 + nr, :], in_=zeros_f[0:nr, :]), reads=[zeros_f], writes=[YACC])
            for ti in range(ntq):
                r0 = ti * 128
                mod = mod_l if ti < NTL else mod_c
                cat, xt, x1, hb2 = cats[ti % 2], xts[ti % 2], x1s[ti % 2], h2b[ti % 2]
                S.dma("sp", lambda: nc.sync.dma_start(out=cat[:, 0:4, :], in_=AT.ap()[:, r0:r0 + 128].rearrange("(j p) t -> p j t", p=128)),
                      reads=[AT], writes=[cat], partial=True)
                S.dma("sp", lambda: nc.sync.dma_start(out=cat[:, 4:6, :], in_=BT.ap()[:, r0:r0 + 128].rearrange("(j p) t -> p j t", p=128)),
                      reads=[BT], writes=[cat], partial=True)
                S.dma("sp", lambda: nc.sync.dma_start(out=cat[:, 6:8, :], in_=CTs.ap()[:, r0:r0 + 128].rearrange("(j p) t -> p j t", p=128)),
                      reads=[CTs], writes=[cat], partial=True)
                S.dma("sp", lambda: nc.sync.dma_start(out=xt[:], in_=X[r0:r0 + 128, :]), reads=[X], writes=[xt])
                for hh in range(2):
                    for k in range(8):
                        S.op("pe", lambda: nc.tensor.matmul(ps5[:, hh * 512:(hh + 1) * 512], lhsT=cat[:, k, :], rhs=wo[:, k, hh * 512:(hh + 1) * 512],
                                                            start=(k == 0), stop=(k == 7)), reads=[cat, wo], writes=[ps5],
                             partial=not (hh == 0 and k == 0))
                S.op("dve", lambda: nc.vector.tensor_tensor(out=tmp5[:], in0=ps5[:], in1=mod[:, 2 * D:3 * D], op=ALU.mult),
                     reads=[ps5, mod], writes=[tmp5])
                S.op("pool", lambda: nc.gpsimd.tensor_tensor(out=x1[:], in0=tmp5[:], in1=xt[:], op=ALU.add), reads=[tmp5, xt], writes=[x1])
                S.dma("sp", lambda: nc.sync.dma_start(out=X[r0:r0 + 128, :], in_=x1[:]), reads=[x1], writes=[X])
                S.op("act", lambda: nc.scalar.activation(out=sq5[:], in_=x1[:], func=AF.Square, accum_out=ss5[:]), reads=[x1], writes=[sq5, ss5])
                S.op("dve", lambda: nc.vector.tensor_scalar(out=rstd5[:], in0=ss5[:], scalar1=1.0 / D, scalar2=EPS, op0=ALU.mult, op1=ALU.add),
                     reads=[ss5], writes=[rstd5])
                S.op("act", lambda: nc.scalar.activation(out=rstd5[:], in_=rstd5[:], func=AF.Sqrt), reads=[rstd5], writes=[rstd5])
                S.op("dve", lambda: nc.vector.reciprocal(out=rstd5[:], in_=rstd5[:]), reads=[rstd5], writes=[rstd5])
                S.op("dve", lambda: nc.vector.scalar_tensor_tensor(out=h2f[:], in0=x1[:], scalar=rstd5[:, 0:1], in1=mod[:, 4 * D:5 * D],
                                                                   op0=ALU.mult, op1=ALU.mult), reads=[x1, rstd5, mod], writes=[h2f])
                S.op("pool", lambda: nc.gpsimd.tensor_tensor(out=h2f[:], in0=h2f[:], in1=mod[:, 3 * D:4 * D], op=ALU.add),
                     reads=[h2f, mod], writes=[h2f])
                S.op("act", lambda: nc.scalar.copy(out=hb2[:], in_=h2f[:]), reads=[h2f], writes=[hb2])
                S.dma("sp", lambda: nc.sync.dma_start(out=H2[r0:r0 + 128, :], in_=hb2[:]), reads=[hb2], writes=[H2])
                for k in range(8):
                    S.op("pe", lambda: nc.tensor.transpose(out=pt5[:, k, :], in_=h2f[:, k * 128:(k + 1) * 128], identity=idf[:]),
                         reads=[h2f, idf], writes=[pt5], partial=(k > 0))
                S.op("dve", lambda: nc.vector.tensor_copy(out=h2T[:], in_=pt5[:]), reads=[pt5], writes=[h2T])
                for k in range(8):
                    S.op("pe", lambda: nc.tensor.matmul(pl5[:], lhsT=h2T[:, k, :], rhs=wr[:, k, :], start=(k == 0), stop=(k == 7)),
                         reads=[h2T, wr], writes=[pl5], partial=(k > 0))
                S.op("dve", lambda: nc.vector.tensor_reduce(out=mx5[:], in_=pl5[:], axis=AX.X, op=ALU.max), reads=[pl5], writes=[mx5])
                S.op("dve", lambda: nc.vector.tensor_scalar(out=mx5[:], in0=mx5[:], scalar1=-1.0, scalar2=None, op0=ALU.mult),
                     reads=[mx5], writes=[mx5])
                S.op("act", lambda: nc.scalar.activation(out=lg[:], in_=pl5[:], func=AF.Exp, bias=mx5[:, 0:1], scale=1.0, accum_out=se5[:]),
                     reads=[pl5, mx5], writes=[lg, se5])
                S.op("dve", lambda: nc.vector.reciprocal(out=se5[:], in_=se5[:]), reads=[se5], writes=[se5])
                S.op("dve", lambda: nc.vector.tensor_scalar(out=AFF[:, ti, :], in0=lg[:], scalar1=se5[:, 0:1], scalar2=None, op0=ALU.mult),
                     reads=[lg, se5], writes=[AFF], partial=True)
            S.end_phase()
        if stop_after == "5":
            break

        nrt = ntq
        with ES() as st:
            utb = S.sb([128, 128], BF16, "utb", st)
            utf = S.sb([128, 128], F32, "utf", st)
            S.dma("sp", lambda: nc.sync.dma_start(out=utf[:], in_=utri_in[:, :]), reads=[utri_in], writes=[utf])
            S.op("dve", lambda: nc.vector.tensor_copy(out=utb[:], in_=utf[:]), reads=[utf], writes=[utb])
            lo = S.sb([128, 32], F32, "lo", st)
            hi = S.sb([128, 32], F32, "hi", st)
            mid = S.sb([128, 32], F32, "mid", st)
            tgt = S.sb([128, 32], F32, "tgt", st)
            ge = S.sb([128, 32], F32, "ge", st)
            d1 = S.sb([128, 32], F32, "d1", st)
            cmpb = S.sb([128, NTT, NE], F32, "cmpb", st)
            cntb = S.sb([128, 32], F32, "cntb", st)
            pc = S.ps([128, 32], F32, "pc", st)
            S.op("dve", lambda: nc.vector.memset(lo[:], 0.0), writes=[lo])
            S.op("dve", lambda: nc.vector.memset(hi[:], 1.0), writes=[hi])
            S.op("dve", lambda: nc.vector.memset(tgt[:, 0:16], float(CAP)), writes=[tgt], partial=True)
            S.op("dve", lambda: nc.vector.memset(tgt[:, 16:32], float(CCAP)), writes=[tgt], partial=True)
            S.op("dve", lambda: nc.vector.memset(cntb[:], 0.0), writes=[cntb])
            parts = [(0, NTL, 0)] + ([] if last else [(NTL, NTT, 16)])

            def compare(dst, thr):
                for (a, b_, c0) in parts:
                    S.op("dve", lambda: nc.vector.tensor_tensor(
                        out=dst[:, a:b_, :], in0=AFF[:, a:b_, :],
                        in1=thr[:, c0:c0 + 16].unsqueeze(1).to_broadcast([128, b_ - a, NE]), op=ALU.is_ge),
                        reads=[AFF, thr], writes=[dst], partial=True)

            for itn in range(40):
                S.op("dve", lambda: nc.vector.tensor_tensor(out=mid[:], in0=lo[:], in1=hi[:], op=ALU.add), reads=[lo, hi], writes=[mid])
                S.op("dve", lambda: nc.vector.tensor_scalar(out=mid[:], in0=mid[:], scalar1=0.5, scalar2=None, op0=ALU.mult),
                     reads=[mid], writes=[mid])
                compare(cmpb, mid)
                for (a, b_, c0) in parts:
                    S.op("dve", lambda: nc.vector.tensor_reduce(out=cntb[:, c0:c0 + 16], in_=cmpb[:, a:b_, :].rearrange("p t e -> p e t"),
                                                                axis=AX.X, op=ALU.add), reads=[cmpb], writes=[cntb], partial=True)
                S.op("pe", lambda: nc.tensor.matmul(pc[:], lhsT=ones_f[:], rhs=cntb[:], start=True, stop=True), reads=[ones_f, cntb], writes=[pc])
                S.op("dve", lambda: nc.vector.tensor_tensor(out=ge[:], in0=pc[:], in1=tgt[:], op=ALU.is_ge), reads=[pc, tgt], writes=[ge])
                S.op("dve", lambda: nc.vector.tensor_tensor(out=d1[:], in0=mid[:], in1=lo[:], op=ALU.subtract), reads=[mid, lo], writes=[d1])
                S.op("dve", lambda: nc.vector.tensor_tensor(out=d1[:], in0=d1[:], in1=ge[:], op=ALU.mult), reads=[d1, ge], writes=[d1])
                S.op("dve", lambda: nc.vector.tensor_tensor(out=lo[:], in0=lo[:], in1=d1[:], op=ALU.add), reads=[lo, d1], writes=[lo])
                S.op("dve", lambda: nc.vector.tensor_tensor(out=d1[:], in0=hi[:], in1=mid[:], op=ALU.subtract), reads=[hi, mid], writes=[d1])
                S.op("dve", lambda: nc.vector.tensor_tensor(out=d1[:], in0=d1[:], in1=ge[:], op=ALU.mult), reads=[d1, ge], writes=[d1])
                S.op("dve", lambda: nc.vector.tensor_tensor(out=hi[:], in0=mid[:], in1=d1[:], op=ALU.add), reads=[mid, d1], writes=[hi])
            maskb = S.sb([128, NTT, NE], BF16, "maskb", st)
            pref = S.sb([128, NTT, NE], F32, "pref", st)
            tcnt = S.sb([128, NTT, NE], F32, "tcnt", st)
            tcn2 = S.sb([128, NTT, NE], F32, "tcn2", st)
            S.op("dve", lambda: nc.vector.memset(cmpb[:], 0.0), writes=[cmpb])
            compare(cmpb, lo)
            S.op("dve", lambda: nc.vector.tensor_copy(out=maskb[:], in_=cmpb[:]), reads=[cmpb], writes=[maskb])
            ncol = NTT * NE
            mflat = maskb[:].rearrange("p t e -> p (t e)")
            pflat = pref[:].rearrange("p t e -> p (t e)")
            tflat = tcnt[:].rearrange("p t e -> p (t e)")
            pp = [S.ps([128, 512], F32, "pp", st) for _ in range(2)]
            for ci, c0 in enumerate(range(0, ncol, 512)):
                n = min(512, ncol - c0)
                S.op("pe", lambda: nc.tensor.matmul(pp[0][:, 0:n], lhsT=utb[:], rhs=mflat[:, c0:c0 + n], start=True, stop=True),
                     reads=[utb, maskb], writes=[pp[0]])
                S.op("pe", lambda: nc.tensor.matmul(pp[1][:, 0:n], lhsT=ones_b[:], rhs=mflat[:, c0:c0 + n], start=True, stop=True),
                     reads=[ones_b, maskb], writes=[pp[1]])
                S.op("dve", lambda: nc.vector.tensor_copy(out=pflat[:, c0:c0 + n], in_=pp[0][:, 0:n]), reads=[pp[0]], writes=[pref], partial=True)
                S.op("act", lambda: nc.scalar.copy(out=tflat[:, c0:c0 + n], in_=pp[1][:, 0:n]), reads=[pp[1]], writes=[tcnt], partial=True)
            src, dst = tcnt, tcn2
            S.op("dve", lambda: nc.vector.tensor_copy(out=tcn2[:], in_=tcnt[:]), reads=[tcnt], writes=[tcn2])
            cum = S.sb([128, NTT, NE], F32, "cum", st)
            S.op("dve", lambda: nc.vector.tensor_copy(out=cum[:], in_=tcnt[:]), reads=[tcnt], writes=[cum])
            a_, b2 = cum, tcn2
            sft = 1
            while sft < NTL:
                S.op("dve", lambda: nc.vector.tensor_tensor(out=b2[:, sft:NTL, :], in0=a_[:, sft:NTL, :], in1=a_[:, 0:NTL - sft, :], op=ALU.add),
                     reads=[a_], writes=[b2], partial=True)
                S.op("dve", lambda: nc.vector.tensor_copy(out=b2[:, 0:sft, :], in_=a_[:, 0:sft, :]), reads=[a_], writes=[b2], partial=True)
                a_, b2 = b2, a_
                sft *= 2
            inc = a_
            slot = S.sb([128, NTT, NE], F32, "slot", st)
            S.op("dve", lambda: nc.vector.tensor_tensor(out=slot[:, 0:NTL, :], in0=inc[:, 0:NTL, :], in1=tcnt[:, 0:NTL, :], op=ALU.subtract),
                 reads=[inc, tcnt], writes=[slot], partial=True)
            if not last:
                S.op("dve", lambda: nc.vector.memset(slot[:, NTL, :], float(CAP)), writes=[slot], partial=True)
                S.op("dve", lambda: nc.vector.tensor_scalar(out=slot[:, NTL + 1, :], in0=tcnt[:, NTL, :], scalar1=float(CAP), scalar2=None,
                                                            op0=ALU.add), reads=[tcnt], writes=[slot], partial=True)
            S.op("dve", lambda: nc.vector.tensor_tensor(out=slot[:, 0:nrt, :], in0=slot[:, 0:nrt, :], in1=pref[:, 0:nrt, :], op=ALU.add),
                 reads=[slot, pref], writes=[slot], partial=True)
            ebase = S.sb([128, NE], F32, "ebase", st)
            for e in range(NE):
                S.op("dve", lambda: nc.vector.memset(ebase[:, e:e + 1], float(e * MS)), writes=[ebase], partial=True)
            S.op("dve", lambda: nc.vector.tensor_tensor(out=slot[:, 0:nrt, :], in0=slot[:, 0:nrt, :],
                                                        in1=ebase[:].unsqueeze(1).to_broadcast([128, nrt, NE]), op=ALU.add),
                 reads=[slot, ebase], writes=[slot], partial=True)
            BIGI = float(1 << 20)
            S.op("dve", lambda: nc.vector.scalar_tensor_tensor(out=slot[:, 0:nrt, :].rearrange("p t e -> p (t e)"),
                                                               in0=slot[:, 0:nrt, :].rearrange("p t e -> p (t e)"), scalar=-BIGI,
                                                               in1=cmpb[:, 0:nrt, :].rearrange("p t e -> p (t e)"), op0=ALU.add, op1=ALU.mult),
                 reads=[slot, cmpb], writes=[slot], partial=True)
            S.op("dve", lambda: nc.vector.tensor_scalar(out=slot[:, 0:nrt, :], in0=slot[:, 0:nrt, :], scalar1=BIGI, scalar2=None, op0=ALU.add),
                 reads=[slot], writes=[slot], partial=True)
            idxi = S.sb([128, NTT, NE], I32, "idxi", st)
            S.op("dve", lambda: nc.vector.tensor_copy(out=idxi[:, 0:nrt, :], in_=slot[:, 0:nrt, :]), reads=[slot], writes=[idxi])
            tid = S.sb([128, NTT], I32, "tid", st)
            S.op("pool", lambda: nc.gpsimd.iota(tid[:], pattern=[[128, NTT]], base=0, channel_multiplier=1), writes=[tid])
            pay = S.sb([128, NTT, NE, 2], I32, "pay", st)
            S.op("dve", lambda: nc.vector.tensor_copy(out=pay[:, :, :, 0], in_=tid[:].unsqueeze(2).to_broadcast([128, NTT, NE])),
                 reads=[tid], writes=[pay], partial=True)
            S.op("dve", lambda: nc.vector.tensor_copy(out=pay[:].bitcast(F32)[:, :, :, 1], in_=AFF[:]), reads=[AFF], writes=[pay], partial=True)
            lflat = LST.ap().rearrange("e s c -> (e s) c")
            for ti in range(nrt):
                for e in range(NE):
                    S.dma("pool", lambda: nc.gpsimd.indirect_dma_start(
                        out=lflat, out_offset=bass.IndirectOffsetOnAxis(ap=idxi[:, ti, e:e + 1], axis=0),
                        in_=pay[:, ti, e, :], in_offset=None, bounds_check=bc_reg, oob_is_err=False),
                        reads=[pay, idxi], writes=[LST])
            if "route" in debug and l == 0:
                dbg["thr"] = S.dram("dbg_thr", [128, 32], F32, kind="ExternalOutput")
                S.dma("sp", lambda: nc.sync.dma_start(out=dbg["thr"][:, :], in_=lo[:]), reads=[lo], writes=[dbg["thr"]])
                dbg["idx"] = S.dram("dbg_idx", [128, NTT, NE], I32, kind="ExternalOutput")
                S.dma("sp", lambda: nc.sync.dma_start(out=dbg["idx"].ap(), in_=idxi[:]), reads=[idxi], writes=[dbg["idx"]])
            S.end_phase()
        if stop_after == "6":
            break

        with ES() as st:
            nst7 = 8 if last else 9
            NS = nst7 * 128
            ngr = [(0, 512), (512, 512)] + ([] if last else [(1024, 128)])
            wgs = [S.sb([128, 8, 1024], BF16, "wg", st) for _ in range(2)]
            wus = [S.sb([128, 8, 1024], BF16, "wu", st) for _ in range(2)]
            wd = S.sb([128, 16, D], BF16, "wd", st)
            xeT = S.sb([128, 8, NS], BF16, "xeT", st)
            hTe = S.sb([128, 16, NS], BF16, "hTe", st)
            idts = [S.sb([128, 9, 2], I32, "idt", st) for _ in range(2)]
            xgs = [S.sb([128, D], BF16, "xg", st) for _ in range(nst7)]
            ysbs = [S.sb([128, D], F32, "ysb", st) for _ in range(2)]
            sgl = [S.sb([128, 512], F32, "sgl", st) for _ in range(2)]
            ptx = S.ps([128, 8, 128], BF16, "ptx", st)
            pgs = [S.ps([128, 512], F32, "pg", st) for _ in range(2)]
            pus = [S.ps([128, 512], F32, "pu", st) for _ in range(2)]
            pys = [S.ps([128, 512], F32, "py", st) for _ in range(2)]
            cnt7 = {"fc": 0, "ys": 0}
            sc_state = {"prev": [], "cur": []}

            def dma_wgu(e, half):
                c0 = half * 1024
                S.dma("pool", lambda: nc.gpsimd.dma_start(out=wgs[half][:], in_=w_gate.ap()[l, e][:, c0:c0 + 1024].rearrange("(k p) f -> p k f", p=128)),
                      reads=[w_gate], writes=[wgs[half]])
                S.dma("pool", lambda: nc.gpsimd.dma_start(out=wus[half][:], in_=w_up.ap()[l, e][:, c0:c0 + 1024].rearrange("(k p) f -> p k f", p=128)),
                      reads=[w_up], writes=[wus[half]])

            def dma_wd(e):
                for h in range(2):
                    S.dma("pool", lambda: nc.gpsimd.dma_start(out=wd[:, h * 8:(h + 1) * 8, :],
                                                              in_=w_down.ap()[l, e][h * 1024:(h + 1) * 1024, :].rearrange("(k p) n -> p k n", p=128)),
                          reads=[w_down], writes=[wd], partial=(h > 0))

            def gather(e):
                idt = idts[e % 2]
                S.dma("sp", lambda: nc.sync.dma_start(out=idt[:, 0:nst7, :], in_=LST.ap()[e, 0:NS, :].rearrange("(t p) c -> p t c", p=128)),
                      reads=[LST], writes=[idt])
                for si in range(nst7):
                    S.dma("pool", lambda: nc.gpsimd.indirect_dma_start(
                        out=xgs[si][:], out_offset=None, in_=H2.ap(),
                        in_offset=bass.IndirectOffsetOnAxis(ap=idt[:, si, 0:1], axis=0)), reads=[H2, idt], writes=[xgs[si]])

            def transp(e):
                for si in range(nst7):
                    for k in range(8):
                        S.op("pe", lambda: nc.tensor.transpose(out=ptx[:, k, :], in_=xgs[si][:, k * 128:(k + 1) * 128], identity=idb[:]),
                             reads=[xgs[si], idb], writes=[ptx], partial=(k > 0))
                    S.op("dve", lambda: nc.vector.tensor_copy(out=xeT[:, :, si * 128:(si + 1) * 128], in_=ptx[:]),
                         reads=[ptx], writes=[xeT], partial=True)

            def hphase(e, half):
                wg_, wu_ = wgs[half], wus[half]
                for fl in range(8):
                    fc = half * 8 + fl
                    for (s0, N) in ngr:
                        i_ = cnt7["fc"]
                        cnt7["fc"] += 1
                        pg, pu, sg_ = pgs[i_ % 2], pus[i_ % 2], sgl[i_ % 2]
                        for k in range(8):
                            S.op("pe", lambda: nc.tensor.matmul(pg[:, 0:N], lhsT=wg_[:, k, fl * 128:(fl + 1) * 128], rhs=xeT[:, k, s0:s0 + N],
                                                                start=(k == 0), stop=(k == 7)), reads=[wg_, xeT], writes=[pg], partial=(k > 0))
                        for k in range(8):
                            S.op("pe", lambda: nc.tensor.matmul(pu[:, 0:N], lhsT=wu_[:, k, fl * 128:(fl + 1) * 128], rhs=xeT[:, k, s0:s0 + N],
                                                                start=(k == 0), stop=(k == 7)), reads=[wu_, xeT], writes=[pu], partial=(k > 0))
                        S.op("act", lambda: nc.scalar.activation(out=sg_[:, 0:N], in_=pg[:, 0:N], func=AF.Silu), reads=[pg], writes=[sg_])
                        S.op("dve", lambda: nc.vector.tensor_tensor(out=hTe[:, fc, s0:s0 + N], in0=sg_[:, 0:N], in1=pu[:, 0:N], op=ALU.mult),
                             reads=[sg_, pu], writes=[hTe], partial=True)

            def yphase(e, tiles):
                idt = idts[e % 2]
                for si in tiles:
                    ysb = ysbs[cnt7["ys"] % 2]
                    cnt7["ys"] += 1
                    gate = idt[:].bitcast(F32)[:, si, 1:2]
                    for dh in range(2):
                        py = pys[dh]
                        for fc in range(16):
                            S.op("pe", lambda: nc.tensor.matmul(py[:], lhsT=hTe[:, fc, si * 128:(si + 1) * 128], rhs=wd[:, fc, dh * 512:(dh + 1) * 512],
                                                                start=(fc == 0), stop=(fc == 15)), reads=[hTe, wd], writes=[py], partial=(fc > 0))
                        if dh == 0:
                            S.op("dve", lambda: nc.vector.tensor_scalar(out=ysb[:, 0:512], in0=py[:], scalar1=gate, scalar2=None, op0=ALU.mult),
                                 reads=[py, idt], writes=[ysb], partial=True)
                        else:
                            S.op("act", lambda: nc.scalar.activation(out=ysb[:, 512:1024], in_=py[:], func=AF.Copy, scale=gate),
                                 reads=[py, idt], writes=[ysb], partial=True)
                    tok = S.dma("pool", lambda: nc.gpsimd.indirect_dma_start(
                        out=YACC.ap(), out_offset=bass.IndirectOffsetOnAxis(ap=idt[:, si, 0:1], axis=0),
                        in_=ysb[:], in_offset=None, compute_op=ALU.add), reads=[ysb, idt], writes=[YACC], after=sc_state["prev"])
                    sc_state["cur"].append(tok)

            dma_wgu(0, 0)
            dma_wgu(0, 1)
            dma_wd(0)
            gather(0)
            transp(0)
            for e in range(NE):
                nxt = e + 1 < NE
                hphase(e, 0)
                if nxt:
                    dma_wgu(e + 1, 0)
                hphase(e, 1)
                if nxt:
                    dma_wgu(e + 1, 1)
                    gather(e + 1)
                sc_state["cur"] = []
                yphase(e, range(0, 4))
                if nxt:
                    transp(e + 1)
                yphase(e, range(4, nst7))
                if nxt:
                    dma_wd(e + 1)
                sc_state["prev"] = sc_state["cur"][-2:]
            S.end_phase()
        if stop_after == "7":
            break

        with ES() as st:
            mod_l, mod_c = load_mod(st, 5 * D, D)
            xt8 = [S.sb([128, D], F32, "xt8", st) for _ in range(2)]
            yt8 = [S.sb([128, D], F32, "yt8", st) for _ in range(2)]
            xo8 = [S.sb([128, D], F32, "xo8", st) for _ in range(2)]
            sq8 = S.sb([128, D], F32, "sq8", st)
            ss8 = S.sb([128, 1], F32, "ss8", st)
            fg = S.sb([128, D], F32, "fg", st)
            if last:
                S.dma("sp", lambda: nc.sync.dma_start(out=fg[:], in_=final_g.ap().partition_broadcast(128)), reads=[final_g], writes=[fg])
            for ti in range(ntq):
                r0 = ti * 128
                mod = mod_l if ti < NTL else mod_c
                xt, yt, xo = xt8[ti % 2], yt8[ti % 2], xo8[ti % 2]
                S.dma("sp", lambda: nc.sync.dma_start(out=xt[:], in_=X[r0:r0 + 128, :]), reads=[X], writes=[xt])
                S.dma("sp", lambda: nc.sync.dma_start(out=yt[:], in_=YACC[r0:r0 + 128, :]), reads=[YACC], writes=[yt])
                S.op("dve", lambda: nc.vector.tensor_tensor(out=yt[:], in0=yt[:], in1=mod[:, 5 * D:6 * D], op=ALU.mult), reads=[yt, mod], writes=[yt])
                S.op("pool", lambda: nc.gpsimd.tensor_tensor(out=xo[:], in0=yt[:], in1=xt[:], op=ALU.add), reads=[yt, xt], writes=[xo])
                if not last:
                    S.dma("sp", lambda: nc.sync.dma_start(out=X[r0:r0 + 128, :], in_=xo[:]), reads=[xo], writes=[X])
                else:
                    S.op("act", lambda: nc.scalar.activation(out=sq8[:], in_=xo[:], func=AF.Square, accum_out=ss8[:]), reads=[xo], writes=[sq8, ss8])
                    S.op("dve", lambda: nc.vector.tensor_scalar(out=ss8[:], in0=ss8[:], scalar1=1.0 / D, scalar2=EPS, op0=ALU.mult, op1=ALU.add),
                         reads=[ss8], writes=[ss8])
                    S.op("act", lambda: nc.scalar.activation(out=ss8[:], in_=ss8[:], func=AF.Sqrt), reads=[ss8], writes=[ss8])
                    S.op("dve", lambda: nc.vector.reciprocal(out=ss8[:], in_=ss8[:]), reads=[ss8], writes=[ss8])
                    S.op("dve", lambda: nc.vector.scalar_tensor_tensor(out=xt[:], in0=xo[:], scalar=ss8[:, 0:1], in1=fg[:], op0=ALU.mult, op1=ALU.mult),
                         reads=[xo, ss8, fg], writes=[xt])
                    S.dma("sp", lambda: nc.sync.dma_start(out=out[r0:r0 + 128, :], in_=xt[:]), reads=[xt], writes=[out])
            S.end_phase()
        if stop_after == "8":
            break

    def tap(name, buf, shape, dt):
        o = S.dram("dbg_" + name, shape, dt, kind="ExternalOutput")
        S.dma("sp", lambda: nc.sync.dma_start(out=o.ap(), in_=buf.ap()), reads=[buf], writes=[o])

    for name in debug:
        if name == "QT":
            tap("QT", QT, [128, 4, TT], BF16)
        if name == "KT":
            tap("KT", KT, [128, TT], BF16)
        if name == "VA":
            tap("VA", VA, [TT, 130], BF16)
        if name == "QBT":
            tap("QBT", QBT, [128, 2, TT], BF16)
        if name == "KBT":
            tap("KBT", KBT, [128, 2, TT], BF16)
        if name == "VB":
            tap("VB", VB, [TT, 256], BF16)
        if name == "UT":
            tap("UT", UT, [256, TT], F32)
        if name == "X":
            tap("X", X, [TT, D], F32)
        if name == "AT":
            tap("AT", AT, [512, TT], BF16)
        if name == "BT":
            tap("BT", BT, [256, TT], BF16)
        if name == "CTs":
            tap("CTs", CTs, [256, TT], BF16)
        if name == "H2":
            tap("H2", H2, [TT + NPAD, D], BF16)
        if name == "LST":
            tap("LST", LST, [NE, MS, 2], I32)
        if name == "YACC":
            tap("YACC", YACC, [TT + NPAD, D], F32)
        if name == "AFF":
            o_ = S.dram("dbg_AFF", [128, NTT, NE], F32, kind="ExternalOutput")
            S.dma("sp", lambda: nc.sync.dma_start(out=o_.ap(), in_=AFF[:]), reads=[AFF], writes=[o_])
    S.barrier()
    return nc


def host_consts(na_rpb, n_layers):
    t = np.arange(T)
    row = (t // 64).astype(np.float64)
    col = (t % 64).astype(np.float64)
    inv = 10000.0 ** (-np.arange(16, dtype=np.float64) / 16)
    rc = np.ones((TT, 64), np.float32)
    rs = np.zeros((TT, 64), np.float32)
    for a, pos in enumerate((row, col)):
        ang = (pos.astype(np.float32)[:, None] * inv.astype(np.float32)[None, :]).astype(np.float32)
        cs, sn = np.cos(ang), np.sin(ang)
        rc[:T, a * 32:a * 32 + 16] = cs
        rc[:T, a * 32 + 16:a * 32 + 32] = cs
        rs[:T, a * 32:a * 32 + 16] = -sn
        rs[:T, a * 32 + 16:a * 32 + 32] = sn
    q = np.arange(64)
    cs0 = np.clip(q - 8, 0, 48)
    c = np.arange(64)
    valid = (c[None, :] >= cs0[:, None]) & (c[None, :] < cs0[:, None] + 16)
    dc = np.clip(c[None, :] - q[:, None] + 15, 0, 30)
    nab = np.full((n_layers, 8, 64, 4, 8, 64), NEG, np.float32)
    for off in range(8):
        for j in range(8):
            g = na_rpb[:n_layers, :, off + j, :][:, :, dc]
            g = np.where(valid[None, None], g, np.float32(NEG))
            nab[:, off, :, :, j, :] = np.transpose(g, (0, 2, 1, 3))
    lpad = np.zeros((NPAD, 2), np.int32)
    lpad[:, 0] = TT + np.arange(NPAD)
    return {"ident": np.eye(128, dtype=np.float32), "utri": np.triu(np.ones((128, 128), np.float32), 1), "rope_c": rc, "rope_s": rs,
            "nabias": nab.reshape(n_layers, 8, 64, 4, 512), "lpad": lpad}


_WNAMES = ["w_ada", "b_ada", "norm1_g", "norm2_g", "w_in", "q_norm_g", "k_norm_g", "conv_w", "conv_b",
           "conv_ln_g", "conv_ln_b", "w_out", "w_router", "w_gate", "w_up", "w_down"]


def make_in_maps(inputs, n_layers, samples):
    consts = host_consts(np.asarray(inputs["na_rpb"]), n_layers)
    maps = []
    for b in samples:
        m = {"x": np.ascontiguousarray(inputs["x"][b]), "ctx": np.ascontiguousarray(inputs["ctx"][b]),
             "c": np.ascontiguousarray(inputs["c"][b]), "c_ctx": np.ascontiguousarray(inputs["c_ctx"]),
             "final_norm_g": np.ascontiguousarray(inputs["final_norm_g"])}
        for k in _WNAMES:
            m[k] = np.ascontiguousarray(inputs[k][:n_layers])
        m.update(consts)
        maps.append(m)
    return maps


def kernel(**inputs):
    inputs = {k: np.asarray(v) for k, v in inputs.items()}
    nc = build_program(DEPTH)
    maps = make_in_maps(inputs, DEPTH, range(4))
    res = run_bass_kernel_spmd(nc, maps, core_ids=list(range(4)))
    return np.stack([r["out"] for r in res.results], 0).astype(np.float32)
```

```python
import contextlib
import numpy as np
import concourse.bass as bass
import concourse.mybir as mybir
from concourse.bass_utils import run_bass_kernel_spmd

F32 = mybir.dt.float32
BF16 = mybir.dt.bfloat16
I32 = mybir.dt.int32
AF = mybir.ActivationFunctionType
ALU = mybir.AluOpType
AX = mybir.AxisListType

D = 1024
T = 8192
CT = 256
TT = T + CT
NTL = T // 128
NTT = TT // 128
DEPTH = 4
NE = 16
FF = 2048
CAP = 1024
CCAP = 32
NPAD = 96
MS = CAP + CCAP + NPAD
EPS = 1e-6
NEG = -30000.0
P2_LIMIT = 0
P3_LIMIT = 0


class Buf:
    def __init__(self, t, name, space):
        self.t = t
        self.name = name
        self.space = space
        self.writes = {}
        self.reads = {}
        self.sem = None
        self.total = 0

    def __getitem__(self, idx):
        return self.t[idx]

    def ap(self):
        return self.t.ap()


class ModSlice(Buf):
    def __getitem__(self, idx):
        p, sl = idx
        return self.t[p, sl.start - self.off:sl.stop - self.off]


class Sched:
    def __init__(self, nc):
        self.nc = nc
        self.eng = {"pe": nc.tensor, "act": nc.scalar, "dve": nc.vector,
                    "pool": nc.gpsimd, "sp": nc.sync}
        self.psem = {k: nc.alloc_semaphore("prog_" + k) for k in self.eng}
        self.cnt = {k: 0 for k in self.eng}
        self.seen = {k: {} for k in self.eng}
        self.sem_pool = []
        self.live = []
        self.nbuf = 0
        self.nsem = 0

    def sb(self, shape, dtype, name=None, stack=None):
        self.nbuf += 1
        name = (name or "sb") + "_%d" % self.nbuf
        if stack is None:
            t = self.nc.alloc_sbuf_tensor(name, list(shape), dtype)
        else:
            t = stack.enter_context(self.nc.sbuf_tensor(name, list(shape), dtype))
        b = Buf(t, name, "sb")
        b.local = stack is not None
        return b

    def ps(self, shape, dtype=F32, name=None, stack=None):
        self.nbuf += 1
        name = (name or "ps") + "_%d" % self.nbuf
        if stack is None:
            t = self.nc.alloc_psum_tensor(name, list(shape), dtype)
        else:
            t = stack.enter_context(self.nc.psum_tensor(name, list(shape), dtype))
        b = Buf(t, name, "ps")
        b.local = stack is not None
        return b

    def dram(self, name, shape, dtype, kind="Internal"):
        b = Buf(self.nc.dram_tensor(name, list(shape), dtype, kind=kind), name, "dram")
        b.local = False
        return b

    def _wait(self, e, tok):
        sem, v = tok
        key = id(sem)
        if self.seen[e].get(key, 0) >= v:
            return
        self.seen[e][key] = v
        self.eng[e].wait_ge(sem, v)

    def _deps(self, e, reads, writes):
        own = id(self.psem[e])
        toks = []
        for r in reads:
            if r.space == "dram":
                continue
            for t in r.writes.values():
                if id(t[0]) == own and e == "pe":
                    continue
                toks.append(t)
        for w in writes:
            if w.space == "dram":
                continue
            for t in list(w.writes.values()) + list(w.reads.values()):
                if id(t[0]) == own:
                    continue
                toks.append(t)
        for t in toks:
            self._wait(e, t)

    def _record(self, tok, reads, writes, partial):
        k = id(tok[0])
        for r in reads:
            if r.space != "dram":
                r.reads[k] = tok
        for w in writes:
            if w.space == "dram":
                continue
            if partial:
                w.writes[k] = tok
            else:
                w.writes = {k: tok}
                w.reads = {}

    def op(self, e, ins, reads=(), writes=(), partial=False):
        self._deps(e, reads, writes)
        i = ins()
        self.cnt[e] += 1
        i.then_inc(self.psem[e], 1)
        tok = (self.psem[e], self.cnt[e])
        self._record(tok, reads, writes, partial)
        return tok

    def dma(self, q, mk, reads=(), writes=(), partial=False, after=()):
        self._deps(q, reads, writes)
        for t in after:
            self._wait(q, t)
        owner = None
        for b in list(writes) + list(reads):
            if b.space != "dram":
                owner = b
                break
        if owner is None:
            owner = (list(writes) + list(reads))[0]
        if owner.sem is None:
            if self.sem_pool:
                owner.sem, owner.total = self.sem_pool.pop()
            else:
                self.nsem += 1
                owner.sem = self.nc.alloc_semaphore("dsem%d" % self.nsem)
                owner.total = 0
            self.live.append(owner)
        i = mk()
        owner.total += 16
        i.then_inc(owner.sem, 16)
        tok = (owner.sem, owner.total)
        self._record(tok, reads, writes, partial)
        return tok

    def barrier(self):
        toks = [(self.psem[k], self.cnt[k]) for k in self.eng if self.cnt[k] > 0]
        toks += [(b.sem, b.total) for b in self.live]
        for e in self.eng:
            for t in toks:
                if id(t[0]) == id(self.psem[e]):
                    continue
                self._wait(e, t)

    def end_phase(self):
        self.barrier()
        keep = []
        for b in self.live:
            if b.local:
                self.sem_pool.append((b.sem, b.total))
                b.sem = None
            else:
                keep.append(b)
        self.live = keep


def build_program(n_layers=DEPTH, debug=(), stop_after=None):
    nc = bass.Bass("TRN2", target_bir_lowering=False)
    S = Sched(nc)
    ES = contextlib.ExitStack
    bc_reg = nc.gpsimd.to_reg(NE * MS - 1)
    L = n_layers

    def din(name, shape, dt=F32):
        return S.dram(name, shape, dt, kind="ExternalInput")

    x_in = din("x", [T, D])
    ctx_in = din("ctx", [CT, D])
    c_in = din("c", [D])
    cc_in = din("c_ctx", [D])
    w_ada = din("w_ada", [L, D, 6 * D])
    b_ada = din("b_ada", [L, 6 * D])
    norm1_g = din("norm1_g", [L, D])
    norm2_g = din("norm2_g", [L, D])
    w_in = din("w_in", [L, D, 2048])
    q_norm_g = din("q_norm_g", [L, 64])
    k_norm_g = din("k_norm_g", [L, 64])
    conv_w = din("conv_w", [L, 31, 256])
    conv_b = din("conv_b", [L, 256])
    conv_ln_g = din("conv_ln_g", [L, 256])
    conv_ln_b = din("conv_ln_b", [L, 256])
    w_out = din("w_out", [L, D, D])
    w_router = din("w_router", [L, D, NE])
    has_moe = stop_after is None or stop_after in ("7", "8")
    if has_moe:
        w_gate = din("w_gate", [L, NE, D, FF])
        w_up = din("w_up", [L, NE, D, FF])
        w_down = din("w_down", [L, NE, FF, D])
    final_g = din("final_norm_g", [D])
    ident_in = din("ident", [128, 128])
    rope_c = din("rope_c", [TT, 64])
    rope_s = din("rope_s", [TT, 64])
    nabias = din("nabias", [L, 8, 64, 4, 512])
    lpad = din("lpad", [NPAD, 2], I32)
    utri_in = din("utri", [128, 128])
    out = S.dram("out", [T, D], F32, kind="ExternalOutput")
    dbg = {}

    X = S.dram("X", [TT, D], F32)
    QT = S.dram("QT", [128, 4, TT], BF16)
    KT = S.dram("KT", [128, TT], BF16)
    VA = S.dram("VA", [TT, 130], BF16)
    QBT = S.dram("QBT", [128, 2, TT], BF16)
    KBT = S.dram("KBT", [128, 2, TT], BF16)
    VB = S.dram("VB", [TT, 256], BF16)
    UT = S.dram("UT", [256, TT], F32)
    AT = S.dram("AT", [512, TT], BF16)
    BT = S.dram("BT", [256, TT], BF16)
    CTs = S.dram("CTs", [256, TT], BF16)
    H2 = S.dram("H2", [TT + NPAD, D], BF16)
    YACC = S.dram("YACC", [TT + NPAD, D], F32)
    LST = S.dram("LST", [NE, MS, 2], I32)

    idf = S.sb([128, 128], F32, "idf")
    idb = S.sb([128, 128], BF16, "idb")
    ones_b = S.sb([128, 128], BF16, "ones_b")
    ones_f = S.sb([128, 128], F32, "ones_f")
    zeros_f = S.sb([128, D], F32, "zeros_f")
    MODS = S.dram("MODS", [2, 128, 6 * D], F32)

    def load_mod(st, off, n):
        res_ = []
        for w_ in range(2):
            b_ = S.sb([128, n], F32, "modw", st)
            b_.__class__ = ModSlice
            b_.off = off
            S.dma("sp", lambda: nc.sync.dma_start(out=b_.t[:], in_=MODS[w_, :, off:off + n]), reads=[MODS], writes=[b_])
            res_.append(b_)
        return res_
    crep_l = S.sb([128, 8, 128], BF16, "crep_l")
    crep_c = S.sb([128, 8, 128], BF16, "crep_c")
    AFF = S.sb([128, NTT, NE], F32, "AFF")

    def v_(e):
        return {"dve": nc.vector, "pool": nc.gpsimd}[e]

    S.dma("sp", lambda: nc.sync.dma_start(out=idf[:], in_=ident_in[:, :]), reads=[ident_in], writes=[idf])
    S.op("dve", lambda: nc.vector.tensor_copy(out=idb[:], in_=idf[:]), reads=[idf], writes=[idb])
    S.op("dve", lambda: nc.vector.memset(ones_b[:], 1.0), writes=[ones_b])
    S.op("dve", lambda: nc.vector.memset(ones_f[:], 1.0), writes=[ones_f])
    S.op("dve", lambda: nc.vector.memset(zeros_f[:], 0.0), writes=[zeros_f])
    with ES() as st:
        cT = S.sb([128, 2, 8], F32, "cT", st)
        with nc.allow_non_contiguous_dma(reason="tiny"):
            S.dma("sp", lambda: nc.sync.dma_start(out=cT[:, 0, :], in_=c_in.ap().rearrange("(k p) -> p k", p=128)),
                  reads=[c_in], writes=[cT], partial=True)
            S.dma("sp", lambda: nc.sync.dma_start(out=cT[:, 1, :], in_=cc_in.ap().rearrange("(k p) -> p k", p=128)),
                  reads=[cc_in], writes=[cT], partial=True)
        sT = S.sb([128, 2, 8], F32, "sT", st)
        S.op("act", lambda: nc.scalar.activation(out=sT[:], in_=cT[:], func=AF.Silu), reads=[cT], writes=[sT])
        for k in range(8):
            S.op("dve", lambda: nc.vector.tensor_scalar(out=crep_l[:, k, :], in0=ones_b[:], scalar1=sT[:, 0, k:k + 1],
                                                        scalar2=None, op0=ALU.mult),
                 reads=[ones_b, sT], writes=[crep_l], partial=True)
            S.op("dve", lambda: nc.vector.tensor_scalar(out=crep_c[:, k, :], in0=ones_b[:], scalar1=sT[:, 1, k:k + 1],
                                                        scalar2=None, op0=ALU.mult),
                 reads=[ones_b, sT], writes=[crep_c], partial=True)
        zb = S.sb([NPAD, D], BF16, "zb", st)
        S.op("dve", lambda: nc.vector.memset(zb[:], 0.0), writes=[zb])
        S.dma("sp", lambda: nc.sync.dma_start(out=H2[TT:TT + NPAD, :], in_=zb[:]), reads=[zb], writes=[H2])
        lp = S.sb([NPAD, 2], I32, "lp", st)
        S.dma("sp", lambda: nc.sync.dma_start(out=lp[:], in_=lpad[:, :]), reads=[lpad], writes=[lp])
        for e in range(NE):
            S.dma("sp", lambda: nc.sync.dma_start(out=LST[e, CAP + CCAP:MS, :], in_=lp[:]), reads=[lp], writes=[LST])
        xts = [S.sb([128, D], F32, "xcp", st) for _ in range(2)]
        for i in range(NTT):
            xt = xts[i % 2]
            src = x_in[i * 128:(i + 1) * 128, :] if i < NTL else ctx_in[(i - NTL) * 128:(i - NTL + 1) * 128, :]
            S.dma("sp", lambda: nc.sync.dma_start(out=xt[:], in_=src), reads=[x_in], writes=[xt])
            S.dma("sp", lambda: nc.sync.dma_start(out=X[i * 128:(i + 1) * 128, :], in_=xt[:]), reads=[xt], writes=[X])
        S.end_phase()

    for l in range(L):
        last = (l == DEPTH - 1)
        ntq = NTL if last else NTT

        with ES() as st:
            mod_l = S.sb([128, 6 * D], F32, "mod_l", st)
            mod_c = S.sb([128, 6 * D], F32, "mod_c", st)
            brep = S.sb([128, 6 * D], F32, "brep", st)
            S.dma("sp", lambda: nc.sync.dma_start(out=brep[:], in_=b_ada.ap()[l].partition_broadcast(128)),
                  reads=[b_ada], writes=[brep])
            g1rep = S.sb([128, D], F32, "g1rep", st)
            g2rep = S.sb([128, D], F32, "g2rep", st)
            S.dma("sp", lambda: nc.sync.dma_start(out=g1rep[:], in_=norm1_g.ap()[l].partition_broadcast(128)),
                  reads=[norm1_g], writes=[g1rep])
            S.dma("sp", lambda: nc.sync.dma_start(out=g2rep[:], in_=norm2_g.ap()[l].partition_broadcast(128)),
                  reads=[norm2_g], writes=[g2rep])
            was = [S.sb([128, 8, 512], BF16, "wa", st) for _ in range(2)]
            pms = [S.ps([128, 512], F32, "pm", st) for _ in range(2)]
            for cc in range(12):
                wa = was[cc % 2]
                S.dma("pool", lambda: nc.gpsimd.dma_start(
                    out=wa[:], in_=w_ada.ap()[l][:, cc * 512:(cc + 1) * 512].rearrange("(k p) n -> p k n", p=128)),
                    reads=[w_ada], writes=[wa])
                for which, (crep, mod) in enumerate(((crep_l, mod_l), (crep_c, mod_c))):
                    pm = pms[which]
                    for k in range(8):
                        S.op("pe", lambda: nc.tensor.matmul(pm[:], lhsT=crep[:, k, :], rhs=wa[:, k, :],
                                                            start=(k == 0), stop=(k == 7)),
                             reads=[crep, wa], writes=[pm], partial=(k > 0))
                    S.op("dve", lambda: nc.vector.tensor_tensor(out=mod[:, cc * 512:(cc + 1) * 512], in0=pm[:],
                                                                in1=brep[:, cc * 512:(cc + 1) * 512], op=ALU.add),
                         reads=[pm, brep], writes=[mod], partial=True)
            for mod in (mod_l, mod_c):
                S.op("dve", lambda: nc.vector.scalar_tensor_tensor(out=mod[:, D:2 * D], in0=mod[:, D:2 * D], scalar=1.0,
                                                                   in1=g1rep[:], op0=ALU.add, op1=ALU.mult),
                     reads=[mod, g1rep], writes=[mod], partial=True)
                S.op("dve", lambda: nc.vector.scalar_tensor_tensor(out=mod[:, 4 * D:5 * D], in0=mod[:, 4 * D:5 * D],
                                                                   scalar=1.0, in1=g2rep[:], op0=ALU.add, op1=ALU.mult),
                     reads=[mod, g2rep], writes=[mod], partial=True)
            S.dma("sp", lambda: nc.sync.dma_start(out=MODS[0], in_=mod_l[:]), reads=[mod_l], writes=[MODS])
            S.dma("sp", lambda: nc.sync.dma_start(out=MODS[1], in_=mod_c[:]), reads=[mod_c], writes=[MODS])
            S.end_phase()
        if stop_after == "M":
            break

        with ES() as st:
            mod_l, mod_c = load_mod(st, 0, 2 * D)
            win = S.sb([128, 8, 2048], BF16, "win", st)
            for h in range(4):
                S.dma("pool", lambda: nc.gpsimd.dma_start(
                    out=win[:, :, h * 512:(h + 1) * 512],
                    in_=w_in.ap()[l][:, h * 512:(h + 1) * 512].rearrange("(k p) n -> p k n", p=128)),
                    reads=[w_in], writes=[win], partial=True)
            gq = S.sb([128, 64], F32, "gq", st)
            gk = S.sb([128, 64], F32, "gk", st)
            S.dma("sp", lambda: nc.sync.dma_start(out=gq[:], in_=q_norm_g.ap()[l].partition_broadcast(128)),
                  reads=[q_norm_g], writes=[gq])
            S.dma("sp", lambda: nc.sync.dma_start(out=gk[:], in_=k_norm_g.ap()[l].partition_broadcast(128)),
                  reads=[k_norm_g], writes=[gk])
            S.op("dve", lambda: nc.vector.tensor_scalar(out=gq[:], in0=gq[:], scalar1=0.125, scalar2=None, op0=ALU.mult),
                 reads=[gq], writes=[gq])
            xts = [S.sb([128, D], F32, "xt", st) for _ in range(2)]
            sq = S.sb([128, D], F32, "sq", st)
            ss = S.sb([128, 1], F32, "ss", st)
            rstd = S.sb([128, 1], F32, "rstd", st)
            hf = S.sb([128, D], F32, "hf", st)
            hb = S.sb([128, D], BF16, "hb", st)
            hT = S.sb([128, 8, 512], BF16, "hT", st)
            rc = [S.sb([128, 64], F32, "rc", st) for _ in range(2)]
            rs_ = [S.sb([128, 64], F32, "rs", st) for _ in range(2)]
            qsq = S.sb([128, 640], F32, "qsq", st)
            qss = S.sb([128, 10], F32, "qss", st)
            qn = S.sb([128, 640], F32, "qn", st)
            t1 = S.sb([128, 640], F32, "t1", st)
            t2 = S.sb([128, 640], F32, "t2", st)
            qr = S.sb([128, 640], BF16, "qr", st)
            qbk = S.sb([128, 512], BF16, "qbk", st)
            qTs = S.sb([128, 4, 512], BF16, "qTs", st)
            kTs = S.sb([128, 512], BF16, "kTs", st)
            vas = S.sb([128, 4, 130], BF16, "vas", st)
            qbTs = S.sb([128, 2, 512], BF16, "qbTs", st)
            kbTs = S.sb([128, 2, 512], BF16, "kbTs", st)
            vbs = S.sb([128, 4, 256], BF16, "vbs", st)
            sg = S.sb([128, 512], F32, "sg", st)
            uTs = S.sb([128, 2, 512], F32, "uTs", st)
            pT = S.ps([128, 8, 128], BF16, "pT", st)
            pmm = [S.ps([128, 512], F32, "pmm", st) for _ in range(3)]
            pq = S.ps([128, 4, 128], BF16, "pq", st)
            pk = S.ps([128, 5, 128], BF16, "pk", st)
            pf = [S.ps([128, 512], F32, "pf", st) for _ in range(2)]
            S.op("dve", lambda: nc.vector.memset(vas[:], 1.0), writes=[vas])
            A1 = lambda mod: mod[:, D:2 * D]
            S1 = lambda mod: mod[:, 0:D]

            groups = [(g * 512, 512, mod_l) for g in range(T // 512)] + [(T, CT, mod_c)]
            for (t0, G, mod) in groups:
                nsub = G // 128
                for s in range(nsub):
                    r0 = t0 + s * 128
                    xt = xts[s % 2]
                    S.dma("sp", lambda: nc.sync.dma_start(out=xt[:], in_=X[r0:r0 + 128, :]), reads=[X], writes=[xt])
                    S.op("act", lambda: nc.scalar.activation(out=sq[:], in_=xt[:], func=AF.Square, accum_out=ss[:]),
                         reads=[xt], writes=[sq, ss])
                    S.op("dve", lambda: nc.vector.tensor_scalar(out=rstd[:], in0=ss[:], scalar1=1.0 / D, scalar2=EPS,
                                                                op0=ALU.mult, op1=ALU.add), reads=[ss], writes=[rstd])
                    S.op("act", lambda: nc.scalar.activation(out=rstd[:], in_=rstd[:], func=AF.Sqrt), reads=[rstd], writes=[rstd])
                    S.op("dve", lambda: nc.vector.reciprocal(out=rstd[:], in_=rstd[:]), reads=[rstd], writes=[rstd])
                    S.op("dve", lambda: nc.vector.scalar_tensor_tensor(out=hf[:], in0=xt[:], scalar=rstd[:, 0:1], in1=A1(mod),
                                                                       op0=ALU.mult, op1=ALU.mult),
                         reads=[xt, rstd, mod], writes=[hf])
                    S.op("pool", lambda: nc.gpsimd.tensor_tensor(out=hb[:], in0=hf[:], in1=S1(mod), op=ALU.add),
                         reads=[hf, mod], writes=[hb])
                    for k in range(8):
                        S.op("pe", lambda: nc.tensor.transpose(out=pT[:, k, :], in_=hb[:, k * 128:(k + 1) * 128], identity=idb[:]),
                             reads=[hb, idb], writes=[pT], partial=(k > 0))
                    S.op("act", lambda: nc.scalar.copy(out=hT[:, :, s * 128:(s + 1) * 128], in_=pT[:]),
                         reads=[pT], writes=[hT], partial=True)
                    for cg_ in range(3):
                        pm = pmm[cg_]
                        for k in range(8):
                            S.op("pe", lambda: nc.tensor.matmul(pm[:], lhsT=hT[:, k, s * 128:(s + 1) * 128],
                                                                rhs=win[:, k, cg_ * 512:(cg_ + 1) * 512],
                                                                start=(k == 0), stop=(k == 7)),
                                 reads=[hT, win], writes=[pm], partial=(k > 0))
                    rcb, rsb = rc[s % 2], rs_[s % 2]
                    S.dma("sp", lambda: nc.sync.dma_start(out=rcb[:], in_=rope_c[r0:r0 + 128, :]), reads=[rope_c], writes=[rcb])
                    S.dma("sp", lambda: nc.sync.dma_start(out=rsb[:], in_=rope_s[r0:r0 + 128, :]), reads=[rope_s], writes=[rsb])
                    S.op("act", lambda: nc.scalar.activation(out=qsq[:, 0:512], in_=pmm[0][:], func=AF.Square),
                         reads=[pmm[0]], writes=[qsq], partial=True)
                    S.op("act", lambda: nc.scalar.activation(out=qsq[:, 512:640], in_=pmm[1][:, 0:128], func=AF.Square),
                         reads=[pmm[1]], writes=[qsq], partial=True)
                    S.op("dve", lambda: nc.vector.tensor_reduce(out=qss[:], in_=qsq[:].rearrange("p (h d) -> p h d", d=64),
                                                                axis=AX.X, op=ALU.add), reads=[qsq], writes=[qss])
                    S.op("dve", lambda: nc.vector.tensor_scalar(out=qss[:], in0=qss[:], scalar1=1.0 / 64, scalar2=EPS,
                                                                op0=ALU.mult, op1=ALU.add), reads=[qss], writes=[qss])
                    S.op("act", lambda: nc.scalar.activation(out=qss[:], in_=qss[:], func=AF.Sqrt), reads=[qss], writes=[qss])
                    S.op("dve", lambda: nc.vector.reciprocal(out=qss[:], in_=qss[:]), reads=[qss], writes=[qss])
                    S.op("dve", lambda: nc.vector.tensor_tensor(
                        out=qn[:, 0:512].rearrange("p (h d) -> p h d", d=64), in0=pmm[0][:].rearrange("p (h d) -> p h d", d=64),
                        in1=qss[:, 0:8].unsqueeze(2).to_broadcast([128, 8, 64]), op=ALU.mult),
                        reads=[pmm[0], qss], writes=[qn], partial=True)
                    S.op("dve", lambda: nc.vector.tensor_tensor(
                        out=qn[:, 512:640].rearrange("p (h d) -> p h d", d=64),
                        in0=pmm[1][:, 0:128].rearrange("p (h d) -> p h d", d=64),
                        in1=qss[:, 8:10].unsqueeze(2).to_broadcast([128, 2, 64]), op=ALU.mult),
                        reads=[pmm[1], qss], writes=[qn], partial=True)
                    S.op("pool", lambda: nc.gpsimd.tensor_tensor(
                        out=qn[:, 0:512].rearrange("p (h d) -> p h d", d=64), in0=qn[:, 0:512].rearrange("p (h d) -> p h d", d=64),
                        in1=gq[:].unsqueeze(1).to_broadcast([128, 8, 64]), op=ALU.mult), reads=[qn, gq], writes=[qn], partial=True)
                    S.op("pool", lambda: nc.gpsimd.tensor_tensor(
                        out=qn[:, 512:640].rearrange("p (h d) -> p h d", d=64),
                        in0=qn[:, 512:640].rearrange("p (h d) -> p h d", d=64),
                        in1=gk[:].unsqueeze(1).to_broadcast([128, 2, 64]), op=ALU.mult), reads=[qn, gk], writes=[qn], partial=True)
                    S.op("dve", lambda: nc.vector.tensor_tensor(
                        out=t1[:].rearrange("p (h d) -> p h d", d=64), in0=qn[:].rearrange("p (h d) -> p h d", d=64),
                        in1=rcb[:].unsqueeze(1).to_broadcast([128, 10, 64]), op=ALU.mult), reads=[qn, rcb], writes=[t1])
                    qv = qn[:].rearrange("p (h a b c) -> p h a b c", h=10, a=2, b=2)
                    tv = t2[:].rearrange("p (h a b c) -> p h a b c", h=10, a=2, b=2)
                    sv = rsb[:].rearrange("p (a b c) -> p a b c", a=2, b=2)
                    for a in range(2):
                        for b_ in range(2):
                            S.op("pool", lambda: nc.gpsimd.tensor_tensor(
                                out=tv[:, :, a, b_, :], in0=qv[:, :, a, 1 - b_, :],
                                in1=sv[:, a, b_, :].unsqueeze(1).to_broadcast([128, 10, 16]), op=ALU.mult),
                                reads=[qn, rsb], writes=[t2], partial=True)
                    S.op("dve", lambda: nc.vector.tensor_tensor(
                        out=qr[:, 0:512].rearrange("p (g k d) -> p k g d", g=4, k=2),
                        in0=t1[:, 0:512].rearrange("p (k g d) -> p k g d", k=2, g=4),
                        in1=t2[:, 0:512].rearrange("p (k g d) -> p k g d", k=2, g=4), op=ALU.add),
                        reads=[t1, t2], writes=[qr])
                    S.op("dve", lambda: nc.vector.tensor_tensor(out=qr[:, 512:640], in0=t1[:, 512:640], in1=t2[:, 512:640],
                                                                op=ALU.add), reads=[t1, t2], writes=[qr], partial=True)
                    for g in range(4):
                        S.op("pe", lambda: nc.tensor.transpose(
                            out=pq[:, g, :], in_=qr[:, g * 128:(g + 1) * 128],
                            identity=idb[:]), reads=[qr, idb], writes=[pq], partial=(g > 0))
                    S.op("act", lambda: nc.scalar.copy(out=qTs[:, :, s * 128:(s + 1) * 128], in_=pq[:]),
                         reads=[pq], writes=[qTs], partial=True)
                    S.op("pe", lambda: nc.tensor.transpose(out=pk[:, 0, :], in_=qr[:, 512:640], identity=idb[:]),
                         reads=[qr, idb], writes=[pk], partial=False)
                    S.op("act", lambda: nc.scalar.copy(out=vas[:, s, :].rearrange("p (k e) -> p k e", e=65)[:, :, 0:64],
                                                       in_=pmm[1][:, 128:256].rearrange("p (k d) -> p k d", d=64)),
                         reads=[pmm[1]], writes=[vas], partial=True)
                    S.op("dve", lambda: nc.vector.tensor_scalar(out=qbk[:, 0:256], in0=pmm[1][:, 256:512], scalar1=0.125,
                                                                scalar2=None, op0=ALU.mult),
                         reads=[pmm[1]], writes=[qbk], partial=True)
                    S.op("act", lambda: nc.scalar.copy(out=qbk[:, 256:512], in_=pmm[2][:, 0:256]),
                         reads=[pmm[2]], writes=[qbk], partial=True)
                    S.op("act", lambda: nc.scalar.copy(out=vbs[:, s, :], in_=pmm[2][:, 256:512]),
                         reads=[pmm[2]], writes=[vbs], partial=True)
                    for j in range(4):
                        S.op("pe", lambda: nc.tensor.transpose(out=pk[:, 1 + j, :], in_=qbk[:, j * 128:(j + 1) * 128],
                                                               identity=idb[:]), reads=[qbk, idb], writes=[pk], partial=True)
                    S.op("dve", lambda: nc.vector.tensor_copy(out=kTs[:, s * 128:(s + 1) * 128], in_=pk[:, 0, :]),
                         reads=[pk], writes=[kTs], partial=True)
                    S.op("dve", lambda: nc.vector.tensor_copy(out=qbTs[:, :, s * 128:(s + 1) * 128], in_=pk[:, 1:3, :]),
                         reads=[pk], writes=[qbTs], partial=True)
                    S.op("dve", lambda: nc.vector.tensor_copy(out=kbTs[:, :, s * 128:(s + 1) * 128], in_=pk[:, 3:5, :]),
                         reads=[pk], writes=[kbTs], partial=True)
                for j in range(2):
                    for which in range(2):
                        c0 = 1536 + which * 256 + j * 128
                        for k in range(8):
                            S.op("pe", lambda: nc.tensor.matmul(pf[which][:, 0:G], lhsT=win[:, k, c0:c0 + 128], rhs=hT[:, k, 0:G],
                                                                start=(k == 0), stop=(k == 7)),
                                 reads=[win, hT], writes=[pf[which]], partial=(k > 0))
                    S.op("act", lambda: nc.scalar.activation(out=sg[:, 0:G], in_=pf[1][:, 0:G], func=AF.Sigmoid),
                         reads=[pf[1]], writes=[sg])
                    S.op("dve", lambda: nc.vector.tensor_tensor(out=uTs[:, j, 0:G], in0=pf[0][:, 0:G], in1=sg[:, 0:G], op=ALU.mult),
                         reads=[pf[0], sg], writes=[uTs], partial=True)
                S.dma("sp", lambda: nc.sync.dma_start(out=QT[:, :, t0:t0 + G], in_=qTs[:, :, 0:G]), reads=[qTs], writes=[QT])
                S.dma("sp", lambda: nc.sync.dma_start(out=KT[:, t0:t0 + G], in_=kTs[:, 0:G]), reads=[kTs], writes=[KT])
                S.dma("sp", lambda: nc.sync.dma_start(out=VA.ap()[t0:t0 + G, :].rearrange("(s p) e -> p s e", p=128),
                                                      in_=vas[:, 0:nsub, :]), reads=[vas], writes=[VA])
                S.dma("sp", lambda: nc.sync.dma_start(out=QBT[:, :, t0:t0 + G], in_=qbTs[:, :, 0:G]), reads=[qbTs], writes=[QBT])
                S.dma("sp", lambda: nc.sync.dma_start(out=KBT[:, :, t0:t0 + G], in_=kbTs[:, :, 0:G]), reads=[kbTs], writes=[KBT])
                S.dma("sp", lambda: nc.sync.dma_start(out=VB.ap()[t0:t0 + G, :].rearrange("(s p) e -> p s e", p=128),
                                                      in_=vbs[:, 0:nsub, :]), reads=[vbs], writes=[VB])
                S.dma("sp", lambda: nc.sync.dma_start(out=UT.ap()[:, t0:t0 + G].rearrange("(j p) t -> p j t", p=128),
                                                      in_=uTs[:, :, 0:G]), reads=[uTs], writes=[UT])
            S.end_phase()
        if stop_after == "1":
            break

        with ES() as st:
            ksb = S.sb([128, TT], BF16, "ksb", st)
            vsb = S.sb([128, NTT, 130], BF16, "vsb", st)
            S.dma("sp", lambda: nc.sync.dma_start(out=ksb[:], in_=KT[:, :]), reads=[KT], writes=[ksb])
            S.dma("sp", lambda: nc.sync.dma_start(out=vsb[:], in_=VA.ap().rearrange("(n p) e -> p n e", p=128)),
                  reads=[VA], writes=[vsb])
            gqk = S.sb([128, 2, 64], F32, "gqk", st)
            S.dma("sp", lambda: nc.sync.dma_start(out=gqk[:, 0, :], in_=q_norm_g.ap()[l].partition_broadcast(128)),
                  reads=[q_norm_g], writes=[gqk], partial=True)
            S.dma("sp", lambda: nc.sync.dma_start(out=gqk[:, 1, :], in_=k_norm_g.ap()[l].partition_broadcast(128)),
                  reads=[k_norm_g], writes=[gqk], partial=True)
            gmx = S.sb([128, 2], F32, "gmx", st)
            nb = S.sb([128, 1], F32, "nb", st)
            gng = S.sb([128, 2, 64], F32, "gng", st)
            S.op("dve", lambda: nc.vector.tensor_scalar(out=gng[:], in0=gqk[:], scalar1=-1.0, scalar2=None, op0=ALU.mult),
                 reads=[gqk], writes=[gng])
            S.op("dve", lambda: nc.vector.tensor_tensor(out=gqk[:], in0=gqk[:], in1=gng[:], op=ALU.max),
                 reads=[gqk, gng], writes=[gqk])
            S.op("dve", lambda: nc.vector.tensor_reduce(out=gmx[:], in_=gqk[:], axis=AX.X, op=ALU.max), reads=[gqk], writes=[gmx])
            S.op("dve", lambda: nc.vector.tensor_tensor(out=nb[:], in0=gmx[:, 0:1], in1=gmx[:, 1:2], op=ALU.mult),
                 reads=[gmx], writes=[nb])
            S.op("dve", lambda: nc.vector.tensor_scalar(out=nb[:], in0=nb[:], scalar1=-8.0, scalar2=None, op0=ALU.mult),
                 reads=[nb], writes=[nb])
            qsbs = [S.sb([128, 2, 512], BF16, "qsb", st) for _ in range(2)]
            for qb_ in qsbs:
                S.op("dve", lambda: nc.vector.memset(qb_[:], 0.0), writes=[qb_])
            pbs = [S.sb([128, 512], BF16, "pb", st) for _ in range(3)]
            rsa = S.sb([128, 4], F32, "rsa", st)
            osb = [S.sb([128, 512], BF16, "osb", st) for _ in range(2)]
            aTs = [S.sb([128, 4, 128], BF16, "aTs", st) for _ in range(2)]
            pss = [S.ps([128, 512], F32, "pss", st) for _ in range(3)]
            pos = [S.ps([128, 512], F32, "po", st) for _ in range(4)]
            pa = S.ps([128, 4, 128], BF16, "pa", st)
            steps = []
            nq2 = min(ntq, P2_LIMIT) if P2_LIMIT else ntq
            for qi in range(nq2):
                ktiles = list(range(NTT)) if qi < NTL else [NTL, NTL + 1]
                for kh in range(2):
                    for idx, kt in enumerate(ktiles):
                        steps.append((qi, kh, idx, kt, len(ktiles)))
            nst_ = len(steps)

            def load_q(qi):
                q0 = qi * 128
                qsb = qsbs[qi % 2]
                for kh_ in range(2):
                    S.dma("sp", lambda: nc.sync.dma_start(
                        out=qsb[kh_ * 64:(kh_ + 1) * 64, kh_, :].rearrange("p (g t) -> p g t", g=4),
                        in_=QT[kh_ * 64:(kh_ + 1) * 64, :, q0:q0 + 128]), reads=[QT], writes=[qsb], partial=True)

            def emit_S(i):
                qi, kh, idx, kt, nk = steps[i]
                if kh == 0 and idx == 0 and qi + 1 < nq2:
                    load_q(qi + 1)
                qsb = qsbs[qi % 2]
                ps = pss[i % 3]
                S.op("pe", lambda: nc.tensor.matmul(ps[:], lhsT=ksb[:, kt * 128:(kt + 1) * 128],
                                                    rhs=qsb[:, kh, :], start=True, stop=True),
                     reads=[ksb, qsb], writes=[ps])

            def finish_q(qi):
                q0 = qi * 128
                ob = osb[qi % 2]
                for j in range(4):
                    S.op("pe", lambda: nc.tensor.transpose(out=pa[:, j, :], in_=ob[:, j * 128:(j + 1) * 128], identity=idb[:]),
                         reads=[ob, idb], writes=[pa], partial=(j > 0))
                aT = aTs[qi % 2]
                S.op("dve", lambda: nc.vector.tensor_copy(out=aT[:], in_=pa[:]), reads=[pa], writes=[aT])
                S.dma("sp", lambda: nc.sync.dma_start(out=AT.ap()[:, q0:q0 + 128].rearrange("(j p) t -> p j t", p=128), in_=aT[:]),
                      reads=[aT], writes=[AT])

            load_q(0)
            emit_S(0)
            if nst_ > 1:
                emit_S(1)
            deferred = {}
            for i in range(nst_):
                qi, kh, idx, kt, nk = steps[i]
                ps, pb = pss[i % 3], pbs[i % 3]
                ob = osb[qi % 2]
                S.op("act", lambda: nc.scalar.activation(out=pb[:], in_=ps[:], func=AF.Exp, bias=nb[:, 0:1], scale=1.0),
                     reads=[ps, nb], writes=[pb])
                for g in range(4):
                    S.op("pe", lambda: nc.tensor.matmul(pos[g][:, 0:65], lhsT=pb[:, g * 128:(g + 1) * 128],
                                                        rhs=vsb[:, kt, kh * 65:(kh + 1) * 65],
                                                        start=(idx == 0), stop=(idx == nk - 1)),
                         reads=[pb, vsb], writes=[pos[g]], partial=(idx > 0))
                if i + 2 < nst_:
                    emit_S(i + 2)
                if idx == nk - 1:
                    for g in range(4):
                        S.op("dve", lambda: nc.vector.reciprocal(out=rsa[:, g:g + 1], in_=pos[g][:, 64:65]),
                             reads=[pos[g]], writes=[rsa], partial=True)
                        S.op("dve", lambda: nc.vector.tensor_scalar(
                            out=ob[:, kh * 256 + g * 64:kh * 256 + (g + 1) * 64], in0=pos[g][:, 0:64], scalar1=rsa[:, g:g + 1],
                            scalar2=None, op0=ALU.mult), reads=[pos[g], rsa], writes=[ob], partial=True)
                    if kh == 1:
                        deferred[min(i + 4, nst_ - 1)] = deferred.get(min(i + 4, nst_ - 1), []) + [qi]
                for qd in deferred.pop(i, []):
                    finish_q(qd)
            S.end_phase()
        if stop_after == "2":
            break

        with ES() as st:
            kc = S.sb([128, 2, 256], BF16, "kc", st)
            vc = S.sb([128, 2, 256], BF16, "vc", st)
            S.dma("sp", lambda: nc.sync.dma_start(out=kc[:], in_=KBT[:, :, T:TT]), reads=[KBT], writes=[kc])
            S.dma("sp", lambda: nc.sync.dma_start(out=vc[:], in_=VB.ap()[T:TT, :].rearrange("(n p) e -> p n e", p=128)),
                  reads=[VB], writes=[vc])
            bint = S.sb([64, 4, 512], F32, "bint", st)
            bedges = [S.sb([64, 4, 512], F32, "bedge", st) for _ in range(2)]
            S.dma("sp", lambda: nc.sync.dma_start(out=bint[:], in_=nabias[l, 3]), reads=[nabias], writes=[bint])
            qrows = [S.sb([128, 2, 64], BF16, "qrow", st) for _ in range(2)]
            kwins = [S.sb([128, 2, 512], BF16, "kwin", st) for _ in range(2)]
            vwins = [S.sb([64, 8, 256], BF16, "vwin", st) for _ in range(2)]
            ssbs = [S.sb([64, 768], F32, "ssb", st) for _ in range(2)]
            mxs = [S.sb([64, 1], F32, "mx", st) for _ in range(2)]
            sms = [S.sb([64, 1], F32, "sm", st) for _ in range(2)]
            pexps = [S.sb([64, 768], BF16, "pexp", st) for _ in range(2)]
            ptw_ss = [S.sb([64, 8, 64], BF16, "ptw_s", st) for _ in range(2)]
            ptc_ss = [S.sb([128, 2, 64], BF16, "ptc_s", st) for _ in range(2)]
            brows = [S.sb([64, 256], BF16, "brow", st) for _ in range(2)]
            bts = [S.sb([128, 2, 64], BF16, "bts", st) for _ in range(2)]
            psn = [S.ps([64, 1024], F32, "psn", st) for _ in range(2)]
            ptps = [S.ps([64, 512], BF16, "ptp", st) for _ in range(1)] * 2
            ptcs_ = [S.ps([128, 2, 64], BF16, "ptcx", st) for _ in range(1)] * 2
            pbts_ = [S.ps([128, 2, 64], BF16, "pbtx", st) for _ in range(1)] * 2
            pons = [S.ps([64, 512], F32, "pon", st) for _ in range(1)] * 2
            NR = min(T // 64, P3_LIMIT) if P3_LIMIT else T // 64
            units = [(r, hb) for r in range(NR) for hb in range(4)]
            NU = len(units)

            def row_info(r):
                rs0 = min(max(r - 4, 0), 120)
                return rs0, rs0 - r + 7

            def load_row(r):
                rs0, off = row_info(r)
                qrow, kwin, vwin = qrows[r % 2], kwins[r % 2], vwins[r % 2]
                S.dma("sp", lambda: nc.sync.dma_start(out=qrow[:], in_=QBT[:, :, r * 64:(r + 1) * 64]), reads=[QBT], writes=[qrow])
                S.dma("sp", lambda: nc.sync.dma_start(out=kwin[:], in_=KBT[:, :, rs0 * 64:(rs0 + 8) * 64]), reads=[KBT], writes=[kwin])
                S.dma("sp", lambda: nc.sync.dma_start(out=vwin[:], in_=VB.ap()[rs0 * 64:(rs0 + 8) * 64, :].rearrange("(j p) e -> p j e", p=64)),
                      reads=[VB], writes=[vwin])
                if off != 3:
                    be = bedges[r % 2]
                    S.dma("sp", lambda: nc.sync.dma_start(out=be[:], in_=nabias[l, off]), reads=[nabias], writes=[be])

            def st_scores(t):
                r, hb = units[t]
                if hb == 1 and r + 1 < NR:
                    load_row(r + 1)
                hp, pr = hb // 2, (hb % 2) * 64
                qrow, kwin = qrows[r % 2], kwins[r % 2]
                ps = psn[t % 2]
                S.op("pe", lambda: nc.tensor.matmul(ps[:, 0:512], lhsT=qrow[pr:pr + 64, hp, :], rhs=kwin[pr:pr + 64, hp, :],
                                                    start=True, stop=True), reads=[qrow, kwin], writes=[ps])
                S.op("pe", lambda: nc.tensor.matmul(ps[:, 512:768], lhsT=qrow[pr:pr + 64, hp, :], rhs=kc[pr:pr + 64, hp, :],
                                                    start=True, stop=True), reads=[qrow, kc], writes=[ps], partial=True)

            def st_softmax(t):
                r, hb = units[t]
                rs0, off = row_info(r)
                bias = bint if off == 3 else bedges[r % 2]
                ps, ssb, mx, sm, pexp = psn[t % 2], ssbs[t % 2], mxs[t % 2], sms[t % 2], pexps[t % 2]
                S.op("dve", lambda: nc.vector.tensor_tensor(out=ssb[:, 0:512], in0=ps[:, 0:512], in1=bias[:, hb, :], op=ALU.add),
                     reads=[ps, bias], writes=[ssb])
                S.op("act", lambda: nc.scalar.copy(out=ssb[:, 512:768], in_=ps[:, 512:768]), reads=[ps], writes=[ssb], partial=True)
                S.op("dve", lambda: nc.vector.tensor_reduce(out=mx[:], in_=ssb[:], axis=AX.X, op=ALU.max), reads=[ssb], writes=[mx])
                S.op("dve", lambda: nc.vector.tensor_scalar(out=mx[:], in0=mx[:], scalar1=-1.0, scalar2=None, op0=ALU.mult),
                     reads=[mx], writes=[mx])
                S.op("act", lambda: nc.scalar.activation(out=pexp[:], in_=ssb[:], func=AF.Exp, bias=mx[:, 0:1], scale=1.0,
                                                         accum_out=sm[:]), reads=[ssb, mx], writes=[pexp, sm])

            def st_transp(t):
                pexp, ptp = pexps[t % 2], ptps[t % 2]
                for j in range(8):
                    S.op("pe", lambda: nc.tensor.transpose(out=ptp[0:64, j * 64:(j + 1) * 64], in_=pexp[:, j * 64:(j + 1) * 64],
                                                           identity=idb[0:64, 0:64]), reads=[pexp, idb], writes=[ptp], partial=(j > 0))
                for j in range(2):
                    S.op("pe", lambda: nc.tensor.transpose(out=ptcs_[t % 2][:, j, :],
                                                           in_=pexp[:, 512 + j * 128:512 + (j + 1) * 128],
                                                           identity=idb[0:64, 0:64]), reads=[pexp, idb], writes=[ptcs_[t % 2]], partial=(j > 0))
                S.op("dve", lambda: nc.vector.tensor_copy(out=ptw_ss[t % 2][:].rearrange("p j q -> p (j q)"), in_=ptp[0:64, 0:512]),
                     reads=[ptp], writes=[ptw_ss[t % 2]])
                S.op("act", lambda: nc.scalar.copy(out=ptc_ss[t % 2][:], in_=ptcs_[t % 2][:]),
                     reads=[ptcs_[t % 2]], writes=[ptc_ss[t % 2]])

            def st_pv(t):
                r, hb = units[t]
                vwin = vwins[r % 2]
                pon, ptw_s, ptc_s, sm, brow = pons[t % 2], ptw_ss[t % 2], ptc_ss[t % 2], sms[t % 2], brows[r % 2]
                for j in range(8):
                    S.op("pe", lambda: nc.tensor.matmul(pon[:, 0:64], lhsT=ptw_s[:, j, :], rhs=vwin[:, j, hb * 64:(hb + 1) * 64],
                                                        start=(j == 0), stop=False), reads=[ptw_s, vwin], writes=[pon], partial=(j > 0))
                for j in range(2):
                    S.op("pe", lambda: nc.tensor.matmul(pon[:, 0:64], lhsT=ptc_s[:, j, :], rhs=vc[:, j, hb * 64:(hb + 1) * 64],
                                                        start=False, stop=(j == 1)), reads=[ptc_s, vc], writes=[pon], partial=True)
                S.op("dve", lambda: nc.vector.reciprocal(out=sm[:], in_=sm[:]), reads=[sm], writes=[sm])
                S.op("dve", lambda: nc.vector.tensor_scalar(out=brow[:, hb * 64:(hb + 1) * 64], in0=pon[:, 0:64], scalar1=sm[:, 0:1],
                                                            scalar2=None, op0=ALU.mult), reads=[pon, sm], writes=[brow], partial=True)

            def st_rowout(r):
                brow, bt, ptp = brows[r % 2], bts[r % 2], ptps[r % 2]
                for j in range(2):
                    S.op("pe", lambda: nc.tensor.transpose(out=pbts_[0][:, j, :], in_=brow[:, j * 128:(j + 1) * 128],
                                                           identity=idb[0:64, 0:64]), reads=[brow, idb], writes=[pbts_[0]], partial=(j > 0))
                S.op("act", lambda: nc.scalar.copy(out=bt[:], in_=pbts_[0][:]), reads=[pbts_[0]], writes=[bt])
                S.dma("sp", lambda: nc.sync.dma_start(out=BT.ap()[:, r * 64:(r + 1) * 64].rearrange("(j p) t -> p j t", p=128), in_=bt[:]),
                      reads=[bt], writes=[BT])

            load_row(0)
            st_scores(0)
            for t in range(NU + 2):
                if 1 <= t <= NU:
                    st_pv(t - 1)
                if t >= 2 and units[t - 2][1] == 3:
                    st_rowout(units[t - 2][0])
                if t + 1 < NU:
                    st_scores(t + 1)
                if t < NU:
                    st_softmax(t)
                    st_transp(t)
            S.end_phase()
        if not last:
            with ES() as st:
                kc = S.sb([128, 2, 256], BF16, "kc", st)
                vc = S.sb([128, 2, 256], BF16, "vc", st)
                S.dma("sp", lambda: nc.sync.dma_start(out=kc[:], in_=KBT[:, :, T:TT]), reads=[KBT], writes=[kc])
                S.dma("sp", lambda: nc.sync.dma_start(out=vc[:], in_=VB.ap()[T:TT, :].rearrange("(n p) e -> p n e", p=128)),
                      reads=[VB], writes=[vc])
                qc = S.sb([128, 2, 128], BF16, "qc", st)
                sc_ = S.sb([128, 256], F32, "sc", st)
                mxc = S.sb([128, 1], F32, "mxc", st)
                smc = S.sb([128, 1], F32, "smc", st)
                pxc = S.sb([128, 256], BF16, "pxc", st)
                ptcs = S.sb([128, 2, 128], BF16, "ptcs", st)
                browc = S.sb([128, 256], BF16, "browc", st)
                btc = S.sb([128, 2, 128], BF16, "btc", st)
                psc = S.ps([128, 256], F32, "psc", st)
                ptcp = S.ps([128, 2, 128], BF16, "ptcp", st)
                poc = S.ps([128, 64], F32, "poc", st)
                pbc = S.ps([128, 2, 128], BF16, "pbc", st)
                for ci in range(2):
                    c0 = T + ci * 128
                    S.dma("sp", lambda: nc.sync.dma_start(out=qc[:], in_=QBT[:, :, c0:c0 + 128]), reads=[QBT], writes=[qc])
                    for hb in range(4):
                        hp, pr = hb // 2, (hb % 2) * 64
                        S.op("pe", lambda: nc.tensor.matmul(psc[:], lhsT=qc[pr:pr + 64, hp, :], rhs=kc[pr:pr + 64, hp, :],
                                                            start=True, stop=True), reads=[qc, kc], writes=[psc])
                        S.op("act", lambda: nc.scalar.copy(out=sc_[:], in_=psc[:]), reads=[psc], writes=[sc_])
                        S.op("dve", lambda: nc.vector.tensor_reduce(out=mxc[:], in_=sc_[:], axis=AX.X, op=ALU.max), reads=[sc_], writes=[mxc])
                        S.op("dve", lambda: nc.vector.tensor_scalar(out=mxc[:], in0=mxc[:], scalar1=-1.0, scalar2=None, op0=ALU.mult),
                             reads=[mxc], writes=[mxc])
                        S.op("act", lambda: nc.scalar.activation(out=pxc[:], in_=sc_[:], func=AF.Exp, bias=mxc[:, 0:1], scale=1.0,
                                                                 accum_out=smc[:]), reads=[sc_, mxc], writes=[pxc, smc])
                        for j in range(2):
                            S.op("pe", lambda: nc.tensor.transpose(out=ptcp[:, j, :], in_=pxc[:, j * 128:(j + 1) * 128], identity=idb[:]),
                                 reads=[pxc, idb], writes=[ptcp], partial=(j > 0))
                        S.op("dve", lambda: nc.vector.tensor_copy(out=ptcs[:], in_=ptcp[:]), reads=[ptcp], writes=[ptcs])
                        for j in range(2):
                            S.op("pe", lambda: nc.tensor.matmul(poc[:], lhsT=ptcs[:, j, :], rhs=vc[:, j, hb * 64:(hb + 1) * 64],
                                                                start=(j == 0), stop=(j == 1)), reads=[ptcs, vc], writes=[poc], partial=(j > 0))
                        S.op("dve", lambda: nc.vector.reciprocal(out=smc[:], in_=smc[:]), reads=[smc], writes=[smc])
                        S.op("dve", lambda: nc.vector.tensor_scalar(out=browc[:, hb * 64:(hb + 1) * 64], in0=poc[:], scalar1=smc[:, 0:1],
                                                                    scalar2=None, op0=ALU.mult), reads=[poc, smc], writes=[browc], partial=True)
                    for j in range(2):
                        S.op("pe", lambda: nc.tensor.transpose(out=pbc[:, j, :], in_=browc[:, j * 128:(j + 1) * 128], identity=idb[:]),
                             reads=[browc, idb], writes=[pbc], partial=(j > 0))
                    S.op("act", lambda: nc.scalar.copy(out=btc[:], in_=pbc[:]), reads=[pbc], writes=[btc])
                    S.dma("sp", lambda: nc.sync.dma_start(out=BT.ap()[:, c0:c0 + 128].rearrange("(j p) t -> p j t", p=128), in_=btc[:]),
                          reads=[btc], writes=[BT])
                S.end_phase()
        if stop_after == "3":
            break

        seqs = [(0, T)] + ([] if last else [(T, CT)])
        for (t0, Ls) in seqs:
            with ES() as st:
                Y = S.sb([128, 2, Ls], F32, "Y", st)
                up = S.sb([128, Ls + 30], F32, "up", st)
                cw = S.sb([128, 2, 31], F32, "cw", st)
                cb = S.sb([128, 2], F32, "cb", st)
                lng = S.sb([128, 2], F32, "lng", st)
                lnb = S.sb([128, 2], F32, "lnb", st)
                ones_s = S.sb([128, 128], F32, "ones_s", st)
                S.op("dve", lambda: nc.vector.memset(ones_s[:], 1.0 / 256), writes=[ones_s])
                with nc.allow_non_contiguous_dma(reason="tiny"):
                    for j in range(2):
                        S.dma("sp", lambda: nc.sync.dma_start(out=cw[:, j, :], in_=conv_w.ap()[l][:, j * 128:(j + 1) * 128].rearrange("w c -> c w")),
                              reads=[conv_w], writes=[cw], partial=True)
                    for (dst, src) in ((cb, conv_b), (lng, conv_ln_g), (lnb, conv_ln_b)):
                        S.dma("sp", lambda: nc.sync.dma_start(out=dst[:], in_=src.ap()[l].rearrange("(j p) -> p j", p=128)),
                              reads=[src], writes=[dst])
                S.op("dve", lambda: nc.vector.memset(up[:, 0:15], 0.0), writes=[up], partial=True)
                S.op("dve", lambda: nc.vector.memset(up[:, Ls + 15:Ls + 30], 0.0), writes=[up], partial=True)
                for j in range(2):
                    S.dma("sp", lambda: nc.sync.dma_start(out=up[:, 15:15 + Ls], in_=UT[j * 128:(j + 1) * 128, t0:t0 + Ls]),
                          reads=[UT], writes=[up], partial=True)
                    S.op("dve", lambda: nc.vector.tensor_scalar(out=Y[:, j, :], in0=up[:, 0:Ls], scalar1=cw[:, j, 0:1], scalar2=cb[:, j:j + 1],
                                                                op0=ALU.mult, op1=ALU.add), reads=[up, cw, cb], writes=[Y], partial=True)
                    for w in range(1, 31):
                        S.op("dve", lambda: nc.vector.scalar_tensor_tensor(out=Y[:, j, :], in0=up[:, w:w + Ls], scalar=cw[:, j, w:w + 1],
                                                                           in1=Y[:, j, :], op0=ALU.mult, op1=ALU.add),
                             reads=[up, cw, Y], writes=[Y], partial=True)
                BL = min(512, Ls)
                ysq = S.sb([128, 2, BL], F32, "ysq", st)
                mean = S.sb([128, BL], F32, "mean", st)
                var = S.sb([128, BL], F32, "var", st)
                tmpc = S.sb([128, 2, BL], F32, "tmpc", st)
                ctsb = [S.sb([128, 2, BL], BF16, "ctsb", st) for _ in range(2)]
                pmn = S.ps([128, BL], F32, "pmn", st)
                pe2 = S.ps([128, BL], F32, "pe2", st)
                for bi in range(Ls // BL):
                    b0 = bi * BL
                    S.op("act", lambda: nc.scalar.activation(out=ysq[:], in_=Y[:, :, b0:b0 + BL], func=AF.Square), reads=[Y], writes=[ysq])
                    for j in range(2):
                        S.op("pe", lambda: nc.tensor.matmul(pmn[:], lhsT=ones_s[:], rhs=Y[:, j, b0:b0 + BL], start=(j == 0), stop=(j == 1)),
                             reads=[ones_s, Y], writes=[pmn], partial=(j > 0))
                    for j in range(2):
                        S.op("pe", lambda: nc.tensor.matmul(pe2[:], lhsT=ones_s[:], rhs=ysq[:, j, :], start=(j == 0), stop=(j == 1)),
                             reads=[ones_s, ysq], writes=[pe2], partial=(j > 0))
                    S.op("act", lambda: nc.scalar.copy(out=mean[:], in_=pmn[:]), reads=[pmn], writes=[mean])
                    S.op("dve", lambda: nc.vector.tensor_tensor(out=var[:], in0=mean[:], in1=mean[:], op=ALU.mult), reads=[mean], writes=[var])
                    S.op("dve", lambda: nc.vector.tensor_tensor(out=var[:], in0=pe2[:], in1=var[:], op=ALU.subtract), reads=[pe2, var], writes=[var])
                    S.op("dve", lambda: nc.vector.tensor_scalar(out=var[:], in0=var[:], scalar1=EPS, scalar2=None, op0=ALU.add),
                         reads=[var], writes=[var])
                    S.op("act", lambda: nc.scalar.activation(out=var[:], in_=var[:], func=AF.Sqrt), reads=[var], writes=[var])
                    S.op("dve", lambda: nc.vector.reciprocal(out=var[:], in_=var[:]), reads=[var], writes=[var])
                    cts_ = ctsb[bi % 2]
                    for j in range(2):
                        S.op("dve", lambda: nc.vector.tensor_tensor(out=tmpc[:, j, :], in0=Y[:, j, b0:b0 + BL], in1=mean[:], op=ALU.subtract),
                             reads=[Y, mean], writes=[tmpc], partial=True)
                        S.op("dve", lambda: nc.vector.tensor_tensor(out=tmpc[:, j, :], in0=tmpc[:, j, :], in1=var[:], op=ALU.mult),
                             reads=[tmpc, var], writes=[tmpc], partial=True)
                        S.op("act", lambda: nc.scalar.activation(out=cts_[:, j, :], in_=tmpc[:, j, :], func=AF.Silu,
                                                                 bias=lnb[:, j:j + 1], scale=lng[:, j:j + 1]),
                             reads=[tmpc, lnb, lng], writes=[cts_], partial=True)
                    S.dma("sp", lambda: nc.sync.dma_start(out=CTs.ap()[:, t0 + b0:t0 + b0 + BL].rearrange("(j p) t -> p j t", p=128), in_=cts_[:]),
                          reads=[cts_], writes=[CTs])
                S.end_phase()
        if stop_after == "4":
            break

        with ES() as st:
            mod_l, mod_c = load_mod(st, 2 * D, 3 * D)
            wo = S.sb([128, 8, D], BF16, "wo", st)
            for h in range(2):
                S.dma("pool", lambda: nc.gpsimd.dma_start(out=wo[:, :, h * 512:(h + 1) * 512],
                                                          in_=w_out.ap()[l][:, h * 512:(h + 1) * 512].rearrange("(k p) n -> p k n", p=128)),
                      reads=[w_out], writes=[wo], partial=True)
            wr = S.sb([128, 8, NE], F32, "wr", st)
            S.dma("sp", lambda: nc.sync.dma_start(out=wr[:], in_=w_router.ap()[l].rearrange("(k p) e -> p k e", p=128)),
                  reads=[w_router], writes=[wr])
            cats = [S.sb([128, 8, 128], BF16, "cat", st) for _ in range(2)]
            xts = [S.sb([128, D], F32, "xt5", st) for _ in range(2)]
            tmp5 = S.sb([128, D], F32, "tmp5", st)
            x1s = [S.sb([128, D], F32, "x1", st) for _ in range(2)]
            sq5 = S.sb([128, D], F32, "sq5", st)
            ss5 = S.sb([128, 1], F32, "ss5", st)
            rstd5 = S.sb([128, 1], F32, "rstd5", st)
            h2f = S.sb([128, D], F32, "h2f", st)
            h2b = [S.sb([128, D], BF16, "h2b", st) for _ in range(2)]
            h2T = S.sb([128, 8, 128], F32, "h2T", st)
            lg = S.sb([128, NE], F32, "lg", st)
            mx5 = S.sb([128, 1], F32, "mx5", st)
            se5 = S.sb([128, 1], F32, "se5", st)
            ps5 = S.ps([128, D], F32, "ps5", st)
            pt5 = S.ps([128, 8, 128], F32, "pt5", st)
            pl5 = S.ps([128, NE], F32, "pl5", st)
            for i in range(NTT + 1):
                r0 = i * 128
                nr = 128 if i < NTT else NPAD
                S.dma("sp", lambda: nc.sync.dma_start(out=YACC[r0:r0 + nr, :], in_=zeros_f[0:nr, :]), reads=[zeros_f], writes=[YACC])
            for ti in range(ntq):
                r0 = ti * 128
                mod = mod_l if ti < NTL else mod_c
                cat, xt, x1, hb2 = cats[ti % 2], xts[ti % 2], x1s[ti % 2], h2b[ti % 2]
                S.dma("sp", lambda: nc.sync.dma_start(out=cat[:, 0:4, :], in_=AT.ap()[:, r0:r0 + 128].rearrange("(j p) t -> p j t", p=128)),
                      reads=[AT], writes=[cat], partial=True)
                S.dma("sp", lambda: nc.sync.dma_start(out=cat[:, 4:6, :], in_=BT.ap()[:, r0:r0 + 128].rearrange("(j p) t -> p j t", p=128)),
                      reads=[BT], writes=[cat], partial=True)
                S.dma("sp", lambda: nc.sync.dma_start(out=cat[:, 6:8, :], in_=CTs.ap()[:, r0:r0 + 128].rearrange("(j p) t -> p j t", p=128)),
                      reads=[CTs], writes=[cat], partial=True)
                S.dma("sp", lambda: nc.sync.dma_start(out=xt[:], in_=X[r0:r0 + 128, :]), reads=[X], writes=[xt])
                for hh in range(2):
                    for k in range(8):
                        S.op("pe", lambda: nc.tensor.matmul(ps5[:, hh * 512:(hh + 1) * 512], lhsT=cat[:, k, :], rhs=wo[:, k, hh * 512:(hh + 1) * 512],
                                                            start=(k == 0), stop=(k == 7)), reads=[cat, wo], writes=[ps5],
                             partial=not (hh == 0 and k == 0))
                S.op("dve", lambda: nc.vector.tensor_tensor(out=tmp5[:], in0=ps5[:], in1=mod[:, 2 * D:3 * D], op=ALU.mult),
                     reads=[ps5, mod], writes=[tmp5])
                S.op("pool", lambda: nc.gpsimd.tensor_tensor(out=x1[:], in0=tmp5[:], in1=xt[:], op=ALU.add), reads=[tmp5, xt], writes=[x1])
                S.dma("sp", lambda: nc.sync.dma_start(out=X[r0:r0 + 128, :], in_=x1[:]), reads=[x1], writes=[X])
                S.op("act", lambda: nc.scalar.activation(out=sq5[:], in_=x1[:], func=AF.Square, accum_out=ss5[:]), reads=[x1], writes=[sq5, ss5])
                S.op("dve", lambda: nc.vector.tensor_scalar(out=rstd5[:], in0=ss5[:], scalar1=1.0 / D, scalar2=EPS, op0=ALU.mult, op1=ALU.add),
                     reads=[ss5], writes=[rstd5])
                S.op("act", lambda: nc.scalar.activation(out=rstd5[:], in_=rstd5[:], func=AF.Sqrt), reads=[rstd5], writes=[rstd5])
                S.op("dve", lambda: nc.vector.reciprocal(out=rstd5[:], in_=rstd5[:]), reads=[rstd5], writes=[rstd5])
                S.op("dve", lambda: nc.vector.scalar_tensor_tensor(out=h2f[:], in0=x1[:], scalar=rstd5[:, 0:1], in1=mod[:, 4 * D:5 * D],
                                                                   op0=ALU.mult, op1=ALU.mult), reads=[x1, rstd5, mod], writes=[h2f])
                S.op("pool", lambda: nc.gpsimd.tensor_tensor(out=h2f[:], in0=h2f[:], in1=mod[:, 3 * D:4 * D], op=ALU.add),
                     reads=[h2f, mod], writes=[h2f])
                S.op("act", lambda: nc.scalar.copy(out=hb2[:], in_=h2f[:]), reads=[h2f], writes=[hb2])
                S.dma("sp", lambda: nc.sync.dma_start(out=H2[r0:r0 + 128, :], in_=hb2[:]), reads=[hb2], writes=[H2])
                for k in range(8):
                    S.op("pe", lambda: nc.tensor.transpose(out=pt5[:, k, :], in_=h2f[:, k * 128:(k + 1) * 128], identity=idf[:]),
                         reads=[h2f, idf], writes=[pt5], partial=(k > 0))
                S.op("dve", lambda: nc.vector.tensor_copy(out=h2T[:], in_=pt5[:]), reads=[pt5], writes=[h2T])
                for k in range(8):
                    S.op("pe", lambda: nc.tensor.matmul(pl5[:], lhsT=h2T[:, k, :], rhs=wr[:, k, :], start=(k == 0), stop=(k == 7)),
                         reads=[h2T, wr], writes=[pl5], partial=(k > 0))
                S.op("dve", lambda: nc.vector.tensor_reduce(out=mx5[:], in_=pl5[:], axis=AX.X, op=ALU.max), reads=[pl5], writes=[mx5])
                S.op("dve", lambda: nc.vector.tensor_scalar(out=mx5[:], in0=mx5[:], scalar1=-1.0, scalar2=None, op0=ALU.mult),
                     reads=[mx5], writes=[mx5])
                S.op("act", lambda: nc.scalar.activation(out=lg[:], in_=pl5[:], func=AF.Exp, bias=mx5[:, 0:1], scale=1.0, accum_out=se5[:]),
                     reads=[pl5, mx5], writes=[lg, se5])
                S.op("dve", lambda: nc.vector.reciprocal(out=se5[:], in_=se5[:]), reads=[se5], writes=[se5])
                S.op("dve", lambda: nc.vector.tensor_scalar(out=AFF[:, ti, :], in0=lg[:], scalar1=se5[:, 0:1], scalar2=None, op0=ALU.mult),
                     reads=[lg, se5], writes=[AFF], partial=True)
            S.end_phase()
        if stop_after == "5":
            break

        nrt = ntq
        with ES() as st:
            utb = S.sb([128, 128], BF16, "utb", st)
            utf = S.sb([128, 128], F32, "utf", st)
            S.dma("sp", lambda: nc.sync.dma_start(out=utf[:], in_=utri_in[:, :]), reads=[utri_in], writes=[utf])
            S.op("dve", lambda: nc.vector.tensor_copy(out=utb[:], in_=utf[:]), reads=[utf], writes=[utb])
            lo = S.sb([128, 32], F32, "lo", st)
            hi = S.sb([128, 32], F32, "hi", st)
            mid = S.sb([128, 32], F32, "mid", st)
            tgt = S.sb([128, 32], F32, "tgt", st)
            ge = S.sb([128, 32], F32, "ge", st)
            d1 = S.sb([128, 32], F32, "d1", st)
            cmpb = S.sb([128, NTT, NE], F32, "cmpb", st)
            cntb = S.sb([128, 32], F32, "cntb", st)
            pc = S.ps([128, 32], F32, "pc", st)
            S.op("dve", lambda: nc.vector.memset(lo[:], 0.0), writes=[lo])
            S.op("dve", lambda: nc.vector.memset(hi[:], 1.0), writes=[hi])
            S.op("dve", lambda: nc.vector.memset(tgt[:, 0:16], float(CAP)), writes=[tgt], partial=True)
            S.op("dve", lambda: nc.vector.memset(tgt[:, 16:32], float(CCAP)), writes=[tgt], partial=True)
            S.op("dve", lambda: nc.vector.memset(cntb[:], 0.0), writes=[cntb])
            parts = [(0, NTL, 0)] + ([] if last else [(NTL, NTT, 16)])

            def compare(dst, thr):
                for (a, b_, c0) in parts:
                    S.op("dve", lambda: nc.vector.tensor_tensor(
                        out=dst[:, a:b_, :], in0=AFF[:, a:b_, :],
                        in1=thr[:, c0:c0 + 16].unsqueeze(1).to_broadcast([128, b_ - a, NE]), op=ALU.is_ge),
                        reads=[AFF, thr], writes=[dst], partial=True)

            for itn in range(40):
                S.op("dve", lambda: nc.vector.tensor_tensor(out=mid[:], in0=lo[:], in1=hi[:], op=ALU.add), reads=[lo, hi], writes=[mid])
                S.op("dve", lambda: nc.vector.tensor_scalar(out=mid[:], in0=mid[:], scalar1=0.5, scalar2=None, op0=ALU.mult),
                     reads=[mid], writes=[mid])
                compare(cmpb, mid)
                for (a, b_, c0) in parts:
                    S.op("dve", lambda: nc.vector.tensor_reduce(out=cntb[:, c0:c0 + 16], in_=cmpb[:, a:b_, :].rearrange("p t e -> p e t"),
                                                                axis=AX.X, op=ALU.add), reads=[cmpb], writes=[cntb], partial=True)
                S.op("pe", lambda: nc.tensor.matmul(pc[:], lhsT=ones_f[:], rhs=cntb[:], start=True, stop=True), reads=[ones_f, cntb], writes=[pc])
                S.op("dve", lambda: nc.vector.tensor_tensor(out=ge[:], in0=pc[:], in1=tgt[:], op=ALU.is_ge), reads=[pc, tgt], writes=[ge])
                S.op("dve", lambda: nc.vector.tensor_tensor(out=d1[:], in0=mid[:], in1=lo[:], op=ALU.subtract), reads=[mid, lo], writes=[d1])
                S.op("dve", lambda: nc.vector.tensor_tensor(out=d1[:], in0=d1[:], in1=ge[:], op=ALU.mult), reads=[d1, ge], writes=[d1])
                S.op("dve", lambda: nc.vector.tensor_tensor(out=lo[:], in0=lo[:], in1=d1[:], op=ALU.add), reads=[lo, d1], writes=[lo])
                S.op("dve", lambda: nc.vector.tensor_tensor(out=d1[:], in0=hi[:], in1=mid[:], op=ALU.subtract), reads=[hi, mid], writes=[d1])
                S.op("dve", lambda: nc.vector.tensor_tensor(out=d1[:], in0=d1[:], in1=ge[:], op=ALU.mult), reads=[d1, ge], writes=[d1])
                S.op("dve", lambda: nc.vector.tensor_tensor(out=hi[:], in0=mid[:], in1=d1[:], op=ALU.add), reads=[mid, d1], writes=[hi])
            maskb = S.sb([128, NTT, NE], BF16, "maskb", st)
            pref = S.sb([128, NTT, NE], F32, "pref", st)
            tcnt = S.sb([128, NTT, NE], F32, "tcnt", st)
            tcn2 = S.sb([128, NTT, NE], F32, "tcn2", st)
            S.op("dve", lambda: nc.vector.memset(cmpb[:], 0.0), writes=[cmpb])
            compare(cmpb, lo)
            S.op("dve", lambda: nc.vector.tensor_copy(out=maskb[:], in_=cmpb[:]), reads=[cmpb], writes=[maskb])
            ncol = NTT * NE
            mflat = maskb[:].rearrange("p t e -> p (t e)")
            pflat = pref[:].rearrange("p t e -> p (t e)")
            tflat = tcnt[:].rearrange("p t e -> p (t e)")
            pp = [S.ps([128, 512], F32, "pp", st) for _ in range(2)]
            for ci, c0 in enumerate(range(0, ncol, 512)):
                n = min(512, ncol - c0)
                S.op("pe", lambda: nc.tensor.matmul(pp[0][:, 0:n], lhsT=utb[:], rhs=mflat[:, c0:c0 + n], start=True, stop=True),
                     reads=[utb, maskb], writes=[pp[0]])
                S.op("pe", lambda: nc.tensor.matmul(pp[1][:, 0:n], lhsT=ones_b[:], rhs=mflat[:, c0:c0 + n], start=True, stop=True),
                     reads=[ones_b, maskb], writes=[pp[1]])
                S.op("dve", lambda: nc.vector.tensor_copy(out=pflat[:, c0:c0 + n], in_=pp[0][:, 0:n]), reads=[pp[0]], writes=[pref], partial=True)
                S.op("act", lambda: nc.scalar.copy(out=tflat[:, c0:c0 + n], in_=pp[1][:, 0:n]), reads=[pp[1]], writes=[tcnt], partial=True)
            src, dst = tcnt, tcn2
            S.op("dve", lambda: nc.vector.tensor_copy(out=tcn2[:], in_=tcnt[:]), reads=[tcnt], writes=[tcn2])
            cum = S.sb([128, NTT, NE], F32, "cum", st)
            S.op("dve", lambda: nc.vector.tensor_copy(out=cum[:], in_=tcnt[:]), reads=[tcnt], writes=[cum])
            a_, b2 = cum, tcn2
            sft = 1
            while sft < NTL:
                S.op("dve", lambda: nc.vector.tensor_tensor(out=b2[:, sft:NTL, :], in0=a_[:, sft:NTL, :], in1=a_[:, 0:NTL - sft, :], op=ALU.add),
                     reads=[a_], writes=[b2], partial=True)
                S.op("dve", lambda: nc.vector.tensor_copy(out=b2[:, 0:sft, :], in_=a_[:, 0:sft, :]), reads=[a_], writes=[b2], partial=True)
                a_, b2 = b2, a_
                sft *= 2
            inc = a_
            slot = S.sb([128, NTT, NE], F32, "slot", st)
            S.op("dve", lambda: nc.vector.tensor_tensor(out=slot[:, 0:NTL, :], in0=inc[:, 0:NTL, :], in1=tcnt[:, 0:NTL, :], op=ALU.subtract),
                 reads=[inc, tcnt], writes=[slot], partial=True)
            if not last:
                S.op("dve", lambda: nc.vector.memset(slot[:, NTL, :], float(CAP)), writes=[slot], partial=True)
                S.op("dve", lambda: nc.vector.tensor_scalar(out=slot[:, NTL + 1, :], in0=tcnt[:, NTL, :], scalar1=float(CAP), scalar2=None,
                                                            op0=ALU.add), reads=[tcnt], writes=[slot], partial=True)
            S.op("dve", lambda: nc.vector.tensor_tensor(out=slot[:, 0:nrt, :], in0=slot[:, 0:nrt, :], in1=pref[:, 0:nrt, :], op=ALU.add),
                 reads=[slot, pref], writes=[slot], partial=True)
            ebase = S.sb([128, NE], F32, "ebase", st)
            for e in range(NE):
                S.op("dve", lambda: nc.vector.memset(ebase[:, e:e + 1], float(e * MS)), writes=[ebase], partial=True)
            S.op("dve", lambda: nc.vector.tensor_tensor(out=slot[:, 0:nrt, :], in0=slot[:, 0:nrt, :],
                                                        in1=ebase[:].unsqueeze(1).to_broadcast([128, nrt, NE]), op=ALU.add),
                 reads=[slot, ebase], writes=[slot], partial=True)
            BIGI = float(1 << 20)
            S.op("dve", lambda: nc.vector.scalar_tensor_tensor(out=slot[:, 0:nrt, :].rearrange("p t e -> p (t e)"),
                                                               in0=slot[:, 0:nrt, :].rearrange("p t e -> p (t e)"), scalar=-BIGI,
                                                               in1=cmpb[:, 0:nrt, :].rearrange("p t e -> p (t e)"), op0=ALU.add, op1=ALU.mult),
                 reads=[slot, cmpb], writes=[slot], partial=True)
            S.op("dve", lambda: nc.vector.tensor_scalar(out=slot[:, 0:nrt, :], in0=slot[:, 0:nrt, :], scalar1=BIGI, scalar2=None, op0=ALU.add),
                 reads=[slot], writes=[slot], partial=True)
            idxi = S.sb([128, NTT, NE], I32, "idxi", st)
            S.op("dve", lambda: nc.vector.tensor_copy(out=idxi[:, 0:nrt, :], in_=slot[:, 0:nrt, :]), reads=[slot], writes=[idxi])
            tid = S.sb([128, NTT], I32, "tid", st)
            S.op("pool", lambda: nc.gpsimd.iota(tid[:], pattern=[[128, NTT]], base=0, channel_multiplier=1), writes=[tid])
            pay = S.sb([128, NTT, NE, 2], I32, "pay", st)
            S.op("dve", lambda: nc.vector.tensor_copy(out=pay[:, :, :, 0], in_=tid[:].unsqueeze(2).to_broadcast([128, NTT, NE])),
                 reads=[tid], writes=[pay], partial=True)
            S.op("dve", lambda: nc.vector.tensor_copy(out=pay[:].bitcast(F32)[:, :, :, 1], in_=AFF[:]), reads=[AFF], writes=[pay], partial=True)
            lflat = LST.ap().rearrange("e s c -> (e s) c")
            for ti in range(nrt):
                for e in range(NE):
                    S.dma("pool", lambda: nc.gpsimd.indirect_dma_start(
                        out=lflat, out_offset=bass.IndirectOffsetOnAxis(ap=idxi[:, ti, e:e + 1], axis=0),
                        in_=pay[:, ti, e, :], in_offset=None, bounds_check=bc_reg, oob_is_err=False),
                        reads=[pay, idxi], writes=[LST])
            if "route" in debug and l == 0:
                dbg["thr"] = S.dram("dbg_thr", [128, 32], F32, kind="ExternalOutput")
                S.dma("sp", lambda: nc.sync.dma_start(out=dbg["thr"][:, :], in_=lo[:]), reads=[lo], writes=[dbg["thr"]])
                dbg["idx"] = S.dram("dbg_idx", [128, NTT, NE], I32, kind="ExternalOutput")
                S.dma("sp", lambda: nc.sync.dma_start(out=dbg["idx"].ap(), in_=idxi[:]), reads=[idxi], writes=[dbg["idx"]])
            S.end_phase()
        if stop_after == "6":
            break

        with ES() as st:
            nst7 = 8 if last else 9
            NS = nst7 * 128
            ngr = [(0, 512), (512, 512)] + ([] if last else [(1024, 128)])
            wgs = [S.sb([128, 8, 1024], BF16, "wg", st) for _ in range(2)]
            wus = [S.sb([128, 8, 1024], BF16, "wu", st) for _ in range(2)]
            wd = S.sb([128, 16, D], BF16, "wd", st)
            xeT = S.sb([128, 8, NS], BF16, "xeT", st)
            hTe = S.sb([128, 16, NS], BF16, "hTe", st)
            idts = [S.sb([128, 9, 2], I32, "idt", st) for _ in range(2)]
            xgs = [S.sb([128, D], BF16, "xg", st) for _ in range(nst7)]
            ysbs = [S.sb([128, D], F32, "ysb", st) for _ in range(2)]
            sgl = [S.sb([128, 512], F32, "sgl", st) for _ in range(2)]
            ptx = S.ps([128, 8, 128], BF16, "ptx", st)
            pgs = [S.ps([128, 512], F32, "pg", st) for _ in range(2)]
            pus = [S.ps([128, 512], F32, "pu", st) for _ in range(2)]
            pys = [S.ps([128, 512], F32, "py", st) for _ in range(2)]
            cnt7 = {"fc": 0, "ys": 0}
            sc_state = {"prev": [], "cur": []}

            def dma_wgu(e, half):
                c0 = half * 1024
                S.dma("pool", lambda: nc.gpsimd.dma_start(out=wgs[half][:], in_=w_gate.ap()[l, e][:, c0:c0 + 1024].rearrange("(k p) f -> p k f", p=128)),
                      reads=[w_gate], writes=[wgs[half]])
                S.dma("pool", lambda: nc.gpsimd.dma_start(out=wus[half][:], in_=w_up.ap()[l, e][:, c0:c0 + 1024].rearrange("(k p) f -> p k f", p=128)),
                      reads=[w_up], writes=[wus[half]])

            def dma_wd(e):
                for h in range(2):
                    S.dma("pool", lambda: nc.gpsimd.dma_start(out=wd[:, h * 8:(h + 1) * 8, :],
                                                              in_=w_down.ap()[l, e][h * 1024:(h + 1) * 1024, :].rearrange("(k p) n -> p k n", p=128)),
                          reads=[w_down], writes=[wd], partial=(h > 0))

            def gather(e):
                idt = idts[e % 2]
                S.dma("sp", lambda: nc.sync.dma_start(out=idt[:, 0:nst7, :], in_=LST.ap()[e, 0:NS, :].rearrange("(t p) c -> p t c", p=128)),
                      reads=[LST], writes=[idt])
                for si in range(nst7):
                    S.dma("pool", lambda: nc.gpsimd.indirect_dma_start(
                        out=xgs[si][:], out_offset=None, in_=H2.ap(),
                        in_offset=bass.IndirectOffsetOnAxis(ap=idt[:, si, 0:1], axis=0)), reads=[H2, idt], writes=[xgs[si]])

            def transp(e):
                for si in range(nst7):
                    for k in range(8):
                        S.op("pe", lambda: nc.tensor.transpose(out=ptx[:, k, :], in_=xgs[si][:, k * 128:(k + 1) * 128], identity=idb[:]),
                             reads=[xgs[si], idb], writes=[ptx], partial=(k > 0))
                    S.op("dve", lambda: nc.vector.tensor_copy(out=xeT[:, :, si * 128:(si + 1) * 128], in_=ptx[:]),
                         reads=[ptx], writes=[xeT], partial=True)

            def hphase(e, half):
                wg_, wu_ = wgs[half], wus[half]
                for fl in range(8):
                    fc = half * 8 + fl
                    for (s0, N) in ngr:
                        i_ = cnt7["fc"]
                        cnt7["fc"] += 1
                        pg, pu, sg_ = pgs[i_ % 2], pus[i_ % 2], sgl[i_ % 2]
                        for k in range(8):
                            S.op("pe", lambda: nc.tensor.matmul(pg[:, 0:N], lhsT=wg_[:, k, fl * 128:(fl + 1) * 128], rhs=xeT[:, k, s0:s0 + N],
                                                                start=(k == 0), stop=(k == 7)), reads=[wg_, xeT], writes=[pg], partial=(k > 0))
                        for k in range(8):
                            S.op("pe", lambda: nc.tensor.matmul(pu[:, 0:N], lhsT=wu_[:, k, fl * 128:(fl + 1) * 128], rhs=xeT[:, k, s0:s0 + N],
                                                                start=(k == 0), stop=(k == 7)), reads=[wu_, xeT], writes=[pu], partial=(k > 0))
                        S.op("act", lambda: nc.scalar.activation(out=sg_[:, 0:N], in_=pg[:, 0:N], func=AF.Silu), reads=[pg], writes=[sg_])
                        S.op("dve", lambda: nc.vector.tensor_tensor(out=hTe[:, fc, s0:s0 + N], in0=sg_[:, 0:N], in1=pu[:, 0:N], op=ALU.mult),
                             reads=[sg_, pu], writes=[hTe], partial=True)

            def yphase(e, tiles):
                idt = idts[e % 2]
                for si in tiles:
                    ysb = ysbs[cnt7["ys"] % 2]
                    cnt7["ys"] += 1
                    gate = idt[:].bitcast(F32)[:, si, 1:2]
                    for dh in range(2):
                        py = pys[dh]
                        for fc in range(16):
                            S.op("pe", lambda: nc.tensor.matmul(py[:], lhsT=hTe[:, fc, si * 128:(si + 1) * 128], rhs=wd[:, fc, dh * 512:(dh + 1) * 512],
                                                                start=(fc == 0), stop=(fc == 15)), reads=[hTe, wd], writes=[py], partial=(fc > 0))
                        if dh == 0:
                            S.op("dve", lambda: nc.vector.tensor_scalar(out=ysb[:, 0:512], in0=py[:], scalar1=gate, scalar2=None, op0=ALU.mult),
                                 reads=[py, idt], writes=[ysb], partial=True)
                        else:
                            S.op("act", lambda: nc.scalar.activation(out=ysb[:, 512:1024], in_=py[:], func=AF.Copy, scale=gate),
                                 reads=[py, idt], writes=[ysb], partial=True)
                    tok = S.dma("pool", lambda: nc.gpsimd.indirect_dma_start(
                        out=YACC.ap(), out_offset=bass.IndirectOffsetOnAxis(ap=idt[:, si, 0:1], axis=0),
                        in_=ysb[:], in_offset=None, compute_op=ALU.add), reads=[ysb, idt], writes=[YACC], after=sc_state["prev"])
                    sc_state["cur"].append(tok)

            dma_wgu(0, 0)
            dma_wgu(0, 1)
            dma_wd(0)
            gather(0)
            transp(0)
            for e in range(NE):
                nxt = e + 1 < NE
                hphase(e, 0)
                if nxt:
                    dma_wgu(e + 1, 0)
                hphase(e, 1)
                if nxt:
                    dma_wgu(e + 1, 1)
                    gather(e + 1)
                sc_state["cur"] = []
                yphase(e, range(0, 4))
                if nxt:
                    transp(e + 1)
                yphase(e, range(4, nst7))
                if nxt:
                    dma_wd(e + 1)
                sc_state["prev"] = sc_state["cur"][-2:]
            S.end_phase()
        if stop_after == "7":
            break

        with ES() as st:
            mod_l, mod_c = load_mod(st, 5 * D, D)
            xt8 = [S.sb([128, D], F32, "xt8", st) for _ in range(2)]
            yt8 = [S.sb([128, D], F32, "yt8", st) for _ in range(2)]
            xo8 = [S.sb([128, D], F32, "xo8", st) for _ in range(2)]
            sq8 = S.sb([128, D], F32, "sq8", st)
            ss8 = S.sb([128, 1], F32, "ss8", st)
            fg = S.sb([128, D], F32, "fg", st)
            if last:
                S.dma("sp", lambda: nc.sync.dma_start(out=fg[:], in_=final_g.ap().partition_broadcast(128)), reads=[final_g], writes=[fg])
            for ti in range(ntq):
                r0 = ti * 128
                mod = mod_l if ti < NTL else mod_c
                xt, yt, xo = xt8[ti % 2], yt8[ti % 2], xo8[ti % 2]
                S.dma("sp", lambda: nc.sync.dma_start(out=xt[:], in_=X[r0:r0 + 128, :]), reads=[X], writes=[xt])
                S.dma("sp", lambda: nc.sync.dma_start(out=yt[:], in_=YACC[r0:r0 + 128, :]), reads=[YACC], writes=[yt])
                S.op("dve", lambda: nc.vector.tensor_tensor(out=yt[:], in0=yt[:], in1=mod[:, 5 * D:6 * D], op=ALU.mult), reads=[yt, mod], writes=[yt])
                S.op("pool", lambda: nc.gpsimd.tensor_tensor(out=xo[:], in0=yt[:], in1=xt[:], op=ALU.add), reads=[yt, xt], writes=[xo])
                if not last:
                    S.dma("sp", lambda: nc.sync.dma_start(out=X[r0:r0 + 128, :], in_=xo[:]), reads=[xo], writes=[X])
                else:
                    S.op("act", lambda: nc.scalar.activation(out=sq8[:], in_=xo[:], func=AF.Square, accum_out=ss8[:]), reads=[xo], writes=[sq8, ss8])
                    S.op("dve", lambda: nc.vector.tensor_scalar(out=ss8[:], in0=ss8[:], scalar1=1.0 / D, scalar2=EPS, op0=ALU.mult, op1=ALU.add),
                         reads=[ss8], writes=[ss8])
                    S.op("act", lambda: nc.scalar.activation(out=ss8[:], in_=ss8[:], func=AF.Sqrt), reads=[ss8], writes=[ss8])
                    S.op("dve", lambda: nc.vector.reciprocal(out=ss8[:], in_=ss8[:]), reads=[ss8], writes=[ss8])
                    S.op("dve", lambda: nc.vector.scalar_tensor_tensor(out=xt[:], in0=xo[:], scalar=ss8[:, 0:1], in1=fg[:], op0=ALU.mult, op1=ALU.mult),
                         reads=[xo, ss8, fg], writes=[xt])
                    S.dma("sp", lambda: nc.sync.dma_start(out=out[r0:r0 + 128, :], in_=xt[:]), reads=[xt], writes=[out])
            S.end_phase()
        if stop_after == "8":
            break

    def tap(name, buf, shape, dt):
        o = S.dram("dbg_" + name, shape, dt, kind="ExternalOutput")
        S.dma("sp", lambda: nc.sync.dma_start(out=o.ap(), in_=buf.ap()), reads=[buf], writes=[o])

    for name in debug:
        if name == "QT":
            tap("QT", QT, [128, 4, TT], BF16)
        if name == "KT":
            tap("KT", KT, [128, TT], BF16)
        if name == "VA":
            tap("VA", VA, [TT, 130], BF16)
        if name == "QBT":
            tap("QBT", QBT, [128, 2, TT], BF16)
        if name == "KBT":
            tap("KBT", KBT, [128, 2, TT], BF16)
        if name == "VB":
            tap("VB", VB, [TT, 256], BF16)
        if name == "UT":
            tap("UT", UT, [256, TT], F32)
        if name == "X":
            tap("X", X, [TT, D], F32)
        if name == "AT":
            tap("AT", AT, [512, TT], BF16)
        if name == "BT":
            tap("BT", BT, [256, TT], BF16)
        if name == "CTs":
            tap("CTs", CTs, [256, TT], BF16)
        if name == "H2":
            tap("H2", H2, [TT + NPAD, D], BF16)
        if name == "LST":
            tap("LST", LST, [NE, MS, 2], I32)
        if name == "YACC":
            tap("YACC", YACC, [TT + NPAD, D], F32)
        if name == "AFF":
            o_ = S.dram("dbg_AFF", [128, NTT, NE], F32, kind="ExternalOutput")
            S.dma("sp", lambda: nc.sync.dma_start(out=o_.ap(), in_=AFF[:]), reads=[AFF], writes=[o_])
    S.barrier()
    return nc


def host_consts(na_rpb, n_layers):
    t = np.arange(T)
    row = (t // 64).astype(np.float64)
    col = (t % 64).astype(np.float64)
    inv = 10000.0 ** (-np.arange(16, dtype=np.float64) / 16)
    rc = np.ones((TT, 64), np.float32)
    rs = np.zeros((TT, 64), np.float32)
    for a, pos in enumerate((row, col)):
        ang = (pos.astype(np.float32)[:, None] * inv.astype(np.float32)[None, :]).astype(np.float32)
        cs, sn = np.cos(ang), np.sin(ang)
        rc[:T, a * 32:a * 32 + 16] = cs
        rc[:T, a * 32 + 16:a * 32 + 32] = cs
        rs[:T, a * 32:a * 32 + 16] = -sn
        rs[:T, a * 32 + 16:a * 32 + 32] = sn
    q = np.arange(64)
    cs0 = np.clip(q - 8, 0, 48)
    c = np.arange(64)
    valid = (c[None, :] >= cs0[:, None]) & (c[None, :] < cs0[:, None] + 16)
    dc = np.clip(c[None, :] - q[:, None] + 15, 0, 30)
    nab = np.full((n_layers, 8, 64, 4, 8, 64), NEG, np.float32)
    for off in range(8):
        for j in range(8):
            g = na_rpb[:n_layers, :, off + j, :][:, :, dc]
            g = np.where(valid[None, None], g, np.float32(NEG))
            nab[:, off, :, :, j, :] = np.transpose(g, (0, 2, 1, 3))
    lpad = np.zeros((NPAD, 2), np.int32)
    lpad[:, 0] = TT + np.arange(NPAD)
    return {"ident": np.eye(128, dtype=np.float32), "utri": np.triu(np.ones((128, 128), np.float32), 1), "rope_c": rc, "rope_s": rs,
            "nabias": nab.reshape(n_layers, 8, 64, 4, 512), "lpad": lpad}


_WNAMES = ["w_ada", "b_ada", "norm1_g", "norm2_g", "w_in", "q_norm_g", "k_norm_g", "conv_w", "conv_b",
           "conv_ln_g", "conv_ln_b", "w_out", "w_router", "w_gate", "w_up", "w_down"]


def make_in_maps(inputs, n_layers, samples):
    consts = host_consts(np.asarray(inputs["na_rpb"]), n_layers)
    maps = []
    for b in samples:
        m = {"x": np.ascontiguousarray(inputs["x"][b]), "ctx": np.ascontiguousarray(inputs["ctx"][b]),
             "c": np.ascontiguousarray(inputs["c"][b]), "c_ctx": np.ascontiguousarray(inputs["c_ctx"]),
             "final_norm_g": np.ascontiguousarray(inputs["final_norm_g"])}
        for k in _WNAMES:
            m[k] = np.ascontiguousarray(inputs[k][:n_layers])
        m.update(consts)
        maps.append(m)
    return maps


def kernel(**inputs):
    inputs = {k: np.asarray(v) for k, v in inputs.items()}
    nc = build_program(DEPTH)
    maps = make_in_maps(inputs, DEPTH, range(4))
    res = run_bass_kernel_spmd(nc, maps, core_ids=list(range(4)))
    return np.stack([r["out"] for r in res.results], 0).astype(np.float32)
```

```python
import contextlib
import numpy as np
import concourse.bass as bass
import concourse.mybir as mybir
from concourse.bass_utils import run_bass_kernel_spmd

F32 = mybir.dt.float32
BF16 = mybir.dt.bfloat16
I32 = mybir.dt.int32
AF = mybir.ActivationFunctionType
ALU = mybir.AluOpType
AX = mybir.AxisListType

D = 1024
T = 8192
CT = 256
TT = T + CT
NTL = T // 128
NTT = TT // 128
DEPTH = 4
NE = 16
FF = 2048
CAP = 1024
CCAP = 32
NPAD = 96
MS = CAP + CCAP + NPAD
EPS = 1e-6
NEG = -30000.0
P2_LIMIT = 0
P3_LIMIT = 0


class Buf:
    def __init__(self, t, name, space):
        self.t = t
        self.name = name
        self.space = space
        self.writes = {}
        self.reads = {}
        self.sem = None
        self.total = 0

    def __getitem__(self, idx):
        return self.t[idx]

    def ap(self):
        return self.t.ap()


class ModSlice(Buf):
    def __getitem__(self, idx):
        p, sl = idx
        return self.t[p, sl.start - self.off:sl.stop - self.off]


class Sched:
    def __init__(self, nc):
        self.nc = nc
        self.eng = {"pe": nc.tensor, "act": nc.scalar, "dve": nc.vector,
                    "pool": nc.gpsimd, "sp": nc.sync}
        self.psem = {k: nc.alloc_semaphore("prog_" + k) for k in self.eng}
        self.cnt = {k: 0 for k in self.eng}
        self.seen = {k: {} for k in self.eng}
        self.sem_pool = []
        self.live = []
        self.nbuf = 0
        self.nsem = 0

    def sb(self, shape, dtype, name=None, stack=None):
        self.nbuf += 1
        name = (name or "sb") + "_%d" % self.nbuf
        if stack is None:
            t = self.nc.alloc_sbuf_tensor(name, list(shape), dtype)
        else:
            t = stack.enter_context(self.nc.sbuf_tensor(name, list(shape), dtype))
        b = Buf(t, name, "sb")
        b.local = stack is not None
        return b

    def ps(self, shape, dtype=F32, name=None, stack=None):
        self.nbuf += 1
        name = (name or "ps") + "_%d" % self.nbuf
        if stack is None:
            t = self.nc.alloc_psum_tensor(name, list(shape), dtype)
        else:
            t = stack.enter_context(self.nc.psum_tensor(name, list(shape), dtype))
        b = Buf(t, name, "ps")
        b.local = stack is not None
        return b

    def dram(self, name, shape, dtype, kind="Internal"):
        b = Buf(self.nc.dram_tensor(name, list(shape), dtype, kind=kind), name, "dram")
        b.local = False
        return b

    def _wait(self, e, tok):
        sem, v = tok
        key = id(sem)
        if self.seen[e].get(key, 0) >= v:
            return
        self.seen[e][key] = v
        self.eng[e].wait_ge(sem, v)

    def _deps(self, e, reads, writes):
        own = id(self.psem[e])
        toks = []
        for r in reads:
            if r.space == "dram":
                continue
            for t in r.writes.values():
                if id(t[0]) == own and e == "pe":
                    continue
                toks.append(t)
        for w in writes:
            if w.space == "dram":
                continue
            for t in list(w.writes.values()) + list(w.reads.values()):
                if id(t[0]) == own:
                    continue
                toks.append(t)
        for t in toks:
            self._wait(e, t)

    def _record(self, tok, reads, writes, partial):
        k = id(tok[0])
        for r in reads:
            if r.space != "dram":
                r.reads[k] = tok
        for w in writes:
            if w.space == "dram":
                continue
            if partial:
                w.writes[k] = tok
            else:
                w.writes = {k: tok}
                w.reads = {}

    def op(self, e, ins, reads=(), writes=(), partial=False):
        self._deps(e, reads, writes)
        i = ins()
        self.cnt[e] += 1
        i.then_inc(self.psem[e], 1)
        tok = (self.psem[e], self.cnt[e])
        self._record(tok, reads, writes, partial)
        return tok

    def dma(self, q, mk, reads=(), writes=(), partial=False, after=()):
        self._deps(q, reads, writes)
        for t in after:
            self._wait(q, t)
        owner = None
        for b in list(writes) + list(reads):
            if b.space != "dram":
                owner = b
                break
        if owner is None:
            owner = (list(writes) + list(reads))[0]
        if owner.sem is None:
            if self.sem_pool:
                owner.sem, owner.total = self.sem_pool.pop()
            else:
                self.nsem += 1
                owner.sem = self.nc.alloc_semaphore("dsem%d" % self.nsem)
                owner.total = 0
            self.live.append(owner)
        i = mk()
        owner.total += 16
        i.then_inc(owner.sem, 16)
        tok = (owner.sem, owner.total)
        self._record(tok, reads, writes, partial)
        return tok

    def barrier(self):
        toks = [(self.psem[k], self.cnt[k]) for k in self.eng if self.cnt[k] > 0]
        toks += [(b.sem, b.total) for b in self.live]
        for e in self.eng:
            for t in toks:
                if id(t[0]) == id(self.psem[e]):
                    continue
                self._wait(e, t)

    def end_phase(self):
        self.barrier()
        keep = []
        for b in self.live:
            if b.local:
                self.sem_pool.append((b.sem, b.total))
                b.sem = None
            else:
                keep.append(b)
        self.live = keep


def build_program(n_layers=DEPTH, debug=(), stop_after=None):
    nc = bass.Bass("TRN2", target_bir_lowering=False)
    S = Sched(nc)
    ES = contextlib.ExitStack
    bc_reg = nc.gpsimd.to_reg(NE * MS - 1)
    L = n_layers

    def din(name, shape, dt=F32):
        return S.dram(name, shape, dt, kind="ExternalInput")

    x_in = din("x", [T, D])
    ctx_in = din("ctx", [CT, D])
    c_in = din("c", [D])
    cc_in = din("c_ctx", [D])
    w_ada = din("w_ada", [L, D, 6 * D])
    b_ada = din("b_ada", [L, 6 * D])
    norm1_g = din("norm1_g", [L, D])
    norm2_g = din("norm2_g", [L, D])
    w_in = din("w_in", [L, D, 2048])
    q_norm_g = din("q_norm_g", [L, 64])
    k_norm_g = din("k_norm_g", [L, 64])
    conv_w = din("conv_w", [L, 31, 256])
    conv_b = din("conv_b", [L, 256])
    conv_ln_g = din("conv_ln_g", [L, 256])
    conv_ln_b = din("conv_ln_b", [L, 256])
    w_out = din("w_out", [L, D, D])
    w_router = din("w_router", [L, D, NE])
    has_moe = stop_after is None or stop_after in ("7", "8")
    if has_moe:
        w_gate = din("w_gate", [L, NE, D, FF])
        w_up = din("w_up", [L, NE, D, FF])
        w_down = din("w_down", [L, NE, FF, D])
    final_g = din("final_norm_g", [D])
    ident_in = din("ident", [128, 128])
    rope_c = din("rope_c", [TT, 64])
    rope_s = din("rope_s", [TT, 64])
    nabias = din("nabias", [L, 8, 64, 4, 512])
    lpad = din("lpad", [NPAD, 2], I32)
    utri_in = din("utri", [128, 128])
    out = S.dram("out", [T, D], F32, kind="ExternalOutput")
    dbg = {}

    X = S.dram("X", [TT, D], F32)
    QT = S.dram("QT", [128, 4, TT], BF16)
    KT = S.dram("KT", [128, TT], BF16)
    VA = S.dram("VA", [TT, 130], BF16)
    QBT = S.dram("QBT", [128, 2, TT], BF16)
    KBT = S.dram("KBT", [128, 2, TT], BF16)
    VB = S.dram("VB", [TT, 256], BF16)
    UT = S.dram("UT", [256, TT], F32)
    AT = S.dram("AT", [512, TT], BF16)
    BT = S.dram("BT", [256, TT], BF16)
    CTs = S.dram("CTs", [256, TT], BF16)
    H2 = S.dram("H2", [TT + NPAD, D], BF16)
    YACC = S.dram("YACC", [TT + NPAD, D], F32)
    LST = S.dram("LST", [NE, MS, 2], I32)

    idf = S.sb([128, 128], F32, "idf")
    idb = S.sb([128, 128], BF16, "idb")
    ones_b = S.sb([128, 128], BF16, "ones_b")
    ones_f = S.sb([128, 128], F32, "ones_f")
    zeros_f = S.sb([128, D], F32, "zeros_f")
    MODS = S.dram("MODS", [2, 128, 6 * D], F32)

    def load_mod(st, off, n):
        res_ = []
        for w_ in range(2):
            b_ = S.sb([128, n], F32, "modw", st)
            b_.__class__ = ModSlice
            b_.off = off
            S.dma("sp", lambda: nc.sync.dma_start(out=b_.t[:], in_=MODS[w_, :, off:off + n]), reads=[MODS], writes=[b_])
            res_.append(b_)
        return res_
    crep_l = S.sb([128, 8, 128], BF16, "crep_l")
    crep_c = S.sb([128, 8, 128], BF16, "crep_c")
    AFF = S.sb([128, NTT, NE], F32, "AFF")

    def v_(e):
        return {"dve": nc.vector, "pool": nc.gpsimd}[e]

    S.dma("sp", lambda: nc.sync.dma_start(out=idf[:], in_=ident_in[:, :]), reads=[ident_in], writes=[idf])
    S.op("dve", lambda: nc.vector.tensor_copy(out=idb[:], in_=idf[:]), reads=[idf], writes=[idb])
    S.op("dve", lambda: nc.vector.memset(ones_b[:], 1.0), writes=[ones_b])
    S.op("dve", lambda: nc.vector.memset(ones_f[:], 1.0), writes=[ones_f])
    S.op("dve", lambda: nc.vector.memset(zeros_f[:], 0.0), writes=[zeros_f])
    with ES() as st:
        cT = S.sb([128, 2, 8], F32, "cT", st)
        with nc.allow_non_contiguous_dma(reason="tiny"):
            S.dma("sp", lambda: nc.sync.dma_start(out=cT[:, 0, :], in_=c_in.ap().rearrange("(k p) -> p k", p=128)),
                  reads=[c_in], writes=[cT], partial=True)
            S.dma("sp", lambda: nc.sync.dma_start(out=cT[:, 1, :], in_=cc_in.ap().rearrange("(k p) -> p k", p=128)),
                  reads=[cc_in], writes=[cT], partial=True)
        sT = S.sb([128, 2, 8], F32, "sT", st)
        S.op("act", lambda: nc.scalar.activation(out=sT[:], in_=cT[:], func=AF.Silu), reads=[cT], writes=[sT])
        for k in range(8):
            S.op("dve", lambda: nc.vector.tensor_scalar(out=crep_l[:, k, :], in0=ones_b[:], scalar1=sT[:, 0, k:k + 1],
                                                        scalar2=None, op0=ALU.mult),
                 reads=[ones_b, sT], writes=[crep_l], partial=True)
            S.op("dve", lambda: nc.vector.tensor_scalar(out=crep_c[:, k, :], in0=ones_b[:], scalar1=sT[:, 1, k:k + 1],
                                                        scalar2=None, op0=ALU.mult),
                 reads=[ones_b, sT], writes=[crep_c], partial=True)
        zb = S.sb([NPAD, D], BF16, "zb", st)
        S.op("dve", lambda: nc.vector.memset(zb[:], 0.0), writes=[zb])
        S.dma("sp", lambda: nc.sync.dma_start(out=H2[TT:TT + NPAD, :], in_=zb[:]), reads=[zb], writes=[H2])
        lp = S.sb([NPAD, 2], I32, "lp", st)
        S.dma("sp", lambda: nc.sync.dma_start(out=lp[:], in_=lpad[:, :]), reads=[lpad], writes=[lp])
        for e in range(NE):
            S.dma("sp", lambda: nc.sync.dma_start(out=LST[e, CAP + CCAP:MS, :], in_=lp[:]), reads=[lp], writes=[LST])
        xts = [S.sb([128, D], F32, "xcp", st) for _ in range(2)]
        for i in range(NTT):
            xt = xts[i % 2]
            src = x_in[i * 128:(i + 1) * 128, :] if i < NTL else ctx_in[(i - NTL) * 128:(i - NTL + 1) * 128, :]
            S.dma("sp", lambda: nc.sync.dma_start(out=xt[:], in_=src), reads=[x_in], writes=[xt])
            S.dma("sp", lambda: nc.sync.dma_start(out=X[i * 128:(i + 1) * 128, :], in_=xt[:]), reads=[xt], writes=[X])
        S.end_phase()

    for l in range(L):
        last = (l == DEPTH - 1)
        ntq = NTL if last else NTT

        with ES() as st:
            mod_l = S.sb([128, 6 * D], F32, "mod_l", st)
            mod_c = S.sb([128, 6 * D], F32, "mod_c", st)
            brep = S.sb([128, 6 * D], F32, "brep", st)
            S.dma("sp", lambda: nc.sync.dma_start(out=brep[:], in_=b_ada.ap()[l].partition_broadcast(128)),
                  reads=[b_ada], writes=[brep])
            g1rep = S.sb([128, D], F32, "g1rep", st)
            g2rep = S.sb([128, D], F32, "g2rep", st)
            S.dma("sp", lambda: nc.sync.dma_start(out=g1rep[:], in_=norm1_g.ap()[l].partition_broadcast(128)),
                  reads=[norm1_g], writes=[g1rep])
            S.dma("sp", lambda: nc.sync.dma_start(out=g2rep[:], in_=norm2_g.ap()[l].partition_broadcast(128)),
                  reads=[norm2_g], writes=[g2rep])
            was = [S.sb([128, 8, 512], BF16, "wa", st) for _ in range(2)]
            pms = [S.ps([128, 512], F32, "pm", st) for _ in range(2)]
            for cc in range(12):
                wa = was[cc % 2]
                S.dma("pool", lambda: nc.gpsimd.dma_start(
                    out=wa[:], in_=w_ada.ap()[l][:, cc * 512:(cc + 1) * 512].rearrange("(k p) n -> p k n", p=128)),
                    reads=[w_ada], writes=[wa])
                for which, (crep, mod) in enumerate(((crep_l, mod_l), (crep_c, mod_c))):
                    pm = pms[which]
                    for k in range(8):
                        S.op("pe", lambda: nc.tensor.matmul(pm[:], lhsT=crep[:, k, :], rhs=wa[:, k, :],
                                                            start=(k == 0), stop=(k == 7)),
                             reads=[crep, wa], writes=[pm], partial=(k > 0))
                    S.op("dve", lambda: nc.vector.tensor_tensor(out=mod[:, cc * 512:(cc + 1) * 512], in0=pm[:],
                                                                in1=brep[:, cc * 512:(cc + 1) * 512], op=ALU.add),
                         reads=[pm, brep], writes=[mod], partial=True)
            for mod in (mod_l, mod_c):
                S.op("dve", lambda: nc.vector.scalar_tensor_tensor(out=mod[:, D:2 * D], in0=mod[:, D:2 * D], scalar=1.0,
                                                                   in1=g1rep[:], op0=ALU.add, op1=ALU.mult),
                     reads=[mod, g1rep], writes=[mod], partial=True)
                S.op("dve", lambda: nc.vector.scalar_tensor_tensor(out=mod[:, 4 * D:5 * D], in0=mod[:, 4 * D:5 * D],
                                                                   scalar=1.0, in1=g2rep[:], op0=ALU.add, op1=ALU.mult),
                     reads=[mod, g2rep], writes=[mod], partial=True)
            S.dma("sp", lambda: nc.sync.dma_start(out=MODS[0], in_=mod_l[:]), reads=[mod_l], writes=[MODS])
            S.dma("sp", lambda: nc.sync.dma_start(out=MODS[1], in_=mod_c[:]), reads=[mod_c], writes=[MODS])
            S.end_phase()
        if stop_after == "M":
            break

        with ES() as st:
            mod_l, mod_c = load_mod(st, 0, 2 * D)
            win = S.sb([128, 8, 2048], BF16, "win", st)
            for h in range(4):
                S.dma("pool", lambda: nc.gpsimd.dma_start(
                    out=win[:, :, h * 512:(h + 1) * 512],
                    in_=w_in.ap()[l][:, h * 512:(h + 1) * 512].rearrange("(k p) n -> p k n", p=128)),
                    reads=[w_in], writes=[win], partial=True)
            gq = S.sb([128, 64], F32, "gq", st)
            gk = S.sb([128, 64], F32, "gk", st)
            S.dma("sp", lambda: nc.sync.dma_start(out=gq[:], in_=q_norm_g.ap()[l].partition_broadcast(128)),
                  reads=[q_norm_g], writes=[gq])
            S.dma("sp", lambda: nc.sync.dma_start(out=gk[:], in_=k_norm_g.ap()[l].partition_broadcast(128)),
                  reads=[k_norm_g], writes=[gk])
            S.op("dve", lambda: nc.vector.tensor_scalar(out=gq[:], in0=gq[:], scalar1=0.125, scalar2=None, op0=ALU.mult),
                 reads=[gq], writes=[gq])
            xts = [S.sb([128, D], F32, "xt", st) for _ in range(2)]
            sq = S.sb([128, D], F32, "sq", st)
            ss = S.sb([128, 1], F32, "ss", st)
            rstd = S.sb([128, 1], F32, "rstd", st)
            hf = S.sb([128, D], F32, "hf", st)
            hb = S.sb([128, D], BF16, "hb", st)
            hT = S.sb([128, 8, 512], BF16, "hT", st)
            rc = [S.sb([128, 64], F32, "rc", st) for _ in range(2)]
            rs_ = [S.sb([128, 64], F32, "rs", st) for _ in range(2)]
            qsq = S.sb([128, 640], F32, "qsq", st)
            qss = S.sb([128, 10], F32, "qss", st)
            qn = S.sb([128, 640], F32, "qn", st)
            t1 = S.sb([128, 640], F32, "t1", st)
            t2 = S.sb([128, 640], F32, "t2", st)
            qr = S.sb([128, 640], BF16, "qr", st)
            qbk = S.sb([128, 512], BF16, "qbk", st)
            qTs = S.sb([128, 4, 512], BF16, "qTs", st)
            kTs = S.sb([128, 512], BF16, "kTs", st)
            vas = S.sb([128, 4, 130], BF16, "vas", st)
            qbTs = S.sb([128, 2, 512], BF16, "qbTs", st)
            kbTs = S.sb([128, 2, 512], BF16, "kbTs", st)
            vbs = S.sb([128, 4, 256], BF16, "vbs", st)
            sg = S.sb([128, 512], F32, "sg", st)
            uTs = S.sb([128, 2, 512], F32, "uTs", st)
            pT = S.ps([128, 8, 128], BF16, "pT", st)
            pmm = [S.ps([128, 512], F32, "pmm", st) for _ in range(3)]
            pq = S.ps([128, 4, 128], BF16, "pq", st)
            pk = S.ps([128, 5, 128], BF16, "pk", st)
            pf = [S.ps([128, 512], F32, "pf", st) for _ in range(2)]
            S.op("dve", lambda: nc.vector.memset(vas[:], 1.0), writes=[vas])
            A1 = lambda mod: mod[:, D:2 * D]
            S1 = lambda mod: mod[:, 0:D]

            groups = [(g * 512, 512, mod_l) for g in range(T // 512)] + [(T, CT, mod_c)]
            hTs = [hT, S.sb([128, 8, 512], BF16, "hT2", st)]
            tiles1 = []
            for gi_, (t0_, G_, mod_) in enumerate(groups):
                for s_ in range(G_ // 128):
                    tiles1.append((gi_, t0_, G_, mod_, s_))

            def stA1(i):
                gi, t0, G, mod, s = tiles1[i]
                r0 = t0 + s * 128
                xt = xts[i % 2]
                rcb, rsb = rc[i % 2], rs_[i % 2]
                if True:
                    S.dma("sp", lambda: nc.sync.dma_start(out=xt[:], in_=X[r0:r0 + 128, :]), reads=[X], writes=[xt])
                    S.op("act", lambda: nc.scalar.activation(out=sq[:], in_=xt[:], func=AF.Square, accum_out=ss[:]),
                         reads=[xt], writes=[sq, ss])
                    S.op("dve", lambda: nc.vector.tensor_scalar(out=rstd[:], in0=ss[:], scalar1=1.0 / D, scalar2=EPS,
                                                                op0=ALU.mult, op1=ALU.add), reads=[ss], writes=[rstd])
                    S.op("act", lambda: nc.scalar.activation(out=rstd[:], in_=rstd[:], func=AF.Sqrt), reads=[rstd], writes=[rstd])
                    S.op("dve", lambda: nc.vector.reciprocal(out=rstd[:], in_=rstd[:]), reads=[rstd], writes=[rstd])
                    S.op("dve", lambda: nc.vector.scalar_tensor_tensor(out=hf[:], in0=xt[:], scalar=rstd[:, 0:1], in1=A1(mod),
                                                                       op0=ALU.mult, op1=ALU.mult),
                         reads=[xt, rstd, mod], writes=[hf])
                    S.op("pool", lambda: nc.gpsimd.tensor_tensor(out=hb[:], in0=hf[:], in1=S1(mod), op=ALU.add),
                         reads=[hf, mod], writes=[hb])
                    S.dma("sp", lambda: nc.sync.dma_start(out=rcb[:], in_=rope_c[r0:r0 + 128, :]), reads=[rope_c], writes=[rcb])
                    S.dma("sp", lambda: nc.sync.dma_start(out=rsb[:], in_=rope_s[r0:r0 + 128, :]), reads=[rope_s], writes=[rsb])

            def stA2(i):
                gi, t0, G, mod, s = tiles1[i]
                hT = hTs[gi % 2]
                if True:
                    for k in range(8):
                        S.op("pe", lambda: nc.tensor.transpose(out=pT[:, k, :], in_=hb[:, k * 128:(k + 1) * 128], identity=idb[:]),
                             reads=[hb, idb], writes=[pT], partial=(k > 0))
                    S.op("act", lambda: nc.scalar.copy(out=hT[:, :, s * 128:(s + 1) * 128], in_=pT[:]),
                         reads=[pT], writes=[hT], partial=True)

            def stBmm(i):
                gi, t0, G, mod, s = tiles1[i]
                hT = hTs[gi % 2]
                if True:
                    for cg_ in range(3):
                        pm = pmm[cg_]
                        for k in range(8):
                            S.op("pe", lambda: nc.tensor.matmul(pm[:], lhsT=hT[:, k, s * 128:(s + 1) * 128],
                                                                rhs=win[:, k, cg_ * 512:(cg_ + 1) * 512],
                                                                start=(k == 0), stop=(k == 7)),
                                 reads=[hT, win], writes=[pm], partial=(k > 0))

            def stBpost(i):
                gi, t0, G, mod, s = tiles1[i]
                rcb, rsb = rc[i % 2], rs_[i % 2]
                if True:
                    S.op("act", lambda: nc.scalar.activation(out=qsq[:, 0:512], in_=pmm[0][:], func=AF.Square),
                         reads=[pmm[0]], writes=[qsq], partial=True)
                    S.op("act", lambda: nc.scalar.activation(out=qsq[:, 512:640], in_=pmm[1][:, 0:128], func=AF.Square),
                         reads=[pmm[1]], writes=[qsq], partial=True)
                    S.op("dve", lambda: nc.vector.tensor_reduce(out=qss[:], in_=qsq[:].rearrange("p (h d) -> p h d", d=64),
                                                                axis=AX.X, op=ALU.add), reads=[qsq], writes=[qss])
                    S.op("dve", lambda: nc.vector.tensor_scalar(out=qss[:], in0=qss[:], scalar1=1.0 / 64, scalar2=EPS,
                                                                op0=ALU.mult, op1=ALU.add), reads=[qss], writes=[qss])
                    S.op("act", lambda: nc.scalar.activation(out=qss[:], in_=qss[:], func=AF.Sqrt), reads=[qss], writes=[qss])
                    S.op("dve", lambda: nc.vector.reciprocal(out=qss[:], in_=qss[:]), reads=[qss], writes=[qss])
                    S.op("dve", lambda: nc.vector.tensor_tensor(
                        out=qn[:, 0:512].rearrange("p (h d) -> p h d", d=64), in0=pmm[0][:].rearrange("p (h d) -> p h d", d=64),
                        in1=qss[:, 0:8].unsqueeze(2).to_broadcast([128, 8, 64]), op=ALU.mult),
                        reads=[pmm[0], qss], writes=[qn], partial=True)
                    S.op("dve", lambda: nc.vector.tensor_tensor(
                        out=qn[:, 512:640].rearrange("p (h d) -> p h d", d=64),
                        in0=pmm[1][:, 0:128].rearrange("p (h d) -> p h d", d=64),
                        in1=qss[:, 8:10].unsqueeze(2).to_broadcast([128, 2, 64]), op=ALU.mult),
                        reads=[pmm[1], qss], writes=[qn], partial=True)
                    S.op("pool", lambda: nc.gpsimd.tensor_tensor(
                        out=qn[:, 0:512].rearrange("p (h d) -> p h d", d=64), in0=qn[:, 0:512].rearrange("p (h d) -> p h d", d=64),
                        in1=gq[:].unsqueeze(1).to_broadcast([128, 8, 64]), op=ALU.mult), reads=[qn, gq], writes=[qn], partial=True)
                    S.op("pool", lambda: nc.gpsimd.tensor_tensor(
                        out=qn[:, 512:640].rearrange("p (h d) -> p h d", d=64),
                        in0=qn[:, 512:640].rearrange("p (h d) -> p h d", d=64),
                        in1=gk[:].unsqueeze(1).to_broadcast([128, 2, 64]), op=ALU.mult), reads=[qn, gk], writes=[qn], partial=True)
                    S.op("dve", lambda: nc.vector.tensor_tensor(
                        out=t1[:].rearrange("p (h d) -> p h d", d=64), in0=qn[:].rearrange("p (h d) -> p h d", d=64),
                        in1=rcb[:].unsqueeze(1).to_broadcast([128, 10, 64]), op=ALU.mult), reads=[qn, rcb], writes=[t1])
                    qv = qn[:].rearrange("p (h a b c) -> p h a b c", h=10, a=2, b=2)
                    tv = t2[:].rearrange("p (h a b c) -> p h a b c", h=10, a=2, b=2)
                    sv = rsb[:].rearrange("p (a b c) -> p a b c", a=2, b=2)
                    for a in range(2):
                        for b_ in range(2):
                            S.op("pool", lambda: nc.gpsimd.tensor_tensor(
                                out=tv[:, :, a, b_, :], in0=qv[:, :, a, 1 - b_, :],
                                in1=sv[:, a, b_, :].unsqueeze(1).to_broadcast([128, 10, 16]), op=ALU.mult),
                                reads=[qn, rsb], writes=[t2], partial=True)
                    S.op("dve", lambda: nc.vector.tensor_tensor(
                        out=qr[:, 0:512].rearrange("p (g k d) -> p k g d", g=4, k=2),
                        in0=t1[:, 0:512].rearrange("p (k g d) -> p k g d", k=2, g=4),
                        in1=t2[:, 0:512].rearrange("p (k g d) -> p k g d", k=2, g=4), op=ALU.add),
                        reads=[t1, t2], writes=[qr])
                    S.op("dve", lambda: nc.vector.tensor_tensor(out=qr[:, 512:640], in0=t1[:, 512:640], in1=t2[:, 512:640],
                                                                op=ALU.add), reads=[t1, t2], writes=[qr], partial=True)
                    for g in range(4):
                        S.op("pe", lambda: nc.tensor.transpose(
                            out=pq[:, g, :], in_=qr[:, g * 128:(g + 1) * 128],
                            identity=idb[:]), reads=[qr, idb], writes=[pq], partial=(g > 0))
                    S.op("act", lambda: nc.scalar.copy(out=qTs[:, :, s * 128:(s + 1) * 128], in_=pq[:]),
                         reads=[pq], writes=[qTs], partial=True)
                    S.op("pe", lambda: nc.tensor.transpose(out=pk[:, 0, :], in_=qr[:, 512:640], identity=idb[:]),
                         reads=[qr, idb], writes=[pk], partial=False)
                    S.op("act", lambda: nc.scalar.copy(out=vas[:, s, :].rearrange("p (k e) -> p k e", e=65)[:, :, 0:64],
                                                       in_=pmm[1][:, 128:256].rearrange("p (k d) -> p k d", d=64)),
                         reads=[pmm[1]], writes=[vas], partial=True)
                    S.op("dve", lambda: nc.vector.tensor_scalar(out=qbk[:, 0:256], in0=pmm[1][:, 256:512], scalar1=0.125,
                                                                scalar2=None, op0=ALU.mult),
                         reads=[pmm[1]], writes=[qbk], partial=True)
                    S.op("act", lambda: nc.scalar.copy(out=qbk[:, 256:512], in_=pmm[2][:, 0:256]),
                         reads=[pmm[2]], writes=[qbk], partial=True)
                    S.op("act", lambda: nc.scalar.copy(out=vbs[:, s, :], in_=pmm[2][:, 256:512]),
                         reads=[pmm[2]], writes=[vbs], partial=True)
                    for j in range(4):
                        S.op("pe", lambda: nc.tensor.transpose(out=pk[:, 1 + j, :], in_=qbk[:, j * 128:(j + 1) * 128],
                                                               identity=idb[:]), reads=[qbk, idb], writes=[pk], partial=True)
                    S.op("dve", lambda: nc.vector.tensor_copy(out=kTs[:, s * 128:(s + 1) * 128], in_=pk[:, 0, :]),
                         reads=[pk], writes=[kTs], partial=True)
                    S.op("dve", lambda: nc.vector.tensor_copy(out=qbTs[:, :, s * 128:(s + 1) * 128], in_=pk[:, 1:3, :]),
                         reads=[pk], writes=[qbTs], partial=True)
                    S.op("dve", lambda: nc.vector.tensor_copy(out=kbTs[:, :, s * 128:(s + 1) * 128], in_=pk[:, 3:5, :]),
                         reads=[pk], writes=[kbTs], partial=True)

            def stGend(gi):
                t0, G, mod = groups[gi]
                nsub = G // 128
                hT = hTs[gi % 2]
                for j in range(2):
                    for which in range(2):
                        c0 = 1536 + which * 256 + j * 128
                        for k in range(8):
                            S.op("pe", lambda: nc.tensor.matmul(pf[which][:, 0:G], lhsT=win[:, k, c0:c0 + 128], rhs=hT[:, k, 0:G],
                                                                start=(k == 0), stop=(k == 7)),
                                 reads=[win, hT], writes=[pf[which]], partial=(k > 0))
                    S.op("act", lambda: nc.scalar.activation(out=sg[:, 0:G], in_=pf[1][:, 0:G], func=AF.Sigmoid),
                         reads=[pf[1]], writes=[sg])
                    S.op("dve", lambda: nc.vector.tensor_tensor(out=uTs[:, j, 0:G], in0=pf[0][:, 0:G], in1=sg[:, 0:G], op=ALU.mult),
                         reads=[pf[0], sg], writes=[uTs], partial=True)
                S.dma("sp", lambda: nc.sync.dma_start(out=QT[:, :, t0:t0 + G], in_=qTs[:, :, 0:G]), reads=[qTs], writes=[QT])
                S.dma("sp", lambda: nc.sync.dma_start(out=KT[:, t0:t0 + G], in_=kTs[:, 0:G]), reads=[kTs], writes=[KT])
                S.dma("sp", lambda: nc.sync.dma_start(out=VA.ap()[t0:t0 + G, :].rearrange("(s p) e -> p s e", p=128),
                                                      in_=vas[:, 0:nsub, :]), reads=[vas], writes=[VA])
                S.dma("sp", lambda: nc.sync.dma_start(out=QBT[:, :, t0:t0 + G], in_=qbTs[:, :, 0:G]), reads=[qbTs], writes=[QBT])
                S.dma("sp", lambda: nc.sync.dma_start(out=KBT[:, :, t0:t0 + G], in_=kbTs[:, :, 0:G]), reads=[kbTs], writes=[KBT])
                S.dma("sp", lambda: nc.sync.dma_start(out=VB.ap()[t0:t0 + G, :].rearrange("(s p) e -> p s e", p=128),
                                                      in_=vbs[:, 0:nsub, :]), reads=[vbs], writes=[VB])
                S.dma("sp", lambda: nc.sync.dma_start(out=UT.ap()[:, t0:t0 + G].rearrange("(j p) t -> p j t", p=128),
                                                      in_=uTs[:, :, 0:G]), reads=[uTs], writes=[UT])

            n1 = len(tiles1)
            stA1(0)
            stA2(0)
            for i in range(n1):
                if i + 1 < n1:
                    stA1(i + 1)
                stBmm(i)
                if i + 1 < n1:
                    stA2(i + 1)
                stBpost(i)
                if i + 1 == n1 or tiles1[i + 1][0] != tiles1[i][0]:
                    stGend(tiles1[i][0])
            S.end_phase()
        if stop_after == "1":
            break

        with ES() as st:
            ksb = S.sb([128, TT], BF16, "ksb", st)
            vsb = S.sb([128, NTT, 130], BF16, "vsb", st)
            S.dma("sp", lambda: nc.sync.dma_start(out=ksb[:], in_=KT[:, :]), reads=[KT], writes=[ksb])
            S.dma("sp", lambda: nc.sync.dma_start(out=vsb[:], in_=VA.ap().rearrange("(n p) e -> p n e", p=128)),
                  reads=[VA], writes=[vsb])
            gqk = S.sb([128, 2, 64], F32, "gqk", st)
            S.dma("sp", lambda: nc.sync.dma_start(out=gqk[:, 0, :], in_=q_norm_g.ap()[l].partition_broadcast(128)),
                  reads=[q_norm_g], writes=[gqk], partial=True)
            S.dma("sp", lambda: nc.sync.dma_start(out=gqk[:, 1, :], in_=k_norm_g.ap()[l].partition_broadcast(128)),
                  reads=[k_norm_g], writes=[gqk], partial=True)
            gmx = S.sb([128, 2], F32, "gmx", st)
            nb = S.sb([128, 1], F32, "nb", st)
            gng = S.sb([128, 2, 64], F32, "gng", st)
            S.op("dve", lambda: nc.vector.tensor_scalar(out=gng[:], in0=gqk[:], scalar1=-1.0, scalar2=None, op0=ALU.mult),
                 reads=[gqk], writes=[gng])
            S.op("dve", lambda: nc.vector.tensor_tensor(out=gqk[:], in0=gqk[:], in1=gng[:], op=ALU.max),
                 reads=[gqk, gng], writes=[gqk])
            S.op("dve", lambda: nc.vector.tensor_reduce(out=gmx[:], in_=gqk[:], axis=AX.X, op=ALU.max), reads=[gqk], writes=[gmx])
            S.op("dve", lambda: nc.vector.tensor_tensor(out=nb[:], in0=gmx[:, 0:1], in1=gmx[:, 1:2], op=ALU.mult),
                 reads=[gmx], writes=[nb])
            S.op("dve", lambda: nc.vector.tensor_scalar(out=nb[:], in0=nb[:], scalar1=-8.0, scalar2=None, op0=ALU.mult),
                 reads=[nb], writes=[nb])
            qsbs = [S.sb([128, 2, 512], BF16, "qsb", st) for _ in range(2)]
            for qb_ in qsbs:
                S.op("dve", lambda: nc.vector.memset(qb_[:], 0.0), writes=[qb_])
            pbs = [S.sb([128, 512], BF16, "pb", st) for _ in range(3)]
            rsa = S.sb([128, 4], F32, "rsa", st)
            osb = [S.sb([128, 512], BF16, "osb", st) for _ in range(2)]
            aTs = [S.sb([128, 4, 128], BF16, "aTs", st) for _ in range(2)]
            pss = [S.ps([128, 512], F32, "pss", st) for _ in range(3)]
            pos = [S.ps([128, 512], F32, "po", st) for _ in range(4)]
            pa = S.ps([128, 4, 128], BF16, "pa", st)
            steps = []
            nq2 = min(ntq, P2_LIMIT) if P2_LIMIT else ntq
            for qi in range(nq2):
                ktiles = list(range(NTT)) if qi < NTL else [NTL, NTL + 1]
                for kh in range(2):
                    for idx, kt in enumerate(ktiles):
                        steps.append((qi, kh, idx, kt, len(ktiles)))
            nst_ = len(steps)

            def load_q(qi):
                q0 = qi * 128
                qsb = qsbs[qi % 2]
                for kh_ in range(2):
                    S.dma("sp", lambda: nc.sync.dma_start(
                        out=qsb[kh_ * 64:(kh_ + 1) * 64, kh_, :].rearrange("p (g t) -> p g t", g=4),
                        in_=QT[kh_ * 64:(kh_ + 1) * 64, :, q0:q0 + 128]), reads=[QT], writes=[qsb], partial=True)

            def emit_S(i):
                qi, kh, idx, kt, nk = steps[i]
                if kh == 0 and idx == 0 and qi + 1 < nq2:
                    load_q(qi + 1)
                qsb = qsbs[qi % 2]
                ps = pss[i % 3]
                S.op("pe", lambda: nc.tensor.matmul(ps[:], lhsT=ksb[:, kt * 128:(kt + 1) * 128],
                                                    rhs=qsb[:, kh, :], start=True, stop=True),
                     reads=[ksb, qsb], writes=[ps])

            def finish_q(qi):
                q0 = qi * 128
                ob = osb[qi % 2]
                for j in range(4):
                    S.op("pe", lambda: nc.tensor.transpose(out=pa[:, j, :], in_=ob[:, j * 128:(j + 1) * 128], identity=idb[:]),
                         reads=[ob, idb], writes=[pa], partial=(j > 0))
                aT = aTs[qi % 2]
                S.op("dve", lambda: nc.vector.tensor_copy(out=aT[:], in_=pa[:]), reads=[pa], writes=[aT])
                S.dma("sp", lambda: nc.sync.dma_start(out=AT.ap()[:, q0:q0 + 128].rearrange("(j p) t -> p j t", p=128), in_=aT[:]),
                      reads=[aT], writes=[AT])

            load_q(0)
            emit_S(0)
            if nst_ > 1:
                emit_S(1)
            deferred = {}
            for i in range(nst_):
                qi, kh, idx, kt, nk = steps[i]
                ps, pb = pss[i % 3], pbs[i % 3]
                ob = osb[qi % 2]
                S.op("act", lambda: nc.scalar.activation(out=pb[:], in_=ps[:], func=AF.Exp, bias=nb[:, 0:1], scale=1.0),
                     reads=[ps, nb], writes=[pb])
                for g in range(4):
                    S.op("pe", lambda: nc.tensor.matmul(pos[g][:, 0:65], lhsT=pb[:, g * 128:(g + 1) * 128],
                                                        rhs=vsb[:, kt, kh * 65:(kh + 1) * 65],
                                                        start=(idx == 0), stop=(idx == nk - 1)),
                         reads=[pb, vsb], writes=[pos[g]], partial=(idx > 0))
                if i + 2 < nst_:
                    emit_S(i + 2)
                if idx == nk - 1:
                    for g in range(4):
                        S.op("dve", lambda: nc.vector.reciprocal(out=rsa[:, g:g + 1], in_=pos[g][:, 64:65]),
                             reads=[pos[g]], writes=[rsa], partial=True)
                        S.op("dve", lambda: nc.vector.tensor_scalar(
                            out=ob[:, kh * 256 + g * 64:kh * 256 + (g + 1) * 64], in0=pos[g][:, 0:64], scalar1=rsa[:, g:g + 1],
                            scalar2=None, op0=ALU.mult), reads=[pos[g], rsa], writes=[ob], partial=True)
                    if kh == 1:
                        deferred[min(i + 4, nst_ - 1)] = deferred.get(min(i + 4, nst_ - 1), []) + [qi]
                for qd in deferred.pop(i, []):
                    finish_q(qd)
            S.end_phase()
        if stop_after == "2":
            break

        with ES() as st:
            kc = S.sb([128, 2, 256], BF16, "kc", st)
            vc = S.sb([128, 2, 256], BF16, "vc", st)
            S.dma("sp", lambda: nc.sync.dma_start(out=kc[:], in_=KBT[:, :, T:TT]), reads=[KBT], writes=[kc])
            S.dma("sp", lambda: nc.sync.dma_start(out=vc[:], in_=VB.ap()[T:TT, :].rearrange("(n p) e -> p n e", p=128)),
                  reads=[VB], writes=[vc])
            bint = S.sb([64, 4, 512], F32, "bint", st)
            bedges = [S.sb([64, 4, 512], F32, "bedge", st) for _ in range(2)]
            S.dma("sp", lambda: nc.sync.dma_start(out=bint[:], in_=nabias[l, 3]), reads=[nabias], writes=[bint])
            qrows = [S.sb([128, 2, 64], BF16, "qrow", st) for _ in range(2)]
            kwins = [S.sb([128, 2, 512], BF16, "kwin", st) for _ in range(2)]
            vwins = [S.sb([64, 8, 256], BF16, "vwin", st) for _ in range(2)]
            ssbs = [S.sb([64, 768], F32, "ssb", st) for _ in range(2)]
            mxs = [S.sb([64, 1], F32, "mx", st) for _ in range(2)]
            sms = [S.sb([64, 1], F32, "sm", st) for _ in range(2)]
            pexps = [S.sb([64, 768], BF16, "pexp", st) for _ in range(2)]
            ptw_ss = [S.sb([64, 8, 64], BF16, "ptw_s", st) for _ in range(2)]
            ptc_ss = [S.sb([128, 2, 64], BF16, "ptc_s", st) for _ in range(2)]
            brows = [S.sb([64, 256], BF16, "brow", st) for _ in range(2)]
            bts = [S.sb([128, 2, 64], BF16, "bts", st) for _ in range(2)]
            psn = [S.ps([64, 1024], F32, "psn", st) for _ in range(2)]
            ptps = [S.ps([64, 512], BF16, "ptp", st) for _ in range(1)] * 2
            ptcs_ = [S.ps([128, 2, 64], BF16, "ptcx", st) for _ in range(1)] * 2
            pbts_ = [S.ps([128, 2, 64], BF16, "pbtx", st) for _ in range(1)] * 2
            pons = [S.ps([64, 512], F32, "pon", st) for _ in range(1)] * 2
            NR = min(T // 64, P3_LIMIT) if P3_LIMIT else T // 64
            units = [(r, hb) for r in range(NR) for hb in range(4)]
            NU = len(units)

            def row_info(r):
                rs0 = min(max(r - 4, 0), 120)
                return rs0, rs0 - r + 7

            def load_row(r):
                rs0, off = row_info(r)
                qrow, kwin, vwin = qrows[r % 2], kwins[r % 2], vwins[r % 2]
                S.dma("sp", lambda: nc.sync.dma_start(out=qrow[:], in_=QBT[:, :, r * 64:(r + 1) * 64]), reads=[QBT], writes=[qrow])
                S.dma("sp", lambda: nc.sync.dma_start(out=kwin[:], in_=KBT[:, :, rs0 * 64:(rs0 + 8) * 64]), reads=[KBT], writes=[kwin])
                S.dma("sp", lambda: nc.sync.dma_start(out=vwin[:], in_=VB.ap()[rs0 * 64:(rs0 + 8) * 64, :].rearrange("(j p) e -> p j e", p=64)),
                      reads=[VB], writes=[vwin])
                if off != 3:
                    be = bedges[r % 2]
                    S.dma("sp", lambda: nc.sync.dma_start(out=be[:], in_=nabias[l, off]), reads=[nabias], writes=[be])

            def st_scores(t):
                r, hb = units[t]
                if hb == 1 and r + 1 < NR:
                    load_row(r + 1)
                hp, pr = hb // 2, (hb % 2) * 64
                qrow, kwin = qrows[r % 2], kwins[r % 2]
                ps = psn[t % 2]
                S.op("pe", lambda: nc.tensor.matmul(ps[:, 0:512], lhsT=qrow[pr:pr + 64, hp, :], rhs=kwin[pr:pr + 64, hp, :],
                                                    start=True, stop=True), reads=[qrow, kwin], writes=[ps])
                S.op("pe", lambda: nc.tensor.matmul(ps[:, 512:768], lhsT=qrow[pr:pr + 64, hp, :], rhs=kc[pr:pr + 64, hp, :],
                                                    start=True, stop=True), reads=[qrow, kc], writes=[ps], partial=True)

            def st_softmax(t):
                r, hb = units[t]
                rs0, off = row_info(r)
                bias = bint if off == 3 else bedges[r % 2]
                ps, ssb, mx, sm, pexp = psn[t % 2], ssbs[t % 2], mxs[t % 2], sms[t % 2], pexps[t % 2]
                S.op("dve", lambda: nc.vector.tensor_tensor(out=ssb[:, 0:512], in0=ps[:, 0:512], in1=bias[:, hb, :], op=ALU.add),
                     reads=[ps, bias], writes=[ssb])
                S.op("act", lambda: nc.scalar.copy(out=ssb[:, 512:768], in_=ps[:, 512:768]), reads=[ps], writes=[ssb], partial=True)
                S.op("dve", lambda: nc.vector.tensor_reduce(out=mx[:], in_=ssb[:], axis=AX.X, op=ALU.max), reads=[ssb], writes=[mx])
                S.op("dve", lambda: nc.vector.tensor_scalar(out=mx[:], in0=mx[:], scalar1=-1.0, scalar2=None, op0=ALU.mult),
                     reads=[mx], writes=[mx])
                S.op("act", lambda: nc.scalar.activation(out=pexp[:], in_=ssb[:], func=AF.Exp, bias=mx[:, 0:1], scale=1.0,
                                                         accum_out=sm[:]), reads=[ssb, mx], writes=[pexp, sm])

            def st_transp(t):
                pexp, ptp = pexps[t % 2], ptps[t % 2]
                for j in range(8):
                    S.op("pe", lambda: nc.tensor.transpose(out=ptp[0:64, j * 64:(j + 1) * 64], in_=pexp[:, j * 64:(j + 1) * 64],
                                                           identity=idb[0:64, 0:64]), reads=[pexp, idb], writes=[ptp], partial=(j > 0))
                for j in range(2):
                    S.op("pe", lambda: nc.tensor.transpose(out=ptcs_[t % 2][:, j, :],
                                                           in_=pexp[:, 512 + j * 128:512 + (j + 1) * 128],
                                                           identity=idb[0:64, 0:64]), reads=[pexp, idb], writes=[ptcs_[t % 2]], partial=(j > 0))
                S.op("dve", lambda: nc.vector.tensor_copy(out=ptw_ss[t % 2][:].rearrange("p j q -> p (j q)"), in_=ptp[0:64, 0:512]),
                     reads=[ptp], writes=[ptw_ss[t % 2]])
                S.op("act", lambda: nc.scalar.copy(out=ptc_ss[t % 2][:], in_=ptcs_[t % 2][:]),
                     reads=[ptcs_[t % 2]], writes=[ptc_ss[t % 2]])

            def st_pv(t):
                r, hb = units[t]
                vwin = vwins[r % 2]
                pon, ptw_s, ptc_s, sm, brow = pons[t % 2], ptw_ss[t % 2], ptc_ss[t % 2], sms[t % 2], brows[r % 2]
                for j in range(8):
                    S.op("pe", lambda: nc.tensor.matmul(pon[:, 0:64], lhsT=ptw_s[:, j, :], rhs=vwin[:, j, hb * 64:(hb + 1) * 64],
                                                        start=(j == 0), stop=False), reads=[ptw_s, vwin], writes=[pon], partial=(j > 0))
                for j in range(2):
                    S.op("pe", lambda: nc.tensor.matmul(pon[:, 0:64], lhsT=ptc_s[:, j, :], rhs=vc[:, j, hb * 64:(hb + 1) * 64],
                                                        start=False, stop=(j == 1)), reads=[ptc_s, vc], writes=[pon], partial=True)
                S.op("dve", lambda: nc.vector.reciprocal(out=sm[:], in_=sm[:]), reads=[sm], writes=[sm])
                S.op("dve", lambda: nc.vector.tensor_scalar(out=brow[:, hb * 64:(hb + 1) * 64], in0=pon[:, 0:64], scalar1=sm[:, 0:1],
                                                            scalar2=None, op0=ALU.mult), reads=[pon, sm], writes=[brow], partial=True)

            def st_rowout(r):
                brow, bt, ptp = brows[r % 2], bts[r % 2], ptps[r % 2]
                for j in range(2):
                    S.op("pe", lambda: nc.tensor.transpose(out=pbts_[0][:, j, :], in_=brow[:, j * 128:(j + 1) * 128],
                                                           identity=idb[0:64, 0:64]), reads=[brow, idb], writes=[pbts_[0]], partial=(j > 0))
                S.op("act", lambda: nc.scalar.copy(out=bt[:], in_=pbts_[0][:]), reads=[pbts_[0]], writes=[bt])
                S.dma("sp", lambda: nc.sync.dma_start(out=BT.ap()[:, r * 64:(r + 1) * 64].rearrange("(j p) t -> p j t", p=128), in_=bt[:]),
                      reads=[bt], writes=[BT])

            load_row(0)
            st_scores(0)
            for t in range(NU + 2):
                if 1 <= t <= NU:
                    st_pv(t - 1)
                if t >= 2 and units[t - 2][1] == 3:
                    st_rowout(units[t - 2][0])
                if t + 1 < NU:
                    st_scores(t + 1)
                if t < NU:
                    st_softmax(t)
                    st_transp(t)
            S.end_phase()
        if not last:
            with ES() as st:
                kc = S.sb([128, 2, 256], BF16, "kc", st)
                vc = S.sb([128, 2, 256], BF16, "vc", st)
                S.dma("sp", lambda: nc.sync.dma_start(out=kc[:], in_=KBT[:, :, T:TT]), reads=[KBT], writes=[kc])
                S.dma("sp", lambda: nc.sync.dma_start(out=vc[:], in_=VB.ap()[T:TT, :].rearrange("(n p) e -> p n e", p=128)),
                      reads=[VB], writes=[vc])
                qc = S.sb([128, 2, 128], BF16, "qc", st)
                sc_ = S.sb([128, 256], F32, "sc", st)
                mxc = S.sb([128, 1], F32, "mxc", st)
                smc = S.sb([128, 1], F32, "smc", st)
                pxc = S.sb([128, 256], BF16, "pxc", st)
                ptcs = S.sb([128, 2, 128], BF16, "ptcs", st)
                browc = S.sb([128, 256], BF16, "browc", st)
                btc = S.sb([128, 2, 128], BF16, "btc", st)
                psc = S.ps([128, 256], F32, "psc", st)
                ptcp = S.ps([128, 2, 128], BF16, "ptcp", st)
                poc = S.ps([128, 64], F32, "poc", st)
                pbc = S.ps([128, 2, 128], BF16, "pbc", st)
                for ci in range(2):
                    c0 = T + ci * 128
                    S.dma("sp", lambda: nc.sync.dma_start(out=qc[:], in_=QBT[:, :, c0:c0 + 128]), reads=[QBT], writes=[qc])
                    for hb in range(4):
                        hp, pr = hb // 2, (hb % 2) * 64
                        S.op("pe", lambda: nc.tensor.matmul(psc[:], lhsT=qc[pr:pr + 64, hp, :], rhs=kc[pr:pr + 64, hp, :],
                                                            start=True, stop=True), reads=[qc, kc], writes=[psc])
                        S.op("act", lambda: nc.scalar.copy(out=sc_[:], in_=psc[:]), reads=[psc], writes=[sc_])
                        S.op("dve", lambda: nc.vector.tensor_reduce(out=mxc[:], in_=sc_[:], axis=AX.X, op=ALU.max), reads=[sc_], writes=[mxc])
                        S.op("dve", lambda: nc.vector.tensor_scalar(out=mxc[:], in0=mxc[:], scalar1=-1.0, scalar2=None, op0=ALU.mult),
                             reads=[mxc], writes=[mxc])
                        S.op("act", lambda: nc.scalar.activation(out=pxc[:], in_=sc_[:], func=AF.Exp, bias=mxc[:, 0:1], scale=1.0,
                                                                 accum_out=smc[:]), reads=[sc_, mxc], writes=[pxc, smc])
                        for j in range(2):
                            S.op("pe", lambda: nc.tensor.transpose(out=ptcp[:, j, :], in_=pxc[:, j * 128:(j + 1) * 128], identity=idb[:]),
                                 reads=[pxc, idb], writes=[ptcp], partial=(j > 0))
                        S.op("dve", lambda: nc.vector.tensor_copy(out=ptcs[:], in_=ptcp[:]), reads=[ptcp], writes=[ptcs])
                        for j in range(2):
                            S.op("pe", lambda: nc.tensor.matmul(poc[:], lhsT=ptcs[:, j, :], rhs=vc[:, j, hb * 64:(hb + 1) * 64],
                                                                start=(j == 0), stop=(j == 1)), reads=[ptcs, vc], writes=[poc], partial=(j > 0))
                        S.op("dve", lambda: nc.vector.reciprocal(out=smc[:], in_=smc[:]), reads=[smc], writes=[smc])
                        S.op("dve", lambda: nc.vector.tensor_scalar(out=browc[:, hb * 64:(hb + 1) * 64], in0=poc[:], scalar1=smc[:, 0:1],
                                                                    scalar2=None, op0=ALU.mult), reads=[poc, smc], writes=[browc], partial=True)
                    for j in range(2):
                        S.op("pe", lambda: nc.tensor.transpose(out=pbc[:, j, :], in_=browc[:, j * 128:(j + 1) * 128], identity=idb[:]),
                             reads=[browc, idb], writes=[pbc], partial=(j > 0))
                    S.op("act", lambda: nc.scalar.copy(out=btc[:], in_=pbc[:]), reads=[pbc], writes=[btc])
                    S.dma("sp", lambda: nc.sync.dma_start(out=BT.ap()[:, c0:c0 + 128].rearrange("(j p) t -> p j t", p=128), in_=btc[:]),
                          reads=[btc], writes=[BT])
                S.end_phase()
        if stop_after == "3":
            break

        seqs = [(0, T)] + ([] if last else [(T, CT)])
        for (t0, Ls) in seqs:
            with ES() as st:
                Y = S.sb([128, 2, Ls], F32, "Y", st)
                up = S.sb([128, Ls + 30], F32, "up", st)
                cw = S.sb([128, 2, 31], F32, "cw", st)
                cb = S.sb([128, 2], F32, "cb", st)
                lng = S.sb([128, 2], F32, "lng", st)
                lnb = S.sb([128, 2], F32, "lnb", st)
                ones_s = S.sb([128, 128], F32, "ones_s", st)
                S.op("dve", lambda: nc.vector.memset(ones_s[:], 1.0 / 256), writes=[ones_s])
                with nc.allow_non_contiguous_dma(reason="tiny"):
                    for j in range(2):
                        S.dma("sp", lambda: nc.sync.dma_start(out=cw[:, j, :], in_=conv_w.ap()[l][:, j * 128:(j + 1) * 128].rearrange("w c -> c w")),
                              reads=[conv_w], writes=[cw], partial=True)
                    for (dst, src) in ((cb, conv_b), (lng, conv_ln_g), (lnb, conv_ln_b)):
                        S.dma("sp", lambda: nc.sync.dma_start(out=dst[:], in_=src.ap()[l].rearrange("(j p) -> p j", p=128)),
                              reads=[src], writes=[dst])
                S.op("dve", lambda: nc.vector.memset(up[:, 0:15], 0.0), writes=[up], partial=True)
                S.op("dve", lambda: nc.vector.memset(up[:, Ls + 15:Ls + 30], 0.0), writes=[up], partial=True)
                for j in range(2):
                    S.dma("sp", lambda: nc.sync.dma_start(out=up[:, 15:15 + Ls], in_=UT[j * 128:(j + 1) * 128, t0:t0 + Ls]),
                          reads=[UT], writes=[up], partial=True)
                    S.op("dve", lambda: nc.vector.tensor_scalar(out=Y[:, j, :], in0=up[:, 0:Ls], scalar1=cw[:, j, 0:1], scalar2=cb[:, j:j + 1],
                                                                op0=ALU.mult, op1=ALU.add), reads=[up, cw, cb], writes=[Y], partial=True)
                    for w in range(1, 31):
                        S.op("dve", lambda: nc.vector.scalar_tensor_tensor(out=Y[:, j, :], in0=up[:, w:w + Ls], scalar=cw[:, j, w:w + 1],
                                                                           in1=Y[:, j, :], op0=ALU.mult, op1=ALU.add),
                             reads=[up, cw, Y], writes=[Y], partial=True)
                BL = min(512, Ls)
                ysq = S.sb([128, 2, BL], F32, "ysq", st)
                mean = S.sb([128, BL], F32, "mean", st)
                var = S.sb([128, BL], F32, "var", st)
                tmpc = S.sb([128, 2, BL], F32, "tmpc", st)
                ctsb = [S.sb([128, 2, BL], BF16, "ctsb", st) for _ in range(2)]
                pmn = S.ps([128, BL], F32, "pmn", st)
                pe2 = S.ps([128, BL], F32, "pe2", st)
                for bi in range(Ls // BL):
                    b0 = bi * BL
                    S.op("act", lambda: nc.scalar.activation(out=ysq[:], in_=Y[:, :, b0:b0 + BL], func=AF.Square), reads=[Y], writes=[ysq])
                    for j in range(2):
                        S.op("pe", lambda: nc.tensor.matmul(pmn[:], lhsT=ones_s[:], rhs=Y[:, j, b0:b0 + BL], start=(j == 0), stop=(j == 1)),
                             reads=[ones_s, Y], writes=[pmn], partial=(j > 0))
                    for j in range(2):
                        S.op("pe", lambda: nc.tensor.matmul(pe2[:], lhsT=ones_s[:], rhs=ysq[:, j, :], start=(j == 0), stop=(j == 1)),
                             reads=[ones_s, ysq], writes=[pe2], partial=(j > 0))
                    S.op("act", lambda: nc.scalar.copy(out=mean[:], in_=pmn[:]), reads=[pmn], writes=[mean])
                    S.op("dve", lambda: nc.vector.tensor_tensor(out=var[:], in0=mean[:], in1=mean[:], op=ALU.mult), reads=[mean], writes=[var])
                    S.op("dve", lambda: nc.vector.tensor_tensor(out=var[:], in0=pe2[:], in1=var[:], op=ALU.subtract), reads=[pe2, var], writes=[var])
                    S.op("dve", lambda: nc.vector.tensor_scalar(out=var[:], in0=var[:], scalar1=EPS, scalar2=None, op0=ALU.add),
                         reads=[var], writes=[var])
                    S.op("act", lambda: nc.scalar.activation(out=var[:], in_=var[:], func=AF.Sqrt), reads=[var], writes=[var])
                    S.op("dve", lambda: nc.vector.reciprocal(out=var[:], in_=var[:]), reads=[var], writes=[var])
                    cts_ = ctsb[bi % 2]
                    for j in range(2):
                        S.op("dve", lambda: nc.vector.tensor_tensor(out=tmpc[:, j, :], in0=Y[:, j, b0:b0 + BL], in1=mean[:], op=ALU.subtract),
                             reads=[Y, mean], writes=[tmpc], partial=True)
                        S.op("dve", lambda: nc.vector.tensor_tensor(out=tmpc[:, j, :], in0=tmpc[:, j, :], in1=var[:], op=ALU.mult),
                             reads=[tmpc, var], writes=[tmpc], partial=True)
                        S.op("act", lambda: nc.scalar.activation(out=cts_[:, j, :], in_=tmpc[:, j, :], func=AF.Silu,
                                                                 bias=lnb[:, j:j + 1], scale=lng[:, j:j + 1]),
                             reads=[tmpc, lnb, lng], writes=[cts_], partial=True)
                    S.dma("sp", lambda: nc.sync.dma_start(out=CTs.ap()[:, t0 + b0:t0 + b0 + BL].rearrange("(j p) t -> p j t", p=128), in_=cts_[:]),
                          reads=[cts_], writes=[CTs])
                S.end_phase()
        if stop_after == "4":
            break

        with ES() as st:
            mod_l, mod_c = load_mod(st, 2 * D, 3 * D)
            wo = S.sb([128, 8, D], BF16, "wo", st)
            for h in range(2):
                S.dma("pool", lambda: nc.gpsimd.dma_start(out=wo[:, :, h * 512:(h + 1) * 512],
                                                          in_=w_out.ap()[l][:, h * 512:(h + 1) * 512].rearrange("(k p) n -> p k n", p=128)),
                      reads=[w_out], writes=[wo], partial=True)
            wr = S.sb([128, 8, NE], F32, "wr", st)
            S.dma("sp", lambda: nc.sync.dma_start(out=wr[:], in_=w_router.ap()[l].rearrange("(k p) e -> p k e", p=128)),
                  reads=[w_router], writes=[wr])
            cats = [S.sb([128, 8, 128], BF16, "cat", st) for _ in range(3)]
            xts = [S.sb([128, D], F32, "xt5", st) for _ in range(3)]
            tmp5 = S.sb([128, D], F32, "tmp5", st)
            x1s = [S.sb([128, D], F32, "x1", st) for _ in range(2)]
            sq5 = S.sb([128, D], F32, "sq5", st)
            ss5 = S.sb([128, 1], F32, "ss5", st)
            rstd5 = S.sb([128, 1], F32, "rstd5", st)
            h2f = S.sb([128, D], F32, "h2f", st)
            h2b = [S.sb([128, D], BF16, "h2b", st) for _ in range(2)]
            h2T = S.sb([128, 8, 128], F32, "h2T", st)
            lg = S.sb([128, NE], F32, "lg", st)
            mx5 = S.sb([128, 1], F32, "mx5", st)
            se5 = S.sb([128, 1], F32, "se5", st)
            ps5 = S.ps([128, D], F32, "ps5", st)
            pt5 = S.ps([128, 8, 128], F32, "pt5", st)
            pl5 = S.ps([128, NE], F32, "pl5", st)
            for i in range(NTT + 1):
                r0 = i * 128
                nr = 128 if i < NTT else NPAD
                S.dma("sp", lambda: nc.sync.dma_start(out=YACC[r0:r0 + nr, :], in_=zeros_f[0:nr, :]), reads=[zeros_f], writes=[YACC])
            h2fs = [h2f, S.sb([128, D], F32, "h2f2", st)]

            def st5Lload(ti):
                r0 = ti * 128
                mod = mod_l if ti < NTL else mod_c
                cat, xt, x1, hb2 = cats[ti % 3], xts[ti % 3], x1s[ti % 2], h2b[ti % 2]
                h2f = h2fs[ti % 2]
                S.dma("sp", lambda: nc.sync.dma_start(out=cat[:, 0:4, :], in_=AT.ap()[:, r0:r0 + 128].rearrange("(j p) t -> p j t", p=128)),
                      reads=[AT], writes=[cat], partial=True)
                S.dma("sp", lambda: nc.sync.dma_start(out=cat[:, 4:6, :], in_=BT.ap()[:, r0:r0 + 128].rearrange("(j p) t -> p j t", p=128)),
                      reads=[BT], writes=[cat], partial=True)
                S.dma("sp", lambda: nc.sync.dma_start(out=cat[:, 6:8, :], in_=CTs.ap()[:, r0:r0 + 128].rearrange("(j p) t -> p j t", p=128)),
                      reads=[CTs], writes=[cat], partial=True)
                S.dma("sp", lambda: nc.sync.dma_start(out=xt[:], in_=X[r0:r0 + 128, :]), reads=[X], writes=[xt])

            def st5Lmm(ti):
                r0 = ti * 128
                mod = mod_l if ti < NTL else mod_c
                cat, xt, x1, hb2 = cats[ti % 3], xts[ti % 3], x1s[ti % 2], h2b[ti % 2]
                h2f = h2fs[ti % 2]
                for hh in range(2):
                    for k in range(8):
                        S.op("pe", lambda: nc.tensor.matmul(ps5[:, hh * 512:(hh + 1) * 512], lhsT=cat[:, k, :], rhs=wo[:, k, hh * 512:(hh + 1) * 512],
                                                            start=(k == 0), stop=(k == 7)), reads=[cat, wo], writes=[ps5],
                             partial=not (hh == 0 and k == 0))

            def st5R(ti):
                r0 = ti * 128
                mod = mod_l if ti < NTL else mod_c
                cat, xt, x1, hb2 = cats[ti % 3], xts[ti % 3], x1s[ti % 2], h2b[ti % 2]
                h2f = h2fs[ti % 2]
                S.op("dve", lambda: nc.vector.tensor_tensor(out=tmp5[:], in0=ps5[:], in1=mod[:, 2 * D:3 * D], op=ALU.mult),
                     reads=[ps5, mod], writes=[tmp5])
                S.op("pool", lambda: nc.gpsimd.tensor_tensor(out=x1[:], in0=tmp5[:], in1=xt[:], op=ALU.add), reads=[tmp5, xt], writes=[x1])
                S.dma("sp", lambda: nc.sync.dma_start(out=X[r0:r0 + 128, :], in_=x1[:]), reads=[x1], writes=[X])
                S.op("act", lambda: nc.scalar.activation(out=sq5[:], in_=x1[:], func=AF.Square, accum_out=ss5[:]), reads=[x1], writes=[sq5, ss5])
                S.op("dve", lambda: nc.vector.tensor_scalar(out=rstd5[:], in0=ss5[:], scalar1=1.0 / D, scalar2=EPS, op0=ALU.mult, op1=ALU.add),
                     reads=[ss5], writes=[rstd5])
                S.op("act", lambda: nc.scalar.activation(out=rstd5[:], in_=rstd5[:], func=AF.Sqrt), reads=[rstd5], writes=[rstd5])
                S.op("dve", lambda: nc.vector.reciprocal(out=rstd5[:], in_=rstd5[:]), reads=[rstd5], writes=[rstd5])
                S.op("dve", lambda: nc.vector.scalar_tensor_tensor(out=h2f[:], in0=x1[:], scalar=rstd5[:, 0:1], in1=mod[:, 4 * D:5 * D],
                                                                   op0=ALU.mult, op1=ALU.mult), reads=[x1, rstd5, mod], writes=[h2f])
                S.op("pool", lambda: nc.gpsimd.tensor_tensor(out=h2f[:], in0=h2f[:], in1=mod[:, 3 * D:4 * D], op=ALU.add),
                     reads=[h2f, mod], writes=[h2f])
                S.op("act", lambda: nc.scalar.copy(out=hb2[:], in_=h2f[:]), reads=[h2f], writes=[hb2])
                S.dma("sp", lambda: nc.sync.dma_start(out=H2[r0:r0 + 128, :], in_=hb2[:]), reads=[hb2], writes=[H2])

            def st5Q(ti):
                r0 = ti * 128
                mod = mod_l if ti < NTL else mod_c
                cat, xt, x1, hb2 = cats[ti % 3], xts[ti % 3], x1s[ti % 2], h2b[ti % 2]
                h2f = h2fs[ti % 2]
                for k in range(8):
                    S.op("pe", lambda: nc.tensor.transpose(out=pt5[:, k, :], in_=h2f[:, k * 128:(k + 1) * 128], identity=idf[:]),
                         reads=[h2f, idf], writes=[pt5], partial=(k > 0))
                S.op("dve", lambda: nc.vector.tensor_copy(out=h2T[:], in_=pt5[:]), reads=[pt5], writes=[h2T])
                for k in range(8):
                    S.op("pe", lambda: nc.tensor.matmul(pl5[:], lhsT=h2T[:, k, :], rhs=wr[:, k, :], start=(k == 0), stop=(k == 7)),
                         reads=[h2T, wr], writes=[pl5], partial=(k > 0))
                S.op("dve", lambda: nc.vector.tensor_reduce(out=mx5[:], in_=pl5[:], axis=AX.X, op=ALU.max), reads=[pl5], writes=[mx5])
                S.op("dve", lambda: nc.vector.tensor_scalar(out=mx5[:], in0=mx5[:], scalar1=-1.0, scalar2=None, op0=ALU.mult),
                     reads=[mx5], writes=[mx5])
                S.op("act", lambda: nc.scalar.activation(out=lg[:], in_=pl5[:], func=AF.Exp, bias=mx5[:, 0:1], scale=1.0, accum_out=se5[:]),
                     reads=[pl5, mx5], writes=[lg, se5])
                S.op("dve", lambda: nc.vector.reciprocal(out=se5[:], in_=se5[:]), reads=[se5], writes=[se5])
                S.op("dve", lambda: nc.vector.tensor_scalar(out=AFF[:, ti, :], in0=lg[:], scalar1=se5[:, 0:1], scalar2=None, op0=ALU.mult),
                     reads=[lg, se5], writes=[AFF], partial=True)

            st5Lload(0)
            st5Lload(1)
            st5Lmm(0)
            for ti in range(ntq):
                if ti + 2 < ntq:
                    st5Lload(ti + 2)
                st5R(ti)
                if ti + 1 < ntq:
                    st5Lmm(ti + 1)
                if ti >= 1:
                    st5Q(ti - 1)
            st5Q(ntq - 1)
            S.end_phase()
        if stop_after == "5":
            break

        wst = ES()
        wst.__enter__()
        st = wst
        wgs = [S.sb([128, 8, 1024], BF16, "wg", st) for _ in range(2)]
        wus = [S.sb([128, 8, 1024], BF16, "wu", st) for _ in range(2)]
        wd = S.sb([128, 16, D], BF16, "wd", st)

        def dma_wgu(e, half):
            c0 = half * 1024
            S.dma("pool", lambda: nc.gpsimd.dma_start(out=wgs[half][:], in_=w_gate.ap()[l, e][:, c0:c0 + 1024].rearrange("(k p) f -> p k f", p=128)),
                  reads=[w_gate], writes=[wgs[half]])
            S.dma("pool", lambda: nc.gpsimd.dma_start(out=wus[half][:], in_=w_up.ap()[l, e][:, c0:c0 + 1024].rearrange("(k p) f -> p k f", p=128)),
                  reads=[w_up], writes=[wus[half]])

        def dma_wd(e):
            for h in range(2):
                S.dma("pool", lambda: nc.gpsimd.dma_start(out=wd[:, h * 8:(h + 1) * 8, :],
                                                          in_=w_down.ap()[l, e][h * 1024:(h + 1) * 1024, :].rearrange("(k p) n -> p k n", p=128)),
                      reads=[w_down], writes=[wd], partial=(h > 0))


        if has_moe:
            dma_wgu(0, 0)
            dma_wgu(0, 1)
            dma_wd(0)
        nrt = ntq
        with ES() as st:
            utb = S.sb([128, 128], BF16, "utb", st)
            utf = S.sb([128, 128], F32, "utf", st)
            S.dma("sp", lambda: nc.sync.dma_start(out=utf[:], in_=utri_in[:, :]), reads=[utri_in], writes=[utf])
            S.op("dve", lambda: nc.vector.tensor_copy(out=utb[:], in_=utf[:]), reads=[utf], writes=[utb])
            lo = S.sb([128, 32], F32, "lo", st)
            hi = S.sb([128, 32], F32, "hi", st)
            mid = S.sb([128, 32], F32, "mid", st)
            tgt = S.sb([128, 32], F32, "tgt", st)
            ge = S.sb([128, 32], F32, "ge", st)
            d1 = S.sb([128, 32], F32, "d1", st)
            cmpb = S.sb([128, NTT, NE], F32, "cmpb", st)
            cntb = S.sb([128, 32], F32, "cntb", st)
            pc = S.ps([128, 32], F32, "pc", st)
            S.op("dve", lambda: nc.vector.memset(lo[:], 0.0), writes=[lo])
            S.op("dve", lambda: nc.vector.memset(hi[:], 1.0), writes=[hi])
            S.op("dve", lambda: nc.vector.memset(tgt[:, 0:16], float(CAP)), writes=[tgt], partial=True)
            S.op("dve", lambda: nc.vector.memset(tgt[:, 16:32], float(CCAP)), writes=[tgt], partial=True)
            S.op("dve", lambda: nc.vector.memset(cntb[:], 0.0), writes=[cntb])
            parts = [(0, NTL, 0)] + ([] if last else [(NTL, NTT, 16)])

            def compare(dst, thr):
                for (a, b_, c0) in parts:
                    S.op("dve", lambda: nc.vector.tensor_tensor(
                        out=dst[:, a:b_, :], in0=AFF[:, a:b_, :],
                        in1=thr[:, c0:c0 + 16].unsqueeze(1).to_broadcast([128, b_ - a, NE]), op=ALU.is_ge),
                        reads=[AFF, thr], writes=[dst], partial=True)

            for itn in range(40):
                S.op("dve", lambda: nc.vector.tensor_tensor(out=mid[:], in0=lo[:], in1=hi[:], op=ALU.add), reads=[lo, hi], writes=[mid])
                S.op("dve", lambda: nc.vector.tensor_scalar(out=mid[:], in0=mid[:], scalar1=0.5, scalar2=None, op0=ALU.mult),
                     reads=[mid], writes=[mid])
                compare(cmpb, mid)
                for (a, b_, c0) in parts:
                    S.op("dve", lambda: nc.vector.tensor_reduce(out=cntb[:, c0:c0 + 16], in_=cmpb[:, a:b_, :].rearrange("p t e -> p e t"),
                                                                axis=AX.X, op=ALU.add), reads=[cmpb], writes=[cntb], partial=True)
                S.op("pe", lambda: nc.tensor.matmul(pc[:], lhsT=ones_f[:], rhs=cntb[:], start=True, stop=True), reads=[ones_f, cntb], writes=[pc])
                S.op("dve", lambda: nc.vector.tensor_tensor(out=ge[:], in0=pc[:], in1=tgt[:], op=ALU.is_ge), reads=[pc, tgt], writes=[ge])
                S.op("dve", lambda: nc.vector.tensor_tensor(out=d1[:], in0=mid[:], in1=lo[:], op=ALU.subtract), reads=[mid, lo], writes=[d1])
                S.op("dve", lambda: nc.vector.tensor_tensor(out=d1[:], in0=d1[:], in1=ge[:], op=ALU.mult), reads=[d1, ge], writes=[d1])
                S.op("dve", lambda: nc.vector.tensor_tensor(out=lo[:], in0=lo[:], in1=d1[:], op=ALU.add), reads=[lo, d1], writes=[lo])
                S.op("dve", lambda: nc.vector.tensor_tensor(out=d1[:], in0=hi[:], in1=mid[:], op=ALU.subtract), reads=[hi, mid], writes=[d1])
                S.op("dve", lambda: nc.vector.tensor_tensor(out=d1[:], in0=d1[:], in1=ge[:], op=ALU.mult), reads=[d1, ge], writes=[d1])
                S.op("dve", lambda: nc.vector.tensor_tensor(out=hi[:], in0=mid[:], in1=d1[:], op=ALU.add), reads=[mid, d1], writes=[hi])
            maskb = S.sb([128, NTT, NE], BF16, "maskb", st)
            pref = S.sb([128, NTT, NE], F32, "pref", st)
            tcnt = S.sb([128, NTT, NE], F32, "tcnt", st)
            tcn2 = S.sb([128, NTT, NE], F32, "tcn2", st)
            S.op("dve", lambda: nc.vector.memset(cmpb[:], 0.0), writes=[cmpb])
            compare(cmpb, lo)
            S.op("dve", lambda: nc.vector.tensor_copy(out=maskb[:], in_=cmpb[:]), reads=[cmpb], writes=[maskb])
            ncol = NTT * NE
            mflat = maskb[:].rearrange("p t e -> p (t e)")
            pflat = pref[:].rearrange("p t e -> p (t e)")
            tflat = tcnt[:].rearrange("p t e -> p (t e)")
            pp = [S.ps([128, 512], F32, "pp", st) for _ in range(2)]
            for ci, c0 in enumerate(range(0, ncol, 512)):
                n = min(512, ncol - c0)
                S.op("pe", lambda: nc.tensor.matmul(pp[0][:, 0:n], lhsT=utb[:], rhs=mflat[:, c0:c0 + n], start=True, stop=True),
                     reads=[utb, maskb], writes=[pp[0]])
                S.op("pe", lambda: nc.tensor.matmul(pp[1][:, 0:n], lhsT=ones_b[:], rhs=mflat[:, c0:c0 + n], start=True, stop=True),
                     reads=[ones_b, maskb], writes=[pp[1]])
                S.op("dve", lambda: nc.vector.tensor_copy(out=pflat[:, c0:c0 + n], in_=pp[0][:, 0:n]), reads=[pp[0]], writes=[pref], partial=True)
                S.op("act", lambda: nc.scalar.copy(out=tflat[:, c0:c0 + n], in_=pp[1][:, 0:n]), reads=[pp[1]], writes=[tcnt], partial=True)
            src, dst = tcnt, tcn2
            S.op("dve", lambda: nc.vector.tensor_copy(out=tcn2[:], in_=tcnt[:]), reads=[tcnt], writes=[tcn2])
            cum = S.sb([128, NTT, NE], F32, "cum", st)
            S.op("dve", lambda: nc.vector.tensor_copy(out=cum[:], in_=tcnt[:]), reads=[tcnt], writes=[cum])
            a_, b2 = cum, tcn2
            sft = 1
            while sft < NTL:
                S.op("dve", lambda: nc.vector.tensor_tensor(out=b2[:, sft:NTL, :], in0=a_[:, sft:NTL, :], in1=a_[:, 0:NTL - sft, :], op=ALU.add),
                     reads=[a_], writes=[b2], partial=True)
                S.op("dve", lambda: nc.vector.tensor_copy(out=b2[:, 0:sft, :], in_=a_[:, 0:sft, :]), reads=[a_], writes=[b2], partial=True)
                a_, b2 = b2, a_
                sft *= 2
            inc = a_
            slot = S.sb([128, NTT, NE], F32, "slot", st)
            S.op("dve", lambda: nc.vector.tensor_tensor(out=slot[:, 0:NTL, :], in0=inc[:, 0:NTL, :], in1=tcnt[:, 0:NTL, :], op=ALU.subtract),
                 reads=[inc, tcnt], writes=[slot], partial=True)
            if not last:
                S.op("dve", lambda: nc.vector.memset(slot[:, NTL, :], float(CAP)), writes=[slot], partial=True)
                S.op("dve", lambda: nc.vector.tensor_scalar(out=slot[:, NTL + 1, :], in0=tcnt[:, NTL, :], scalar1=float(CAP), scalar2=None,
                                                            op0=ALU.add), reads=[tcnt], writes=[slot], partial=True)
            S.op("dve", lambda: nc.vector.tensor_tensor(out=slot[:, 0:nrt, :], in0=slot[:, 0:nrt, :], in1=pref[:, 0:nrt, :], op=ALU.add),
                 reads=[slot, pref], writes=[slot], partial=True)
            ebase = S.sb([128, NE], F32, "ebase", st)
            for e in range(NE):
                S.op("dve", lambda: nc.vector.memset(ebase[:, e:e + 1], float(e * MS)), writes=[ebase], partial=True)
            S.op("dve", lambda: nc.vector.tensor_tensor(out=slot[:, 0:nrt, :], in0=slot[:, 0:nrt, :],
                                                        in1=ebase[:].unsqueeze(1).to_broadcast([128, nrt, NE]), op=ALU.add),
                 reads=[slot, ebase], writes=[slot], partial=True)
            BIGI = float(1 << 20)
            S.op("dve", lambda: nc.vector.scalar_tensor_tensor(out=slot[:, 0:nrt, :].rearrange("p t e -> p (t e)"),
                                                               in0=slot[:, 0:nrt, :].rearrange("p t e -> p (t e)"), scalar=-BIGI,
                                                               in1=cmpb[:, 0:nrt, :].rearrange("p t e -> p (t e)"), op0=ALU.add, op1=ALU.mult),
                 reads=[slot, cmpb], writes=[slot], partial=True)
            S.op("dve", lambda: nc.vector.tensor_scalar(out=slot[:, 0:nrt, :], in0=slot[:, 0:nrt, :], scalar1=BIGI, scalar2=None, op0=ALU.add),
                 reads=[slot], writes=[slot], partial=True)
            idxi = S.sb([128, NTT, NE], I32, "idxi", st)
            S.op("dve", lambda: nc.vector.tensor_copy(out=idxi[:, 0:nrt, :], in_=slot[:, 0:nrt, :]), reads=[slot], writes=[idxi])
            tid = S.sb([128, NTT], I32, "tid", st)
            S.op("pool", lambda: nc.gpsimd.iota(tid[:], pattern=[[128, NTT]], base=0, channel_multiplier=1), writes=[tid])
            pay = S.sb([128, NTT, NE, 2], I32, "pay", st)
            S.op("dve", lambda: nc.vector.tensor_copy(out=pay[:, :, :, 0], in_=tid[:].unsqueeze(2).to_broadcast([128, NTT, NE])),
                 reads=[tid], writes=[pay], partial=True)
            S.op("dve", lambda: nc.vector.tensor_copy(out=pay[:].bitcast(F32)[:, :, :, 1], in_=AFF[:]), reads=[AFF], writes=[pay], partial=True)
            lflat = LST.ap().rearrange("e s c -> (e s) c")
            for ti in range(nrt):
                for e in range(NE):
                    S.dma("pool", lambda: nc.gpsimd.indirect_dma_start(
                        out=lflat, out_offset=bass.IndirectOffsetOnAxis(ap=idxi[:, ti, e:e + 1], axis=0),
                        in_=pay[:, ti, e, :], in_offset=None, bounds_check=bc_reg, oob_is_err=False),
                        reads=[pay, idxi], writes=[LST])
            if "route" in debug and l == 0:
                dbg["thr"] = S.dram("dbg_thr", [128, 32], F32, kind="ExternalOutput")
                S.dma("sp", lambda: nc.sync.dma_start(out=dbg["thr"][:, :], in_=lo[:]), reads=[lo], writes=[dbg["thr"]])
                dbg["idx"] = S.dram("dbg_idx", [128, NTT, NE], I32, kind="ExternalOutput")
                S.dma("sp", lambda: nc.sync.dma_start(out=dbg["idx"].ap(), in_=idxi[:]), reads=[idxi], writes=[dbg["idx"]])
            S.end_phase()
        if stop_after == "6":
            break

        with ES() as st:
            nst7 = 8 if last else 9
            NS = nst7 * 128
            ngr = [(0, 512), (512, 512)] + ([] if last else [(1024, 128)])
            xeT = S.sb([128, 8, NS], BF16, "xeT", st)
            hTe = S.sb([128, 16, NS], BF16, "hTe", st)
            idts = [S.sb([128, 9, 2], I32, "idt", st) for _ in range(2)]
            xgs = [S.sb([128, D], BF16, "xg", st) for _ in range(nst7)]
            ysbs = [S.sb([128, D], F32, "ysb", st) for _ in range(2)]
            sgl = [S.sb([128, 512], F32, "sgl", st) for _ in range(2)]
            ptx = S.ps([128, 8, 128], BF16, "ptx", st)
            pgs = [S.ps([128, 512], F32, "pg", st) for _ in range(2)]
            pus = [S.ps([128, 512], F32, "pu", st) for _ in range(2)]
            pys = [S.ps([128, 512], F32, "py", st) for _ in range(2)]
            cnt7 = {"fc": 0, "ys": 0}
            sc_state = {"prev": [], "cur": []}

            def gather(e):
                idt = idts[e % 2]
                S.dma("sp", lambda: nc.sync.dma_start(out=idt[:, 0:nst7, :], in_=LST.ap()[e, 0:NS, :].rearrange("(t p) c -> p t c", p=128)),
                      reads=[LST], writes=[idt])
                for si in range(nst7):
                    S.dma("pool", lambda: nc.gpsimd.indirect_dma_start(
                        out=xgs[si][:], out_offset=None, in_=H2.ap(),
                        in_offset=bass.IndirectOffsetOnAxis(ap=idt[:, si, 0:1], axis=0)), reads=[H2, idt], writes=[xgs[si]])

            def transp(e):
                for si in range(nst7):
                    for k in range(8):
                        S.op("pe", lambda: nc.tensor.transpose(out=ptx[:, k, :], in_=xgs[si][:, k * 128:(k + 1) * 128], identity=idb[:]),
                             reads=[xgs[si], idb], writes=[ptx], partial=(k > 0))
                    S.op("dve", lambda: nc.vector.tensor_copy(out=xeT[:, :, si * 128:(si + 1) * 128], in_=ptx[:]),
                         reads=[ptx], writes=[xeT], partial=True)

            def hphase(e, half):
                wg_, wu_ = wgs[half], wus[half]
                for fl in range(8):
                    fc = half * 8 + fl
                    for (s0, N) in ngr:
                        i_ = cnt7["fc"]
                        cnt7["fc"] += 1
                        pg, pu, sg_ = pgs[i_ % 2], pus[i_ % 2], sgl[i_ % 2]
                        for k in range(8):
                            S.op("pe", lambda: nc.tensor.matmul(pg[:, 0:N], lhsT=wg_[:, k, fl * 128:(fl + 1) * 128], rhs=xeT[:, k, s0:s0 + N],
                                                                start=(k == 0), stop=(k == 7)), reads=[wg_, xeT], writes=[pg], partial=(k > 0))
                        for k in range(8):
                            S.op("pe", lambda: nc.tensor.matmul(pu[:, 0:N], lhsT=wu_[:, k, fl * 128:(fl + 1) * 128], rhs=xeT[:, k, s0:s0 + N],
                                                                start=(k == 0), stop=(k == 7)), reads=[wu_, xeT], writes=[pu], partial=(k > 0))
                        S.op("act", lambda: nc.scalar.activation(out=sg_[:, 0:N], in_=pg[:, 0:N], func=AF.Silu), reads=[pg], writes=[sg_])
                        S.op("dve", lambda: nc.vector.tensor_tensor(out=hTe[:, fc, s0:s0 + N], in0=sg_[:, 0:N], in1=pu[:, 0:N], op=ALU.mult),
                             reads=[sg_, pu], writes=[hTe], partial=True)

            def yphase(e, tiles):
                idt = idts[e % 2]
                for si in tiles:
                    ysb = ysbs[cnt7["ys"] % 2]
                    cnt7["ys"] += 1
                    gate = idt[:].bitcast(F32)[:, si, 1:2]
                    for dh in range(2):
                        py = pys[dh]
                        for fc in range(16):
                            S.op("pe", lambda: nc.tensor.matmul(py[:], lhsT=hTe[:, fc, si * 128:(si + 1) * 128], rhs=wd[:, fc, dh * 512:(dh + 1) * 512],
                                                                start=(fc == 0), stop=(fc == 15)), reads=[hTe, wd], writes=[py], partial=(fc > 0))
                        if dh == 0:
                            S.op("dve", lambda: nc.vector.tensor_scalar(out=ysb[:, 0:512], in0=py[:], scalar1=gate, scalar2=None, op0=ALU.mult),
                                 reads=[py, idt], writes=[ysb], partial=True)
                        else:
                            S.op("act", lambda: nc.scalar.activation(out=ysb[:, 512:1024], in_=py[:], func=AF.Copy, scale=gate),
                                 reads=[py, idt], writes=[ysb], partial=True)
                    tok = S.dma("pool", lambda: nc.gpsimd.indirect_dma_start(
                        out=YACC.ap(), out_offset=bass.IndirectOffsetOnAxis(ap=idt[:, si, 0:1], axis=0),
                        in_=ysb[:], in_offset=None, compute_op=ALU.add), reads=[ysb, idt], writes=[YACC], after=sc_state["prev"])
                    sc_state["cur"].append(tok)

            gather(0)
            transp(0)
            for e in range(NE):
                nxt = e + 1 < NE
                hphase(e, 0)
                if nxt:
                    dma_wgu(e + 1, 0)
                hphase(e, 1)
                if nxt:
                    dma_wgu(e + 1, 1)
                    gather(e + 1)
                sc_state["cur"] = []
                yphase(e, range(0, 4))
                if nxt:
                    transp(e + 1)
                yphase(e, range(4, nst7))
                if nxt:
                    dma_wd(e + 1)
                sc_state["prev"] = sc_state["cur"][-2:]
            S.end_phase()
        wst.__exit__(None, None, None)
        if stop_after == "7":
            break

        with ES() as st:
            mod_l, mod_c = load_mod(st, 5 * D, D)
            xt8 = [S.sb([128, D], F32, "xt8", st) for _ in range(3)]
            yt8 = [S.sb([128, D], F32, "yt8", st) for _ in range(3)]
            xo8 = [S.sb([128, D], F32, "xo8", st) for _ in range(2)]
            sq8 = S.sb([128, D], F32, "sq8", st)
            ss8 = S.sb([128, 1], F32, "ss8", st)
            fg = S.sb([128, D], F32, "fg", st)
            if last:
                S.dma("sp", lambda: nc.sync.dma_start(out=fg[:], in_=final_g.ap().partition_broadcast(128)), reads=[final_g], writes=[fg])
            def ld8(ti):
                r0 = ti * 128
                xt, yt = xt8[ti % 3], yt8[ti % 3]
                S.dma("sp", lambda: nc.sync.dma_start(out=xt[:], in_=X[r0:r0 + 128, :]), reads=[X], writes=[xt])
                S.dma("sp", lambda: nc.sync.dma_start(out=yt[:], in_=YACC[r0:r0 + 128, :]), reads=[YACC], writes=[yt])

            ld8(0)
            if ntq > 1:
                ld8(1)
            for ti in range(ntq):
                r0 = ti * 128
                mod = mod_l if ti < NTL else mod_c
                xt, yt, xo = xt8[ti % 3], yt8[ti % 3], xo8[ti % 2]
                if ti + 2 < ntq:
                    ld8(ti + 2)
                S.op("dve", lambda: nc.vector.tensor_tensor(out=yt[:], in0=yt[:], in1=mod[:, 5 * D:6 * D], op=ALU.mult), reads=[yt, mod], writes=[yt])
                S.op("pool", lambda: nc.gpsimd.tensor_tensor(out=xo[:], in0=yt[:], in1=xt[:], op=ALU.add), reads=[yt, xt], writes=[xo])
                if not last:
                    S.dma("sp", lambda: nc.sync.dma_start(out=X[r0:r0 + 128, :], in_=xo[:]), reads=[xo], writes=[X])
                else:
                    S.op("act", lambda: nc.scalar.activation(out=sq8[:], in_=xo[:], func=AF.Square, accum_out=ss8[:]), reads=[xo], writes=[sq8, ss8])
                    S.op("dve", lambda: nc.vector.tensor_scalar(out=ss8[:], in0=ss8[:], scalar1=1.0 / D, scalar2=EPS, op0=ALU.mult, op1=ALU.add),
                         reads=[ss8], writes=[ss8])
                    S.op("act", lambda: nc.scalar.activation(out=ss8[:], in_=ss8[:], func=AF.Sqrt), reads=[ss8], writes=[ss8])
                    S.op("dve", lambda: nc.vector.reciprocal(out=ss8[:], in_=ss8[:]), reads=[ss8], writes=[ss8])
                    S.op("dve", lambda: nc.vector.scalar_tensor_tensor(out=xt[:], in0=xo[:], scalar=ss8[:, 0:1], in1=fg[:], op0=ALU.mult, op1=ALU.mult),
                         reads=[xo, ss8, fg], writes=[xt])
                    S.dma("sp", lambda: nc.sync.dma_start(out=out[r0:r0 + 128, :], in_=xt[:]), reads=[xt], writes=[out])
            S.end_phase()
        if stop_after == "8":
            break

    def tap(name, buf, shape, dt):
        o = S.dram("dbg_" + name, shape, dt, kind="ExternalOutput")
        S.dma("sp", lambda: nc.sync.dma_start(out=o.ap(), in_=buf.ap()), reads=[buf], writes=[o])

    for name in debug:
        if name == "QT":
            tap("QT", QT, [128, 4, TT], BF16)
        if name == "KT":
            tap("KT", KT, [128, TT], BF16)
        if name == "VA":
            tap("VA", VA, [TT, 130], BF16)
        if name == "QBT":
            tap("QBT", QBT, [128, 2, TT], BF16)
        if name == "KBT":
            tap("KBT", KBT, [128, 2, TT], BF16)
        if name == "VB":
            tap("VB", VB, [TT, 256], BF16)
        if name == "UT":
            tap("UT", UT, [256, TT], F32)
        if name == "X":
            tap("X", X, [TT, D], F32)
        if name == "AT":
            tap("AT", AT, [512, TT], BF16)
        if name == "BT":
            tap("BT", BT, [256, TT], BF16)
        if name == "CTs":
            tap("CTs", CTs, [256, TT], BF16)
        if name == "H2":
            tap("H2", H2, [TT + NPAD, D], BF16)
        if name == "LST":
            tap("LST", LST, [NE, MS, 2], I32)
        if name == "YACC":
            tap("YACC", YACC, [TT + NPAD, D], F32)
        if name == "AFF":
            o_ = S.dram("dbg_AFF", [128, NTT, NE], F32, kind="ExternalOutput")
            S.dma("sp", lambda: nc.sync.dma_start(out=o_.ap(), in_=AFF[:]), reads=[AFF], writes=[o_])
    S.barrier()
    return nc


def host_consts(na_rpb, n_layers):
    t = np.arange(T)
    row = (t // 64).astype(np.float64)
    col = (t % 64).astype(np.float64)
    inv = 10000.0 ** (-np.arange(16, dtype=np.float64) / 16)
    rc = np.ones((TT, 64), np.float32)
    rs = np.zeros((TT, 64), np.float32)
    for a, pos in enumerate((row, col)):
        ang = (pos.astype(np.float32)[:, None] * inv.astype(np.float32)[None, :]).astype(np.float32)
        cs, sn = np.cos(ang), np.sin(ang)
        rc[:T, a * 32:a * 32 + 16] = cs
        rc[:T, a * 32 + 16:a * 32 + 32] = cs
        rs[:T, a * 32:a * 32 + 16] = -sn
        rs[:T, a * 32 + 16:a * 32 + 32] = sn
    q = np.arange(64)
    cs0 = np.clip(q - 8, 0, 48)
    c = np.arange(64)
    valid = (c[None, :] >= cs0[:, None]) & (c[None, :] < cs0[:, None] + 16)
    dc = np.clip(c[None, :] - q[:, None] + 15, 0, 30)
    nab = np.full((n_layers, 8, 64, 4, 8, 64), NEG, np.float32)
    for off in range(8):
        for j in range(8):
            g = na_rpb[:n_layers, :, off + j, :][:, :, dc]
            g = np.where(valid[None, None], g, np.float32(NEG))
            nab[:, off, :, :, j, :] = np.transpose(g, (0, 2, 1, 3))
    lpad = np.zeros((NPAD, 2), np.int32)
    lpad[:, 0] = TT + np.arange(NPAD)
    return {"ident": np.eye(128, dtype=np.float32), "utri": np.triu(np.ones((128, 128), np.float32), 1), "rope_c": rc, "rope_s": rs,
            "nabias": nab.reshape(n_layers, 8, 64, 4, 512), "lpad": lpad}


_WNAMES = ["w_ada", "b_ada", "norm1_g", "norm2_g", "w_in", "q_norm_g", "k_norm_g", "conv_w", "conv_b",
           "conv_ln_g", "conv_ln_b", "w_out", "w_router", "w_gate", "w_up", "w_down"]


def make_in_maps(inputs, n_layers, samples):
    consts = host_consts(np.asarray(inputs["na_rpb"]), n_layers)
    maps = []
    for b in samples:
        m = {"x": np.ascontiguousarray(inputs["x"][b]), "ctx": np.ascontiguousarray(inputs["ctx"][b]),
             "c": np.ascontiguousarray(inputs["c"][b]), "c_ctx": np.ascontiguousarray(inputs["c_ctx"]),
             "final_norm_g": np.ascontiguousarray(inputs["final_norm_g"])}
        for k in _WNAMES:
            m[k] = np.ascontiguousarray(inputs[k][:n_layers])
        m.update(consts)
        maps.append(m)
    return maps


def kernel(**inputs):
    inputs = {k: np.asarray(v) for k, v in inputs.items()}
    nc = build_program(DEPTH)
    maps = make_in_maps(inputs, DEPTH, range(4))
    res = run_bass_kernel_spmd(nc, maps, core_ids=list(range(4)))
    return np.stack([r["out"] for r in res.results], 0).astype(np.float32)
```
